# Optimizing a Trainium2 kernel written in Bass

```python
import math
import jax, jax.numpy as jnp
from jax import lax
import numpy as np

D_MODEL = 1024
BATCH = 16
SEQ = 4096
DEPTH = 1
DEC_BATCH = 128
DEC_SEQ = 1
PAST_LEN = 8192
PAGE_SIZE = 128

SSM_GROUP = 16
D_SSM = D_MODEL // 2
N_SSM_GROUPS = D_SSM // SSM_GROUP
SSM_STATE = 64
DT_MIN = 1e-3
DT_MAX = 1e-1
N_HEADS = 8
N_KV_HEADS = 2
HEAD_DIM = 64
D_ATTN = N_HEADS * HEAD_DIM
ROT_DIM = HEAD_DIM // 4
ROPE_THETA = 500000.0
N_IDX_HEADS = 8
IDX_DIM = 64
IDX_ROT_DIM = IDX_DIM // 4
TOPK_MAX = 256
Q_BLOCK = 128
D_FF = 4 * D_MODEL
EPS = 1e-6
SPLITS = (D_SSM, D_ATTN, N_KV_HEADS * HEAD_DIM, N_KV_HEADS * HEAD_DIM,
          N_IDX_HEADS * IDX_DIM, IDX_DIM, N_IDX_HEADS, D_MODEL, D_MODEL)
IN_COLS = D_SSM + D_ATTN + 2 * N_KV_HEADS * HEAD_DIM + N_IDX_HEADS * IDX_DIM + IDX_DIM + N_IDX_HEADS + 2 * D_MODEL

kernel_name = 'hybrid_s5_dsa_gated_decoder_step'


def rms_norm(x, g):
    xf = x.astype(jnp.float32)
    y = xf * lax.rsqrt(jnp.mean(xf * xf, axis=-1, keepdims=True) + EPS)
    return (y * g.astype(jnp.float32)).astype(x.dtype)


def partial_rope(x, pos, rot_dim):
    half = rot_dim // 2
    inv = ROPE_THETA ** (-jnp.arange(half, dtype=jnp.float32) / half)
    ang = pos.astype(jnp.float32)[:, None] * inv[None, :]
    cos = jnp.cos(ang)[:, None, :]
    sin = jnp.sin(ang)[:, None, :]
    xr = x[..., :rot_dim].astype(jnp.float32)
    x1, x2 = xr[..., :half], xr[..., half:]
    rot = jnp.concatenate([x1 * cos - x2 * sin, x1 * sin + x2 * cos], axis=-1)
    return jnp.concatenate([rot.astype(x.dtype), x[..., rot_dim:]], axis=-1)


def _ssm_combine(e1, e2):
    a1, b1 = e1
    a2, b2 = e2
    return a1 * a2, a2 * b1 + b2


def s5_branch(u, h0_re, h0_im, a_re, a_im, log_dt, b_re, b_im, c_re, c_im, d_skip, w_glu, b_glu):
    f32 = jnp.float32
    bsz, t_len, _ = u.shape
    a_c = lax.complex(a_re.astype(f32), a_im.astype(f32))
    dt = jnp.exp(log_dt.astype(f32))[:, None]
    a_bar = jnp.exp(a_c * dt)
    b_mat = lax.complex(b_re.astype(f32), b_im.astype(f32))
    b_bar = ((a_bar - 1.0) / a_c)[:, :, None] * b_mat
    c_mat = lax.complex(c_re.astype(f32), c_im.astype(f32))
    uf = u.astype(f32)
    ug = uf.reshape(bsz, t_len, N_SSM_GROUPS, SSM_GROUP).astype(jnp.complex64)
    bu = jnp.einsum('gpn,btgn->btgp', b_bar, ug)
    if h0_re is not None:
        h0 = lax.complex(h0_re.astype(f32), h0_im.astype(f32))
        bu = bu.at[:, 0].add(a_bar * h0)
    a_seq = jnp.broadcast_to(a_bar, bu.shape)
    _, h = lax.associative_scan(_ssm_combine, (a_seq, bu), axis=1)
    y = jnp.real(jnp.einsum('gnp,btgp->btgn', c_mat, h)).reshape(bsz, t_len, D_SSM)
    y = y + d_skip.astype(f32) * uf
    z = jax.nn.gelu(y).astype(u.dtype) @ w_glu + b_glu
    val, gate = jnp.split(z, 2, axis=-1)
    out = val * jax.nn.sigmoid(gate)
    h_last = h[:, -1]
    return out, jnp.real(h_last), jnp.imag(h_last)


def index_topk(qi, wi, ki, q_pos, k_pos, k_top):
    f32 = jnp.float32
    dots = jnp.einsum('bthd,bsd->btsh', qi.astype(f32), ki.astype(f32)) * (IDX_DIM ** -0.5)
    score = jnp.einsum('btsh,bth->bts', jax.nn.relu(dots), wi.astype(f32))
    causal = k_pos[None, None, :] <= q_pos[None, :, None]
    score = jnp.where(causal, score, -jnp.inf)
    _, idx = lax.top_k(score, k_top)
    return idx


def sparse_attend(q, k_sel, v_sel, valid):
    f32 = jnp.float32
    bsz, t_len = q.shape[:2]
    qg = q.reshape(bsz, t_len, N_KV_HEADS, N_HEADS // N_KV_HEADS, HEAD_DIM).astype(f32)
    s = jnp.einsum('btgrd,btkgd->btgrk', qg, k_sel.astype(f32)) * (HEAD_DIM ** -0.5)
    s = jnp.where(valid[:, :, None, None, :], s, -jnp.inf)
    p = jax.nn.softmax(s, axis=-1)
    o = jnp.einsum('btgrk,btkgd->btgrd', p, v_sel.astype(f32))
    return o.reshape(bsz, t_len, D_ATTN).astype(q.dtype)


def prompt_sparse_attention(q, k, v, qi, ki, wi, pos):
    bsz, s_len = q.shape[:2]
    k_top = min(TOPK_MAX, s_len // 4)
    nb = s_len // Q_BLOCK
    bidx = jnp.arange(bsz)[:, None, None]

    def to_blocks(a):
        return jnp.moveaxis(a.reshape(bsz, nb, Q_BLOCK, *a.shape[2:]), 1, 0)

    def block(args):
        q_b, qi_b, wi_b, pos_b = args
        idx = index_topk(qi_b, wi_b, ki, pos_b, pos, k_top)
        valid = idx <= pos_b[None, :, None]
        return sparse_attend(q_b, k[bidx, idx], v[bidx, idx], valid)

    out = lax.map(block, (to_blocks(q), to_blocks(qi), to_blocks(wi), pos.reshape(nb, Q_BLOCK)))
    return jnp.moveaxis(out, 0, 1).reshape(bsz, s_len, D_ATTN)


def sample_sparse_attention(q, k, v, qi, ki, wi, pos, cache_k, cache_v, cache_idx_k, page_table):
    bsz, t_len = q.shape[:2]
    n_pages = PAST_LEN // PAGE_SIZE
    l_keys = PAST_LEN + t_len
    k_top = min(TOPK_MAX, l_keys // 4)
    ki_past = cache_idx_k[page_table].reshape(bsz, n_pages * PAGE_SIZE, IDX_DIM)
    ki_all = jnp.concatenate([ki_past.astype(ki.dtype), ki], axis=1)
    k_pos = jnp.arange(l_keys, dtype=jnp.int32)
    idx = index_topk(qi, wi, ki_all, pos, k_pos, k_top)
    bidx = jnp.arange(bsz)[:, None, None]
    in_past = (idx < PAST_LEN)[..., None, None]
    pidx = jnp.minimum(idx, PAST_LEN - 1)
    phys = page_table[bidx, pidx // PAGE_SIZE]
    off = pidx % PAGE_SIZE
    nidx = jnp.clip(idx - PAST_LEN, 0, t_len - 1)
    k_sel = jnp.where(in_past, cache_k[phys, off], k[bidx, nidx])
    v_sel = jnp.where(in_past, cache_v[phys, off], v[bidx, nidx])
    valid = idx <= pos[None, :, None]
    return sparse_attend(q, k_sel, v_sel, valid)


def decoder_layer(x, pos, attend, h0_re, h0_im, p):
    bsz, t_len, _ = x.shape
    h = rms_norm(x, p['norm1_g'])
    proj = h @ p['w_in']
    split_points = [int(c) for c in np.cumsum(SPLITS)[:-1]]
    u, q, k, v, qi, ki, wi, g_ssm, g_attn = jnp.split(proj, split_points, axis=-1)
    q = partial_rope(q.reshape(bsz, t_len, N_HEADS, HEAD_DIM), pos, ROT_DIM)
    k = partial_rope(k.reshape(bsz, t_len, N_KV_HEADS, HEAD_DIM), pos, ROT_DIM)
    v = v.reshape(bsz, t_len, N_KV_HEADS, HEAD_DIM)
    qi = partial_rope(qi.reshape(bsz, t_len, N_IDX_HEADS, IDX_DIM), pos, IDX_ROT_DIM)
    ki = partial_rope(ki.reshape(bsz, t_len, 1, IDX_DIM), pos, IDX_ROT_DIM)[:, :, 0]
    wi = wi * (N_IDX_HEADS ** -0.5)
    ssm_out, hT_re, hT_im = s5_branch(u, h0_re, h0_im, p['ssm_a_re'], p['ssm_a_im'], p['ssm_log_dt'],
                                      p['ssm_b_re'], p['ssm_b_im'], p['ssm_c_re'], p['ssm_c_im'],
                                      p['ssm_d'], p['w_glu'], p['b_glu'])
    attn_out = attend(q, k, v, qi, ki, wi, pos) @ p['w_attn_out']
    mix = jax.nn.sigmoid(g_ssm) * ssm_out + jax.nn.sigmoid(g_attn) * attn_out
    x = x + mix @ p['w_o']
    hh = rms_norm(x, p['norm2_g'])
    x = x + jnp.square(jax.nn.relu(hh @ p['w_up'])) @ p['w_down']
    return x, k, v, ki, hT_re, hT_im


def setup_inputs(seed: int = 0) -> dict:
    key = jax.random.key(seed)
    ks = jax.random.split(key, 32)
    f32 = jnp.float32
    n_pages = PAST_LEN // PAGE_SIZE
    n_used = DEC_BATCH * n_pages
    n_pool = n_used + max(1, n_used // 4)

    def nrm(k, shape, scale):
        return scale * jax.random.normal(k, shape, f32)

    g, pst, n = N_SSM_GROUPS, SSM_STATE, SSM_GROUP
    page_table = jax.random.permutation(ks[7], n_pool)[:n_used].reshape(DEC_BATCH, n_pages).astype(jnp.int32)
    return {
        'x_prompt': nrm(ks[0], (BATCH, SEQ, D_MODEL), 1.0),
        'x_sample': nrm(ks[1], (DEC_BATCH, DEC_SEQ, D_MODEL), 1.0),
        'cache_k': nrm(ks[2], (n_pool, PAGE_SIZE, N_KV_HEADS, HEAD_DIM), 1.0),
        'cache_v': nrm(ks[3], (n_pool, PAGE_SIZE, N_KV_HEADS, HEAD_DIM), 1.0),
        'cache_idx_k': nrm(ks[4], (n_pool, PAGE_SIZE, IDX_DIM), 1.0),
        'state_ssm_re': nrm(ks[5], (DEC_BATCH, g, pst), 0.5),
        'state_ssm_im': nrm(ks[6], (DEC_BATCH, g, pst), 0.5),
        'page_table': page_table,
        'norm1_g': 1.0 + nrm(ks[8], (D_MODEL,), 0.01),
        'w_in': nrm(ks[9], (D_MODEL, IN_COLS), D_MODEL ** -0.5),
        'ssm_a_re': -0.5 + nrm(ks[10], (g, pst), 0.01),
        'ssm_a_im': math.pi * jnp.arange(pst, dtype=f32)[None, :] + nrm(ks[11], (g, pst), 0.01),
        'ssm_log_dt': jax.random.uniform(ks[12], (g,), f32, math.log(DT_MIN), math.log(DT_MAX)),
        'ssm_b_re': nrm(ks[13], (g, pst, n), (2 * n) ** -0.5),
        'ssm_b_im': nrm(ks[14], (g, pst, n), (2 * n) ** -0.5),
        'ssm_c_re': nrm(ks[15], (g, n, pst), pst ** -0.5),
        'ssm_c_im': nrm(ks[16], (g, n, pst), pst ** -0.5),
        'ssm_d': nrm(ks[17], (D_SSM,), 1.0),
        'w_glu': nrm(ks[18], (D_SSM, 2 * D_MODEL), D_SSM ** -0.5),
        'b_glu': nrm(ks[19], (2 * D_MODEL,), 0.01),
        'w_attn_out': nrm(ks[20], (D_ATTN, D_MODEL), D_ATTN ** -0.5),
        'w_o': nrm(ks[21], (D_MODEL, D_MODEL), D_MODEL ** -0.5),
        'norm2_g': 1.0 + nrm(ks[22], (D_MODEL,), 0.01),
        'w_up': nrm(ks[23], (D_MODEL, D_FF), D_MODEL ** -0.5),
        'w_down': nrm(ks[24], (D_FF, D_MODEL), D_FF ** -0.5),
        'normf_g': 1.0 + nrm(ks[25], (D_MODEL,), 0.01),
    }


def reference(x_prompt, x_sample, cache_k, cache_v, cache_idx_k, state_ssm_re, state_ssm_im, page_table,
              norm1_g, w_in, ssm_a_re, ssm_a_im, ssm_log_dt, ssm_b_re, ssm_b_im, ssm_c_re, ssm_c_im,
              ssm_d, w_glu, b_glu, w_attn_out, w_o, norm2_g, w_up, w_down, normf_g):
    p = {'norm1_g': norm1_g, 'w_in': w_in, 'ssm_a_re': ssm_a_re, 'ssm_a_im': ssm_a_im,
         'ssm_log_dt': ssm_log_dt, 'ssm_b_re': ssm_b_re, 'ssm_b_im': ssm_b_im,
         'ssm_c_re': ssm_c_re, 'ssm_c_im': ssm_c_im, 'ssm_d': ssm_d, 'w_glu': w_glu, 'b_glu': b_glu,
         'w_attn_out': w_attn_out, 'w_o': w_o, 'norm2_g': norm2_g, 'w_up': w_up, 'w_down': w_down}
    pos_p = jnp.arange(x_prompt.shape[1], dtype=jnp.int32)
    pos_s = PAST_LEN + jnp.arange(x_sample.shape[1], dtype=jnp.int32)

    def attend_sample(q, k, v, qi, ki, wi, pos):
        return sample_sparse_attention(q, k, v, qi, ki, wi, pos, cache_k, cache_v, cache_idx_k, page_table)

    hp, hs = x_prompt, x_sample
    for _ in range(DEPTH):
        hp, k_p, v_p, ki_p, re_p, im_p = decoder_layer(hp, pos_p, prompt_sparse_attention, None, None, p)
        hs, k_s, v_s, ki_s, re_s, im_s = decoder_layer(hs, pos_s, attend_sample, state_ssm_re, state_ssm_im, p)
    y_prompt = rms_norm(hp, normf_g)
    y_sample = rms_norm(hs, normf_g)
    return (y_prompt, y_sample, k_p, v_p, ki_p, re_p, im_p, k_s, v_s, ki_s, re_s, im_s)
```

```python
import math
import contextlib
import numpy as np
import concourse.bass as bass
import concourse.mybir as mybir
from concourse.bass_utils import run_bass_kernel_spmd

F32 = mybir.dt.float32
BF16 = mybir.dt.bfloat16
I32 = mybir.dt.int32
U32 = mybir.dt.uint32
ALU = mybir.AluOpType
AF = mybir.ActivationFunctionType
AX = mybir.AxisListType

D_MODEL = 1024
D_SSM = 512
N_G = 32
P_ST = 64
N_HEADS = 8
N_KV = 2
HD = 64
D_ATTN = 512
N_IH = 8
IDX_D = 64
D_FF = 4096
IN_COLS = 3912
EPS = 1e-6
ROPE_THETA = 500000.0
NEG = -1.0e30
BIGM = -240000.0
C_U, C_Q, C_K, C_V, C_QI, C_KI, C_WI, C_GS, C_GA = 0, 512, 1024, 1152, 1280, 1792, 1856, 1864, 2888


class Sync:
    def __init__(self, nc):
        self.nc = nc
        self.eng = {"pe": nc.tensor, "dve": nc.vector, "act": nc.scalar, "pool": nc.gpsimd, "sp": nc.sync}
        self.sem = {k: nc.alloc_semaphore("sem_" + k) for k in self.eng}
        self.cnt = {k: 0 for k in self.eng}
        self.waited = {k: {} for k in self.eng}
        self.R = 12
        self.dring = {q: [nc.alloc_semaphore(f"dsem_{q}{i}") for i in range(self.R)] for q in ("sp", "pool", "act")}
        self.dn = {q: 0 for q in self.dring}
        self.lastw = {}
        self.readers = {}
        self.semobj = {}
        for k, s in self.sem.items():
            self.semobj[id(s)] = s
        for q in self.dring:
            for s in self.dring[q]:
                self.semobj[id(s)] = s
        self.out_events = []

    def _wait(self, e, ev):
        s, v = ev
        w = self.waited[e]
        if w.get(id(s), 0) >= v:
            return
        self.eng[e].wait_ge(s, v)
        w[id(s)] = v

    def _deps(self, e, reads, writes, skip_same=False):
        evs = []
        for k in reads:
            if k in self.lastw:
                evs.append(self.lastw[k])
        for k in writes:
            if k in self.lastw:
                evs.append(self.lastw[k])
            for r in self.readers.get(k, ()):
                evs.append(r)
        for ev in evs:
            if skip_same and ev[0] is self.sem[e]:
                continue
            self._wait(e, ev)

    def _record(self, ev, reads, writes):
        for k in reads:
            self.readers.setdefault(k, []).append(ev)
            if len(self.readers[k]) > 24:
                best = {}
                for s, v in self.readers[k]:
                    if id(s) not in best or best[id(s)][1] < v:
                        best[id(s)] = (s, v)
                self.readers[k] = list(best.values())
        for k in writes:
            self.lastw[k] = ev
            self.readers[k] = []

    def op(self, e, fn, reads=(), writes=(), skip_same=False):
        self._deps(e, reads, writes, skip_same)
        ins = fn()
        self.cnt[e] += 1
        ins.then_inc(self.sem[e], 1)
        ev = (self.sem[e], self.cnt[e])
        self._record(ev, reads, writes)
        return ev

    def dma(self, q, fn, reads=(), writes=(), is_output=False):
        n = self.dn[q]
        slot = n % self.R
        s = self.dring[q][slot]
        prev = 16 * (n // self.R)
        if prev > 0:
            self._wait(q, (s, prev))
        self._deps(q, reads, writes)
        ins = fn()
        ins.then_inc(s, 16)
        self.dn[q] = n + 1
        ev = (s, prev + 16)
        self._record(ev, reads, writes)
        if is_output:
            self.out_events.append(ev)
        return ev

    def barrier(self):
        evs = []
        for q in self.dring:
            n = self.dn[q]
            for slot in range(self.R):
                cnt = (n - slot + self.R - 1) // self.R if n > slot else 0
                if cnt > 0:
                    evs.append((self.dring[q][slot], 16 * cnt))
        for e in self.eng:
            if self.cnt[e] > 0:
                evs.append((self.sem[e], self.cnt[e]))
        for e in self.eng:
            for ev in evs:
                if ev[0] is self.sem[e]:
                    continue
                self._wait(e, ev)

    def finish(self):
        for q in self.dring:
            n = self.dn[q]
            for slot in range(self.R):
                cnt = (n - slot + self.R - 1) // self.R if n > slot else 0
                if cnt > 0:
                    self._wait("sp", (self.dring[q][slot], 16 * cnt))
        for e in self.eng:
            if e != "sp" and self.cnt[e] > 0:
                self._wait("sp", (self.sem[e], self.cnt[e]))


class Cfg:
    def __init__(self, seq=4096, past=8192, nb=2, ns=16, n_pool=10240):
        self.SEQ = seq
        self.PAST = past
        self.NB = nb
        self.NS = ns
        self.NPOOL = n_pool
        self.NT = seq // 128
        self.NPG = past // 128
        self.TOPK = min(256, seq // 4)
        self.TOPK_S = min(256, (past + 1) // 4)
        self.NTOK = nb * seq
        self.NTT = nb * self.NT + 1
        self.NTOKP = self.NTT * 128


def build(cfg, phases=("p1",), debug=False):
    nc = bass.Bass("TRN2", target_bir_lowering=False)
    S = Sync(nc)
    NB, SEQ, NS, NT = cfg.NB, cfg.SEQ, cfg.NS, cfg.NT
    NTT, NTOKP = cfg.NTT, cfg.NTOKP

    def din(name, shape, dt=F32):
        return nc.dram_tensor(name, list(shape), dt, kind="ExternalInput").ap()

    def dout(name, shape, dt=F32):
        return nc.dram_tensor(name, list(shape), dt, kind="ExternalOutput").ap()

    def dscr(name, shape, dt=F32):
        return nc.dram_tensor(name, list(shape), dt, kind="ExternalOutput" if debug else "Internal").ap()

    xp = din("xp", [NB * SEQ, D_MODEL])
    xs = din("xs", [NS, D_MODEL])
    cache_k = din("cache_k", [cfg.NPOOL, 128 * 128])
    cache_v = din("cache_v", [cfg.NPOOL, 128 * 128])
    cache_ik = din("cache_ik", [cfg.NPOOL, 128 * 64])
    st_re = din("st_re", [NS, N_G * P_ST])
    st_im = din("st_im", [NS, N_G * P_ST])
    ptab = din("ptab", [NS * cfg.NPG, 1], I32)
    norm1_g = din("norm1_g", [D_MODEL])
    w_in = din("w_in", [D_MODEL, IN_COLS])
    a_re = din("ssm_a_re", [N_G * P_ST])
    a_im = din("ssm_a_im", [N_G * P_ST])
    log_dt = din("ssm_log_dt", [N_G])
    b_re = din("ssm_b_re", [N_G * P_ST, 16])
    b_im = din("ssm_b_im", [N_G * P_ST, 16])
    c_re = din("ssm_c_re", [N_G * 16, P_ST])
    c_im = din("ssm_c_im", [N_G * 16, P_ST])
    ssm_d = din("ssm_d", [D_SSM])
    w_glu = din("w_glu", [D_SSM, 2 * D_MODEL])
    b_glu = din("b_glu", [2 * D_MODEL])
    w_ao = din("w_attn_out", [D_ATTN, D_MODEL])
    w_o = din("w_o", [D_MODEL, D_MODEL])
    norm2_g = din("norm2_g", [D_MODEL])
    w_up = din("w_up", [D_MODEL, D_FF])
    w_down = din("w_down", [D_FF, D_MODEL])
    normf_g = din("normf_g", [D_MODEL])

    y_p = dout("y_p", [NB * SEQ, D_MODEL])
    y_s = dout("y_s", [NS, D_MODEL])
    k_p = dout("k_p", [NB * SEQ, 128])
    v_p = dout("v_p", [NB * SEQ, 128])
    ki_p = dout("ki_p", [NB * SEQ, 64])
    re_p = dout("re_p", [NB, N_G * P_ST])
    im_p = dout("im_p", [NB, N_G * P_ST])
    k_s = dout("k_s", [NS, 128])
    v_s = dout("v_s", [NS, 128])
    ki_s = dout("ki_s", [NS, 64])
    re_s = dout("re_s", [NS, N_G * P_ST])
    im_s = dout("im_s", [NS, N_G * P_ST])

    qT_scr = dscr("qT_scr", [NTT, 65, 8, 128], BF16)
    qiT_scr = dscr("qiT_scr", [NTT, 64, 8, 128], BF16)
    kT_scr = dscr("kT_scr", [64, 2, NTOKP], BF16)
    kiT_scr = dscr("kiT_scr", [64, NTOKP], BF16)
    vb_scr = dscr("vb_scr", [NTOKP, 2, 64], BF16)
    uT_scr = dscr("uT_scr", [D_SSM, NTOKP], F32)
    gate_scr = dscr("gate_scr", [NTOKP, 2048], F32)
    gyT_scr = dscr("gyT_scr", [D_SSM, NTOKP], BF16)
    ao_scr = dscr("ao_scr", [NTOKP, D_ATTN], BF16)
    x2_scr = dscr("x2_scr", [NTOKP, D_MODEL], F32)

    if debug:
        dbg_mb = dscr("dbg_mb", [128, SEQ], BF16)
        dbg_sc = dscr("dbg_sc", [128, SEQ], F32)
        dbg_thr = dscr("dbg_thr", [128, 1], F32)
    ident = nc.alloc_sbuf_tensor("ident", [128, 128], BF16)
    identf = nc.alloc_sbuf_tensor("identf", [128, 128], F32)
    cmask = nc.alloc_sbuf_tensor("cmask", [128, 128], F32)
    eps_t = nc.alloc_sbuf_tensor("eps_t", [128, 1], F32)
    cosT = nc.alloc_sbuf_tensor("cosT", [128, NT + 1, 8], F32)
    sinT = nc.alloc_sbuf_tensor("sinT", [128, NT + 1, 8], F32)
    lohi = nc.alloc_sbuf_tensor("lohi", [128, NTT, 16], F32)
    kn2 = nc.alloc_sbuf_tensor("kn2", [128, NTT, 2], F32)

    def V(fn, r=(), w=(), **kw):
        return S.op("dve", fn, r, w, **kw)

    def A(fn, r=(), w=(), **kw):
        return S.op("act", fn, r, w, **kw)

    def G(fn, r=(), w=(), **kw):
        return S.op("pool", fn, r, w, **kw)

    def PE(fn, r=(), w=(), **kw):
        return S.op("pe", fn, r, w, **kw)

    def DMA(out, in_, r=(), w=(), q="sp", is_output=False, **kw):
        return S.dma(q, lambda: S.eng[q].dma_start(out=out, in_=in_, **kw), r, w, is_output=is_output)

    ones_f = nc.alloc_sbuf_tensor("ones_f", [128, 128], F32)
    G(lambda: nc.gpsimd.memset(ones_f[:], 1.0), w=["ones_f"])
    G(lambda: nc.gpsimd.memset(eps_t[:], EPS), w=["eps_t"])
    G(lambda: nc.gpsimd.affine_select(out=identf[:], in_=ones_f[:], pattern=[[-1, 128]], compare_op=ALU.is_equal,
                                      fill=0.0, base=0, channel_multiplier=1), r=["ones_f"], w=["identf"])
    V(lambda: nc.vector.tensor_copy(out=ident[:], in_=identf[:]), r=["identf"], w=["ident"])
    zer_f = nc.alloc_sbuf_tensor("zer_f", [128, 128], F32)
    G(lambda: nc.gpsimd.memset(zer_f[:], 0.0), w=["zer_f"])
    G(lambda: nc.gpsimd.affine_select(out=cmask[:], in_=zer_f[:], pattern=[[-1, 128]], compare_op=ALU.is_ge,
                                      fill=NEG, base=0, channel_multiplier=1), r=["zer_f"], w=["cmask"])

    with contextlib.ExitStack() as es:
        posi = es.enter_context(nc.sbuf_tensor("posi", [128, NT + 1], I32))
        posf = es.enter_context(nc.sbuf_tensor("posf", [128, NT + 1], F32))
        ang = es.enter_context(nc.sbuf_tensor("ang", [128, NT + 1, 8], F32))
        ki_ = es.enter_context(nc.sbuf_tensor("kint", [128, NT + 1, 8], I32))
        kf = es.enter_context(nc.sbuf_tensor("kf", [128, NT + 1, 8], F32))
        rr = es.enter_context(nc.sbuf_tensor("rr", [128, NT + 1, 8], F32))
        r2 = es.enter_context(nc.sbuf_tensor("r2", [128, NT + 1, 8], F32))
        mk = es.enter_context(nc.sbuf_tensor("mk", [128, NT + 1, 8], F32))
        G(lambda: nc.gpsimd.iota(posi[:, 0:NT], pattern=[[128, NT]], base=0, channel_multiplier=1), w=["posi"])
        G(lambda: nc.gpsimd.iota(posi[:, NT:NT + 1], pattern=[[0, 1]], base=cfg.PAST, channel_multiplier=0), w=["posi"])
        V(lambda: nc.vector.tensor_copy(out=posf[:], in_=posi[:]), r=["posi"], w=["posf"])
        for j in range(8):
            inv = ROPE_THETA ** (-j / 8.0)
            V(lambda j=j, inv=inv: nc.vector.tensor_scalar(out=ang[:, :, j], in0=posf[:], scalar1=float(np.float32(inv)),
                                                           scalar2=float(1.0 / (2 * math.pi)), op0=ALU.mult, op1=ALU.mult),
              r=["posf"], w=["ang"])

        def wrap_sin(dst, off):
            V(lambda: nc.vector.tensor_scalar(out=rr[:], in0=ang[:], scalar1=float(off), scalar2=None, op0=ALU.add),
              r=["ang"], w=["rr"])
            V(lambda: nc.vector.tensor_copy(out=ki_[:], in_=rr[:]), r=["rr"], w=["kint"])
            V(lambda: nc.vector.tensor_copy(out=kf[:], in_=ki_[:]), r=["kint"], w=["kf"])
            V(lambda: nc.vector.tensor_tensor(out=r2[:], in0=rr[:], in1=kf[:], op=ALU.subtract), r=["rr", "kf"], w=["r2"])
            V(lambda: nc.vector.tensor_scalar(out=mk[:], in0=r2[:], scalar1=0.5, scalar2=None, op0=ALU.is_gt), r=["r2"], w=["mk"])
            V(lambda: nc.vector.tensor_tensor(out=r2[:], in0=r2[:], in1=mk[:], op=ALU.subtract), r=["r2", "mk"], w=["r2"])
            V(lambda: nc.vector.tensor_scalar(out=mk[:], in0=r2[:], scalar1=-0.5, scalar2=None, op0=ALU.is_lt), r=["r2"], w=["mk"])
            V(lambda: nc.vector.tensor_tensor(out=r2[:], in0=r2[:], in1=mk[:], op=ALU.add), r=["r2", "mk"], w=["r2"])
            A(lambda: nc.scalar.activation(out=dst[:], in_=r2[:], func=AF.Sin, scale=float(2 * math.pi)), r=["r2"], w=[dst.name])

        wrap_sin(sinT, 0.0)
        wrap_sin(cosT, 0.25)
        S.barrier()

    es_all = contextlib.ExitStack()

    def sb(name, shape, dt=F32, stack=None):
        return (stack or es_all).enter_context(nc.sbuf_tensor(name, list(shape), dt))

    def ps(name, shape, dt=F32, stack=None):
        return (stack or es_all).enter_context(nc.psum_tensor(name, list(shape), dt))

    def load_weight_bf16(dst, src, K, N, stack, gcol=None, nm="w"):
        KC = K // 128
        CH = 2048
        stg = [sb(f"stg_{nm}{i}", [128, CH], F32, stack) for i in range(2)]
        i = 0
        for kc in range(KC):
            for c0 in range(0, N, CH):
                cw = min(CH, N - c0)
                st = stg[i % 2]
                DMA(st[:, 0:cw], src[kc * 128:(kc + 1) * 128, c0:c0 + cw], w=[st.name])
                eng = ("dve", "pool")[i % 2]
                if gcol is None:
                    if eng == "dve":
                        V(lambda st=st, kc=kc, c0=c0, cw=cw: nc.vector.tensor_copy(out=dst[:, kc, c0:c0 + cw], in_=st[:, 0:cw]),
                          r=[st.name], w=[dst.name])
                    else:
                        G(lambda st=st, kc=kc, c0=c0, cw=cw: nc.gpsimd.tensor_copy(out=dst[:, kc, c0:c0 + cw], in_=st[:, 0:cw]),
                          r=[st.name], w=[dst.name])
                else:
                    V(lambda st=st, kc=kc, c0=c0, cw=cw: nc.vector.tensor_scalar(out=dst[:, kc, c0:c0 + cw], in0=st[:, 0:cw],
                                                                              scalar1=gcol[:, kc:kc + 1], scalar2=None, op0=ALU.mult),
                      r=[st.name, gcol.name], w=[dst.name])
                i += 1

    def tile_rows(T):
        if T < NB * NT:
            return T * 128, 128, False, T % NT
        return 0, NS, True, NT

    if "p1" in phases:
        with contextlib.ExitStack() as e1:
            Win = sb("Win", [128, 8, IN_COLS], BF16, e1)
            g1c = sb("g1c", [128, 8], F32, e1)
            with nc.allow_non_contiguous_dma(reason="tiny gain vector"):
                DMA(g1c[:], norm1_g.rearrange("(k p) -> p k", p=128), w=["g1c"])
            with contextlib.ExitStack() as e1w:
                load_weight_bf16(Win, w_in, D_MODEL, IN_COLS, e1w, gcol=g1c, nm="win")
                S.barrier()
            xt = [sb(f"xt{i}", [128, D_MODEL], F32, e1) for i in range(2)]
            junk = sb("junk1", [128, D_MODEL], F32, e1)
            ss = sb("ss", [128, 1], F32, e1)
            rstd = sb("rstd", [128, 1], F32, e1)
            hb = sb("hb", [128, D_MODEL], BF16, e1)
            hT = sb("hT", [128, 8, 128], BF16, e1)
            pt = [sb(f"pt{i}", [128, IN_COLS - 512], F32, e1) for i in range(2)]
            uTs = sb("uTs", [128, 4, 128], F32, e1)
            tA = [sb(f"tA{i}", [128, 10, 8], F32, e1) for i in range(6)]
            qn = sb("qn", [128, 8], F32, e1)
            sq = sb("sq", [128, 10, 64], F32, e1)
            wp = sb("wp", [128, 8], F32, e1)
            qa = sb("qa", [128, 8, 65], BF16, e1)
            kb_ = sb("kb_", [128, 2, 64], BF16, e1)
            qib = sb("qib", [128, 8, 64], BF16, e1)
            kib = sb("kib", [128, 64], BF16, e1)
            vbt = sb("vbt", [128, 2, 64], BF16, e1)
            gts = sb("gts", [128, 2048], F32, e1)
            trs = sb("trs", [65, 19, 128], BF16, e1)
            p_tr = ps("p_tr", [128, 8, 128], BF16, e1)
            p_mm = [ps(f"p_mm{i}", [128, 512], F32, e1) for i in range(3)]
            p_u = ps("p_u", [128, 4, 128], F32, e1)
            p_t2 = ps("p_t2", [65, 19, 128], BF16, e1)

            for T in range(NTT):
                r0, nr, is_s, pti = tile_rows(T)
                x_ = xt[T % 2]
                p_ = pt[T % 2]
                xn, pn = x_.name, p_.name
                if is_s:
                    V(lambda x_=x_: nc.vector.memset(x_[:], 0.0), w=[xn])
                    DMA(x_[0:NS, :], xs[:, :], w=[xn])
                else:
                    DMA(x_[:], xp[r0:r0 + 128, :], w=[xn])
                A(lambda x_=x_: nc.scalar.activation(out=junk[:], in_=x_[:], func=AF.Square, accum_out=ss[:]),
                  r=[xn], w=["junk1", "ss"])
                A(lambda: nc.scalar.activation(out=rstd[:], in_=ss[:], func=AF.Sqrt, bias=eps_t[:], scale=1.0 / D_MODEL),
                  r=["ss", "eps_t"], w=["rstd"])
                V(lambda: nc.vector.reciprocal(out=rstd[:], in_=rstd[:]), r=["rstd"], w=["rstd"])
                V(lambda x_=x_: nc.vector.tensor_scalar(out=hb[:], in0=x_[:], scalar1=rstd[:, 0:1], scalar2=None, op0=ALU.mult),
                  r=[xn, "rstd"], w=["hb"])
                for kc in range(8):
                    PE(lambda kc=kc: nc.tensor.transpose(out=p_tr[:, kc, :], in_=hb[:, kc * 128:(kc + 1) * 128], identity=ident[:]),
                       r=["hb", "ident"], w=["p_tr"], skip_same=True)
                A(lambda: nc.scalar.copy(out=hT[:], in_=p_tr[:]), r=["p_tr"], w=["hT"])
                for ct in range(4):
                    for kc in range(8):
                        PE(lambda ct=ct, kc=kc: nc.tensor.matmul(p_u[:, ct, :], lhsT=Win[:, kc, ct * 128:(ct + 1) * 128],
                                                                 rhs=hT[:, kc, :], start=(kc == 0), stop=(kc == 7)),
                           r=["Win", "hT"], w=["p_u"], skip_same=True)
                V(lambda: nc.vector.tensor_copy(out=uTs[:], in_=p_u[:]), r=["p_u"], w=["uTs"])
                DMA(uT_scr.rearrange("(c p) t -> p c t", p=128)[:, :, T * 128:(T + 1) * 128], uTs[:], r=["uTs"], w=[("uT", T)])
                ci = 0
                for c0 in range(512, IN_COLS, 512):
                    cw = min(512, IN_COLS - c0)
                    pm = p_mm[ci % 3]
                    for kc in range(8):
                        PE(lambda pm=pm, kc=kc, c0=c0, cw=cw: nc.tensor.matmul(pm[:, 0:cw], lhsT=hT[:, kc, :], rhs=Win[:, kc, c0:c0 + cw],
                                                                               start=(kc == 0), stop=(kc == 7)),
                           r=["Win", "hT"], w=[pm.name], skip_same=True)
                    if ci % 2 == 0:
                        A(lambda pm=pm, c0=c0, cw=cw, p_=p_: nc.scalar.copy(out=p_[:, c0 - 512:c0 - 512 + cw], in_=pm[:, 0:cw]),
                          r=[pm.name], w=[pn])
                    else:
                        V(lambda pm=pm, c0=c0, cw=cw, p_=p_: nc.vector.tensor_copy(out=p_[:, c0 - 512:c0 - 512 + cw], in_=pm[:, 0:cw]),
                          r=[pm.name], w=[pn])
                    ci += 1
                cs_c = cosT[:, pti, :]
                cs_s = sinT[:, pti, :]
                for (cb, H) in ((C_Q - 512, 10), (C_QI - 512, 9)):
                    X = p_[:, cb:cb + H * 64].rearrange("p (h d) -> p h d", d=64)
                    x1 = X[:, :, 0:8]
                    x2 = X[:, :, 8:16]
                    cB = cs_c.unsqueeze(1).to_broadcast([128, H, 8])
                    sB = cs_s.unsqueeze(1).to_broadcast([128, H, 8])
                    t = [tt[:, 0:H, :] for tt in tA]
                    V(lambda x1=x1, cB=cB, t=t: nc.vector.tensor_tensor(out=t[0], in0=x1, in1=cB, op=ALU.mult), r=[pn, "cosT"], w=["tA0"])
                    V(lambda x2=x2, sB=sB, t=t: nc.vector.tensor_tensor(out=t[1], in0=x2, in1=sB, op=ALU.mult), r=[pn, "sinT"], w=["tA1"])
                    V(lambda x1=x1, sB=sB, t=t: nc.vector.tensor_tensor(out=t[2], in0=x1, in1=sB, op=ALU.mult), r=[pn, "sinT"], w=["tA2"])
                    V(lambda x2=x2, cB=cB, t=t: nc.vector.tensor_tensor(out=t[3], in0=x2, in1=cB, op=ALU.mult), r=[pn, "cosT"], w=["tA3"])
                    V(lambda x1=x1, t=t: nc.vector.tensor_tensor(out=x1, in0=t[0], in1=t[1], op=ALU.subtract), r=["tA0", "tA1"], w=[pn])
                    V(lambda x2=x2, t=t: nc.vector.tensor_tensor(out=x2, in0=t[2], in1=t[3], op=ALU.add), r=["tA2", "tA3"], w=[pn])
                kcol, vcol, kicol = C_K - 512, C_V - 512, C_KI - 512
                if is_s:
                    DMA(k_s[:, :], p_[0:NS, kcol:kcol + 128], r=[pn], q="pool", is_output=True)
                    DMA(v_s[:, :], p_[0:NS, vcol:vcol + 128], r=[pn], q="pool", is_output=True)
                    DMA(ki_s[:, :], p_[0:NS, kicol:kicol + 64], r=[pn], q="pool", is_output=True)
                else:
                    DMA(k_p[r0:r0 + 128, :], p_[:, kcol:kcol + 128], r=[pn], q="pool", is_output=True)
                    DMA(v_p[r0:r0 + 128, :], p_[:, vcol:vcol + 128], r=[pn], q="pool", is_output=True)
                    DMA(ki_p[r0:r0 + 128, :], p_[:, kicol:kicol + 64], r=[pn], q="pool", is_output=True)
                QK = p_[:, C_Q - 512:C_Q - 512 + 640].rearrange("p (h d) -> p h d", d=64)
                G(lambda QK=QK: nc.gpsimd.tensor_tensor(out=sq[:], in0=QK, in1=QK, op=ALU.mult), r=[pn], w=["sq"])
                V(lambda: nc.vector.tensor_reduce(out=qn[:], in_=sq[:, 0:8, :], axis=AX.X, op=ALU.add), r=["sq"], w=["qn"])
                V(lambda T=T: nc.vector.tensor_reduce(out=kn2[:, T, :], in_=sq[:, 8:10, :], axis=AX.X, op=ALU.add), r=["sq"], w=["kn2"])
                A(lambda: nc.scalar.activation(out=qn[:], in_=qn[:], func=AF.Sqrt), r=["qn"], w=["qn"])
                V(lambda: nc.vector.tensor_scalar(out=qa[:, :, 64], in0=qn[:], scalar1=-1.0, scalar2=None, op0=ALU.mult),
                  r=["qn"], w=["qa"])
                V(lambda QK=QK: nc.vector.tensor_copy(out=qa[:, :, 0:64], in_=QK[:, 0:8, :]), r=[pn], w=["qa"])
                G(lambda QK=QK: nc.gpsimd.tensor_copy(out=kb_[:], in_=QK[:, 8:10, :]), r=[pn], w=["kb_"])
                wic = C_WI - 512
                V(lambda p_=p_, wic=wic: nc.vector.tensor_scalar(out=wp[:], in0=p_[:, wic:wic + 8], scalar1=float(8 ** -0.5 * 0.125),
                                                                 scalar2=None, op0=ALU.mult), r=[pn], w=["wp"])
                V(lambda T=T: nc.vector.tensor_scalar(out=lohi[:, T, 0:8], in0=wp[:], scalar1=0.0, scalar2=-3.0e38,
                                                      op0=ALU.is_le, op1=ALU.mult), r=["wp"], w=["lohi"])
                V(lambda T=T: nc.vector.tensor_scalar(out=lohi[:, T, 8:16], in0=wp[:], scalar1=0.0, scalar2=3.0e38,
                                                      op0=ALU.is_gt, op1=ALU.mult), r=["wp"], w=["lohi"])
                QI = p_[:, C_QI - 512:C_QI - 512 + 512].rearrange("p (h d) -> p h d", d=64)
                V(lambda QI=QI: nc.vector.tensor_tensor(out=qib[:], in0=QI, in1=wp[:].unsqueeze(2).to_broadcast([128, 8, 64]), op=ALU.mult),
                  r=[pn, "wp"], w=["qib"])
                G(lambda p_=p_: nc.gpsimd.tensor_copy(out=kib[:], in_=p_[:, C_KI - 512:C_KI - 512 + 64]), r=[pn], w=["kib"])
                G(lambda p_=p_: nc.gpsimd.tensor_copy(out=vbt[:], in_=p_[:, vcol:vcol + 128].rearrange("p (g d) -> p g d", d=64)),
                  r=[pn], w=["vbt"])
                DMA(vb_scr[T * 128:(T + 1) * 128, :, :], vbt[:], r=["vbt"], w=[("vb", T)])
                A(lambda p_=p_: nc.scalar.activation(out=gts[:], in_=p_[:, C_GS - 512:C_GS - 512 + 2048], func=AF.Sigmoid),
                  r=[pn], w=["gts"])
                DMA(gate_scr[T * 128:(T + 1) * 128, :], gts[:], r=["gts"], w=[("gate", T)])
                for h in range(8):
                    PE(lambda h=h: nc.tensor.transpose(out=p_t2[0:65, h, :], in_=qa[:, h, :], identity=ident[:]),
                       r=["qa", "ident"], w=["p_t2"], skip_same=True)
                for g in range(2):
                    PE(lambda g=g: nc.tensor.transpose(out=p_t2[0:64, 8 + g, :], in_=kb_[:, g, :], identity=ident[:]),
                       r=["kb_", "ident"], w=["p_t2"], skip_same=True)
                for h in range(8):
                    PE(lambda h=h: nc.tensor.transpose(out=p_t2[0:64, 10 + h, :], in_=qib[:, h, :], identity=ident[:]),
                       r=["qib", "ident"], w=["p_t2"], skip_same=True)
                PE(lambda: nc.tensor.transpose(out=p_t2[0:64, 18, :], in_=kib[:], identity=ident[:]),
                   r=["kib", "ident"], w=["p_t2"], skip_same=True)
                V(lambda: nc.vector.tensor_copy(out=trs[0:65, 0:8, :], in_=p_t2[0:65, 0:8, :]), r=["p_t2"], w=["trs"])
                A(lambda: nc.scalar.copy(out=trs[0:64, 8:19, :], in_=p_t2[0:64, 8:19, :]), r=["p_t2"], w=["trs"])
                DMA(qT_scr[T, :, :, :], trs[0:65, 0:8, :], r=["trs"], w=[("qT", T)])
                DMA(kT_scr[:, :, T * 128:(T + 1) * 128], trs[0:64, 8:10, :], r=["trs"], w=[("kT", T)])
                DMA(qiT_scr[T, :, :, :], trs[0:64, 10:18, :], r=["trs"], w=[("qiT", T)])
                DMA(kiT_scr[:, T * 128:(T + 1) * 128], trs[0:64, 18, :], r=["trs"], w=[("kiT", T)])
            S.barrier()


    if "p2" in phases:
        with contextlib.ExitStack() as e2:
            NLEV = int(math.log2(SEQ))
            assert (1 << NLEV) == SEQ
            NCH = SEQ // 512
            sm = {}

            def small(name, shape=(128, 16), dt=F32):
                t_ = sb("s2_" + name, list(shape), dt, e2)
                sm[name] = t_
                return t_

            def vtt(o, a, b, op):
                V(lambda: nc.vector.tensor_tensor(out=o[:], in0=a[:], in1=b[:], op=op), r=[a.name, b.name], w=[o.name])

            def vts(o, a, s1, op, s2=None, op1=None):
                if op1 is None:
                    V(lambda: nc.vector.tensor_scalar(out=o[:], in0=a[:], scalar1=s1, scalar2=None, op0=op), r=[a.name], w=[o.name])
                else:
                    V(lambda: nc.vector.tensor_scalar(out=o[:], in0=a[:], scalar1=s1, scalar2=s2, op0=op, op1=op1), r=[a.name], w=[o.name])

            are_t = small("are"); aim_t = small("aim"); ldt = small("ldt"); dtt = small("dt")
            xre = small("xre"); th = small("th"); lam_abs = small("lam_abs")
            cs_ = small("cs"); sn_ = small("sn"); lam_re = small("lam_re"); lam_im = small("lam_im")
            tq = [small(f"tq{i}") for i in range(6)]
            tqi = small("tqi", dt=I32)
            cf_re = small("cf_re"); cf_im = small("cf_im")
            with nc.allow_non_contiguous_dma(reason="tiny ssm params"):
                DMA(are_t[:], a_re.rearrange("(ft f) -> f ft", f=128), w=[are_t.name])
                DMA(aim_t[:], a_im.rearrange("(ft f) -> f ft", f=128), w=[aim_t.name])
                lv = log_dt.rearrange("(ft two) -> two ft", two=2)
                DMA(ldt[0:64, :], lv[0:1, :].partition_broadcast(64), w=[ldt.name])
                DMA(ldt[64:128, :], lv[1:2, :].partition_broadcast(64), w=[ldt.name])
            A(lambda: nc.scalar.activation(out=dtt[:], in_=ldt[:], func=AF.Exp), r=[ldt.name], w=[dtt.name])
            vtt(xre, are_t, dtt, ALU.mult)
            vtt(th, aim_t, dtt, ALU.mult)
            A(lambda: nc.scalar.activation(out=lam_abs[:], in_=xre[:], func=AF.Exp), r=[xre.name], w=[lam_abs.name])

            def sincos_turns(dst, src_turns, off):
                a_, k_, r_, m_ = tq[0], tq[1], tq[2], tq[3]
                vts(a_, src_turns, float(off), ALU.add)
                V(lambda: nc.vector.tensor_copy(out=tqi[:], in_=a_[:]), r=[a_.name], w=[tqi.name])
                V(lambda: nc.vector.tensor_copy(out=k_[:], in_=tqi[:]), r=[tqi.name], w=[k_.name])
                vtt(r_, a_, k_, ALU.subtract)
                vts(m_, r_, 0.5, ALU.is_gt)
                vtt(r_, r_, m_, ALU.subtract)
                vts(m_, r_, -0.5, ALU.is_lt)
                vtt(r_, r_, m_, ALU.add)
                A(lambda: nc.scalar.activation(out=dst[:], in_=r_[:], func=AF.Sin, scale=float(2 * math.pi)), r=[r_.name], w=[dst.name])

            thn = small("thn")
            vts(thn, th, float(1.0 / (2 * math.pi)), ALU.mult)
            sincos_turns(sn_, thn, 0.0)
            sincos_turns(cs_, thn, 0.25)
            vtt(lam_re, lam_abs, cs_, ALU.mult)
            vtt(lam_im, lam_abs, sn_, ALU.mult)
            nr, den, t1_, t2_ = tq[0], tq[1], tq[2], tq[3]
            vts(nr, lam_re, -1.0, ALU.add)
            vtt(t1_, are_t, are_t, ALU.mult)
            vtt(t2_, aim_t, aim_t, ALU.mult)
            vtt(den, t1_, t2_, ALU.add)
            V(lambda: nc.vector.reciprocal(out=den[:], in_=den[:]), r=[den.name], w=[den.name])
            vtt(t1_, nr, are_t, ALU.mult)
            vtt(t2_, lam_im, aim_t, ALU.mult)
            vtt(t1_, t1_, t2_, ALU.add)
            vtt(cf_re, t1_, den, ALU.mult)
            vtt(t1_, lam_im, are_t, ALU.mult)
            vtt(t2_, nr, aim_t, ALU.mult)
            vtt(t1_, t1_, t2_, ALU.subtract)
            vtt(cf_im, t1_, den, ALU.mult)
            wre = small("wre", (128, 16, NLEV)); wim = small("wim", (128, 16, NLEV))
            V(lambda: nc.vector.tensor_copy(out=wre[:, :, 0], in_=cs_[:]), r=[cs_.name], w=[wre.name])
            V(lambda: nc.vector.tensor_copy(out=wim[:, :, 0], in_=sn_[:]), r=[sn_.name], w=[wim.name])
            for k in range(1, NLEV):
                V(lambda k=k: nc.vector.tensor_tensor(out=tq[0][:], in0=wre[:, :, k - 1], in1=wre[:, :, k - 1], op=ALU.mult), r=[wre.name], w=[tq[0].name])
                V(lambda k=k: nc.vector.tensor_tensor(out=tq[1][:], in0=wim[:, :, k - 1], in1=wim[:, :, k - 1], op=ALU.mult), r=[wim.name], w=[tq[1].name])
                V(lambda k=k: nc.vector.tensor_tensor(out=wre[:, :, k], in0=tq[0][:], in1=tq[1][:], op=ALU.subtract), r=[tq[0].name, tq[1].name], w=[wre.name])
                V(lambda k=k: nc.vector.tensor_tensor(out=tq[2][:], in0=wre[:, :, k - 1], in1=wim[:, :, k - 1], op=ALU.mult), r=[wre.name, wim.name], w=[tq[2].name])
                V(lambda k=k: nc.vector.tensor_scalar(out=wim[:, :, k], in0=tq[2][:], scalar1=2.0, scalar2=None, op0=ALU.mult), r=[tq[2].name], w=[wim.name])
            Bre = small("Bre", (128, 16, 16)); Bim = small("Bim", (128, 16, 16))
            bbr = small("bbr", (128, 16, 16)); bbi = small("bbi", (128, 16, 16)); btmp = small("btmp", (128, 16, 16))
            DMA(Bre[:], b_re.rearrange("(ft f) m -> f ft m", f=128), w=[Bre.name])
            DMA(Bim[:], b_im.rearrange("(ft f) m -> f ft m", f=128), w=[Bim.name])
            cfr_b = cf_re[:].unsqueeze(2).to_broadcast([128, 16, 16])
            cfi_b = cf_im[:].unsqueeze(2).to_broadcast([128, 16, 16])
            V(lambda: nc.vector.tensor_tensor(out=bbr[:], in0=Bre[:], in1=cfr_b, op=ALU.mult), r=[Bre.name, cf_re.name], w=[bbr.name])
            V(lambda: nc.vector.tensor_tensor(out=btmp[:], in0=Bim[:], in1=cfi_b, op=ALU.mult), r=[Bim.name, cf_im.name], w=[btmp.name])
            vtt(bbr, bbr, btmp, ALU.subtract)
            V(lambda: nc.vector.tensor_tensor(out=bbi[:], in0=Bim[:], in1=cfr_b, op=ALU.mult), r=[Bim.name, cf_re.name], w=[bbi.name])
            V(lambda: nc.vector.tensor_tensor(out=btmp[:], in0=Bre[:], in1=cfi_b, op=ALU.mult), r=[Bre.name, cf_im.name], w=[btmp.name])
            vtt(bbi, bbi, btmp, ALU.add)
            BT = small("BT", (128, 16, 2, 128), BF16)
            CT = small("CT", (128, 16, 2, 128), BF16)
            V(lambda: nc.vector.memset(CT[:], 0.0), w=[CT.name])
            Xb = small("Xb", (128, 128))
            p_s = ps("p2_s", [128, 512], F32, e2)
            for ft in range(16):
                ct, q = ft // 4, ft % 4
                for ri, bb in enumerate((bbr, bbi)):
                    V(lambda: nc.vector.memset(Xb[:], 0.0), w=[Xb.name])
                    V(lambda bb=bb, ft=ft, q=q: nc.vector.tensor_copy(out=Xb[0:64, 32 * q:32 * q + 16], in_=bb[0:64, ft, :]), r=[bb.name], w=[Xb.name])
                    V(lambda bb=bb, ft=ft, q=q: nc.vector.tensor_copy(out=Xb[64:128, 32 * q + 16:32 * q + 32], in_=bb[64:128, ft, :]), r=[bb.name], w=[Xb.name])
                    PE(lambda: nc.tensor.transpose(out=p_s[:, 0:128], in_=Xb[:], identity=identf[:]), r=[Xb.name, "identf"], w=[p_s.name])
                    V(lambda ft=ft, ri=ri: nc.vector.tensor_copy(out=BT[:, ft, ri, :], in_=p_s[:, 0:128]),
                      r=[p_s.name], w=[BT.name])
            Cre = small("Cre", (32, 16, 64)); Cim = small("Cim", (32, 16, 64))
            mA = small("mA", (32, 1)); mB = small("mB", (32, 1)); Xc = small("Xc", (32, 128))
            DMA(Cre[:], c_re.rearrange("(ft r) p -> r ft p", r=32), w=[Cre.name])
            DMA(Cim[:], c_im.rearrange("(ft r) p -> r ft p", r=32), w=[Cim.name])
            V(lambda: nc.vector.memset(mA[:], 0.0), w=[mA.name])
            V(lambda: nc.vector.memset(mA[0:16, :], 1.0), w=[mA.name])
            V(lambda: nc.vector.tensor_scalar(out=mB[:], in0=mA[:], scalar1=-1.0, scalar2=1.0, op0=ALU.mult, op1=ALU.add), r=[mA.name], w=[mB.name])
            for ft in range(16):
                for ri, cc in enumerate((Cre, Cim)):
                    sgn = 1.0 if ri == 0 else -1.0
                    V(lambda cc=cc, ft=ft, sgn=sgn: nc.vector.tensor_scalar(out=Xc[:, 0:64], in0=cc[:, ft, :], scalar1=mA[:, 0:1], scalar2=sgn,
                                                                          op0=ALU.mult, op1=ALU.mult), r=[cc.name, mA.name], w=[Xc.name])
                    V(lambda cc=cc, ft=ft, sgn=sgn: nc.vector.tensor_scalar(out=Xc[:, 64:128], in0=cc[:, ft, :], scalar1=mB[:, 0:1], scalar2=sgn,
                                                                          op0=ALU.mult, op1=ALU.mult), r=[cc.name, mB.name], w=[Xc.name])
                    PE(lambda: nc.tensor.transpose(out=p_s[:, 0:32], in_=Xc[:], identity=identf[0:32, 0:32]), r=[Xc.name, "identf"], w=[p_s.name])
                    V(lambda ft=ft, ri=ri: nc.vector.tensor_copy(out=CT[:, ft, ri, 32 * (ft % 4):32 * (ft % 4) + 32], in_=p_s[:, 0:32]), r=[p_s.name], w=[CT.name])
            dcol = small("dcol", (128, 4))
            with nc.allow_non_contiguous_dma(reason="tiny"):
                DMA(dcol[:], ssm_d.rearrange("(c p) -> p c", p=128), w=[dcol.name])
            h0r = small("h0r", (128, 16, NS)); h0i = small("h0i", (128, 16, NS))
            h1r = small("h1r", (128, 16, NS)); h1i = small("h1i", (128, 16, NS))
            sst = small("sst", (NS, 2048))
            for (src, dst) in ((st_re, h0r), (st_im, h0i)):
                DMA(sst[:], src[:, :], w=[sst.name])
                for ft in range(16):
                    PE(lambda ft=ft: nc.tensor.transpose(out=p_s[:, ft * NS:(ft + 1) * NS], in_=sst[:, ft * 128:(ft + 1) * 128],
                                                         identity=identf[0:NS, 0:NS]), r=[sst.name, "identf"], w=[p_s.name])
                V(lambda dst=dst: nc.vector.tensor_copy(out=dst[:].rearrange("p a b -> p (a b)"), in_=p_s[:, 0:16 * NS]), r=[p_s.name], w=[dst.name])

            WTOK = SEQ
            ufst = [sb(f"ufst{i}", [128, 512], F32, e2) for i in range(2)]
            ub = sb("ub", [128, NB, SEQ], BF16, e2)
            ufs = sb("ufs", [128, NS], F32, e2)
            ubs = sb("ubs", [128, NS], BF16, e2)
            Ec = sb("Ec", [128, SEQ], F32, e2)
            Es = sb("Es", [128, SEQ], F32, e2)
            rdec = sb("rdec", [128, 512], F32, e2)
            yT = sb("yT", [128, NB, SEQ], F32, e2)
            yTs = sb("yTs", [128, NS], F32, e2)
            fin = sb("fin", [128, 16, NB, 2], F32, e2)
            tm = [sb(f"tm{i}", [128, 512], F32, e2) for i in range(4)]
            gre = [sb(f"gre{i}", [128, 512], F32, e2) for i in range(2)]
            gim = [sb(f"gim{i}", [128, 512], F32, e2) for i in range(2)]
            Gre = [sb(f"Gre{i}", [128, 512], F32, e2) for i in range(2)]
            Gim = [sb(f"Gim{i}", [128, 512], F32, e2) for i in range(2)]
            dm = [sb(f"dm{i}", [128, 512], F32, e2) for i in range(4)]
            hre = [sb(f"hre{i}", [128, 512], BF16, e2) for i in range(2)]
            him = [sb(f"him{i}", [128, 512], BF16, e2) for i in range(2)]
            gl = [sb(f"gl{i}", [128, 512], F32, e2) for i in range(3)]
            gyb = sb("gyb", [128, 512], BF16, e2)
            p_br = [ps(f"p_br{i}", [128, 512], F32, e2) for i in range(2)]
            p_bi = [ps(f"p_bi{i}", [128, 512], F32, e2) for i in range(2)]
            p_y = [ps(f"p_y{i}", [128, 512], F32, e2) for i in range(2)]
            samp0 = (NTT - 1) * 128
            it = 0
            for ct in range(4):
                for b in range(NB):
                    for c in range(NCH):
                        us_ = ufst[(b * NCH + c) % 2]
                        DMA(us_[:], uT_scr[ct * 128:(ct + 1) * 128, b * SEQ + c * 512: b * SEQ + (c + 1) * 512],
                            r=[("uT", (b * SEQ + c * 512) // 128 + i_) for i_ in range(4)], w=[us_.name])
                        G(lambda b=b, c=c, us_=us_: nc.gpsimd.tensor_copy(out=ub[:, b, c * 512:(c + 1) * 512], in_=us_[:]), r=[us_.name], w=["ub"])
                DMA(ufs[:], uT_scr[ct * 128:(ct + 1) * 128, samp0:samp0 + NS], r=[("uT", NTT - 1)], w=["ufs"])
                G(lambda: nc.gpsimd.tensor_copy(out=ubs[:], in_=ufs[:]), r=["ufs"], w=["ubs"])
                for q in range(4):
                    ft = ct * 4 + q
                    qs = slice(32 * q, 32 * q + 32)
                    V(lambda: nc.vector.memset(Ec[:, 0:1], 1.0), w=["Ec"])
                    V(lambda: nc.vector.memset(Es[:, 0:1], 0.0), w=["Es"])
                    for k in range(NLEV):
                        n = 1 << k
                        wr = wre[:, ft, k:k + 1]
                        wi = wim[:, ft, k:k + 1]
                        for n0 in range(0, n, 512):
                            nn = min(512, n - n0)
                            lo_ = slice(n0, n0 + nn)
                            hi_ = slice(n + n0, n + n0 + nn)
                            V(lambda wi=wi, lo_=lo_, nn=nn: nc.vector.tensor_scalar(out=tm[0][:, 0:nn], in0=Es[:, lo_], scalar1=wi, scalar2=None, op0=ALU.mult),
                              r=["Es", wim.name], w=[tm[0].name])
                            V(lambda wr=wr, lo_=lo_, hi_=hi_, nn=nn: nc.vector.scalar_tensor_tensor(out=Ec[:, hi_], in0=Ec[:, lo_], scalar=wr, in1=tm[0][:, 0:nn],
                                                                                                  op0=ALU.mult, op1=ALU.subtract),
                              r=["Ec", tm[0].name, wre.name], w=["Ec"])
                            V(lambda wi=wi, lo_=lo_, nn=nn: nc.vector.tensor_scalar(out=tm[1][:, 0:nn], in0=Ec[:, lo_], scalar1=wi, scalar2=None, op0=ALU.mult),
                              r=["Ec", wim.name], w=[tm[1].name])
                            V(lambda wr=wr, lo_=lo_, hi_=hi_, nn=nn: nc.vector.scalar_tensor_tensor(out=Es[:, hi_], in0=Es[:, lo_], scalar=wr, in1=tm[1][:, 0:nn],
                                                                                                  op0=ALU.mult, op1=ALU.add),
                              r=["Es", tm[1].name, wre.name], w=["Es"])
                    V(lambda ft=ft: nc.vector.tensor_scalar(out=rdec[:], in0=Ec[:, 0:512], scalar1=0.0, scalar2=lam_abs[:, ft:ft + 1],
                                                            op0=ALU.mult, op1=ALU.add), r=["Ec", lam_abs.name], w=["rdec"])
                    for b in range(NB):
                        for c in range(NCH):
                            cs = slice(c * 512, (c + 1) * 512)
                            pr, pi = p_br[it % 2], p_bi[it % 2]
                            PE(lambda pr=pr, ft=ft, b=b, cs=cs: nc.tensor.matmul(pr[:], lhsT=BT[:, ft, 0, :], rhs=ub[:, b, cs], start=True, stop=True),
                               r=[BT.name, "ub"], w=[pr.name])
                            PE(lambda pi=pi, ft=ft, b=b, cs=cs: nc.tensor.matmul(pi[:], lhsT=BT[:, ft, 1, :], rhs=ub[:, b, cs], start=True, stop=True),
                               r=[BT.name, "ub"], w=[pi.name])
                            g_r, g_i, G_r, G_i = gre[it % 2], gim[it % 2], Gre[it % 2], Gim[it % 2]
                            V(lambda pr=pr, cs=cs: nc.vector.tensor_tensor(out=tm[0][:], in0=pr[:], in1=Ec[:, cs], op=ALU.mult), r=[pr.name, "Ec"], w=[tm[0].name])
                            V(lambda pi=pi, cs=cs: nc.vector.tensor_tensor(out=tm[1][:], in0=pi[:], in1=Es[:, cs], op=ALU.mult), r=[pi.name, "Es"], w=[tm[1].name])
                            V(lambda g_r=g_r: nc.vector.tensor_tensor(out=g_r[:], in0=tm[0][:], in1=tm[1][:], op=ALU.add), r=[tm[0].name, tm[1].name], w=[g_r.name])
                            V(lambda pi=pi, cs=cs: nc.vector.tensor_tensor(out=tm[2][:], in0=pi[:], in1=Ec[:, cs], op=ALU.mult), r=[pi.name, "Ec"], w=[tm[2].name])
                            V(lambda pr=pr, cs=cs: nc.vector.tensor_tensor(out=tm[3][:], in0=pr[:], in1=Es[:, cs], op=ALU.mult), r=[pr.name, "Es"], w=[tm[3].name])
                            V(lambda g_i=g_i: nc.vector.tensor_tensor(out=g_i[:], in0=tm[2][:], in1=tm[3][:], op=ALU.subtract), r=[tm[2].name, tm[3].name], w=[g_i.name])
                            if c == 0:
                                ini_r, ini_i = 0.0, 0.0
                                rd_extra = []
                            else:
                                pG_r, pG_i = Gre[(it - 1) % 2], Gim[(it - 1) % 2]
                                ini_r, ini_i = pG_r[:, 511:512], pG_i[:, 511:512]
                                rd_extra = [pG_r.name, pG_i.name]
                            V(lambda G_r=G_r, g_r=g_r, ini_r=ini_r: nc.vector.tensor_tensor_scan(out=G_r[:], data0=rdec[:], data1=g_r[:], initial=ini_r,
                                                                                                op0=ALU.mult, op1=ALU.add),
                              r=["rdec", g_r.name] + rd_extra, w=[G_r.name])
                            V(lambda G_i=G_i, g_i=g_i, ini_i=ini_i: nc.vector.tensor_tensor_scan(out=G_i[:], data0=rdec[:], data1=g_i[:], initial=ini_i,
                                                                                                op0=ALU.mult, op1=ALU.add),
                              r=["rdec", g_i.name] + rd_extra, w=[G_i.name])
                            h_r, h_i = hre[it % 2], him[it % 2]
                            G(lambda G_r=G_r, cs=cs: nc.gpsimd.tensor_tensor(out=dm[0][:], in0=G_r[:], in1=Ec[:, cs], op=ALU.mult), r=[G_r.name, "Ec"], w=[dm[0].name])
                            G(lambda G_i=G_i, cs=cs: nc.gpsimd.tensor_tensor(out=dm[1][:], in0=G_i[:], in1=Es[:, cs], op=ALU.mult), r=[G_i.name, "Es"], w=[dm[1].name])
                            G(lambda h_r=h_r: nc.gpsimd.tensor_tensor(out=h_r[:], in0=dm[0][:], in1=dm[1][:], op=ALU.subtract), r=[dm[0].name, dm[1].name], w=[h_r.name])
                            G(lambda G_r=G_r, cs=cs: nc.gpsimd.tensor_tensor(out=dm[2][:], in0=G_r[:], in1=Es[:, cs], op=ALU.mult), r=[G_r.name, "Es"], w=[dm[2].name])
                            G(lambda G_i=G_i, cs=cs: nc.gpsimd.tensor_tensor(out=dm[3][:], in0=G_i[:], in1=Ec[:, cs], op=ALU.mult), r=[G_i.name, "Ec"], w=[dm[3].name])
                            G(lambda h_i=h_i: nc.gpsimd.tensor_tensor(out=h_i[:], in0=dm[2][:], in1=dm[3][:], op=ALU.add), r=[dm[2].name, dm[3].name], w=[h_i.name])
                            if c == NCH - 1:
                                G(lambda ft=ft, b=b: nc.gpsimd.tensor_tensor(out=fin[:, ft, b, 0:1], in0=dm[0][:, 511:512], in1=dm[1][:, 511:512], op=ALU.subtract),
                                  r=[dm[0].name, dm[1].name], w=["fin"])
                                G(lambda ft=ft, b=b: nc.gpsimd.tensor_tensor(out=fin[:, ft, b, 1:2], in0=dm[2][:, 511:512], in1=dm[3][:, 511:512], op=ALU.add),
                                  r=[dm[2].name, dm[3].name], w=["fin"])
                            py = p_y[it % 2]
                            PE(lambda py=py, h_r=h_r, ft=ft, qs=qs: nc.tensor.matmul(py[:, :], lhsT=CT[:, ft, 0, :], rhs=h_r[:], start=True, stop=False),
                               r=[CT.name, h_r.name], w=[py.name])
                            PE(lambda py=py, h_i=h_i, ft=ft, qs=qs: nc.tensor.matmul(py[:, :], lhsT=CT[:, ft, 1, :], rhs=h_i[:], start=False, stop=True),
                               r=[CT.name, h_i.name], w=[py.name], skip_same=True)
                            if q == 0:
                                A(lambda py=py, b=b, cs=cs: nc.scalar.copy(out=yT[:, b, cs], in_=py[:, :]), r=[py.name], w=["yT"])
                            else:
                                V(lambda py=py, b=b, cs=cs: nc.vector.tensor_tensor(out=yT[:, b, cs], in0=py[:, :], in1=yT[:, b, cs], op=ALU.add), r=[py.name, "yT"], w=["yT"])
                            it += 1
                    pr, pi = p_br[it % 2], p_bi[it % 2]
                    PE(lambda pr=pr, ft=ft: nc.tensor.matmul(pr[:, 0:NS], lhsT=BT[:, ft, 0, :], rhs=ubs[:, :], start=True, stop=True),
                       r=[BT.name, "ubs"], w=[pr.name])
                    PE(lambda pi=pi, ft=ft: nc.tensor.matmul(pi[:, 0:NS], lhsT=BT[:, ft, 1, :], rhs=ubs[:, :], start=True, stop=True),
                       r=[BT.name, "ubs"], w=[pi.name])
                    lr, li = lam_re[:, ft:ft + 1], lam_im[:, ft:ft + 1]
                    V(lambda ft=ft, li=li: nc.vector.tensor_scalar(out=tm[0][:, 0:NS], in0=h0i[:, ft, :], scalar1=li, scalar2=None, op0=ALU.mult),
                      r=[h0i.name, lam_im.name], w=[tm[0].name])
                    V(lambda ft=ft, lr=lr: nc.vector.scalar_tensor_tensor(out=tm[1][:, 0:NS], in0=h0r[:, ft, :], scalar=lr, in1=tm[0][:, 0:NS], op0=ALU.mult, op1=ALU.subtract),
                      r=[h0r.name, lam_re.name, tm[0].name], w=[tm[1].name])
                    V(lambda ft=ft, pr=pr: nc.vector.tensor_tensor(out=h1r[:, ft, :], in0=pr[:, 0:NS], in1=tm[1][:, 0:NS], op=ALU.add), r=[pr.name, tm[1].name], w=[h1r.name])
                    V(lambda ft=ft, li=li: nc.vector.tensor_scalar(out=tm[2][:, 0:NS], in0=h0r[:, ft, :], scalar1=li, scalar2=None, op0=ALU.mult),
                      r=[h0r.name, lam_im.name], w=[tm[2].name])
                    V(lambda ft=ft, lr=lr: nc.vector.scalar_tensor_tensor(out=tm[3][:, 0:NS], in0=h0i[:, ft, :], scalar=lr, in1=tm[2][:, 0:NS], op0=ALU.mult, op1=ALU.add),
                      r=[h0i.name, lam_re.name, tm[2].name], w=[tm[3].name])
                    V(lambda ft=ft, pi=pi: nc.vector.tensor_tensor(out=h1i[:, ft, :], in0=pi[:, 0:NS], in1=tm[3][:, 0:NS], op=ALU.add), r=[pi.name, tm[3].name], w=[h1i.name])
                    h_r, h_i = hre[it % 2], him[it % 2]
                    V(lambda h_r=h_r, ft=ft: nc.vector.tensor_copy(out=h_r[:, 0:NS], in_=h1r[:, ft, :]), r=[h1r.name], w=[h_r.name])
                    V(lambda h_i=h_i, ft=ft: nc.vector.tensor_copy(out=h_i[:, 0:NS], in_=h1i[:, ft, :]), r=[h1i.name], w=[h_i.name])
                    py = p_y[it % 2]
                    PE(lambda py=py, h_r=h_r, ft=ft, qs=qs: nc.tensor.matmul(py[:, 0:NS], lhsT=CT[:, ft, 0, :], rhs=h_r[:, 0:NS], start=True, stop=False),
                       r=[CT.name, h_r.name], w=[py.name])
                    PE(lambda py=py, h_i=h_i, ft=ft, qs=qs: nc.tensor.matmul(py[:, 0:NS], lhsT=CT[:, ft, 1, :], rhs=h_i[:, 0:NS], start=False, stop=True),
                       r=[CT.name, h_i.name], w=[py.name], skip_same=True)
                    if q == 0:
                        A(lambda py=py: nc.scalar.copy(out=yTs[:, :], in_=py[:, 0:NS]), r=[py.name], w=["yTs"])
                    else:
                        V(lambda py=py: nc.vector.tensor_tensor(out=yTs[:, :], in0=py[:, 0:NS], in1=yTs[:, :], op=ALU.add), r=[py.name, "yTs"], w=["yTs"])
                    it += 1
                dsc = dcol[:, ct:ct + 1]

                def gelu_block(ysrc, usrc, n, dst_dram, rkeys, wkey):
                    a0, a1, a2 = gl[0][:, 0:n], gl[1][:, 0:n], gl[2][:, 0:n]
                    V(lambda: nc.vector.scalar_tensor_tensor(out=a0, in0=usrc, scalar=dsc, in1=ysrc, op0=ALU.mult, op1=ALU.add),
                      r=rkeys + [dcol.name], w=[gl[0].name])
                    A(lambda: nc.scalar.activation(out=a1, in_=a0, func=AF.Square), r=[gl[0].name], w=[gl[1].name])
                    V(lambda: nc.vector.tensor_scalar(out=a1, in0=a1, scalar1=0.044715, scalar2=1.0, op0=ALU.mult, op1=ALU.add), r=[gl[1].name], w=[gl[1].name])
                    G(lambda: nc.gpsimd.tensor_tensor(out=a2, in0=a1, in1=a0, op=ALU.mult), r=[gl[0].name, gl[1].name], w=[gl[2].name])
                    A(lambda: nc.scalar.activation(out=a2, in_=a2, func=AF.Sigmoid, scale=1.5957691216057308), r=[gl[2].name], w=[gl[2].name])
                    V(lambda: nc.vector.tensor_tensor(out=gyb[:, 0:n], in0=a0, in1=a2, op=ALU.mult), r=[gl[0].name, gl[2].name], w=["gyb"])
                    DMA(dst_dram, gyb[:, 0:n], r=["gyb"], w=[wkey])

                for b in range(NB):
                    for c in range(NCH):
                        cs = slice(c * 512, (c + 1) * 512)
                        t0 = b * SEQ + c * 512
                        us_ = ufst[(b * NCH + c) % 2]
                        DMA(us_[:], uT_scr[ct * 128:(ct + 1) * 128, t0:t0 + 512], r=[("uT", t0 // 128 + i_) for i_ in range(4)], w=[us_.name])
                        gelu_block(yT[:, b, cs], us_[:], 512, gyT_scr[ct * 128:(ct + 1) * 128, t0:t0 + 512], ["yT", us_.name], ("gyT", ct, b, c))
                gelu_block(yTs[:], ufs[:], NS, gyT_scr[ct * 128:(ct + 1) * 128, samp0:samp0 + NS], ["yTs", "ufs"], ("gyT", ct, "s"))
            with nc.allow_non_contiguous_dma(reason="state out"):
                for b in range(NB):
                    DMA(re_p[b:b + 1, :].rearrange("o (ft f) -> f (o ft)", f=128), fin[:, :, b, 0], r=["fin"], q="pool", is_output=True)
                    DMA(im_p[b:b + 1, :].rearrange("o (ft f) -> f (o ft)", f=128), fin[:, :, b, 1], r=["fin"], q="pool", is_output=True)
            for (src, dst) in ((h1r, re_s), (h1i, im_s)):
                for half in range(4):
                    for j in range(4):
                        ft = half * 4 + j
                        PE(lambda src=src, ft=ft, j=j: nc.tensor.transpose(out=p_s[0:NS, j * 128:(j + 1) * 128], in_=src[:, ft, :], identity=identf[:]),
                           r=[src.name, "identf"], w=[p_s.name])
                    V(lambda half=half: nc.vector.tensor_copy(out=sst[:, half * 512:(half + 1) * 512], in_=p_s[0:NS, :]), r=[p_s.name], w=[sst.name])
                DMA(dst[:, :], sst[:], r=[sst.name], q="pool", is_output=True)
            S.barrier()

    NIT = 22
    if "p3" in phases:
        with contextlib.ExitStack() as e3:
            TOPK = cfg.TOPK
            kTa = sb("kTa", [65, 2, SEQ], BF16, e3)
            kiT = sb("kiT", [64, SEQ], BF16, e3)
            V1 = sb("V1", [128, NT, 2, 65], BF16, e3)
            Irep = sb("Irep", [128, 4, 128], BF16, e3)
            pw2 = sb("pw2", [128, NIT + 1], F32, e3)
            kmaxb = sb("kmaxb", [128, 1], F32, e3)
            km1 = sb("km1", [128, 1], F32, e3)
            km2 = sb("km2", [1, 128], F32, e3)
            zrow = sb("zrow", [1, 512], BF16, e3)
            qTa = [sb(f"qTa{i}", [65, 8, 128], BF16, e3) for i in range(2)]
            qiT = [sb(f"qiT{i}", [64, 8, 128], BF16, e3) for i in range(2)]
            score = sb("score", [128, SEQ], F32, e3)
            tmpc = [sb(f"tmpc{i}", [128, 512], F32, e3) for i in range(2)]
            junkb = sb("junkb", [128, SEQ], BF16, e3)
            mb = sb("mb", [128, SEQ], BF16, e3)
            pT = [sb(f"pT{i}", [128, 512], BF16, e3) for i in range(2)]
            ao = sb("ao", [128, 8, 64], BF16, e3)
            amax = sb("amax", [128, 1], F32, e3)
            stp = sb("stp", [128, NIT + 1], F32, e3)
            mid = sb("mid", [128, 1], F32, e3)
            cnt = sb("cnt", [128, 1], F32, e3)
            c2 = sb("c2", [128, 1], F32, e3)
            thr = sb("thr", [128, 1], F32, e3)
            rec = sb("rec", [128, 4], F32, e3)
            p_ix = [ps(f"p_ix{i}", [128, 512], F32, e3) for i in range(2)]
            p_st = [ps(f"p_st{i}", [128, 512], F32, e3) for i in range(2)]
            p_o = ps("p_o", [128, 4, 65], F32, e3)
            p_m = ps("p_m3", [128, 512], F32, e3)

            for k in range(NIT + 1):
                G(lambda k=k: nc.gpsimd.memset(pw2[:, k:k + 1], float(2.0 ** (-k))), w=["pw2"])
            G(lambda: nc.gpsimd.memset(zrow[:], 0.0), w=["zrow"])
            for j in range(4):
                V(lambda j=j: nc.vector.tensor_copy(out=Irep[:, j, :], in_=ident[:]), r=["ident"], w=["Irep"])
            V(lambda: nc.vector.tensor_reduce(out=km1[:], in_=kn2[:].rearrange("p a b -> p (a b)"), axis=AX.X, op=ALU.max), r=["kn2"], w=["km1"])
            PE(lambda: nc.tensor.transpose(out=p_m[0:1, 0:128], in_=km1[:], identity=identf[:]), r=["km1", "identf"], w=[p_m.name])
            V(lambda: nc.vector.tensor_reduce(out=km2[0:1, 0:1], in_=p_m[0:1, 0:128], axis=AX.X, op=ALU.max), r=[p_m.name], w=["km2"])
            A(lambda: nc.scalar.activation(out=km2[0:1, 0:1], in_=km2[0:1, 0:1], func=AF.Sqrt), r=["km2"], w=["km2"])
            PE(lambda: nc.tensor.matmul(p_m[:, 0:1], lhsT=ones_f[0:1, :], rhs=km2[0:1, 0:1], start=True, stop=True), r=["ones_f", "km2"], w=[p_m.name])
            V(lambda: nc.vector.tensor_copy(out=kmaxb[:], in_=p_m[:, 0:1]), r=[p_m.name], w=["kmaxb"])

            it = 0
            for b in range(NB):
                t0 = b * SEQ
                for g in range(2):
                    DMA(kTa[0:64, g, :], kT_scr[:, g, t0:t0 + SEQ], r=[("kT", b * NT + i_) for i_ in range(NT)], w=["kTa"])
                V(lambda: nc.vector.memset(kTa[64:65, :, :], 1.0), w=["kTa"])
                DMA(kiT[:, :], kiT_scr[:, t0:t0 + SEQ], r=[("kiT", b * NT + i_) for i_ in range(NT)], w=["kiT"])
                for g in range(2):
                    DMA(V1[:, :, g, 0:64], vb_scr[t0:t0 + SEQ, g, :].rearrange("(kb p) d -> p kb d", p=128),
                        r=[("vb", b * NT + i_) for i_ in range(NT)], w=["V1"])
                V(lambda: nc.vector.memset(V1[:, :, :, 64:65], 1.0), w=["V1"])
                for i in range(NT):
                    T = b * NT + i
                    Sk = (i + 1) * 128
                    qa_, qi_ = qTa[T % 2], qiT[T % 2]
                    DMA(qa_[:], qT_scr[T, :, :, :], r=[("qT", T)], w=[qa_.name])
                    DMA(qi_[:], qiT_scr[T, :, :, :], r=[("qiT", T)], w=[qi_.name])
                    V(lambda qa_=qa_: nc.vector.tensor_scalar(out=qa_[64:65, :, :], in0=qa_[64:65, :, :], scalar1=kmaxb[64:65, 0:1], scalar2=None, op0=ALU.mult),
                      r=[qa_.name, "kmaxb"], w=[qa_.name])
                    for c0 in range(0, Sk, 512):
                        cw = min(512, Sk - c0)
                        for h in range(8):
                            px = p_ix[it % 2]
                            PE(lambda px=px, qi_=qi_, h=h, c0=c0, cw=cw: nc.tensor.matmul(px[:, 0:cw], lhsT=qi_[:, h, :], rhs=kiT[:, c0:c0 + cw], start=True, stop=True),
                               r=[qi_.name, "kiT"], w=[px.name])
                            lo_ = lohi[:, T, h:h + 1]
                            hi_ = lohi[:, T, 8 + h:9 + h]
                            if h == 0:
                                V(lambda px=px, c0=c0, cw=cw, lo_=lo_, hi_=hi_: nc.vector.tensor_scalar(out=score[:, c0:c0 + cw], in0=px[:, 0:cw], scalar1=lo_, scalar2=hi_,
                                                                                                     op0=ALU.max, op1=ALU.min), r=[px.name, "lohi"], w=["score"])
                            else:
                                tc_ = tmpc[it % 2]
                                V(lambda px=px, tc_=tc_, cw=cw, lo_=lo_, hi_=hi_: nc.vector.tensor_scalar(out=tc_[:, 0:cw], in0=px[:, 0:cw], scalar1=lo_, scalar2=hi_,
                                                                                                       op0=ALU.max, op1=ALU.min), r=[px.name, "lohi"], w=[tc_.name])
                                G(lambda tc_=tc_, c0=c0, cw=cw: nc.gpsimd.tensor_tensor(out=score[:, c0:c0 + cw], in0=score[:, c0:c0 + cw], in1=tc_[:, 0:cw], op=ALU.add),
                                  r=[tc_.name, "score"], w=["score"])
                            it += 1
                    if Sk > TOPK:
                        V(lambda Sk=Sk: nc.vector.tensor_reduce(out=amax[:], in_=score[:, 0:Sk], axis=AX.X, op=ALU.max, apply_absolute_value=True),
                          r=["score"], w=["amax"])
                    G(lambda Sk=Sk: nc.gpsimd.tensor_tensor(out=score[:, Sk - 128:Sk], in0=score[:, Sk - 128:Sk], in1=cmask[:], op=ALU.add),
                      r=["score", "cmask"], w=["score"])
                    if Sk > TOPK:
                        V(lambda: nc.vector.tensor_scalar(out=amax[:], in0=amax[:], scalar1=1.0, scalar2=None, op0=ALU.add), r=["amax"], w=["amax"])
                        V(lambda: nc.vector.tensor_scalar(out=stp[:], in0=pw2[:], scalar1=amax[:, 0:1], scalar2=None, op0=ALU.mult), r=["pw2", "amax"], w=["stp"])
                        V(lambda: nc.vector.memset(mid[:], 0.0), w=["mid"])
                        for k in range(NIT):
                            V(lambda Sk=Sk: nc.vector.tensor_scalar(out=junkb[:, 0:Sk], in0=score[:, 0:Sk], scalar1=mid[:, 0:1], scalar2=None, op0=ALU.is_ge,
                                                                    op1=ALU.add, accum_out=cnt[:]), r=["score", "mid"], w=["junkb", "cnt"])
                            V(lambda: nc.vector.tensor_scalar(out=c2[:], in0=cnt[:], scalar1=float(TOPK), scalar2=-0.5, op0=ALU.is_ge, op1=ALU.add), r=["cnt"], w=["c2"])
                            V(lambda k=k: nc.vector.scalar_tensor_tensor(out=mid[:], in0=c2[:], scalar=stp[:, k:k + 1], in1=mid[:], op0=ALU.mult, op1=ALU.add),
                              r=["c2", "stp", "mid"], w=["mid"])
                        V(lambda: nc.vector.tensor_tensor(out=thr[:], in0=mid[:], in1=stp[:, NIT:NIT + 1], op=ALU.subtract), r=["mid", "stp"], w=["thr"])
                    else:
                        V(lambda: nc.vector.memset(thr[:], -1.0e29), w=["thr"])
                    V(lambda Sk=Sk: nc.vector.tensor_scalar(out=mb[:, 0:Sk], in0=score[:, 0:Sk], scalar1=thr[:, 0:1], scalar2=BIGM, op0=ALU.is_lt, op1=ALU.mult),
                      r=["score", "thr"], w=["mb"])
                    if debug and T == NT - 1:
                        DMA(dbg_mb[:, 0:Sk], mb[:, 0:Sk], r=["mb"], w=["dbg_mb"])
                        DMA(dbg_sc[:, 0:Sk], score[:, 0:Sk], r=["score"], w=["dbg_sc"])
                        DMA(dbg_thr[:, :], thr[:], r=["thr"], w=["dbg_thr"])
                    for g in range(2):
                        PE(lambda: nc.tensor.matmul(p_o[:].rearrange("p a b -> p (a b)"), lhsT=zrow[0:1, 0:128], rhs=zrow[0:1, 0:260], start=True, stop=False,
                                                    skip_group_check=True), r=["zrow"], w=["p_o"])
                        for kb in range(i + 1):
                            pst = p_st[it % 2]
                            pt_ = pT[it % 2]
                            ks = slice(kb * 128, (kb + 1) * 128)
                            PE(lambda pst=pst, g=g, ks=ks, qa_=qa_: nc.tensor.matmul(pst[:], lhsT=kTa[:, g, ks], rhs=qa_[:, 4 * g:4 * g + 4, :].rearrange("p a b -> p (a b)"),
                                                                                    start=True, stop=False), r=["kTa", qa_.name], w=[pst.name])
                            PE(lambda pst=pst, ks=ks: nc.tensor.matmul(pst[:], lhsT=mb[:, ks], rhs=Irep[:].rearrange("p a b -> p (a b)"), start=False, stop=True),
                               r=["mb", "Irep"], w=[pst.name], skip_same=True)
                            A(lambda pst=pst, pt_=pt_: nc.scalar.activation(out=pt_[:], in_=pst[:], func=AF.Exp, scale=0.125), r=[pst.name], w=[pt_.name])
                            for h4 in range(4):
                                PE(lambda pt_=pt_, h4=h4, kb=kb, g=g, i=i: nc.tensor.matmul(p_o[:, h4, :], lhsT=pt_[:, h4 * 128:(h4 + 1) * 128], rhs=V1[:, kb, g, :],
                                                                                         start=False, stop=(kb == i), skip_group_check=True),
                                   r=[pt_.name, "V1"], w=["p_o"], skip_same=True)
                            it += 1
                        V(lambda: nc.vector.reciprocal(out=rec[:], in_=p_o[:, :, 64]), r=["p_o"], w=["rec"])
                        V(lambda g=g: nc.vector.tensor_tensor(out=ao[:, 4 * g:4 * g + 4, :], in0=p_o[:, :, 0:64], in1=rec[:].unsqueeze(2).to_broadcast([128, 4, 64]), op=ALU.mult),
                          r=["p_o", "rec"], w=["ao"])
                    DMA(ao_scr[T * 128:(T + 1) * 128, :], ao[:].rearrange("p a b -> p (a b)"), r=["ao"], w=[("ao", T)])
            S.barrier()

    if "p3s" in phases:
        with contextlib.ExitStack() as e4:
            NPG = cfg.NPG
            assert NPG == 64 and NS % 2 == 0
            TOPK_S = cfg.TOPK_S
            TS = NTT - 1
            samp0 = TS * 128
            NO = 129
            lohi_scr = dscr("lohi_scr", [NS, 16], F32)
            DMA(lohi_scr[:, :], lohi[0:NS, TS, :], r=["lohi"], w=["lohi_scr"])
            lohiB = sb("lohiB", [128, NS, 16], F32, e4)
            DMA(lohiB[:].rearrange("p a b -> p (a b)"), lohi_scr.rearrange("(o a) b -> o (a b)", o=1).partition_broadcast(128),
                r=["lohi_scr"], w=["lohiB"])
            qTs = sb("qTs", [65, 8, 128], BF16, e4)
            qiTs = sb("qiTs", [64, 8, 128], BF16, e4)
            DMA(qTs[:], qT_scr[TS, :, :, :], r=[("qT", TS)], w=["qTs"])
            DMA(qiTs[:], qiT_scr[TS, :, :, :], r=[("qiT", TS)], w=["qiTs"])
            idx = sb("idx", [128, 1], I32, e4)
            idxf = sb("idxf", [128, 1], F32, e4)
            idxa = sb("idxa", [128, 1], I32, e4)
            idxb = sb("idxb", [128, 1], I32, e4)
            KI = sb("KI", [128, 128, 64], F32, e4)
            KIx = sb("KIx", [128, 64], F32, e4)
            Kh = [sb(f"Kh{i}", [128, 64, 128], F32, e4) for i in range(2)]
            Kx = sb("Kx", [128, 128], F32, e4)
            Vh = sb("Vh", [128, 64, 128], F32, e4)
            Vx = sb("Vx", [128, 128], F32, e4)
            V1h = sb("V1h", [128, 64, 2, 65], BF16, e4)
            V1x = sb("V1x", [128, 2, 65], BF16, e4)
            kT4 = [sb(f"kT4_{i}", [65, 4, 128], BF16, e4) for i in range(2)]
            qis = sb("qis", [64, 2, 8], BF16, e4)
            qas = sb("qas", [65, 2, 2, 4], BF16, e4)
            sc = sb("sc_s", [128, NO], F32, e4)
            cl = sb("cl_s", [128, 32, 16], F32, e4)
            red = sb("red_s", [128, 32, 2], F32, e4)
            colmask = sb("colmask", [128, 1], F32, e4)
            cross = sb("cross", [128, 16], F32, e4)
            maskfull = sb("maskfull", [128, NO, 16], F32, e4)
            blk1 = sb("blk1", [128, 128], F32, e4)
            pw2s = sb("pw2s", [128, NIT + 1], F32, e4)
            am1 = sb("am1", [128, 1], F32, e4)
            am2 = sb("am2", [1, 128], F32, e4)
            amb = sb("amb", [128, 1], F32, e4)
            stps = sb("stps", [128, NIT + 1], F32, e4)
            mids = sb("mids", [128, 1], F32, e4)
            cnts = sb("cnts", [128, 1], F32, e4)
            c2s = sb("c2s", [128, 1], F32, e4)
            thrs = sb("thrs", [128, 1], F32, e4)
            junks = sb("junks", [128, NO], F32, e4)
            sqk = Vh
            n2 = sb("n2", [128, 258], F32, e4)
            kmb = sb("kmb", [128, 1], F32, e4)
            tmps = sb("tmps", [128, 32, 16], F32, e4)
            pTs = [sb(f"pTs{i}", [128, 32, 16], BF16, e4) for i in range(2)]
            zrow_s = sb("zrow_s", [1, 512], BF16, e4)
            o8 = sb("o8", [8, 2, 64], BF16, e4)
            rec8 = sb("rec8", [8, 2], F32, e4)
            p_t = [ps(f"p3s_t{i}", [64, 4, 128], F32, e4) for i in range(2)]
            p_sx = [ps(f"p3s_x{i}", [128, 32, 16], F32, e4) for i in range(2)]
            p_os = ps("p3s_o", [8, 2, 65], F32, e4)
            p_ms = ps("p3s_m", [128, 512], F32, e4)

            for k in range(NIT + 1):
                G(lambda k=k: nc.gpsimd.memset(pw2s[:, k:k + 1], float(2.0 ** (-k))), w=["pw2s"])
            G(lambda: nc.gpsimd.memset(zrow_s[:], 0.0), w=["zrow_s"])
            V(lambda: nc.vector.memset(colmask[:], NEG), w=["colmask"])
            V(lambda: nc.vector.memset(colmask[0:1, :], 0.0), w=["colmask"])
            V(lambda: nc.vector.memset(colmask[64:65, :], 0.0), w=["colmask"])
            V(lambda: nc.vector.memset(cross[:], 0.0), w=["cross"])
            crv = cross[:].rearrange("p (g b h) -> p g b h", g=2, b=2)
            V(lambda: nc.vector.memset(crv[0:64, :, 1, :], BIGM), w=["cross"])
            V(lambda: nc.vector.memset(crv[64:128, :, 0, :], BIGM), w=["cross"])
            V(lambda: nc.vector.memset(blk1[:], 0.0), w=["blk1"])
            V(lambda: nc.vector.memset(blk1[0:64, 0:64], 1.0), w=["blk1"])
            V(lambda: nc.vector.memset(blk1[64:128, 64:128], 1.0), w=["blk1"])
            for t_ in kT4:
                V(lambda t_=t_: nc.vector.memset(t_[64:65, :, :], 1.0), w=[t_.name])
            V(lambda: nc.vector.memset(V1h[:, :, :, 64:65], 1.0), w=["V1h"])
            V(lambda: nc.vector.memset(V1x[:, :, 64:65], 1.0), w=["V1x"])

            def gather(dst2d, src2d, idx_t, wkey):
                S.dma("pool", lambda: nc.gpsimd.indirect_dma_start(out=dst2d, out_offset=None, in_=src2d,
                                                                   in_offset=bass.IndirectOffsetOnAxis(ap=idx_t[:, :], axis=0)),
                      reads=[idx_t.name], writes=[wkey])

            cache_kh = cache_k.rearrange("n (h e) -> (n h) e", h=2)
            cache_vh = cache_v.rearrange("n (h e) -> (n h) e", h=2)
            itc = [0]

            def transposed_units(units, consume):
                for u0 in range(0, len(units), 4):
                    grp = units[u0:u0 + 4]
                    pt_ = p_t[itc[0] % 2]
                    kt = kT4[itc[0] % 2]
                    for s_, (src, rk) in enumerate(grp):
                        PE(lambda pt_=pt_, s_=s_, src=src: nc.tensor.transpose(out=pt_[:, s_, :], in_=src, identity=identf[:]),
                           r=list(rk) + ["identf"], w=[pt_.name], skip_same=True)
                    n_ = len(grp)
                    if itc[0] % 2 == 0:
                        V(lambda pt_=pt_, kt=kt, n_=n_: nc.vector.tensor_copy(out=kt[0:64, 0:n_, :], in_=pt_[:, 0:n_, :]), r=[pt_.name], w=[kt.name])
                    else:
                        A(lambda pt_=pt_, kt=kt, n_=n_: nc.scalar.copy(out=kt[0:64, 0:n_, :], in_=pt_[:, 0:n_, :]), r=[pt_.name], w=[kt.name])
                    for s_ in range(n_):
                        consume(kt, s_, u0 + s_)
                    itc[0] += 1

            for pr in range(NS // 2):
                b0 = 2 * pr
                DMA(idx[:], ptab[b0 * NPG:(b0 + 2) * NPG, :], w=["idx"])
                V(lambda: nc.vector.tensor_copy(out=idxf[:], in_=idx[:]), r=["idx"], w=["idxf"])
                V(lambda: nc.vector.tensor_scalar(out=idxa[:], in0=idxf[:], scalar1=2.0, scalar2=None, op0=ALU.mult), r=["idxf"], w=["idxa"])
                V(lambda: nc.vector.tensor_scalar(out=idxb[:], in0=idxf[:], scalar1=2.0, scalar2=1.0, op0=ALU.mult, op1=ALU.add), r=["idxf"], w=["idxb"])
                gather(KI[:].rearrange("p a b -> p (a b)"), cache_ik[:, :], idx, "KI")
                gather(Kh[0][:].rearrange("p a b -> p (a b)"), cache_kh[:, :], idxa, "Kh0")
                gather(Kh[1][:].rearrange("p a b -> p (a b)"), cache_kh[:, :], idxb, "Kh1")
                for (xt_, src_) in ((KIx, ki_s), (Kx, k_s), (Vx, v_s)):
                    V(lambda xt_=xt_: nc.vector.memset(xt_[:], 0.0), w=[xt_.name])
                    DMA(xt_[0:1, :], src_[b0:b0 + 1, :], w=[xt_.name])
                    DMA(xt_[64:65, :], src_[b0 + 1:b0 + 2, :], w=[xt_.name])
                V(lambda b0=b0: nc.vector.tensor_copy(out=qis[:], in_=qiTs[:, :, b0:b0 + 2].rearrange("p h t -> p t h")), r=["qiTs"], w=["qis"])
                for g in range(2):
                    V(lambda b0=b0, g=g: nc.vector.tensor_copy(out=qas[:, g, :, :], in_=qTs[:, 4 * g:4 * g + 4, b0:b0 + 2].rearrange("p h t -> p t h")),
                      r=["qTs"], w=["qas"])
                for hf in range(2):
                    G(lambda hf=hf: nc.gpsimd.tensor_tensor(out=sqk[:], in0=Kh[hf][:], in1=Kh[hf][:], op=ALU.mult), r=[f"Kh{hf}"], w=["Vh"])
                    V(lambda hf=hf: nc.vector.tensor_reduce(out=n2[:, hf * 128:(hf + 1) * 128], in_=sqk[:].rearrange("p o (g d) -> p (o g) d", g=2), axis=AX.X, op=ALU.add),
                      r=["Vh"], w=["n2"])
                G(lambda: nc.gpsimd.tensor_tensor(out=sqk[:, 0, :], in0=Kx[:], in1=Kx[:], op=ALU.mult), r=["Kx"], w=["Vh"])
                V(lambda: nc.vector.tensor_reduce(out=n2[:, 256:258], in_=sqk[:, 0, :].rearrange("p (g d) -> p g d", g=2), axis=AX.X, op=ALU.add), r=["Vh"], w=["n2"])
                V(lambda: nc.vector.tensor_reduce(out=am1[:], in_=n2[:], axis=AX.X, op=ALU.max), r=["n2"], w=["am1"])
                PE(lambda: nc.tensor.transpose(out=p_ms[0:1, 0:128], in_=am1[:], identity=identf[:]), r=["am1", "identf"], w=[p_ms.name])
                V(lambda: nc.vector.tensor_reduce(out=am2[0:1, 0:1], in_=p_ms[0:1, 0:128], axis=AX.X, op=ALU.max), r=[p_ms.name], w=["am2"])
                A(lambda: nc.scalar.activation(out=am2[0:1, 0:1], in_=am2[0:1, 0:1], func=AF.Sqrt), r=["am2"], w=["am2"])
                PE(lambda: nc.tensor.matmul(p_ms[:, 0:1], lhsT=ones_f[0:1, :], rhs=am2[0:1, 0:1], start=True, stop=True), r=["ones_f", "am2"], w=[p_ms.name])
                V(lambda: nc.vector.tensor_copy(out=kmb[:], in_=p_ms[:, 0:1]), r=[p_ms.name], w=["kmb"])
                V(lambda: nc.vector.tensor_scalar(out=qas[64:65, :, :, :], in0=qas[64:65, :, :, :], scalar1=kmb[64:65, 0:1], scalar2=None, op0=ALU.mult),
                  r=["qas", "kmb"], w=["qas"])

                loB = lohiB[:, b0:b0 + 2, 0:8]
                hiB = lohiB[:, b0:b0 + 2, 8:16]

                def idx_group(units, o_lo, n_o):
                    px = p_sx[itc[0] % 2]

                    def consume(kt, s_, ui, px=px):
                        PE(lambda: nc.tensor.matmul(px[:, ui, :], lhsT=kt[0:64, s_, :], rhs=qis[:].rearrange("p a b -> p (a b)"), start=True, stop=True),
                           r=[kt.name, "qis"], w=[px.name], skip_same=True)
                    transposed_units(units, consume)
                    V(lambda: nc.vector.tensor_tensor(out=cl[:, 0:n_o, :].rearrange("p o (b h) -> p o b h", b=2), in0=px[:, 0:n_o, :].rearrange("p o (b h) -> p o b h", b=2),
                                                      in1=loB.unsqueeze(1).to_broadcast([128, n_o, 2, 8]), op=ALU.max), r=[px.name, "lohiB"], w=["cl_s"])
                    V(lambda: nc.vector.tensor_tensor(out=cl[:, 0:n_o, :].rearrange("p o (b h) -> p o b h", b=2), in0=cl[:, 0:n_o, :].rearrange("p o (b h) -> p o b h", b=2),
                                                      in1=hiB.unsqueeze(1).to_broadcast([128, n_o, 2, 8]), op=ALU.min), r=["cl_s", "lohiB"], w=["cl_s"])
                    V(lambda: nc.vector.tensor_reduce(out=red[:, 0:n_o, :], in_=cl[:, 0:n_o, :].rearrange("p o (b h) -> p o b h", b=2), axis=AX.X, op=ALU.add),
                      r=["cl_s"], w=["red_s"])
                    V(lambda: nc.vector.tensor_copy(out=sc[0:64, o_lo:o_lo + n_o], in_=red[0:64, 0:n_o, 0]), r=["red_s"], w=["sc_s"])
                    V(lambda: nc.vector.tensor_copy(out=sc[64:128, o_lo:o_lo + n_o], in_=red[64:128, 0:n_o, 1]), r=["red_s"], w=["sc_s"])

                for og in range(4):
                    idx_group([(KI[:, og * 32 + oo, :], ["KI"]) for oo in range(32)], og * 32, 32)
                idx_group([(KIx[:], ["KIx"])], 128, 1)
                V(lambda: nc.vector.tensor_reduce(out=am1[:], in_=sc[:], axis=AX.X, op=ALU.max, apply_absolute_value=True), r=["sc_s"], w=["am1"])
                V(lambda: nc.vector.tensor_tensor(out=sc[:, 128:129], in0=sc[:, 128:129], in1=colmask[:], op=ALU.add), r=["sc_s", "colmask"], w=["sc_s"])
                PE(lambda: nc.tensor.transpose(out=p_ms[0:1, 0:128], in_=am1[:], identity=identf[:]), r=["am1", "identf"], w=[p_ms.name])
                V(lambda: nc.vector.tensor_reduce(out=am2[0:1, 0:1], in_=p_ms[0:1, 0:128], axis=AX.X, op=ALU.max), r=[p_ms.name], w=["am2"])
                V(lambda: nc.vector.tensor_scalar(out=am2[0:1, 0:1], in0=am2[0:1, 0:1], scalar1=1.0, scalar2=None, op0=ALU.add), r=["am2"], w=["am2"])
                PE(lambda: nc.tensor.matmul(p_ms[:, 0:1], lhsT=ones_f[0:1, :], rhs=am2[0:1, 0:1], start=True, stop=True), r=["ones_f", "am2"], w=[p_ms.name])
                V(lambda: nc.vector.tensor_copy(out=amb[:], in_=p_ms[:, 0:1]), r=[p_ms.name], w=["amb"])
                V(lambda: nc.vector.tensor_scalar(out=stps[:], in0=pw2s[:], scalar1=amb[:, 0:1], scalar2=None, op0=ALU.mult), r=["pw2s", "amb"], w=["stps"])
                V(lambda: nc.vector.memset(mids[:], 0.0), w=["mids"])
                for k in range(NIT):
                    V(lambda: nc.vector.tensor_scalar(out=junks[:], in0=sc[:], scalar1=mids[:, 0:1], scalar2=None, op0=ALU.is_ge, op1=ALU.add, accum_out=cnts[:]),
                      r=["sc_s", "mids"], w=["junks", "cnts"])
                    PE(lambda: nc.tensor.matmul(p_ms[:, 0:1], lhsT=blk1[:], rhs=cnts[:], start=True, stop=True), r=["blk1", "cnts"], w=[p_ms.name])
                    V(lambda: nc.vector.tensor_scalar(out=c2s[:], in0=p_ms[:, 0:1], scalar1=float(TOPK_S), scalar2=-0.5, op0=ALU.is_ge, op1=ALU.add), r=[p_ms.name], w=["c2s"])
                    V(lambda k=k: nc.vector.scalar_tensor_tensor(out=mids[:], in0=c2s[:], scalar=stps[:, k:k + 1], in1=mids[:], op0=ALU.mult, op1=ALU.add),
                      r=["c2s", "stps", "mids"], w=["mids"])
                V(lambda: nc.vector.tensor_tensor(out=thrs[:], in0=mids[:], in1=stps[:, NIT:NIT + 1], op=ALU.subtract), r=["mids", "stps"], w=["thrs"])
                V(lambda: nc.vector.tensor_scalar(out=junks[:], in0=sc[:], scalar1=thrs[:, 0:1], scalar2=BIGM, op0=ALU.is_lt, op1=ALU.mult), r=["sc_s", "thrs"], w=["junks"])
                V(lambda: nc.vector.tensor_tensor(out=maskfull[:], in0=junks[:].unsqueeze(2).to_broadcast([128, NO, 16]),
                                                  in1=cross[:].unsqueeze(1).to_broadcast([128, NO, 16]), op=ALU.add), r=["junks", "cross"], w=["maskfull"])
                PE(lambda: nc.tensor.matmul(p_os[:].rearrange("p a b -> p (a b)"), lhsT=zrow_s[0:1, 0:8], rhs=zrow_s[0:1, 0:130], start=True, stop=False,
                                            skip_group_check=True), r=["zrow_s"], w=["p3s_o"])

                def att_group(kunits, vsrc_fn, o_lo, n_o, last):
                    px = p_sx[itc[0] % 2]
                    pts = pTs[itc[0] % 2]

                    def consume(kt, s_, ui, px=px):
                        ol, g = ui // 2, ui % 2
                        PE(lambda: nc.tensor.matmul(px[:, ol, g * 8:(g + 1) * 8], lhsT=kt[:, s_, :], rhs=qas[:, g, :, :].rearrange("p a b -> p (a b)"), start=True, stop=True),
                           r=[kt.name, "qas"], w=[px.name], skip_same=True)
                    transposed_units(kunits, consume)
                    V(lambda: nc.vector.tensor_tensor(out=tmps[:, 0:n_o, :], in0=px[:, 0:n_o, :], in1=maskfull[:, o_lo:o_lo + n_o, :], op=ALU.add),
                      r=[px.name, "maskfull"], w=["tmps"])
                    A(lambda: nc.scalar.activation(out=pts[:, 0:n_o, :], in_=tmps[:, 0:n_o, :], func=AF.Exp, scale=0.125), r=["tmps"], w=[pts.name])
                    for ol in range(n_o):
                        for g in range(2):
                            vap, vk = vsrc_fn(ol, g)
                            PE(lambda ol=ol, g=g, vap=vap: nc.tensor.matmul(p_os[:, g, :], lhsT=pts[:, ol, g * 8:(g + 1) * 8], rhs=vap, start=False,
                                                                         stop=(last and ol == n_o - 1), skip_group_check=True),
                               r=[pts.name, vk], w=["p3s_o"], skip_same=True)

                for hf in range(2):
                    gather(Vh[:].rearrange("p a b -> p (a b)"), cache_vh[:, :], idxa if hf == 0 else idxb, "Vh")
                    for g in range(2):
                        G(lambda g=g: nc.gpsimd.tensor_copy(out=V1h[:, :, g, 0:64], in_=Vh[:, :, g * 64:(g + 1) * 64]), r=["Vh"], w=["V1h"])
                    for og in range(2):
                        units = []
                        for oo in range(32):
                            for g in range(2):
                                units.append((Kh[hf][:, og * 32 + oo, g * 64:(g + 1) * 64], [f"Kh{hf}"]))
                        att_group(units, lambda ol, g, og=og: (V1h[:, og * 32 + ol, g, :], "V1h"), hf * 64 + og * 32, 32, False)
                for g in range(2):
                    V(lambda g=g: nc.vector.tensor_copy(out=V1x[:, g, 0:64], in_=Vx[:, g * 64:(g + 1) * 64]), r=["Vx"], w=["V1x"])
                att_group([(Kx[:, g * 64:(g + 1) * 64], ["Kx"]) for g in range(2)], lambda ol, g: (V1x[:, g, :], "V1x"), 128, 1, True)
                V(lambda: nc.vector.reciprocal(out=rec8[:], in_=p_os[:, :, 64]), r=["p3s_o"], w=["rec8"])
                V(lambda: nc.vector.tensor_tensor(out=o8[:], in0=p_os[:, :, 0:64], in1=rec8[:].unsqueeze(2).to_broadcast([8, 2, 64]), op=ALU.mult),
                  r=["p3s_o", "rec8"], w=["o8"])
                for b2 in range(2):
                    row = samp0 + b0 + b2
                    DMA(ao_scr[row:row + 1, :].rearrange("o (g h d) -> (o h) g d", g=2, h=4), o8[4 * b2:4 * b2 + 4, :, :], r=["o8"], w=[("ao", TS)])
            S.barrier()

    if "p4" in phases:
        with contextlib.ExitStack() as e5:
            Wglu = sb("Wglu", [128, 4, 2048], BF16, e5)
            Wao = sb("Wao", [128, 4, 1024], BF16, e5)
            Wo = sb("Wo", [128, 8, 1024], BF16, e5)
            bglu = sb("bglu", [128, 2048], F32, e5)
            with contextlib.ExitStack() as e5w:
                load_weight_bf16(Wglu, w_glu, D_SSM, 2048, e5w, nm="wglu")
                load_weight_bf16(Wao, w_ao, D_ATTN, 1024, e5w, nm="wao")
                load_weight_bf16(Wo, w_o, D_MODEL, 1024, e5w, nm="wo")
                S.barrier()
            DMA(bglu[:], b_glu.rearrange("(o n) -> o n", o=1).partition_broadcast(128), w=["bglu"])
            gyt = [sb(f"gyt{i}", [128, 4, 128], BF16, e5) for i in range(2)]
            aot = [sb(f"aot{i}", [128, 512], BF16, e5) for i in range(2)]
            gat = [sb(f"gat{i}", [128, 2048], F32, e5) for i in range(2)]
            xin = [sb(f"xin{i}", [128, 1024], F32, e5) for i in range(2)]
            zv = sb("zv", [128, 512], F32, e5)
            zg = sb("zg", [128, 512], F32, e5)
            so = sb("so", [128, 1024], F32, e5)
            aoT = sb("aoT", [128, 4, 128], BF16, e5)
            m1 = sb("m1", [128, 1024], F32, e5)
            m2 = sb("m2", [128, 512], F32, e5)
            mixb = sb("mixb", [128, 1024], BF16, e5)
            mixT = sb("mixT", [128, 8, 128], BF16, e5)
            x2t = [sb(f"x2t{i}", [128, 1024], F32, e5) for i in range(2)]
            p_a = [ps(f"p4_a{i}", [128, 512], F32, e5) for i in range(4)]
            p_tr4 = ps("p4_tr", [128, 8, 128], BF16, e5)
            for T in range(NTT):
                r0, nr, is_s, pti = tile_rows(T)
                gy_, ao_, ga_, xi_, x2_ = gyt[T % 2], aot[T % 2], gat[T % 2], xin[T % 2], x2t[T % 2]
                gkeys = [("gyT", ct_, "s") for ct_ in range(4)] if is_s else [("gyT", ct_, (T * 128) // SEQ, ((T * 128) % SEQ) // 512) for ct_ in range(4)]
                DMA(gy_[:], gyT_scr.rearrange("(c p) t -> p c t", p=128)[:, :, T * 128:(T + 1) * 128], r=gkeys, w=[gy_.name])
                if is_s:
                    V(lambda ao_=ao_: nc.vector.memset(ao_[:], 0.0), w=[ao_.name])
                    DMA(ao_[0:NS, :], ao_scr[T * 128:T * 128 + NS, :], r=[("ao", T)], w=[ao_.name])
                    V(lambda xi_=xi_: nc.vector.memset(xi_[:], 0.0), w=[xi_.name])
                    DMA(xi_[0:NS, :], xs[:, :], w=[xi_.name])
                else:
                    DMA(ao_[:], ao_scr[T * 128:(T + 1) * 128, :], r=[("ao", T)], w=[ao_.name])
                    DMA(xi_[:], xp[r0:r0 + 128, :], w=[xi_.name])
                DMA(ga_[:], gate_scr[T * 128:(T + 1) * 128, :], r=[("gate", T)], w=[ga_.name])
                for nh in range(2):
                    pv, pg = p_a[0], p_a[1]
                    for (pp, c0) in ((pv, nh * 512), (pg, 1024 + nh * 512)):
                        for kc in range(4):
                            PE(lambda pp=pp, c0=c0, kc=kc, gy_=gy_: nc.tensor.matmul(pp[:], lhsT=gy_[:, kc, :], rhs=Wglu[:, kc, c0:c0 + 512], start=(kc == 0), stop=(kc == 3)),
                               r=[gy_.name, "Wglu"], w=[pp.name], skip_same=True)
                    V(lambda nh=nh: nc.vector.tensor_tensor(out=zv[:], in0=p_a[0][:], in1=bglu[:, nh * 512:(nh + 1) * 512], op=ALU.add), r=[p_a[0].name, "bglu"], w=["zv"])
                    V(lambda nh=nh: nc.vector.tensor_tensor(out=zg[:], in0=p_a[1][:], in1=bglu[:, 1024 + nh * 512:1024 + (nh + 1) * 512], op=ALU.add),
                      r=[p_a[1].name, "bglu"], w=["zg"])
                    A(lambda: nc.scalar.activation(out=zg[:], in_=zg[:], func=AF.Sigmoid), r=["zg"], w=["zg"])
                    G(lambda nh=nh: nc.gpsimd.tensor_tensor(out=so[:, nh * 512:(nh + 1) * 512], in0=zv[:], in1=zg[:], op=ALU.mult), r=["zv", "zg"], w=["so"])
                for kc in range(4):
                    PE(lambda kc=kc, ao_=ao_: nc.tensor.transpose(out=p_tr4[:, kc, :], in_=ao_[:, kc * 128:(kc + 1) * 128], identity=ident[:]),
                       r=[ao_.name, "ident"], w=["p4_tr"], skip_same=True)
                A(lambda: nc.scalar.copy(out=aoT[:], in_=p_tr4[:, 0:4, :]), r=["p4_tr"], w=["aoT"])
                G(lambda ga_=ga_: nc.gpsimd.tensor_tensor(out=m1[:], in0=so[:], in1=ga_[:, 0:1024], op=ALU.mult), r=["so", ga_.name], w=["m1"])
                for nh in range(2):
                    pp = p_a[2 + nh]
                    for kc in range(4):
                        PE(lambda pp=pp, nh=nh, kc=kc: nc.tensor.matmul(pp[:], lhsT=aoT[:, kc, :], rhs=Wao[:, kc, nh * 512:(nh + 1) * 512], start=(kc == 0), stop=(kc == 3)),
                           r=["aoT", "Wao"], w=[pp.name], skip_same=True)
                    V(lambda pp=pp, nh=nh, ga_=ga_: nc.vector.tensor_tensor(out=m2[:], in0=pp[:], in1=ga_[:, 1024 + nh * 512:1024 + (nh + 1) * 512], op=ALU.mult),
                      r=[pp.name, ga_.name], w=["m2"])
                    V(lambda nh=nh: nc.vector.tensor_tensor(out=mixb[:, nh * 512:(nh + 1) * 512], in0=m1[:, nh * 512:(nh + 1) * 512], in1=m2[:], op=ALU.add),
                      r=["m1", "m2"], w=["mixb"])
                for kc in range(8):
                    PE(lambda kc=kc: nc.tensor.transpose(out=p_tr4[:, kc, :], in_=mixb[:, kc * 128:(kc + 1) * 128], identity=ident[:]),
                       r=["mixb", "ident"], w=["p4_tr"], skip_same=True)
                A(lambda: nc.scalar.copy(out=mixT[:], in_=p_tr4[:]), r=["p4_tr"], w=["mixT"])
                for nh in range(2):
                    pp = p_a[nh]
                    for kc in range(8):
                        PE(lambda pp=pp, nh=nh, kc=kc: nc.tensor.matmul(pp[:], lhsT=mixT[:, kc, :], rhs=Wo[:, kc, nh * 512:(nh + 1) * 512], start=(kc == 0), stop=(kc == 7)),
                           r=["mixT", "Wo"], w=[pp.name], skip_same=True)
                    V(lambda pp=pp, nh=nh, xi_=xi_, x2_=x2_: nc.vector.tensor_tensor(out=x2_[:, nh * 512:(nh + 1) * 512], in0=pp[:], in1=xi_[:, nh * 512:(nh + 1) * 512], op=ALU.add),
                      r=[pp.name, xi_.name], w=[x2_.name])
                DMA(x2_scr[T * 128:(T + 1) * 128, :], x2_[:], r=[x2_.name], w=[("x2", T)])
            S.barrier()

    if "p4" in phases:
        with contextlib.ExitStack() as e6:
            Wup = sb("Wup", [128, 8, D_FF], BF16, e6)
            Wdn = sb("Wdn", [128, 32, 1024], BF16, e6)
            g2c = sb("g2c", [128, 8], F32, e6)
            gfb = sb("gfb", [128, 1024], F32, e6)
            with nc.allow_non_contiguous_dma(reason="tiny gain vector"):
                DMA(g2c[:], norm2_g.rearrange("(k p) -> p k", p=128), w=["g2c"])
            DMA(gfb[:], normf_g.rearrange("(o n) -> o n", o=1).partition_broadcast(128), w=["gfb"])
            with contextlib.ExitStack() as e6w:
                load_weight_bf16(Wup, w_up, D_MODEL, D_FF, e6w, gcol=g2c, nm="wup")
                load_weight_bf16(Wdn, w_down, D_FF, 1024, e6w, nm="wdn")
                S.barrier()
            x2i = [sb(f"x2i{i}", [128, 1024], F32, e6) for i in range(2)]
            junk6 = sb("junk6", [128, 1024], F32, e6)
            ss6 = sb("ss6", [128, 1], F32, e6)
            rs6 = sb("rs6", [128, 1], F32, e6)
            hh = sb("hh", [128, 1024], BF16, e6)
            hhT = sb("hhT", [128, 8, 128], BF16, e6)
            rl = [sb(f"rl{i}", [128, 512], F32, e6) for i in range(2)]
            aT = sb("aT", [128, 32, 128], BF16, e6)
            x3 = sb("x3", [128, 1024], F32, e6)
            yt = [sb(f"yt{i}", [128, 1024], F32, e6) for i in range(2)]
            p_tr6 = ps("p6_tr", [128, 8, 128], BF16, e6)
            p_up = [ps(f"p6_up{i}", [128, 4, 128], F32, e6) for i in range(2)]
            p_dn = [ps(f"p6_dn{i}", [128, 512], F32, e6) for i in range(2)]
            for T in range(NTT):
                r0, nr, is_s, pti = tile_rows(T)
                xi_, y_ = x2i[T % 2], yt[T % 2]
                DMA(xi_[:], x2_scr[T * 128:(T + 1) * 128, :], r=[("x2", T)], w=[xi_.name])
                A(lambda xi_=xi_: nc.scalar.activation(out=junk6[:], in_=xi_[:], func=AF.Square, accum_out=ss6[:]), r=[xi_.name], w=["junk6", "ss6"])
                A(lambda: nc.scalar.activation(out=rs6[:], in_=ss6[:], func=AF.Sqrt, bias=eps_t[:], scale=1.0 / D_MODEL), r=["ss6", "eps_t"], w=["rs6"])
                V(lambda: nc.vector.reciprocal(out=rs6[:], in_=rs6[:]), r=["rs6"], w=["rs6"])
                V(lambda xi_=xi_: nc.vector.tensor_scalar(out=hh[:], in0=xi_[:], scalar1=rs6[:, 0:1], scalar2=None, op0=ALU.mult), r=[xi_.name, "rs6"], w=["hh"])
                for kc in range(8):
                    PE(lambda kc=kc: nc.tensor.transpose(out=p_tr6[:, kc, :], in_=hh[:, kc * 128:(kc + 1) * 128], identity=ident[:]),
                       r=["hh", "ident"], w=["p6_tr"], skip_same=True)
                A(lambda: nc.scalar.copy(out=hhT[:], in_=p_tr6[:]), r=["p6_tr"], w=["hhT"])
                for f4 in range(8):
                    pu = p_up[f4 % 2]
                    r_ = rl[f4 % 2]
                    for fi in range(4):
                        f = f4 * 4 + fi
                        for kc in range(8):
                            PE(lambda pu=pu, fi=fi, f=f, kc=kc: nc.tensor.matmul(pu[:, fi, :], lhsT=Wup[:, kc, f * 128:(f + 1) * 128], rhs=hhT[:, kc, :],
                                                                              start=(kc == 0), stop=(kc == 7)), r=["Wup", "hhT"], w=[pu.name], skip_same=True)
                    A(lambda pu=pu, r_=r_: nc.scalar.activation(out=r_[:], in_=pu[:].rearrange("p a b -> p (a b)"), func=AF.Relu), r=[pu.name], w=[r_.name])
                    V(lambda r_=r_, f4=f4: nc.vector.tensor_tensor(out=aT[:, f4 * 4:(f4 + 1) * 4, :].rearrange("p a b -> p (a b)"), in0=r_[:], in1=r_[:], op=ALU.mult),
                      r=[r_.name], w=["aT"])
                for nh in range(2):
                    pd = p_dn[nh]
                    for fk in range(32):
                        PE(lambda pd=pd, nh=nh, fk=fk: nc.tensor.matmul(pd[:], lhsT=aT[:, fk, :], rhs=Wdn[:, fk, nh * 512:(nh + 1) * 512], start=(fk == 0), stop=(fk == 31)),
                           r=["aT", "Wdn"], w=[pd.name], skip_same=True)
                    V(lambda pd=pd, nh=nh, xi_=xi_: nc.vector.tensor_tensor(out=x3[:, nh * 512:(nh + 1) * 512], in0=pd[:], in1=xi_[:, nh * 512:(nh + 1) * 512], op=ALU.add),
                      r=[pd.name, xi_.name], w=["x3"])
                A(lambda: nc.scalar.activation(out=junk6[:], in_=x3[:], func=AF.Square, accum_out=ss6[:]), r=["x3"], w=["junk6", "ss6"])
                A(lambda: nc.scalar.activation(out=rs6[:], in_=ss6[:], func=AF.Sqrt, bias=eps_t[:], scale=1.0 / D_MODEL), r=["ss6", "eps_t"], w=["rs6"])
                V(lambda: nc.vector.reciprocal(out=rs6[:], in_=rs6[:]), r=["rs6"], w=["rs6"])
                V(lambda y_=y_: nc.vector.scalar_tensor_tensor(out=y_[:], in0=x3[:], scalar=rs6[:, 0:1], in1=gfb[:], op0=ALU.mult, op1=ALU.mult),
                  r=["x3", "rs6", "gfb"], w=[y_.name])
                if is_s:
                    DMA(y_s[:, :], y_[0:NS, :], r=[y_.name], q="pool", is_output=True)
                else:
                    DMA(y_p[r0:r0 + 128, :], y_[:], r=[y_.name], q="pool", is_output=True)
            S.barrier()
    S.finish()
    es_all.close()
    dbg = dict(qT_scr=qT_scr, kT_scr=kT_scr)
    return nc


def make_in_map(inp, c, cfg):
    NB, NS = cfg.NB, cfg.NS
    f = lambda a: np.ascontiguousarray(a)
    m = {
        "xp": f(inp["x_prompt"][c * NB:(c + 1) * NB].reshape(NB * cfg.SEQ, D_MODEL)),
        "xs": f(inp["x_sample"][c * NS:(c + 1) * NS].reshape(NS, D_MODEL)),
        "cache_k": inp["cache_k"].reshape(cfg.NPOOL, -1),
        "cache_v": inp["cache_v"].reshape(cfg.NPOOL, -1),
        "cache_ik": inp["cache_idx_k"].reshape(cfg.NPOOL, -1),
        "st_re": f(inp["state_ssm_re"][c * NS:(c + 1) * NS].reshape(NS, -1)),
        "st_im": f(inp["state_ssm_im"][c * NS:(c + 1) * NS].reshape(NS, -1)),
        "ptab": f(inp["page_table"][c * NS:(c + 1) * NS].reshape(-1, 1)),
        "ssm_a_re": inp["ssm_a_re"].reshape(-1), "ssm_a_im": inp["ssm_a_im"].reshape(-1),
        "ssm_b_re": inp["ssm_b_re"].reshape(-1, 16), "ssm_b_im": inp["ssm_b_im"].reshape(-1, 16),
        "ssm_c_re": inp["ssm_c_re"].reshape(-1, 64), "ssm_c_im": inp["ssm_c_im"].reshape(-1, 64),
    }
    for k in ("norm1_g", "w_in", "ssm_log_dt", "ssm_d", "w_glu", "b_glu", "w_attn_out", "w_o", "norm2_g",
              "w_up", "w_down", "normf_g"):
        m[k] = inp[k]
    return m


ALL_PHASES = ("p1", "p2", "p3", "p3s", "p4")
OUT_NAMES = ["y_p", "y_s", "k_p", "v_p", "ki_p", "re_p", "im_p", "k_s", "v_s", "ki_s", "re_s", "im_s"]


def kernel(**inputs):
    inp = {k: np.asarray(v) for k, v in inputs.items()}
    B, SEQ = inp["x_prompt"].shape[0], inp["x_prompt"].shape[1]
    NSAMP = inp["x_sample"].shape[0]
    n_cores = 8
    cfg = Cfg(seq=SEQ, past=inp["page_table"].shape[1] * 128, nb=B // n_cores, ns=NSAMP // n_cores,
              n_pool=inp["cache_k"].shape[0])
    nc = build(cfg, phases=ALL_PHASES)
    shared = {"cache_k": inp["cache_k"].reshape(cfg.NPOOL, -1), "cache_v": inp["cache_v"].reshape(cfg.NPOOL, -1),
              "cache_ik": inp["cache_idx_k"].reshape(cfg.NPOOL, -1)}
    in_maps = []
    for c in range(n_cores):
        m = make_in_map(inp, c, cfg)
        m.update(shared)
        in_maps.append(m)
    res = run_bass_kernel_spmd(nc, in_maps, core_ids=list(range(n_cores)))
    outs = {n: np.concatenate([np.asarray(res.results[c][n]) for c in range(n_cores)], axis=0) for n in OUT_NAMES}
    NB, NS = cfg.NB, cfg.NS
    f32 = np.float32
    return (
        outs["y_p"].reshape(B, SEQ, D_MODEL).astype(f32, copy=False),
        outs["y_s"].reshape(NSAMP, 1, D_MODEL).astype(f32, copy=False),
        outs["k_p"].reshape(B, SEQ, 2, 64).astype(f32, copy=False),
        outs["v_p"].reshape(B, SEQ, 2, 64).astype(f32, copy=False),
        outs["ki_p"].reshape(B, SEQ, 64).astype(f32, copy=False),
        outs["re_p"].reshape(B, N_G, P_ST).astype(f32, copy=False),
        outs["im_p"].reshape(B, N_G, P_ST).astype(f32, copy=False),
        outs["k_s"].reshape(NSAMP, 1, 2, 64).astype(f32, copy=False),
        outs["v_s"].reshape(NSAMP, 1, 2, 64).astype(f32, copy=False),
        outs["ki_s"].reshape(NSAMP, 1, 64).astype(f32, copy=False),
        outs["re_s"].reshape(NSAMP, N_G, P_ST).astype(f32, copy=False),
        outs["im_s"].reshape(NSAMP, N_G, P_ST).astype(f32, copy=False),
    )
```

```python
import math
import contextlib
import numpy as np
import concourse.bass as bass
import concourse.mybir as mybir
from concourse.bass_utils import run_bass_kernel_spmd

F32 = mybir.dt.float32
BF16 = mybir.dt.bfloat16
I32 = mybir.dt.int32
U32 = mybir.dt.uint32
ALU = mybir.AluOpType
AF = mybir.ActivationFunctionType
AX = mybir.AxisListType

D_MODEL = 1024
D_SSM = 512
N_G = 32
P_ST = 64
N_HEADS = 8
N_KV = 2
HD = 64
D_ATTN = 512
N_IH = 8
IDX_D = 64
D_FF = 4096
IN_COLS = 3912
EPS = 1e-6
ROPE_THETA = 500000.0
NEG = -1.0e30
RELAX_SAME = False
RELAX_ENGINES = ('dve', 'act', 'pe')
BIGM = -240000.0
C_U, C_Q, C_K, C_V, C_QI, C_KI, C_WI, C_GS, C_GA = 0, 512, 1024, 1152, 1280, 1792, 1856, 1864, 2888


class Sync:
    def __init__(self, nc):
        self.nc = nc
        self.eng = {"pe": nc.tensor, "dve": nc.vector, "act": nc.scalar, "pool": nc.gpsimd, "sp": nc.sync}
        self.sem = {k: nc.alloc_semaphore("sem_" + k) for k in self.eng}
        self.cnt = {k: 0 for k in self.eng}
        self.waited = {k: {} for k in self.eng}
        self.R = 12
        self.dring = {q: [nc.alloc_semaphore(f"dsem_{q}{i}") for i in range(self.R)] for q in ("sp", "pool", "act")}
        self.dn = {q: 0 for q in self.dring}
        self.lastw = {}
        self.readers = {}
        self.semobj = {}
        for k, s in self.sem.items():
            self.semobj[id(s)] = s
        for q in self.dring:
            for s in self.dring[q]:
                self.semobj[id(s)] = s
        self.out_events = []
        self.relax_same = RELAX_SAME

    def _wait(self, e, ev):
        s, v = ev
        w = self.waited[e]
        if w.get(id(s), 0) >= v:
            return
        self.eng[e].wait_ge(s, v)
        w[id(s)] = v

    def _deps(self, e, reads, writes, skip_same=False):
        evs = []
        for k in reads:
            if k in self.lastw:
                evs.append(self.lastw[k])
        for k in writes:
            if k in self.lastw:
                evs.append(self.lastw[k])
            for r in self.readers.get(k, ()):
                evs.append(r)
        for ev in evs:
            if (skip_same or (self.relax_same and e in RELAX_ENGINES)) and e in self.sem and ev[0] is self.sem[e]:
                continue
            self._wait(e, ev)

    def _record(self, ev, reads, writes):
        for k in reads:
            self.readers.setdefault(k, []).append(ev)
            if len(self.readers[k]) > 24:
                best = {}
                for s, v in self.readers[k]:
                    if id(s) not in best or best[id(s)][1] < v:
                        best[id(s)] = (s, v)
                self.readers[k] = list(best.values())
        for k in writes:
            self.lastw[k] = ev
            self.readers[k] = []

    def op(self, e, fn, reads=(), writes=(), skip_same=False):
        self._deps(e, reads, writes, skip_same)
        ins = fn()
        self.cnt[e] += 1
        ins.then_inc(self.sem[e], 1)
        ev = (self.sem[e], self.cnt[e])
        self._record(ev, reads, writes)
        return ev

    def dma(self, q, fn, reads=(), writes=(), is_output=False):
        n = self.dn[q]
        slot = n % self.R
        s = self.dring[q][slot]
        prev = 16 * (n // self.R)
        if prev > 0:
            self._wait(q, (s, prev))
        self._deps(q, reads, writes)
        ins = fn()
        ins.then_inc(s, 16)
        self.dn[q] = n + 1
        ev = (s, prev + 16)
        self._record(ev, reads, writes)
        if is_output:
            self.out_events.append(ev)
        return ev

    def barrier(self):
        evs = []
        for q in self.dring:
            n = self.dn[q]
            for slot in range(self.R):
                cnt = (n - slot + self.R - 1) // self.R if n > slot else 0
                if cnt > 0:
                    evs.append((self.dring[q][slot], 16 * cnt))
        for e in self.eng:
            if self.cnt[e] > 0:
                evs.append((self.sem[e], self.cnt[e]))
        for e in self.eng:
            for ev in evs:
                if ev[0] is self.sem[e]:
                    continue
                self._wait(e, ev)

    def finish(self):
        for q in self.dring:
            n = self.dn[q]
            for slot in range(self.R):
                cnt = (n - slot + self.R - 1) // self.R if n > slot else 0
                if cnt > 0:
                    self._wait("sp", (self.dring[q][slot], 16 * cnt))
        for e in self.eng:
            if e != "sp" and self.cnt[e] > 0:
                self._wait("sp", (self.sem[e], self.cnt[e]))


class Cfg:
    def __init__(self, seq=4096, past=8192, nb=2, ns=16, n_pool=10240):
        self.SEQ = seq
        self.PAST = past
        self.NB = nb
        self.NS = ns
        self.NPOOL = n_pool
        self.NT = seq // 128
        self.NPG = past // 128
        self.TOPK = min(256, seq // 4)
        self.TOPK_S = min(256, (past + 1) // 4)
        self.NTOK = nb * seq
        self.NTT = nb * self.NT + 1
        self.NTOKP = self.NTT * 128


def build(cfg, phases=("p1",), debug=False):
    nc = bass.Bass("TRN2", target_bir_lowering=False)
    S = Sync(nc)
    NB, SEQ, NS, NT = cfg.NB, cfg.SEQ, cfg.NS, cfg.NT
    NTT, NTOKP = cfg.NTT, cfg.NTOKP

    def din(name, shape, dt=F32):
        return nc.dram_tensor(name, list(shape), dt, kind="ExternalInput").ap()

    def dout(name, shape, dt=F32):
        return nc.dram_tensor(name, list(shape), dt, kind="ExternalOutput").ap()

    def dscr(name, shape, dt=F32):
        return nc.dram_tensor(name, list(shape), dt, kind="ExternalOutput" if debug else "Internal").ap()

    xp = din("xp", [NB * SEQ, D_MODEL])
    xs = din("xs", [NS, D_MODEL])
    cache_k = din("cache_k", [cfg.NPOOL, 128 * 128])
    cache_v = din("cache_v", [cfg.NPOOL, 128 * 128])
    cache_ik = din("cache_ik", [cfg.NPOOL, 128 * 64])
    st_re = din("st_re", [NS, N_G * P_ST])
    st_im = din("st_im", [NS, N_G * P_ST])
    ptab = din("ptab", [NS * cfg.NPG, 1], I32)
    norm1_g = din("norm1_g", [D_MODEL])
    w_in = din("w_in", [D_MODEL, IN_COLS])
    a_re = din("ssm_a_re", [N_G * P_ST])
    a_im = din("ssm_a_im", [N_G * P_ST])
    log_dt = din("ssm_log_dt", [N_G])
    b_re = din("ssm_b_re", [N_G * P_ST, 16])
    b_im = din("ssm_b_im", [N_G * P_ST, 16])
    c_re = din("ssm_c_re", [N_G * 16, P_ST])
    c_im = din("ssm_c_im", [N_G * 16, P_ST])
    ssm_d = din("ssm_d", [D_SSM])
    w_glu = din("w_glu", [D_SSM, 2 * D_MODEL])
    b_glu = din("b_glu", [2 * D_MODEL])
    w_ao = din("w_attn_out", [D_ATTN, D_MODEL])
    w_o = din("w_o", [D_MODEL, D_MODEL])
    norm2_g = din("norm2_g", [D_MODEL])
    w_up = din("w_up", [D_MODEL, D_FF])
    w_down = din("w_down", [D_FF, D_MODEL])
    normf_g = din("normf_g", [D_MODEL])

    y_p = dout("y_p", [NB * SEQ, D_MODEL])
    y_s = dout("y_s", [NS, D_MODEL])
    k_p = dout("k_p", [NB * SEQ, 128])
    v_p = dout("v_p", [NB * SEQ, 128])
    ki_p = dout("ki_p", [NB * SEQ, 64])
    re_p = dout("re_p", [NB, N_G * P_ST])
    im_p = dout("im_p", [NB, N_G * P_ST])
    k_s = dout("k_s", [NS, 128])
    v_s = dout("v_s", [NS, 128])
    ki_s = dout("ki_s", [NS, 64])
    re_s = dout("re_s", [NS, N_G * P_ST])
    im_s = dout("im_s", [NS, N_G * P_ST])

    qT_scr = dscr("qT_scr", [NTT, 65, 8, 128], BF16)
    qiT_scr = dscr("qiT_scr", [NTT, 64, 8, 128], BF16)
    kT_scr = dscr("kT_scr", [64, 2, NTOKP], BF16)
    kiT_scr = dscr("kiT_scr", [64, NTOKP], BF16)
    vb_scr = dscr("vb_scr", [NTOKP, 2, 64], BF16)
    uT_scr = dscr("uT_scr", [D_SSM, NTOKP], F32)
    gate_scr = dscr("gate_scr", [NTOKP, 2048], F32)
    gyT_scr = dscr("gyT_scr", [D_SSM, NTOKP], BF16)
    ao_scr = dscr("ao_scr", [NTOKP, D_ATTN], BF16)
    x2_scr = dscr("x2_scr", [NTOKP, D_MODEL], F32)

    if debug:
        dbg_mb = dscr("dbg_mb", [128, SEQ], BF16)
        dbg_sc = dscr("dbg_sc", [128, SEQ], F32)
        dbg_thr = dscr("dbg_thr", [128, 1], F32)
    ident = nc.alloc_sbuf_tensor("ident", [128, 128], BF16)
    identf = nc.alloc_sbuf_tensor("identf", [128, 128], F32)
    cmask = nc.alloc_sbuf_tensor("cmask", [128, 128], F32)
    eps_t = nc.alloc_sbuf_tensor("eps_t", [128, 1], F32)
    cosT = nc.alloc_sbuf_tensor("cosT", [128, NT + 1, 8], F32)
    sinT = nc.alloc_sbuf_tensor("sinT", [128, NT + 1, 8], F32)
    lohi = nc.alloc_sbuf_tensor("lohi", [128, NTT, 16], F32)
    kn2 = nc.alloc_sbuf_tensor("kn2", [128, NTT, 2], F32)

    def V(fn, r=(), w=(), **kw):
        return S.op("dve", fn, r, w, **kw)

    def A(fn, r=(), w=(), **kw):
        return S.op("act", fn, r, w, **kw)

    def G(fn, r=(), w=(), **kw):
        return S.op("pool", fn, r, w, **kw)

    def PE(fn, r=(), w=(), **kw):
        return S.op("pe", fn, r, w, **kw)

    def DMA(out, in_, r=(), w=(), q="sp", is_output=False, **kw):
        return S.dma(q, lambda: S.eng[q].dma_start(out=out, in_=in_, **kw), r, w, is_output=is_output)

    ones_f = nc.alloc_sbuf_tensor("ones_f", [128, 128], F32)
    G(lambda: nc.gpsimd.memset(ones_f[:], 1.0), w=["ones_f"])
    G(lambda: nc.gpsimd.memset(eps_t[:], EPS), w=["eps_t"])
    G(lambda: nc.gpsimd.affine_select(out=identf[:], in_=ones_f[:], pattern=[[-1, 128]], compare_op=ALU.is_equal,
                                      fill=0.0, base=0, channel_multiplier=1), r=["ones_f"], w=["identf"])
    V(lambda: nc.vector.tensor_copy(out=ident[:], in_=identf[:]), r=["identf"], w=["ident"])
    zer_f = nc.alloc_sbuf_tensor("zer_f", [128, 128], F32)
    G(lambda: nc.gpsimd.memset(zer_f[:], 0.0), w=["zer_f"])
    G(lambda: nc.gpsimd.affine_select(out=cmask[:], in_=zer_f[:], pattern=[[-1, 128]], compare_op=ALU.is_ge,
                                      fill=NEG, base=0, channel_multiplier=1), r=["zer_f"], w=["cmask"])

    with contextlib.ExitStack() as es:
        posi = es.enter_context(nc.sbuf_tensor("posi", [128, NT + 1], I32))
        posf = es.enter_context(nc.sbuf_tensor("posf", [128, NT + 1], F32))
        ang = es.enter_context(nc.sbuf_tensor("ang", [128, NT + 1, 8], F32))
        ki_ = es.enter_context(nc.sbuf_tensor("kint", [128, NT + 1, 8], I32))
        kf = es.enter_context(nc.sbuf_tensor("kf", [128, NT + 1, 8], F32))
        rr = es.enter_context(nc.sbuf_tensor("rr", [128, NT + 1, 8], F32))
        r2 = es.enter_context(nc.sbuf_tensor("r2", [128, NT + 1, 8], F32))
        mk = es.enter_context(nc.sbuf_tensor("mk", [128, NT + 1, 8], F32))
        G(lambda: nc.gpsimd.iota(posi[:, 0:NT], pattern=[[128, NT]], base=0, channel_multiplier=1), w=["posi"])
        G(lambda: nc.gpsimd.iota(posi[:, NT:NT + 1], pattern=[[0, 1]], base=cfg.PAST, channel_multiplier=0), w=["posi"])
        V(lambda: nc.vector.tensor_copy(out=posf[:], in_=posi[:]), r=["posi"], w=["posf"])
        for j in range(8):
            inv = ROPE_THETA ** (-j / 8.0)
            V(lambda j=j, inv=inv: nc.vector.tensor_scalar(out=ang[:, :, j], in0=posf[:], scalar1=float(np.float32(inv)),
                                                           scalar2=float(1.0 / (2 * math.pi)), op0=ALU.mult, op1=ALU.mult),
              r=["posf"], w=["ang"])

        def wrap_sin(dst, off):
            V(lambda: nc.vector.tensor_scalar(out=rr[:], in0=ang[:], scalar1=float(off), scalar2=None, op0=ALU.add),
              r=["ang"], w=["rr"])
            V(lambda: nc.vector.tensor_copy(out=ki_[:], in_=rr[:]), r=["rr"], w=["kint"])
            V(lambda: nc.vector.tensor_copy(out=kf[:], in_=ki_[:]), r=["kint"], w=["kf"])
            V(lambda: nc.vector.tensor_tensor(out=r2[:], in0=rr[:], in1=kf[:], op=ALU.subtract), r=["rr", "kf"], w=["r2"])
            V(lambda: nc.vector.tensor_scalar(out=mk[:], in0=r2[:], scalar1=0.5, scalar2=None, op0=ALU.is_gt), r=["r2"], w=["mk"])
            V(lambda: nc.vector.tensor_tensor(out=r2[:], in0=r2[:], in1=mk[:], op=ALU.subtract), r=["r2", "mk"], w=["r2"])
            V(lambda: nc.vector.tensor_scalar(out=mk[:], in0=r2[:], scalar1=-0.5, scalar2=None, op0=ALU.is_lt), r=["r2"], w=["mk"])
            V(lambda: nc.vector.tensor_tensor(out=r2[:], in0=r2[:], in1=mk[:], op=ALU.add), r=["r2", "mk"], w=["r2"])
            A(lambda: nc.scalar.activation(out=dst[:], in_=r2[:], func=AF.Sin, scale=float(2 * math.pi)), r=["r2"], w=[dst.name])

        wrap_sin(sinT, 0.0)
        wrap_sin(cosT, 0.25)
        S.barrier()

    es_all = contextlib.ExitStack()

    def sb(name, shape, dt=F32, stack=None):
        return (stack or es_all).enter_context(nc.sbuf_tensor(name, list(shape), dt))

    def ps(name, shape, dt=F32, stack=None):
        return (stack or es_all).enter_context(nc.psum_tensor(name, list(shape), dt))

    def load_weight_bf16(dst, src, K, N, stack, gcol=None, nm="w", NSTG=2):
        KC = K // 128
        CH = 2048
        stg = [sb(f"stg_{nm}{i}", [128, CH], F32, stack) for i in range(NSTG)]
        i = 0
        for kc in range(KC):
            for c0 in range(0, N, CH):
                cw = min(CH, N - c0)
                st = stg[i % NSTG]
                DMA(st[:, 0:cw], src[kc * 128:(kc + 1) * 128, c0:c0 + cw], w=[st.name], q=("sp", "act")[i % 2])
                eng = ("dve", "pool")[i % 2]
                if gcol is None:
                    if eng == "dve":
                        V(lambda st=st, kc=kc, c0=c0, cw=cw: nc.vector.tensor_copy(out=dst[:, kc, c0:c0 + cw], in_=st[:, 0:cw]),
                          r=[st.name], w=[dst.name])
                    else:
                        G(lambda st=st, kc=kc, c0=c0, cw=cw: nc.gpsimd.tensor_copy(out=dst[:, kc, c0:c0 + cw], in_=st[:, 0:cw]),
                          r=[st.name], w=[dst.name])
                else:
                    V(lambda st=st, kc=kc, c0=c0, cw=cw: nc.vector.tensor_scalar(out=dst[:, kc, c0:c0 + cw], in0=st[:, 0:cw],
                                                                              scalar1=gcol[:, kc:kc + 1], scalar2=None, op0=ALU.mult),
                      r=[st.name, gcol.name], w=[dst.name])
                i += 1

    def tile_rows(T):
        if T < NB * NT:
            return T * 128, 128, False, T % NT
        return 0, NS, True, NT

    if "p1" in phases:
        with contextlib.ExitStack() as e1:
            Win = sb("Win", [128, 8, IN_COLS], BF16, e1)
            g1c = sb("g1c", [128, 8], F32, e1)
            with nc.allow_non_contiguous_dma(reason="tiny gain vector"):
                DMA(g1c[:], norm1_g.rearrange("(k p) -> p k", p=128), w=["g1c"])
            with contextlib.ExitStack() as e1w:
                load_weight_bf16(Win, w_in, D_MODEL, IN_COLS, e1w, gcol=g1c, nm="win", NSTG=6)
                S.barrier()
            xt = [sb(f"xt{i}", [128, D_MODEL], F32, e1) for i in range(2)]
            junk = sb("junk1", [128, D_MODEL], F32, e1)
            ss = sb("ss", [128, 1], F32, e1)
            rstd = sb("rstd", [128, 1], F32, e1)
            hb = sb("hb", [128, D_MODEL], BF16, e1)
            hT = sb("hT", [128, 8, 128], BF16, e1)
            pt = [sb(f"pt{i}", [128, IN_COLS - 512], F32, e1) for i in range(2)]
            uTs = sb("uTs", [128, 4, 128], F32, e1)
            tA = [sb(f"tA{i}", [128, 10, 8], F32, e1) for i in range(6)]
            qn = sb("qn", [128, 8], F32, e1)
            sq = sb("sq", [128, 10, 64], F32, e1)
            wp = sb("wp", [128, 8], F32, e1)
            qa = sb("qa", [128, 8, 65], BF16, e1)
            kb_ = sb("kb_", [128, 2, 64], BF16, e1)
            qib = sb("qib", [128, 8, 64], BF16, e1)
            kib = sb("kib", [128, 64], BF16, e1)
            vbt = sb("vbt", [128, 2, 64], BF16, e1)
            gts = sb("gts", [128, 2048], F32, e1)
            trs = sb("trs", [65, 19, 128], BF16, e1)
            p_tr = ps("p_tr", [128, 8, 128], BF16, e1)
            p_mm = [ps(f"p_mm{i}", [128, 512], F32, e1) for i in range(3)]
            p_u = ps("p_u", [128, 4, 128], F32, e1)
            p_t2 = ps("p_t2", [65, 19, 128], BF16, e1)

            for T in range(NTT):
                r0, nr, is_s, pti = tile_rows(T)
                x_ = xt[T % 2]
                p_ = pt[T % 2]
                xn, pn = x_.name, p_.name
                if is_s:
                    V(lambda x_=x_: nc.vector.memset(x_[:], 0.0), w=[xn])
                    DMA(x_[0:NS, :], xs[:, :], w=[xn])
                else:
                    DMA(x_[:], xp[r0:r0 + 128, :], w=[xn])
                A(lambda x_=x_: nc.scalar.activation(out=junk[:], in_=x_[:], func=AF.Square, accum_out=ss[:]),
                  r=[xn], w=["junk1", "ss"])
                A(lambda: nc.scalar.activation(out=rstd[:], in_=ss[:], func=AF.Sqrt, bias=eps_t[:], scale=1.0 / D_MODEL),
                  r=["ss", "eps_t"], w=["rstd"])
                V(lambda: nc.vector.reciprocal(out=rstd[:], in_=rstd[:]), r=["rstd"], w=["rstd"])
                V(lambda x_=x_: nc.vector.tensor_scalar(out=hb[:], in0=x_[:], scalar1=rstd[:, 0:1], scalar2=None, op0=ALU.mult),
                  r=[xn, "rstd"], w=["hb"])
                for kc in range(8):
                    PE(lambda kc=kc: nc.tensor.transpose(out=p_tr[:, kc, :], in_=hb[:, kc * 128:(kc + 1) * 128], identity=ident[:]),
                       r=["hb", "ident"], w=["p_tr"], skip_same=True)
                A(lambda: nc.scalar.copy(out=hT[:], in_=p_tr[:]), r=["p_tr"], w=["hT"])
                for ct in range(4):
                    for kc in range(8):
                        PE(lambda ct=ct, kc=kc: nc.tensor.matmul(p_u[:, ct, :], lhsT=Win[:, kc, ct * 128:(ct + 1) * 128],
                                                                 rhs=hT[:, kc, :], start=(kc == 0), stop=(kc == 7)),
                           r=["Win", "hT"], w=["p_u"], skip_same=True)
                V(lambda: nc.vector.tensor_copy(out=uTs[:], in_=p_u[:]), r=["p_u"], w=["uTs"])
                DMA(uT_scr.rearrange("(c p) t -> p c t", p=128)[:, :, T * 128:(T + 1) * 128], uTs[:], r=["uTs"], w=[("uT", T)])
                ci = 0
                for c0 in range(512, IN_COLS, 512):
                    cw = min(512, IN_COLS - c0)
                    pm = p_mm[ci % 3]
                    for kc in range(8):
                        PE(lambda pm=pm, kc=kc, c0=c0, cw=cw: nc.tensor.matmul(pm[:, 0:cw], lhsT=hT[:, kc, :], rhs=Win[:, kc, c0:c0 + cw],
                                                                               start=(kc == 0), stop=(kc == 7)),
                           r=["Win", "hT"], w=[pm.name], skip_same=True)
                    if ci % 2 == 0:
                        A(lambda pm=pm, c0=c0, cw=cw, p_=p_: nc.scalar.copy(out=p_[:, c0 - 512:c0 - 512 + cw], in_=pm[:, 0:cw]),
                          r=[pm.name], w=[pn])
                    else:
                        V(lambda pm=pm, c0=c0, cw=cw, p_=p_: nc.vector.tensor_copy(out=p_[:, c0 - 512:c0 - 512 + cw], in_=pm[:, 0:cw]),
                          r=[pm.name], w=[pn])
                    ci += 1
                cs_c = cosT[:, pti, :]
                cs_s = sinT[:, pti, :]
                for (cb, H) in ((C_Q - 512, 10), (C_QI - 512, 9)):
                    X = p_[:, cb:cb + H * 64].rearrange("p (h d) -> p h d", d=64)
                    x1 = X[:, :, 0:8]
                    x2 = X[:, :, 8:16]
                    cB = cs_c.unsqueeze(1).to_broadcast([128, H, 8])
                    sB = cs_s.unsqueeze(1).to_broadcast([128, H, 8])
                    t = [tt[:, 0:H, :] for tt in tA]
                    V(lambda x1=x1, cB=cB, t=t: nc.vector.tensor_tensor(out=t[0], in0=x1, in1=cB, op=ALU.mult), r=[pn, "cosT"], w=["tA0"])
                    V(lambda x2=x2, sB=sB, t=t: nc.vector.tensor_tensor(out=t[1], in0=x2, in1=sB, op=ALU.mult), r=[pn, "sinT"], w=["tA1"])
                    V(lambda x1=x1, sB=sB, t=t: nc.vector.tensor_tensor(out=t[2], in0=x1, in1=sB, op=ALU.mult), r=[pn, "sinT"], w=["tA2"])
                    V(lambda x2=x2, cB=cB, t=t: nc.vector.tensor_tensor(out=t[3], in0=x2, in1=cB, op=ALU.mult), r=[pn, "cosT"], w=["tA3"])
                    V(lambda x1=x1, t=t: nc.vector.tensor_tensor(out=x1, in0=t[0], in1=t[1], op=ALU.subtract), r=["tA0", "tA1"], w=[pn])
                    V(lambda x2=x2, t=t: nc.vector.tensor_tensor(out=x2, in0=t[2], in1=t[3], op=ALU.add), r=["tA2", "tA3"], w=[pn])
                kcol, vcol, kicol = C_K - 512, C_V - 512, C_KI - 512
                if is_s:
                    DMA(k_s[:, :], p_[0:NS, kcol:kcol + 128], r=[pn], q="pool", is_output=True)
                    DMA(v_s[:, :], p_[0:NS, vcol:vcol + 128], r=[pn], q="pool", is_output=True)
                    DMA(ki_s[:, :], p_[0:NS, kicol:kicol + 64], r=[pn], q="pool", is_output=True)
                else:
                    DMA(k_p[r0:r0 + 128, :], p_[:, kcol:kcol + 128], r=[pn], q="pool", is_output=True)
                    DMA(v_p[r0:r0 + 128, :], p_[:, vcol:vcol + 128], r=[pn], q="pool", is_output=True)
                    DMA(ki_p[r0:r0 + 128, :], p_[:, kicol:kicol + 64], r=[pn], q="pool", is_output=True)
                QK = p_[:, C_Q - 512:C_Q - 512 + 640].rearrange("p (h d) -> p h d", d=64)
                G(lambda QK=QK: nc.gpsimd.tensor_tensor(out=sq[:], in0=QK, in1=QK, op=ALU.mult), r=[pn], w=["sq"])
                V(lambda: nc.vector.tensor_reduce(out=qn[:], in_=sq[:, 0:8, :], axis=AX.X, op=ALU.add), r=["sq"], w=["qn"])
                V(lambda T=T: nc.vector.tensor_reduce(out=kn2[:, T, :], in_=sq[:, 8:10, :], axis=AX.X, op=ALU.add), r=["sq"], w=["kn2"])
                A(lambda: nc.scalar.activation(out=qn[:], in_=qn[:], func=AF.Sqrt), r=["qn"], w=["qn"])
                V(lambda: nc.vector.tensor_scalar(out=qa[:, :, 64], in0=qn[:], scalar1=-1.0, scalar2=None, op0=ALU.mult),
                  r=["qn"], w=["qa"])
                V(lambda QK=QK: nc.vector.tensor_copy(out=qa[:, :, 0:64], in_=QK[:, 0:8, :]), r=[pn], w=["qa"])
                G(lambda QK=QK: nc.gpsimd.tensor_copy(out=kb_[:], in_=QK[:, 8:10, :]), r=[pn], w=["kb_"])
                wic = C_WI - 512
                V(lambda p_=p_, wic=wic: nc.vector.tensor_scalar(out=wp[:], in0=p_[:, wic:wic + 8], scalar1=float(8 ** -0.5 * 0.125),
                                                                 scalar2=None, op0=ALU.mult), r=[pn], w=["wp"])
                V(lambda T=T: nc.vector.tensor_scalar(out=lohi[:, T, 0:8], in0=wp[:], scalar1=0.0, scalar2=-3.0e38,
                                                      op0=ALU.is_le, op1=ALU.mult), r=["wp"], w=["lohi"])
                V(lambda T=T: nc.vector.tensor_scalar(out=lohi[:, T, 8:16], in0=wp[:], scalar1=0.0, scalar2=3.0e38,
                                                      op0=ALU.is_gt, op1=ALU.mult), r=["wp"], w=["lohi"])
                QI = p_[:, C_QI - 512:C_QI - 512 + 512].rearrange("p (h d) -> p h d", d=64)
                V(lambda QI=QI: nc.vector.tensor_tensor(out=qib[:], in0=QI, in1=wp[:].unsqueeze(2).to_broadcast([128, 8, 64]), op=ALU.mult),
                  r=[pn, "wp"], w=["qib"])
                G(lambda p_=p_: nc.gpsimd.tensor_copy(out=kib[:], in_=p_[:, C_KI - 512:C_KI - 512 + 64]), r=[pn], w=["kib"])
                G(lambda p_=p_: nc.gpsimd.tensor_copy(out=vbt[:], in_=p_[:, vcol:vcol + 128].rearrange("p (g d) -> p g d", d=64)),
                  r=[pn], w=["vbt"])
                DMA(vb_scr[T * 128:(T + 1) * 128, :, :], vbt[:], r=["vbt"], w=[("vb", T)])
                A(lambda p_=p_: nc.scalar.activation(out=gts[:], in_=p_[:, C_GS - 512:C_GS - 512 + 2048], func=AF.Sigmoid),
                  r=[pn], w=["gts"])
                DMA(gate_scr[T * 128:(T + 1) * 128, :], gts[:], r=["gts"], w=[("gate", T)])
                for h in range(8):
                    PE(lambda h=h: nc.tensor.transpose(out=p_t2[0:65, h, :], in_=qa[:, h, :], identity=ident[:]),
                       r=["qa", "ident"], w=["p_t2"], skip_same=True)
                for g in range(2):
                    PE(lambda g=g: nc.tensor.transpose(out=p_t2[0:64, 8 + g, :], in_=kb_[:, g, :], identity=ident[:]),
                       r=["kb_", "ident"], w=["p_t2"], skip_same=True)
                for h in range(8):
                    PE(lambda h=h: nc.tensor.transpose(out=p_t2[0:64, 10 + h, :], in_=qib[:, h, :], identity=ident[:]),
                       r=["qib", "ident"], w=["p_t2"], skip_same=True)
                PE(lambda: nc.tensor.transpose(out=p_t2[0:64, 18, :], in_=kib[:], identity=ident[:]),
                   r=["kib", "ident"], w=["p_t2"], skip_same=True)
                V(lambda: nc.vector.tensor_copy(out=trs[0:65, 0:8, :], in_=p_t2[0:65, 0:8, :]), r=["p_t2"], w=["trs"])
                A(lambda: nc.scalar.copy(out=trs[0:64, 8:19, :], in_=p_t2[0:64, 8:19, :]), r=["p_t2"], w=["trs"])
                DMA(qT_scr[T, :, :, :], trs[0:65, 0:8, :], r=["trs"], w=[("qT", T)])
                DMA(kT_scr[:, :, T * 128:(T + 1) * 128], trs[0:64, 8:10, :], r=["trs"], w=[("kT", T)])
                DMA(qiT_scr[T, :, :, :], trs[0:64, 10:18, :], r=["trs"], w=[("qiT", T)])
                DMA(kiT_scr[:, T * 128:(T + 1) * 128], trs[0:64, 18, :], r=["trs"], w=[("kiT", T)])
            S.barrier()


    if "p2" in phases:
        with contextlib.ExitStack() as e2:
            NLEV = int(math.log2(SEQ))
            assert (1 << NLEV) == SEQ
            NCH = SEQ // 512
            sm = {}

            def small(name, shape=(128, 16), dt=F32):
                t_ = sb("s2_" + name, list(shape), dt, e2)
                sm[name] = t_
                return t_

            def vtt(o, a, b, op):
                V(lambda: nc.vector.tensor_tensor(out=o[:], in0=a[:], in1=b[:], op=op), r=[a.name, b.name], w=[o.name])

            def vts(o, a, s1, op, s2=None, op1=None):
                if op1 is None:
                    V(lambda: nc.vector.tensor_scalar(out=o[:], in0=a[:], scalar1=s1, scalar2=None, op0=op), r=[a.name], w=[o.name])
                else:
                    V(lambda: nc.vector.tensor_scalar(out=o[:], in0=a[:], scalar1=s1, scalar2=s2, op0=op, op1=op1), r=[a.name], w=[o.name])

            are_t = small("are"); aim_t = small("aim"); ldt = small("ldt"); dtt = small("dt")
            xre = small("xre"); th = small("th"); lam_abs = small("lam_abs")
            cs_ = small("cs"); sn_ = small("sn"); lam_re = small("lam_re"); lam_im = small("lam_im")
            tq = [small(f"tq{i}") for i in range(6)]
            tqi = small("tqi", dt=I32)
            cf_re = small("cf_re"); cf_im = small("cf_im")
            with nc.allow_non_contiguous_dma(reason="tiny ssm params"):
                DMA(are_t[:], a_re.rearrange("(ft f) -> f ft", f=128), w=[are_t.name])
                DMA(aim_t[:], a_im.rearrange("(ft f) -> f ft", f=128), w=[aim_t.name])
                lv = log_dt.rearrange("(ft two) -> two ft", two=2)
                DMA(ldt[0:64, :], lv[0:1, :].partition_broadcast(64), w=[ldt.name])
                DMA(ldt[64:128, :], lv[1:2, :].partition_broadcast(64), w=[ldt.name])
            A(lambda: nc.scalar.activation(out=dtt[:], in_=ldt[:], func=AF.Exp), r=[ldt.name], w=[dtt.name])
            vtt(xre, are_t, dtt, ALU.mult)
            vtt(th, aim_t, dtt, ALU.mult)
            A(lambda: nc.scalar.activation(out=lam_abs[:], in_=xre[:], func=AF.Exp), r=[xre.name], w=[lam_abs.name])

            def sincos_turns(dst, src_turns, off):
                a_, k_, r_, m_ = tq[0], tq[1], tq[2], tq[3]
                vts(a_, src_turns, float(off), ALU.add)
                V(lambda: nc.vector.tensor_copy(out=tqi[:], in_=a_[:]), r=[a_.name], w=[tqi.name])
                V(lambda: nc.vector.tensor_copy(out=k_[:], in_=tqi[:]), r=[tqi.name], w=[k_.name])
                vtt(r_, a_, k_, ALU.subtract)
                vts(m_, r_, 0.5, ALU.is_gt)
                vtt(r_, r_, m_, ALU.subtract)
                vts(m_, r_, -0.5, ALU.is_lt)
                vtt(r_, r_, m_, ALU.add)
                A(lambda: nc.scalar.activation(out=dst[:], in_=r_[:], func=AF.Sin, scale=float(2 * math.pi)), r=[r_.name], w=[dst.name])

            thn = small("thn")
            vts(thn, th, float(1.0 / (2 * math.pi)), ALU.mult)
            sincos_turns(sn_, thn, 0.0)
            sincos_turns(cs_, thn, 0.25)
            vtt(lam_re, lam_abs, cs_, ALU.mult)
            vtt(lam_im, lam_abs, sn_, ALU.mult)
            nr, den, t1_, t2_ = tq[0], tq[1], tq[2], tq[3]
            vts(nr, lam_re, -1.0, ALU.add)
            vtt(t1_, are_t, are_t, ALU.mult)
            vtt(t2_, aim_t, aim_t, ALU.mult)
            vtt(den, t1_, t2_, ALU.add)
            V(lambda: nc.vector.reciprocal(out=den[:], in_=den[:]), r=[den.name], w=[den.name])
            vtt(t1_, nr, are_t, ALU.mult)
            vtt(t2_, lam_im, aim_t, ALU.mult)
            vtt(t1_, t1_, t2_, ALU.add)
            vtt(cf_re, t1_, den, ALU.mult)
            vtt(t1_, lam_im, are_t, ALU.mult)
            vtt(t2_, nr, aim_t, ALU.mult)
            vtt(t1_, t1_, t2_, ALU.subtract)
            vtt(cf_im, t1_, den, ALU.mult)
            wre = small("wre", (128, 16, NLEV)); wim = small("wim", (128, 16, NLEV))
            V(lambda: nc.vector.tensor_copy(out=wre[:, :, 0], in_=cs_[:]), r=[cs_.name], w=[wre.name])
            V(lambda: nc.vector.tensor_copy(out=wim[:, :, 0], in_=sn_[:]), r=[sn_.name], w=[wim.name])
            for k in range(1, NLEV):
                V(lambda k=k: nc.vector.tensor_tensor(out=tq[0][:], in0=wre[:, :, k - 1], in1=wre[:, :, k - 1], op=ALU.mult), r=[wre.name], w=[tq[0].name])
                V(lambda k=k: nc.vector.tensor_tensor(out=tq[1][:], in0=wim[:, :, k - 1], in1=wim[:, :, k - 1], op=ALU.mult), r=[wim.name], w=[tq[1].name])
                V(lambda k=k: nc.vector.tensor_tensor(out=wre[:, :, k], in0=tq[0][:], in1=tq[1][:], op=ALU.subtract), r=[tq[0].name, tq[1].name], w=[wre.name])
                V(lambda k=k: nc.vector.tensor_tensor(out=tq[2][:], in0=wre[:, :, k - 1], in1=wim[:, :, k - 1], op=ALU.mult), r=[wre.name, wim.name], w=[tq[2].name])
                V(lambda k=k: nc.vector.tensor_scalar(out=wim[:, :, k], in0=tq[2][:], scalar1=2.0, scalar2=None, op0=ALU.mult), r=[tq[2].name], w=[wim.name])
            Bre = small("Bre", (128, 16, 16)); Bim = small("Bim", (128, 16, 16))
            bbr = small("bbr", (128, 16, 16)); bbi = small("bbi", (128, 16, 16)); btmp = small("btmp", (128, 16, 16))
            DMA(Bre[:], b_re.rearrange("(ft f) m -> f ft m", f=128), w=[Bre.name])
            DMA(Bim[:], b_im.rearrange("(ft f) m -> f ft m", f=128), w=[Bim.name])
            cfr_b = cf_re[:].unsqueeze(2).to_broadcast([128, 16, 16])
            cfi_b = cf_im[:].unsqueeze(2).to_broadcast([128, 16, 16])
            V(lambda: nc.vector.tensor_tensor(out=bbr[:], in0=Bre[:], in1=cfr_b, op=ALU.mult), r=[Bre.name, cf_re.name], w=[bbr.name])
            V(lambda: nc.vector.tensor_tensor(out=btmp[:], in0=Bim[:], in1=cfi_b, op=ALU.mult), r=[Bim.name, cf_im.name], w=[btmp.name])
            vtt(bbr, bbr, btmp, ALU.subtract)
            V(lambda: nc.vector.tensor_tensor(out=bbi[:], in0=Bim[:], in1=cfr_b, op=ALU.mult), r=[Bim.name, cf_re.name], w=[bbi.name])
            V(lambda: nc.vector.tensor_tensor(out=btmp[:], in0=Bre[:], in1=cfi_b, op=ALU.mult), r=[Bre.name, cf_im.name], w=[btmp.name])
            vtt(bbi, bbi, btmp, ALU.add)
            BT = small("BT", (128, 16, 2, 128), BF16)
            CT = small("CT", (128, 16, 2, 128), BF16)
            V(lambda: nc.vector.memset(CT[:], 0.0), w=[CT.name])
            Xb = small("Xb", (128, 128))
            p_s = ps("p2_s", [128, 512], F32, e2)
            for ft in range(16):
                ct, q = ft // 4, ft % 4
                for ri, bb in enumerate((bbr, bbi)):
                    V(lambda: nc.vector.memset(Xb[:], 0.0), w=[Xb.name])
                    V(lambda bb=bb, ft=ft, q=q: nc.vector.tensor_copy(out=Xb[0:64, 32 * q:32 * q + 16], in_=bb[0:64, ft, :]), r=[bb.name], w=[Xb.name])
                    V(lambda bb=bb, ft=ft, q=q: nc.vector.tensor_copy(out=Xb[64:128, 32 * q + 16:32 * q + 32], in_=bb[64:128, ft, :]), r=[bb.name], w=[Xb.name])
                    PE(lambda: nc.tensor.transpose(out=p_s[:, 0:128], in_=Xb[:], identity=identf[:]), r=[Xb.name, "identf"], w=[p_s.name])
                    V(lambda ft=ft, ri=ri: nc.vector.tensor_copy(out=BT[:, ft, ri, :], in_=p_s[:, 0:128]),
                      r=[p_s.name], w=[BT.name])
            Cre = small("Cre", (32, 16, 64)); Cim = small("Cim", (32, 16, 64))
            mA = small("mA", (32, 1)); mB = small("mB", (32, 1)); Xc = small("Xc", (32, 128))
            DMA(Cre[:], c_re.rearrange("(ft r) p -> r ft p", r=32), w=[Cre.name])
            DMA(Cim[:], c_im.rearrange("(ft r) p -> r ft p", r=32), w=[Cim.name])
            V(lambda: nc.vector.memset(mA[:], 0.0), w=[mA.name])
            V(lambda: nc.vector.memset(mA[0:16, :], 1.0), w=[mA.name])
            V(lambda: nc.vector.tensor_scalar(out=mB[:], in0=mA[:], scalar1=-1.0, scalar2=1.0, op0=ALU.mult, op1=ALU.add), r=[mA.name], w=[mB.name])
            for ft in range(16):
                for ri, cc in enumerate((Cre, Cim)):
                    sgn = 1.0 if ri == 0 else -1.0
                    V(lambda cc=cc, ft=ft, sgn=sgn: nc.vector.tensor_scalar(out=Xc[:, 0:64], in0=cc[:, ft, :], scalar1=mA[:, 0:1], scalar2=sgn,
                                                                          op0=ALU.mult, op1=ALU.mult), r=[cc.name, mA.name], w=[Xc.name])
                    V(lambda cc=cc, ft=ft, sgn=sgn: nc.vector.tensor_scalar(out=Xc[:, 64:128], in0=cc[:, ft, :], scalar1=mB[:, 0:1], scalar2=sgn,
                                                                          op0=ALU.mult, op1=ALU.mult), r=[cc.name, mB.name], w=[Xc.name])
                    PE(lambda: nc.tensor.transpose(out=p_s[:, 0:32], in_=Xc[:], identity=identf[0:32, 0:32]), r=[Xc.name, "identf"], w=[p_s.name])
                    V(lambda ft=ft, ri=ri: nc.vector.tensor_copy(out=CT[:, ft, ri, 32 * (ft % 4):32 * (ft % 4) + 32], in_=p_s[:, 0:32]), r=[p_s.name], w=[CT.name])
            dcol = small("dcol", (128, 4))
            with nc.allow_non_contiguous_dma(reason="tiny"):
                DMA(dcol[:], ssm_d.rearrange("(c p) -> p c", p=128), w=[dcol.name])
            h0r = small("h0r", (128, 16, NS)); h0i = small("h0i", (128, 16, NS))
            h1r = small("h1r", (128, 16, NS)); h1i = small("h1i", (128, 16, NS))
            sst = small("sst", (NS, 2048))
            for (src, dst) in ((st_re, h0r), (st_im, h0i)):
                DMA(sst[:], src[:, :], w=[sst.name])
                for ft in range(16):
                    PE(lambda ft=ft: nc.tensor.transpose(out=p_s[:, ft * NS:(ft + 1) * NS], in_=sst[:, ft * 128:(ft + 1) * 128],
                                                         identity=identf[0:NS, 0:NS]), r=[sst.name, "identf"], w=[p_s.name])
                V(lambda dst=dst: nc.vector.tensor_copy(out=dst[:].rearrange("p a b -> p (a b)"), in_=p_s[:, 0:16 * NS]), r=[p_s.name], w=[dst.name])

            WTOK = SEQ
            ufst = [sb(f"ufst{i}", [128, 512], F32, e2) for i in range(2)]
            ub = sb("ub", [128, NB, SEQ], BF16, e2)
            ufs = sb("ufs", [128, NS], F32, e2)
            ubs = sb("ubs", [128, NS], BF16, e2)
            Ec = sb("Ec", [128, SEQ], F32, e2)
            Es = sb("Es", [128, SEQ], F32, e2)
            rdec = sb("rdec", [128, 512], F32, e2)
            yT = sb("yT", [128, NB, SEQ], F32, e2)
            yTs = sb("yTs", [128, NS], F32, e2)
            fin = sb("fin", [128, 16, NB, 2], F32, e2)
            tm8 = [sb(f"tm{i}", [128, 512], F32, e2) for i in range(8)]
            tm = tm8[0:4]
            gre = [sb(f"gre{i}", [128, 512], F32, e2) for i in range(2)]
            gim = [sb(f"gim{i}", [128, 512], F32, e2) for i in range(2)]
            Gre = [sb(f"Gre{i}", [128, 512], F32, e2) for i in range(2)]
            Gim = [sb(f"Gim{i}", [128, 512], F32, e2) for i in range(2)]
            dm8 = [sb(f"dm{i}", [128, 512], F32, e2) for i in range(8)]
            dm = dm8[0:4]
            hre = [sb(f"hre{i}", [128, 512], BF16, e2) for i in range(2)]
            him = [sb(f"him{i}", [128, 512], BF16, e2) for i in range(2)]
            gl = [sb(f"gl{i}", [128, 512], F32, e2) for i in range(3)]
            gyb = sb("gyb", [128, 512], BF16, e2)
            p_br = [ps(f"p_br{i}", [128, 512], F32, e2) for i in range(2)]
            p_bi = [ps(f"p_bi{i}", [128, 512], F32, e2) for i in range(2)]
            p_y = [ps(f"p_y{i}", [128, 512], F32, e2) for i in range(2)]
            samp0 = (NTT - 1) * 128
            it = 0
            for ct in range(4):
                for b in range(NB):
                    for c in range(NCH):
                        us_ = ufst[(b * NCH + c) % 2]
                        DMA(us_[:], uT_scr[ct * 128:(ct + 1) * 128, b * SEQ + c * 512: b * SEQ + (c + 1) * 512],
                            r=[("uT", (b * SEQ + c * 512) // 128 + i_) for i_ in range(4)], w=[us_.name])
                        G(lambda b=b, c=c, us_=us_: nc.gpsimd.tensor_copy(out=ub[:, b, c * 512:(c + 1) * 512], in_=us_[:]), r=[us_.name], w=["ub"])
                DMA(ufs[:], uT_scr[ct * 128:(ct + 1) * 128, samp0:samp0 + NS], r=[("uT", NTT - 1)], w=["ufs"])
                G(lambda: nc.gpsimd.tensor_copy(out=ubs[:], in_=ufs[:]), r=["ufs"], w=["ubs"])
                for q in range(4):
                    ft = ct * 4 + q
                    qs = slice(32 * q, 32 * q + 32)
                    V(lambda: nc.vector.memset(Ec[:, 0:1], 1.0), w=["Ec"])
                    V(lambda: nc.vector.memset(Es[:, 0:1], 0.0), w=["Es"])
                    for k in range(NLEV):
                        n = 1 << k
                        wr = wre[:, ft, k:k + 1]
                        wi = wim[:, ft, k:k + 1]
                        for n0 in range(0, n, 512):
                            nn = min(512, n - n0)
                            lo_ = slice(n0, n0 + nn)
                            hi_ = slice(n + n0, n + n0 + nn)
                            V(lambda wi=wi, lo_=lo_, nn=nn: nc.vector.tensor_scalar(out=tm[0][:, 0:nn], in0=Es[:, lo_], scalar1=wi, scalar2=None, op0=ALU.mult),
                              r=["Es", wim.name], w=[tm[0].name])
                            V(lambda wr=wr, lo_=lo_, hi_=hi_, nn=nn: nc.vector.scalar_tensor_tensor(out=Ec[:, hi_], in0=Ec[:, lo_], scalar=wr, in1=tm[0][:, 0:nn],
                                                                                                  op0=ALU.mult, op1=ALU.subtract),
                              r=["Ec", tm[0].name, wre.name], w=["Ec"])
                            V(lambda wi=wi, lo_=lo_, nn=nn: nc.vector.tensor_scalar(out=tm[1][:, 0:nn], in0=Ec[:, lo_], scalar1=wi, scalar2=None, op0=ALU.mult),
                              r=["Ec", wim.name], w=[tm[1].name])
                            V(lambda wr=wr, lo_=lo_, hi_=hi_, nn=nn: nc.vector.scalar_tensor_tensor(out=Es[:, hi_], in0=Es[:, lo_], scalar=wr, in1=tm[1][:, 0:nn],
                                                                                                  op0=ALU.mult, op1=ALU.add),
                              r=["Es", tm[1].name, wre.name], w=["Es"])
                    V(lambda ft=ft: nc.vector.tensor_scalar(out=rdec[:], in0=Ec[:, 0:512], scalar1=0.0, scalar2=lam_abs[:, ft:ft + 1],
                                                            op0=ALU.mult, op1=ALU.add), r=["Ec", lam_abs.name], w=["rdec"])
                    chunks = [(b, c) for b in range(NB) for c in range(NCH)]
                    base_it = it

                    def stage_A(j):
                        b, c = chunks[j]
                        itj = base_it + j
                        cs = slice(c * 512, (c + 1) * 512)
                        pr, pi = p_br[itj % 2], p_bi[itj % 2]
                        PE(lambda: nc.tensor.matmul(pr[:], lhsT=BT[:, ft, 0, :], rhs=ub[:, b, cs], start=True, stop=True), r=[BT.name, "ub"], w=[pr.name])
                        PE(lambda: nc.tensor.matmul(pi[:], lhsT=BT[:, ft, 1, :], rhs=ub[:, b, cs], start=True, stop=True), r=[BT.name, "ub"], w=[pi.name])
                        g_r, g_i, G_r, G_i = gre[itj % 2], gim[itj % 2], Gre[itj % 2], Gim[itj % 2]
                        tm = tm8[4 * (itj % 2):4 * (itj % 2) + 4]
                        V(lambda: nc.vector.tensor_tensor(out=tm[0][:], in0=pr[:], in1=Ec[:, cs], op=ALU.mult), r=[pr.name, "Ec"], w=[tm[0].name])
                        V(lambda: nc.vector.tensor_tensor(out=tm[1][:], in0=pi[:], in1=Es[:, cs], op=ALU.mult), r=[pi.name, "Es"], w=[tm[1].name])
                        V(lambda: nc.vector.tensor_tensor(out=tm[2][:], in0=pi[:], in1=Ec[:, cs], op=ALU.mult), r=[pi.name, "Ec"], w=[tm[2].name])
                        V(lambda: nc.vector.tensor_tensor(out=tm[3][:], in0=pr[:], in1=Es[:, cs], op=ALU.mult), r=[pr.name, "Es"], w=[tm[3].name])
                        V(lambda: nc.vector.tensor_tensor(out=g_r[:], in0=tm[0][:], in1=tm[1][:], op=ALU.add), r=[tm[0].name, tm[1].name], w=[g_r.name])
                        V(lambda: nc.vector.tensor_tensor(out=g_i[:], in0=tm[2][:], in1=tm[3][:], op=ALU.subtract), r=[tm[2].name, tm[3].name], w=[g_i.name])
                        if c == 0:
                            ini_r, ini_i, rd_extra = 0.0, 0.0, []
                        else:
                            pG_r, pG_i = Gre[(itj - 1) % 2], Gim[(itj - 1) % 2]
                            ini_r, ini_i = pG_r[:, 511:512], pG_i[:, 511:512]
                            rd_extra = [pG_r.name, pG_i.name]
                        V(lambda: nc.vector.tensor_tensor_scan(out=G_r[:], data0=rdec[:], data1=g_r[:], initial=ini_r, op0=ALU.mult, op1=ALU.add),
                          r=["rdec", g_r.name] + rd_extra, w=[G_r.name])
                        V(lambda: nc.vector.tensor_tensor_scan(out=G_i[:], data0=rdec[:], data1=g_i[:], initial=ini_i, op0=ALU.mult, op1=ALU.add),
                          r=["rdec", g_i.name] + rd_extra, w=[G_i.name])

                    def stage_B(j):
                        b, c = chunks[j]
                        itj = base_it + j
                        cs = slice(c * 512, (c + 1) * 512)
                        G_r, G_i = Gre[itj % 2], Gim[itj % 2]
                        dm = dm8[4 * (itj % 2):4 * (itj % 2) + 4]
                        h_r, h_i = hre[itj % 2], him[itj % 2]
                        G(lambda: nc.gpsimd.tensor_tensor(out=dm[0][:], in0=G_r[:], in1=Ec[:, cs], op=ALU.mult), r=[G_r.name, "Ec"], w=[dm[0].name])
                        G(lambda: nc.gpsimd.tensor_tensor(out=dm[1][:], in0=G_i[:], in1=Es[:, cs], op=ALU.mult), r=[G_i.name, "Es"], w=[dm[1].name])
                        G(lambda: nc.gpsimd.tensor_tensor(out=dm[2][:], in0=G_r[:], in1=Es[:, cs], op=ALU.mult), r=[G_r.name, "Es"], w=[dm[2].name])
                        G(lambda: nc.gpsimd.tensor_tensor(out=dm[3][:], in0=G_i[:], in1=Ec[:, cs], op=ALU.mult), r=[G_i.name, "Ec"], w=[dm[3].name])
                        G(lambda: nc.gpsimd.tensor_tensor(out=h_r[:], in0=dm[0][:], in1=dm[1][:], op=ALU.subtract), r=[dm[0].name, dm[1].name], w=[h_r.name])
                        G(lambda: nc.gpsimd.tensor_tensor(out=h_i[:], in0=dm[2][:], in1=dm[3][:], op=ALU.add), r=[dm[2].name, dm[3].name], w=[h_i.name])
                        if c == NCH - 1:
                            G(lambda: nc.gpsimd.tensor_tensor(out=fin[:, ft, b, 0:1], in0=dm[0][:, 511:512], in1=dm[1][:, 511:512], op=ALU.subtract),
                              r=[dm[0].name, dm[1].name], w=["fin"])
                            G(lambda: nc.gpsimd.tensor_tensor(out=fin[:, ft, b, 1:2], in0=dm[2][:, 511:512], in1=dm[3][:, 511:512], op=ALU.add),
                              r=[dm[2].name, dm[3].name], w=["fin"])
                        py = p_y[itj % 2]
                        PE(lambda: nc.tensor.matmul(py[:, :], lhsT=CT[:, ft, 0, :], rhs=h_r[:], start=True, stop=False), r=[CT.name, h_r.name], w=[py.name])
                        PE(lambda: nc.tensor.matmul(py[:, :], lhsT=CT[:, ft, 1, :], rhs=h_i[:], start=False, stop=True), r=[CT.name, h_i.name], w=[py.name], skip_same=True)
                        if q == 0:
                            A(lambda: nc.scalar.copy(out=yT[:, b, cs], in_=py[:, :]), r=[py.name], w=["yT"])
                        else:
                            V(lambda: nc.vector.tensor_tensor(out=yT[:, b, cs], in0=py[:, :], in1=yT[:, b, cs], op=ALU.add), r=[py.name, "yT"], w=["yT"])

                    stage_A(0)
                    for j in range(len(chunks)):
                        if j + 1 < len(chunks):
                            stage_A(j + 1)
                        stage_B(j)
                    it += len(chunks)
                    pr, pi = p_br[it % 2], p_bi[it % 2]
                    PE(lambda pr=pr, ft=ft: nc.tensor.matmul(pr[:, 0:NS], lhsT=BT[:, ft, 0, :], rhs=ubs[:, :], start=True, stop=True),
                       r=[BT.name, "ubs"], w=[pr.name])
                    PE(lambda pi=pi, ft=ft: nc.tensor.matmul(pi[:, 0:NS], lhsT=BT[:, ft, 1, :], rhs=ubs[:, :], start=True, stop=True),
                       r=[BT.name, "ubs"], w=[pi.name])
                    lr, li = lam_re[:, ft:ft + 1], lam_im[:, ft:ft + 1]
                    V(lambda ft=ft, li=li: nc.vector.tensor_scalar(out=tm[0][:, 0:NS], in0=h0i[:, ft, :], scalar1=li, scalar2=None, op0=ALU.mult),
                      r=[h0i.name, lam_im.name], w=[tm[0].name])
                    V(lambda ft=ft, lr=lr: nc.vector.scalar_tensor_tensor(out=tm[1][:, 0:NS], in0=h0r[:, ft, :], scalar=lr, in1=tm[0][:, 0:NS], op0=ALU.mult, op1=ALU.subtract),
                      r=[h0r.name, lam_re.name, tm[0].name], w=[tm[1].name])
                    V(lambda ft=ft, pr=pr: nc.vector.tensor_tensor(out=h1r[:, ft, :], in0=pr[:, 0:NS], in1=tm[1][:, 0:NS], op=ALU.add), r=[pr.name, tm[1].name], w=[h1r.name])
                    V(lambda ft=ft, li=li: nc.vector.tensor_scalar(out=tm[2][:, 0:NS], in0=h0r[:, ft, :], scalar1=li, scalar2=None, op0=ALU.mult),
                      r=[h0r.name, lam_im.name], w=[tm[2].name])
                    V(lambda ft=ft, lr=lr: nc.vector.scalar_tensor_tensor(out=tm[3][:, 0:NS], in0=h0i[:, ft, :], scalar=lr, in1=tm[2][:, 0:NS], op0=ALU.mult, op1=ALU.add),
                      r=[h0i.name, lam_re.name, tm[2].name], w=[tm[3].name])
                    V(lambda ft=ft, pi=pi: nc.vector.tensor_tensor(out=h1i[:, ft, :], in0=pi[:, 0:NS], in1=tm[3][:, 0:NS], op=ALU.add), r=[pi.name, tm[3].name], w=[h1i.name])
                    h_r, h_i = hre[it % 2], him[it % 2]
                    V(lambda h_r=h_r, ft=ft: nc.vector.tensor_copy(out=h_r[:, 0:NS], in_=h1r[:, ft, :]), r=[h1r.name], w=[h_r.name])
                    V(lambda h_i=h_i, ft=ft: nc.vector.tensor_copy(out=h_i[:, 0:NS], in_=h1i[:, ft, :]), r=[h1i.name], w=[h_i.name])
                    py = p_y[it % 2]
                    PE(lambda py=py, h_r=h_r, ft=ft, qs=qs: nc.tensor.matmul(py[:, 0:NS], lhsT=CT[:, ft, 0, :], rhs=h_r[:, 0:NS], start=True, stop=False),
                       r=[CT.name, h_r.name], w=[py.name])
                    PE(lambda py=py, h_i=h_i, ft=ft, qs=qs: nc.tensor.matmul(py[:, 0:NS], lhsT=CT[:, ft, 1, :], rhs=h_i[:, 0:NS], start=False, stop=True),
                       r=[CT.name, h_i.name], w=[py.name], skip_same=True)
                    if q == 0:
                        A(lambda py=py: nc.scalar.copy(out=yTs[:, :], in_=py[:, 0:NS]), r=[py.name], w=["yTs"])
                    else:
                        V(lambda py=py: nc.vector.tensor_tensor(out=yTs[:, :], in0=py[:, 0:NS], in1=yTs[:, :], op=ALU.add), r=[py.name, "yTs"], w=["yTs"])
                    it += 1
                dsc = dcol[:, ct:ct + 1]

                def gelu_block(ysrc, usrc, n, dst_dram, rkeys, wkey):
                    a0, a1, a2 = gl[0][:, 0:n], gl[1][:, 0:n], gl[2][:, 0:n]
                    V(lambda: nc.vector.scalar_tensor_tensor(out=a0, in0=usrc, scalar=dsc, in1=ysrc, op0=ALU.mult, op1=ALU.add),
                      r=rkeys + [dcol.name], w=[gl[0].name])
                    A(lambda: nc.scalar.activation(out=a1, in_=a0, func=AF.Square), r=[gl[0].name], w=[gl[1].name])
                    V(lambda: nc.vector.tensor_scalar(out=a1, in0=a1, scalar1=0.044715, scalar2=1.0, op0=ALU.mult, op1=ALU.add), r=[gl[1].name], w=[gl[1].name])
                    G(lambda: nc.gpsimd.tensor_tensor(out=a2, in0=a1, in1=a0, op=ALU.mult), r=[gl[0].name, gl[1].name], w=[gl[2].name])
                    A(lambda: nc.scalar.activation(out=a2, in_=a2, func=AF.Sigmoid, scale=1.5957691216057308), r=[gl[2].name], w=[gl[2].name])
                    V(lambda: nc.vector.tensor_tensor(out=gyb[:, 0:n], in0=a0, in1=a2, op=ALU.mult), r=[gl[0].name, gl[2].name], w=["gyb"])
                    DMA(dst_dram, gyb[:, 0:n], r=["gyb"], w=[wkey])

                for b in range(NB):
                    for c in range(NCH):
                        cs = slice(c * 512, (c + 1) * 512)
                        t0 = b * SEQ + c * 512
                        us_ = ufst[(b * NCH + c) % 2]
                        DMA(us_[:], uT_scr[ct * 128:(ct + 1) * 128, t0:t0 + 512], r=[("uT", t0 // 128 + i_) for i_ in range(4)], w=[us_.name])
                        gelu_block(yT[:, b, cs], us_[:], 512, gyT_scr[ct * 128:(ct + 1) * 128, t0:t0 + 512], ["yT", us_.name], ("gyT", ct, b, c))
                gelu_block(yTs[:], ufs[:], NS, gyT_scr[ct * 128:(ct + 1) * 128, samp0:samp0 + NS], ["yTs", "ufs"], ("gyT", ct, "s"))
            with nc.allow_non_contiguous_dma(reason="state out"):
                for b in range(NB):
                    DMA(re_p[b:b + 1, :].rearrange("o (ft f) -> f (o ft)", f=128), fin[:, :, b, 0], r=["fin"], q="pool", is_output=True)
                    DMA(im_p[b:b + 1, :].rearrange("o (ft f) -> f (o ft)", f=128), fin[:, :, b, 1], r=["fin"], q="pool", is_output=True)
            for (src, dst) in ((h1r, re_s), (h1i, im_s)):
                for half in range(4):
                    for j in range(4):
                        ft = half * 4 + j
                        PE(lambda src=src, ft=ft, j=j: nc.tensor.transpose(out=p_s[0:NS, j * 128:(j + 1) * 128], in_=src[:, ft, :], identity=identf[:]),
                           r=[src.name, "identf"], w=[p_s.name])
                    V(lambda half=half: nc.vector.tensor_copy(out=sst[:, half * 512:(half + 1) * 512], in_=p_s[0:NS, :]), r=[p_s.name], w=[sst.name])
                DMA(dst[:, :], sst[:], r=[sst.name], q="pool", is_output=True)
            S.barrier()

    NIT = 22
    if "p3" in phases:
        assert NB == 2
        with contextlib.ExitStack() as e3:
            TOPK = cfg.TOPK
            kTa = [sb(f"kTa{b}", [65, 2, SEQ], BF16, e3) for b in range(NB)]
            kiT = [sb(f"kiT{b}", [64, SEQ], BF16, e3) for b in range(NB)]
            V1 = [sb(f"V1_{b}", [128, NT, 2, 65], BF16, e3) for b in range(NB)]
            Irep = sb("Irep", [128, 4, 128], BF16, e3)
            pw2 = sb("pw2", [128, NIT + 1], F32, e3)
            kmaxb = sb("kmaxb", [128, 1], F32, e3)
            km1 = sb("km1", [128, 1], F32, e3)
            km2 = sb("km2", [1, 128], F32, e3)
            zrow = sb("zrow", [1, 512], BF16, e3)
            qTa = [[sb(f"qTa{b}_{i}", [65, 8, 128], BF16, e3) for i in range(2)] for b in range(NB)]
            qiT = [[sb(f"qiT{b}_{i}", [64, 8, 128], BF16, e3) for i in range(2)] for b in range(NB)]
            score2 = [[sb(f"score{j}_{b}", [128, SEQ], F32, e3) for b in range(NB)] for j in range(2)]
            eps1 = sb("eps1", [128, 1], F32, e3)
            stph = sb("stph", [128, NIT + 1], F32, e3)
            G(lambda: nc.gpsimd.memset(eps1[:], 1.0), w=["eps1"])
            tmpc = [sb(f"tmpc{i}", [128, 512], F32, e3) for i in range(2)]
            junkb = [sb(f"junkb{b}", [128, SEQ], BF16, e3) for b in range(NB)]
            mb = [sb(f"mb{b}", [128, SEQ], BF16, e3) for b in range(NB)]
            pT = [sb(f"pT{i}", [128, 512], BF16, e3) for i in range(3)]
            ao = [sb(f"ao{b}", [128, 8, 64], BF16, e3) for b in range(NB)]
            amax = [sb(f"amax{b}", [128, 1], F32, e3) for b in range(NB)]
            stp = [sb(f"stp{b}", [128, NIT + 1], F32, e3) for b in range(NB)]
            mid = [sb(f"mid{b}", [128, 1], F32, e3) for b in range(NB)]
            cnt = [sb(f"cnt{b}", [128, 1], F32, e3) for b in range(NB)]
            c2 = [sb(f"c2{b}", [128, 1], F32, e3) for b in range(NB)]
            thr = [sb(f"thr{b}", [128, 1], F32, e3) for b in range(NB)]
            rec = [sb(f"rec{b}", [128, 4], F32, e3) for b in range(NB)]
            p_ix = [ps(f"p_ix{i}", [128, 512], F32, e3) for i in range(2)]
            p_st = [ps(f"p_st{i}", [128, 512], F32, e3) for i in range(2)]
            p_o = [ps(f"p_o{j}", [128, 512], F32, e3) for j in range(2)]
            p_n = ps("p_n", [128, 4, 65], F32, e3)
            oT = [sb(f"oT{b}", [65, 2, 512], F32, e3) for b in range(NB)]
            p_m = p_ix[0]

            for k in range(NIT + 1):
                G(lambda k=k: nc.gpsimd.memset(pw2[:, k:k + 1], float(2.0 ** (-k))), w=["pw2"])
            G(lambda: nc.gpsimd.memset(zrow[:], 0.0), w=["zrow"])
            for j in range(4):
                V(lambda j=j: nc.vector.tensor_copy(out=Irep[:, j, :], in_=ident[:]), r=["ident"], w=["Irep"])
            V(lambda: nc.vector.tensor_reduce(out=km1[:], in_=kn2[:].rearrange("p a b -> p (a b)"), axis=AX.X, op=ALU.max), r=["kn2"], w=["km1"])
            PE(lambda: nc.tensor.transpose(out=p_m[0:1, 0:128], in_=km1[:], identity=identf[:]), r=["km1", "identf"], w=[p_m.name])
            V(lambda: nc.vector.tensor_reduce(out=km2[0:1, 0:1], in_=p_m[0:1, 0:128], axis=AX.X, op=ALU.max), r=[p_m.name], w=["km2"])
            A(lambda: nc.scalar.activation(out=km2[0:1, 0:1], in_=km2[0:1, 0:1], func=AF.Sqrt), r=["km2"], w=["km2"])
            PE(lambda: nc.tensor.matmul(p_m[:, 0:1], lhsT=ones_f[0:1, :], rhs=km2[0:1, 0:1], start=True, stop=True), r=["ones_f", "km2"], w=[p_m.name])
            V(lambda: nc.vector.tensor_copy(out=kmaxb[:], in_=p_m[:, 0:1]), r=[p_m.name], w=["kmaxb"])

            for b in range(NB):
                t0 = b * SEQ
                for g in range(2):
                    DMA(kTa[b][0:64, g, :], kT_scr[:, g, t0:t0 + SEQ], r=[("kT", b * NT + i_) for i_ in range(NT)], w=[kTa[b].name])
                V(lambda b=b: nc.vector.memset(kTa[b][64:65, :, :], 1.0), w=[kTa[b].name])
                DMA(kiT[b][:, :], kiT_scr[:, t0:t0 + SEQ], r=[("kiT", b * NT + i_) for i_ in range(NT)], w=[kiT[b].name])
                for g in range(2):
                    DMA(V1[b][:, :, g, 0:64], vb_scr[t0:t0 + SEQ, g, :].rearrange("(kb p) d -> p kb d", p=128),
                        r=[("vb", b * NT + i_) for i_ in range(NT)], w=[V1[b].name])
                V(lambda b=b: nc.vector.memset(V1[b][:, :, :, 64:65], 1.0), w=[V1[b].name])

            itc3 = [0, 0]
            thrc = [sb(f"thrc{j}", [128, 1], F32, e3) for j in range(2)]

            def slot_bufs(i):
                return [qTa[b][i % 2] for b in range(NB)], [qiT[b][i % 2] for b in range(NB)], [score2[i % 2][b] for b in range(NB)]

            def idx_stage(i):
                Sk = (i + 1) * 128
                search = Sk > TOPK
                Ts = [b * NT + i for b in range(NB)]
                qa_, qi_, sc2 = slot_bufs(i)
                for b in range(NB):
                    DMA(qa_[b][:], qT_scr[Ts[b], :, :, :], r=[("qT", Ts[b])], w=[qa_[b].name])
                    DMA(qi_[b][:], qiT_scr[Ts[b], :, :, :], r=[("qiT", Ts[b])], w=[qi_[b].name])
                    V(lambda b=b: nc.vector.tensor_scalar(out=qa_[b][64:65, :, :], in0=qa_[b][64:65, :, :], scalar1=kmaxb[64:65, 0:1], scalar2=None, op0=ALU.mult),
                      r=[qa_[b].name, "kmaxb"], w=[qa_[b].name])
                for b in range(NB):
                    sc_ = sc2[b]
                    for c0 in range(0, Sk, 512):
                        cw = min(512, Sk - c0)
                        for h in range(8):
                            it = itc3[0]
                            px = p_ix[it % 2]
                            PE(lambda px=px, b=b, h=h, c0=c0, cw=cw: nc.tensor.matmul(px[:, 0:cw], lhsT=qi_[b][:, h, :], rhs=kiT[b][:, c0:c0 + cw], start=True, stop=True),
                               r=[qi_[b].name, kiT[b].name], w=[px.name])
                            lo_ = lohi[:, Ts[b], h:h + 1]
                            hi_ = lohi[:, Ts[b], 8 + h:9 + h]
                            if h == 0:
                                V(lambda px=px, sc_=sc_, c0=c0, cw=cw, lo_=lo_, hi_=hi_: nc.vector.tensor_scalar(out=sc_[:, c0:c0 + cw], in0=px[:, 0:cw], scalar1=lo_, scalar2=hi_,
                                                                                                              op0=ALU.max, op1=ALU.min), r=[px.name, "lohi"], w=[sc_.name])
                            else:
                                tc_ = tmpc[it % 2]
                                V(lambda px=px, tc_=tc_, cw=cw, lo_=lo_, hi_=hi_: nc.vector.tensor_scalar(out=tc_[:, 0:cw], in0=px[:, 0:cw], scalar1=lo_, scalar2=hi_,
                                                                                                       op0=ALU.max, op1=ALU.min), r=[px.name, "lohi"], w=[tc_.name])
                                G(lambda tc_=tc_, sc_=sc_, c0=c0, cw=cw: nc.gpsimd.tensor_tensor(out=sc_[:, c0:c0 + cw], in0=sc_[:, c0:c0 + cw], in1=tc_[:, 0:cw], op=ALU.add),
                                  r=[tc_.name, sc_.name], w=[sc_.name])
                            itc3[0] += 1
                    if search:
                        G(lambda b=b, sc_=sc_: nc.gpsimd.tensor_reduce(out=amax[b][:], in_=sc_[:, 0:Sk], axis=AX.X, op=ALU.max, apply_absolute_value=True),
                          r=[sc_.name], w=[amax[b].name]) if False else \
                        V(lambda b=b, sc_=sc_: nc.vector.tensor_reduce(out=amax[b][:], in_=sc_[:, 0:Sk], axis=AX.X, op=ALU.max, apply_absolute_value=True),
                          r=[sc_.name], w=[amax[b].name])
                    G(lambda sc_=sc_: nc.gpsimd.tensor_tensor(out=sc_[:, Sk - 128:Sk], in0=sc_[:, Sk - 128:Sk], in1=cmask[:], op=ALU.add),
                      r=[sc_.name, "cmask"], w=[sc_.name])

            def srch_stage(i):
                Sk = (i + 1) * 128
                search = Sk > TOPK
                Ts = [b * NT + i for b in range(NB)]
                qa_, qi_, sc2 = slot_bufs(i)
                if search:
                    V(lambda: nc.vector.tensor_scalar(out=amax[0][:], in0=amax[0][:], scalar1=1.0, scalar2=None, op0=ALU.add), r=[amax[0].name], w=[amax[0].name])
                    V(lambda: nc.vector.tensor_scalar(out=stp[0][:], in0=pw2[:], scalar1=amax[0][:, 0:1], scalar2=None, op0=ALU.mult), r=["pw2", amax[0].name], w=[stp[0].name])
                    V(lambda: nc.vector.memset(mid[0][:], 0.0), w=[mid[0].name])
                    A(lambda: nc.scalar.activation(out=amax[1][:], in_=amax[1][:], func=AF.Identity, bias=1.0 if False else eps1[:, 0:1], scale=1.0), r=[amax[1].name, "eps1"], w=[amax[1].name])
                    A(lambda: nc.scalar.activation(out=stp[1][:], in_=pw2[:], func=AF.Identity, scale=amax[1][:, 0:1]), r=["pw2", amax[1].name], w=[stp[1].name])
                    A(lambda: nc.scalar.activation(out=stph[:], in_=stp[1][:], func=AF.Identity, scale=-0.5), r=[stp[1].name], w=["stph"])
                    A(lambda: nc.scalar.activation(out=mid[1][:], in_=zer_f[:, 0:1], func=AF.Identity), r=["zer_f"], w=[mid[1].name])
                    tcst = thrc[i % 2]
                    V(lambda tcst=tcst: nc.vector.memset(tcst[:], -(float(2 * TOPK - Sk) - 0.5)), w=[tcst.name])
                    for k in range(NIT):
                        V(lambda: nc.vector.tensor_scalar(out=junkb[0][:, 0:Sk], in0=sc2[0][:, 0:Sk], scalar1=mid[0][:, 0:1], scalar2=None, op0=ALU.is_ge,
                                                          op1=ALU.add, accum_out=cnt[0][:]), r=[sc2[0].name, mid[0].name], w=[junkb[0].name, cnt[0].name])
                        A(lambda: nc.scalar.activation(out=junkb[1][:, 0:Sk], in_=sc2[1][:, 0:Sk], func=AF.Sign, bias=mid[1][:, 0:1], scale=1.0, accum_out=cnt[1][:]),
                          r=[sc2[1].name, mid[1].name], w=[junkb[1].name, cnt[1].name])
                        V(lambda: nc.vector.tensor_scalar(out=c2[0][:], in0=cnt[0][:], scalar1=float(TOPK), scalar2=-0.5, op0=ALU.is_ge, op1=ALU.add),
                          r=[cnt[0].name], w=[c2[0].name])
                        V(lambda k=k: nc.vector.scalar_tensor_tensor(out=mid[0][:], in0=c2[0][:], scalar=stp[0][:, k:k + 1], in1=mid[0][:], op0=ALU.mult, op1=ALU.add),
                          r=[c2[0].name, stp[0].name, mid[0].name], w=[mid[0].name])
                        A(lambda tcst=tcst: nc.scalar.activation(out=c2[1][:], in_=cnt[1][:], func=AF.Sign, bias=tcst[:, 0:1], scale=1.0), r=[cnt[1].name, tcst.name], w=[c2[1].name])
                        A(lambda k=k: nc.scalar.activation(out=mid[1][:], in_=c2[1][:], func=AF.Identity, scale=stph[:, k:k + 1], bias=mid[1][:, 0:1]),
                          r=[c2[1].name, "stph", mid[1].name], w=[mid[1].name])
                    V(lambda: nc.vector.tensor_tensor(out=thr[0][:], in0=mid[0][:], in1=stp[0][:, NIT:NIT + 1], op=ALU.subtract), r=[mid[0].name, stp[0].name], w=[thr[0].name])
                    V(lambda: nc.vector.scalar_tensor_tensor(out=thr[1][:], in0=mid[1][:], scalar=-1.0, in1=stp[1][:, NIT:NIT + 1], op0=ALU.mult, op1=ALU.subtract),
                      r=[mid[1].name, stp[1].name], w=[thr[1].name])
                else:
                    for b in range(NB):
                        V(lambda b=b: nc.vector.memset(thr[b][:], -1.0e29), w=[thr[b].name])
                for b in range(NB):
                    V(lambda b=b: nc.vector.tensor_scalar(out=mb[b][:, 0:Sk], in0=sc2[b][:, 0:Sk], scalar1=thr[b][:, 0:1], scalar2=BIGM, op0=ALU.is_lt, op1=ALU.mult),
                      r=[sc2[b].name, thr[b].name], w=[mb[b].name])
                    if debug and Ts[b] == NT - 1:
                        DMA(dbg_mb[:, 0:Sk], mb[b][:, 0:Sk], r=[mb[b].name], w=["dbg_mb"])
                        DMA(dbg_sc[:, 0:Sk], sc2[b][:, 0:Sk], r=[sc2[b].name], w=["dbg_sc"])
                        DMA(dbg_thr[:, :], thr[b][:], r=[thr[b].name], w=["dbg_thr"])

            def att_stage(i, b, g):
                qa_, qi_, sc2 = slot_bufs(i)
                po = p_o[(2 * b + g) % 2]
                base = itc3[1]

                def qk(kb):
                    ia = base + kb
                    pst = p_st[ia % 2]
                    pt_ = pT[ia % 3]
                    ks = slice(kb * 128, (kb + 1) * 128)
                    PE(lambda: nc.tensor.matmul(pst[:], lhsT=kTa[b][:, g, ks], rhs=qa_[b][:, 4 * g:4 * g + 4, :].rearrange("p a b -> p (a b)"),
                                                start=True, stop=False), r=[kTa[b].name, qa_[b].name], w=[pst.name])
                    PE(lambda: nc.tensor.matmul(pst[:], lhsT=mb[b][:, ks], rhs=Irep[:].rearrange("p a b -> p (a b)"), start=False, stop=True),
                       r=[mb[b].name, "Irep"], w=[pst.name], skip_same=True)
                    A(lambda: nc.scalar.activation(out=pt_[:], in_=pst[:], func=AF.Exp, scale=0.125), r=[pst.name], w=[pt_.name])

                def pv(kb):
                    ia = base + kb
                    pt_ = pT[ia % 3]
                    PE(lambda: nc.tensor.matmul(po[0:65, :], lhsT=V1[b][:, kb, g, :], rhs=pt_[:], start=(kb == 0), stop=(kb == i)),
                       r=[pt_.name, V1[b].name], w=[po.name], skip_same=True)

                qk(0)
                for kb in range(i + 1):
                    if kb + 1 <= i:
                        qk(kb + 1)
                    pv(kb)
                itc3[1] += i + 1
                A(lambda po=po: nc.scalar.copy(out=oT[b][0:65, g, :], in_=po[0:65, :]), r=[po.name], w=[oT[b].name])

            def norm_stage(i):
                Ts = [b * NT + i for b in range(NB)]
                for b in range(NB):
                    for g in range(2):
                        for h4 in range(4):
                            PE(lambda b=b, g=g, h4=h4: nc.tensor.transpose(out=p_n[:, h4, :], in_=oT[b][0:65, g, h4 * 128:(h4 + 1) * 128], identity=identf[0:65, 0:65]),
                               r=[oT[b].name, "identf"], w=["p_n"], skip_same=True)
                        V(lambda b=b: nc.vector.reciprocal(out=rec[b][:], in_=p_n[:, :, 64]), r=["p_n"], w=[rec[b].name])
                        V(lambda b=b, g=g: nc.vector.tensor_tensor(out=ao[b][:, 4 * g:4 * g + 4, :], in0=p_n[:, :, 0:64], in1=rec[b][:].unsqueeze(2).to_broadcast([128, 4, 64]),
                                                                   op=ALU.mult), r=["p_n", rec[b].name], w=[ao[b].name])
                    DMA(ao_scr[Ts[b] * 128:(Ts[b] + 1) * 128, :], ao[b][:].rearrange("p a b -> p (a b)"), r=[ao[b].name], w=[("ao", Ts[b])])

            idx_stage(0)
            for i in range(NT):
                srch_stage(i)
                if i >= 1:
                    norm_stage(i - 1)
                if i + 1 < NT:
                    idx_stage(i + 1)
                for b in range(NB):
                    for g in range(2):
                        att_stage(i, b, g)
            norm_stage(NT - 1)
            S.barrier()

    if "p3s" in phases:
        with contextlib.ExitStack() as e4:
            NPG = cfg.NPG
            assert NPG == 64 and NS % 2 == 0
            TOPK_S = cfg.TOPK_S
            TS = NTT - 1
            samp0 = TS * 128
            NO = 129
            lohi_scr = dscr("lohi_scr", [NS, 16], F32)
            DMA(lohi_scr[:, :], lohi[0:NS, TS, :], r=["lohi"], w=["lohi_scr"])
            lohiB = sb("lohiB", [128, NS, 16], F32, e4)
            DMA(lohiB[:].rearrange("p a b -> p (a b)"), lohi_scr.rearrange("(o a) b -> o (a b)", o=1).partition_broadcast(128),
                r=["lohi_scr"], w=["lohiB"])
            qTs = sb("qTs", [65, 8, 128], BF16, e4)
            qiTs = sb("qiTs", [64, 8, 128], BF16, e4)
            DMA(qTs[:], qT_scr[TS, :, :, :], r=[("qT", TS)], w=["qTs"])
            DMA(qiTs[:], qiT_scr[TS, :, :, :], r=[("qiT", TS)], w=["qiTs"])
            idx = sb("idx", [128, 1], I32, e4)
            idxf = sb("idxf", [128, 1], F32, e4)
            idxa = sb("idxa", [128, 1], I32, e4)
            idxb = sb("idxb", [128, 1], I32, e4)
            KI = sb("KI", [128, 128, 64], F32, e4)
            KIx = sb("KIx", [128, 64], F32, e4)
            Kh = [sb(f"Kh{i}", [128, 64, 128], F32, e4) for i in range(2)]
            Kx = sb("Kx", [128, 128], F32, e4)
            Vh = sb("Vh", [128, 64, 128], F32, e4)
            Vx = sb("Vx", [128, 128], F32, e4)
            V1h = sb("V1h", [128, 64, 2, 65], BF16, e4)
            V1x = sb("V1x", [128, 2, 65], BF16, e4)
            kT4 = [sb(f"kT4_{i}", [65, 4, 128], BF16, e4) for i in range(2)]
            qis = sb("qis", [64, 2, 8], BF16, e4)
            qas = sb("qas", [65, 2, 2, 4], BF16, e4)
            sc = sb("sc_s", [128, NO], F32, e4)
            cl = sb("cl_s", [128, 32, 16], F32, e4)
            red = sb("red_s", [128, 32, 2], F32, e4)
            colmask = sb("colmask", [128, 1], F32, e4)
            cross = sb("cross", [128, 16], F32, e4)
            maskfull = sb("maskfull", [128, NO, 16], F32, e4)
            blk1 = sb("blk1", [128, 128], F32, e4)
            pw2s = sb("pw2s", [128, NIT + 1], F32, e4)
            am1 = sb("am1", [128, 1], F32, e4)
            am2 = sb("am2", [1, 128], F32, e4)
            amb = sb("amb", [128, 1], F32, e4)
            stps = sb("stps", [128, NIT + 1], F32, e4)
            mids = sb("mids", [128, 1], F32, e4)
            cnts = sb("cnts", [128, 1], F32, e4)
            c2s = sb("c2s", [128, 1], F32, e4)
            thrs = sb("thrs", [128, 1], F32, e4)
            junks = sb("junks", [128, NO], F32, e4)
            sqk = Vh
            n2 = sb("n2", [128, 258], F32, e4)
            kmb = sb("kmb", [128, 1], F32, e4)
            tmps = sb("tmps", [128, 32, 16], F32, e4)
            pTs = [sb(f"pTs{i}", [128, 32, 16], BF16, e4) for i in range(2)]
            zrow_s = sb("zrow_s", [1, 512], BF16, e4)
            o8 = sb("o8", [8, 2, 64], BF16, e4)
            rec8 = sb("rec8", [8, 2], F32, e4)
            p_t = [ps(f"p3s_t{i}", [64, 4, 128], F32, e4) for i in range(2)]
            p_sx = [ps(f"p3s_x{i}", [128, 32, 16], F32, e4) for i in range(2)]
            p_os = ps("p3s_o", [8, 2, 65], F32, e4)
            p_ms = ps("p3s_m", [128, 512], F32, e4)

            for k in range(NIT + 1):
                G(lambda k=k: nc.gpsimd.memset(pw2s[:, k:k + 1], float(2.0 ** (-k))), w=["pw2s"])
            G(lambda: nc.gpsimd.memset(zrow_s[:], 0.0), w=["zrow_s"])
            V(lambda: nc.vector.memset(colmask[:], NEG), w=["colmask"])
            V(lambda: nc.vector.memset(colmask[0:1, :], 0.0), w=["colmask"])
            V(lambda: nc.vector.memset(colmask[64:65, :], 0.0), w=["colmask"])
            V(lambda: nc.vector.memset(cross[:], 0.0), w=["cross"])
            crv = cross[:].rearrange("p (g b h) -> p g b h", g=2, b=2)
            V(lambda: nc.vector.memset(crv[0:64, :, 1, :], BIGM), w=["cross"])
            V(lambda: nc.vector.memset(crv[64:128, :, 0, :], BIGM), w=["cross"])
            V(lambda: nc.vector.memset(blk1[:], 0.0), w=["blk1"])
            V(lambda: nc.vector.memset(blk1[0:64, 0:64], 1.0), w=["blk1"])
            V(lambda: nc.vector.memset(blk1[64:128, 64:128], 1.0), w=["blk1"])
            for t_ in kT4:
                V(lambda t_=t_: nc.vector.memset(t_[64:65, :, :], 1.0), w=[t_.name])
            V(lambda: nc.vector.memset(V1h[:, :, :, 64:65], 1.0), w=["V1h"])
            V(lambda: nc.vector.memset(V1x[:, :, 64:65], 1.0), w=["V1x"])

            def gather(dst2d, src2d, idx_t, wkey):
                S.dma("pool", lambda: nc.gpsimd.indirect_dma_start(out=dst2d, out_offset=None, in_=src2d,
                                                                   in_offset=bass.IndirectOffsetOnAxis(ap=idx_t[:, :], axis=0)),
                      reads=[idx_t.name], writes=[wkey])

            cache_kh = cache_k.rearrange("n (h e) -> (n h) e", h=2)
            cache_vh = cache_v.rearrange("n (h e) -> (n h) e", h=2)
            itc = [0]

            def transposed_units(units, consume):
                for u0 in range(0, len(units), 4):
                    grp = units[u0:u0 + 4]
                    pt_ = p_t[itc[0] % 2]
                    kt = kT4[itc[0] % 2]
                    for s_, (src, rk) in enumerate(grp):
                        PE(lambda pt_=pt_, s_=s_, src=src: nc.tensor.transpose(out=pt_[:, s_, :], in_=src, identity=identf[:]),
                           r=list(rk) + ["identf"], w=[pt_.name], skip_same=True)
                    n_ = len(grp)
                    if itc[0] % 2 == 0:
                        V(lambda pt_=pt_, kt=kt, n_=n_: nc.vector.tensor_copy(out=kt[0:64, 0:n_, :], in_=pt_[:, 0:n_, :]), r=[pt_.name], w=[kt.name])
                    else:
                        A(lambda pt_=pt_, kt=kt, n_=n_: nc.scalar.copy(out=kt[0:64, 0:n_, :], in_=pt_[:, 0:n_, :]), r=[pt_.name], w=[kt.name])
                    for s_ in range(n_):
                        consume(kt, s_, u0 + s_)
                    itc[0] += 1

            for pr in range(NS // 2):
                b0 = 2 * pr
                DMA(idx[:], ptab[b0 * NPG:(b0 + 2) * NPG, :], w=["idx"])
                V(lambda: nc.vector.tensor_copy(out=idxf[:], in_=idx[:]), r=["idx"], w=["idxf"])
                V(lambda: nc.vector.tensor_scalar(out=idxa[:], in0=idxf[:], scalar1=2.0, scalar2=None, op0=ALU.mult), r=["idxf"], w=["idxa"])
                V(lambda: nc.vector.tensor_scalar(out=idxb[:], in0=idxf[:], scalar1=2.0, scalar2=1.0, op0=ALU.mult, op1=ALU.add), r=["idxf"], w=["idxb"])
                gather(KI[:].rearrange("p a b -> p (a b)"), cache_ik[:, :], idx, "KI")
                gather(Kh[0][:].rearrange("p a b -> p (a b)"), cache_kh[:, :], idxa, "Kh0")
                gather(Kh[1][:].rearrange("p a b -> p (a b)"), cache_kh[:, :], idxb, "Kh1")
                for (xt_, src_) in ((KIx, ki_s), (Kx, k_s), (Vx, v_s)):
                    V(lambda xt_=xt_: nc.vector.memset(xt_[:], 0.0), w=[xt_.name])
                    DMA(xt_[0:1, :], src_[b0:b0 + 1, :], w=[xt_.name])
                    DMA(xt_[64:65, :], src_[b0 + 1:b0 + 2, :], w=[xt_.name])
                V(lambda b0=b0: nc.vector.tensor_copy(out=qis[:], in_=qiTs[:, :, b0:b0 + 2].rearrange("p h t -> p t h")), r=["qiTs"], w=["qis"])
                for g in range(2):
                    V(lambda b0=b0, g=g: nc.vector.tensor_copy(out=qas[:, g, :, :], in_=qTs[:, 4 * g:4 * g + 4, b0:b0 + 2].rearrange("p h t -> p t h")),
                      r=["qTs"], w=["qas"])
                for hf in range(2):
                    G(lambda hf=hf: nc.gpsimd.tensor_tensor(out=sqk[:], in0=Kh[hf][:], in1=Kh[hf][:], op=ALU.mult), r=[f"Kh{hf}"], w=["Vh"])
                    V(lambda hf=hf: nc.vector.tensor_reduce(out=n2[:, hf * 128:(hf + 1) * 128], in_=sqk[:].rearrange("p o (g d) -> p (o g) d", g=2), axis=AX.X, op=ALU.add),
                      r=["Vh"], w=["n2"])
                G(lambda: nc.gpsimd.tensor_tensor(out=sqk[:, 0, :], in0=Kx[:], in1=Kx[:], op=ALU.mult), r=["Kx"], w=["Vh"])
                V(lambda: nc.vector.tensor_reduce(out=n2[:, 256:258], in_=sqk[:, 0, :].rearrange("p (g d) -> p g d", g=2), axis=AX.X, op=ALU.add), r=["Vh"], w=["n2"])
                V(lambda: nc.vector.tensor_reduce(out=am1[:], in_=n2[:], axis=AX.X, op=ALU.max), r=["n2"], w=["am1"])
                PE(lambda: nc.tensor.transpose(out=p_ms[0:1, 0:128], in_=am1[:], identity=identf[:]), r=["am1", "identf"], w=[p_ms.name])
                V(lambda: nc.vector.tensor_reduce(out=am2[0:1, 0:1], in_=p_ms[0:1, 0:128], axis=AX.X, op=ALU.max), r=[p_ms.name], w=["am2"])
                A(lambda: nc.scalar.activation(out=am2[0:1, 0:1], in_=am2[0:1, 0:1], func=AF.Sqrt), r=["am2"], w=["am2"])
                PE(lambda: nc.tensor.matmul(p_ms[:, 0:1], lhsT=ones_f[0:1, :], rhs=am2[0:1, 0:1], start=True, stop=True), r=["ones_f", "am2"], w=[p_ms.name])
                V(lambda: nc.vector.tensor_copy(out=kmb[:], in_=p_ms[:, 0:1]), r=[p_ms.name], w=["kmb"])
                V(lambda: nc.vector.tensor_scalar(out=qas[64:65, :, :, :], in0=qas[64:65, :, :, :], scalar1=kmb[64:65, 0:1], scalar2=None, op0=ALU.mult),
                  r=["qas", "kmb"], w=["qas"])

                loB = lohiB[:, b0:b0 + 2, 0:8]
                hiB = lohiB[:, b0:b0 + 2, 8:16]

                def idx_group(units, o_lo, n_o):
                    px = p_sx[itc[0] % 2]

                    def consume(kt, s_, ui, px=px):
                        PE(lambda: nc.tensor.matmul(px[:, ui, :], lhsT=kt[0:64, s_, :], rhs=qis[:].rearrange("p a b -> p (a b)"), start=True, stop=True),
                           r=[kt.name, "qis"], w=[px.name], skip_same=True)
                    transposed_units(units, consume)
                    V(lambda: nc.vector.tensor_tensor(out=cl[:, 0:n_o, :].rearrange("p o (b h) -> p o b h", b=2), in0=px[:, 0:n_o, :].rearrange("p o (b h) -> p o b h", b=2),
                                                      in1=loB.unsqueeze(1).to_broadcast([128, n_o, 2, 8]), op=ALU.max), r=[px.name, "lohiB"], w=["cl_s"])
                    V(lambda: nc.vector.tensor_tensor(out=cl[:, 0:n_o, :].rearrange("p o (b h) -> p o b h", b=2), in0=cl[:, 0:n_o, :].rearrange("p o (b h) -> p o b h", b=2),
                                                      in1=hiB.unsqueeze(1).to_broadcast([128, n_o, 2, 8]), op=ALU.min), r=["cl_s", "lohiB"], w=["cl_s"])
                    V(lambda: nc.vector.tensor_reduce(out=red[:, 0:n_o, :], in_=cl[:, 0:n_o, :].rearrange("p o (b h) -> p o b h", b=2), axis=AX.X, op=ALU.add),
                      r=["cl_s"], w=["red_s"])
                    V(lambda: nc.vector.tensor_copy(out=sc[0:64, o_lo:o_lo + n_o], in_=red[0:64, 0:n_o, 0]), r=["red_s"], w=["sc_s"])
                    V(lambda: nc.vector.tensor_copy(out=sc[64:128, o_lo:o_lo + n_o], in_=red[64:128, 0:n_o, 1]), r=["red_s"], w=["sc_s"])

                for og in range(4):
                    idx_group([(KI[:, og * 32 + oo, :], ["KI"]) for oo in range(32)], og * 32, 32)
                idx_group([(KIx[:], ["KIx"])], 128, 1)
                V(lambda: nc.vector.tensor_reduce(out=am1[:], in_=sc[:], axis=AX.X, op=ALU.max, apply_absolute_value=True), r=["sc_s"], w=["am1"])
                V(lambda: nc.vector.tensor_tensor(out=sc[:, 128:129], in0=sc[:, 128:129], in1=colmask[:], op=ALU.add), r=["sc_s", "colmask"], w=["sc_s"])
                PE(lambda: nc.tensor.transpose(out=p_ms[0:1, 0:128], in_=am1[:], identity=identf[:]), r=["am1", "identf"], w=[p_ms.name])
                V(lambda: nc.vector.tensor_reduce(out=am2[0:1, 0:1], in_=p_ms[0:1, 0:128], axis=AX.X, op=ALU.max), r=[p_ms.name], w=["am2"])
                V(lambda: nc.vector.tensor_scalar(out=am2[0:1, 0:1], in0=am2[0:1, 0:1], scalar1=1.0, scalar2=None, op0=ALU.add), r=["am2"], w=["am2"])
                PE(lambda: nc.tensor.matmul(p_ms[:, 0:1], lhsT=ones_f[0:1, :], rhs=am2[0:1, 0:1], start=True, stop=True), r=["ones_f", "am2"], w=[p_ms.name])
                V(lambda: nc.vector.tensor_copy(out=amb[:], in_=p_ms[:, 0:1]), r=[p_ms.name], w=["amb"])
                V(lambda: nc.vector.tensor_scalar(out=stps[:], in0=pw2s[:], scalar1=amb[:, 0:1], scalar2=None, op0=ALU.mult), r=["pw2s", "amb"], w=["stps"])
                V(lambda: nc.vector.memset(mids[:], 0.0), w=["mids"])
                for k in range(NIT):
                    V(lambda: nc.vector.tensor_scalar(out=junks[:], in0=sc[:], scalar1=mids[:, 0:1], scalar2=None, op0=ALU.is_ge, op1=ALU.add, accum_out=cnts[:]),
                      r=["sc_s", "mids"], w=["junks", "cnts"])
                    PE(lambda: nc.tensor.matmul(p_ms[:, 0:1], lhsT=blk1[:], rhs=cnts[:], start=True, stop=True), r=["blk1", "cnts"], w=[p_ms.name])
                    V(lambda: nc.vector.tensor_scalar(out=c2s[:], in0=p_ms[:, 0:1], scalar1=float(TOPK_S), scalar2=-0.5, op0=ALU.is_ge, op1=ALU.add), r=[p_ms.name], w=["c2s"])
                    V(lambda k=k: nc.vector.scalar_tensor_tensor(out=mids[:], in0=c2s[:], scalar=stps[:, k:k + 1], in1=mids[:], op0=ALU.mult, op1=ALU.add),
                      r=["c2s", "stps", "mids"], w=["mids"])
                V(lambda: nc.vector.tensor_tensor(out=thrs[:], in0=mids[:], in1=stps[:, NIT:NIT + 1], op=ALU.subtract), r=["mids", "stps"], w=["thrs"])
                V(lambda: nc.vector.tensor_scalar(out=junks[:], in0=sc[:], scalar1=thrs[:, 0:1], scalar2=BIGM, op0=ALU.is_lt, op1=ALU.mult), r=["sc_s", "thrs"], w=["junks"])
                V(lambda: nc.vector.tensor_tensor(out=maskfull[:], in0=junks[:].unsqueeze(2).to_broadcast([128, NO, 16]),
                                                  in1=cross[:].unsqueeze(1).to_broadcast([128, NO, 16]), op=ALU.add), r=["junks", "cross"], w=["maskfull"])
                PE(lambda: nc.tensor.matmul(p_os[:].rearrange("p a b -> p (a b)"), lhsT=zrow_s[0:1, 0:8], rhs=zrow_s[0:1, 0:130], start=True, stop=False,
                                            skip_group_check=True), r=["zrow_s"], w=["p3s_o"])

                def att_group(kunits, vsrc_fn, o_lo, n_o, last):
                    px = p_sx[itc[0] % 2]
                    pts = pTs[itc[0] % 2]

                    def consume(kt, s_, ui, px=px):
                        ol, g = ui // 2, ui % 2
                        PE(lambda: nc.tensor.matmul(px[:, ol, g * 8:(g + 1) * 8], lhsT=kt[:, s_, :], rhs=qas[:, g, :, :].rearrange("p a b -> p (a b)"), start=True, stop=True),
                           r=[kt.name, "qas"], w=[px.name], skip_same=True)
                    transposed_units(kunits, consume)
                    V(lambda: nc.vector.tensor_tensor(out=tmps[:, 0:n_o, :], in0=px[:, 0:n_o, :], in1=maskfull[:, o_lo:o_lo + n_o, :], op=ALU.add),
                      r=[px.name, "maskfull"], w=["tmps"])
                    A(lambda: nc.scalar.activation(out=pts[:, 0:n_o, :], in_=tmps[:, 0:n_o, :], func=AF.Exp, scale=0.125), r=["tmps"], w=[pts.name])
                    for ol in range(n_o):
                        for g in range(2):
                            vap, vk = vsrc_fn(ol, g)
                            PE(lambda ol=ol, g=g, vap=vap: nc.tensor.matmul(p_os[:, g, :], lhsT=pts[:, ol, g * 8:(g + 1) * 8], rhs=vap, start=False,
                                                                         stop=(last and ol == n_o - 1), skip_group_check=True),
                               r=[pts.name, vk], w=["p3s_o"], skip_same=True)

                for hf in range(2):
                    gather(Vh[:].rearrange("p a b -> p (a b)"), cache_vh[:, :], idxa if hf == 0 else idxb, "Vh")
                    for g in range(2):
                        G(lambda g=g: nc.gpsimd.tensor_copy(out=V1h[:, :, g, 0:64], in_=Vh[:, :, g * 64:(g + 1) * 64]), r=["Vh"], w=["V1h"])
                    for og in range(2):
                        units = []
                        for oo in range(32):
                            for g in range(2):
                                units.append((Kh[hf][:, og * 32 + oo, g * 64:(g + 1) * 64], [f"Kh{hf}"]))
                        att_group(units, lambda ol, g, og=og: (V1h[:, og * 32 + ol, g, :], "V1h"), hf * 64 + og * 32, 32, False)
                for g in range(2):
                    V(lambda g=g: nc.vector.tensor_copy(out=V1x[:, g, 0:64], in_=Vx[:, g * 64:(g + 1) * 64]), r=["Vx"], w=["V1x"])
                att_group([(Kx[:, g * 64:(g + 1) * 64], ["Kx"]) for g in range(2)], lambda ol, g: (V1x[:, g, :], "V1x"), 128, 1, True)
                V(lambda: nc.vector.reciprocal(out=rec8[:], in_=p_os[:, :, 64]), r=["p3s_o"], w=["rec8"])
                V(lambda: nc.vector.tensor_tensor(out=o8[:], in0=p_os[:, :, 0:64], in1=rec8[:].unsqueeze(2).to_broadcast([8, 2, 64]), op=ALU.mult),
                  r=["p3s_o", "rec8"], w=["o8"])
                for b2 in range(2):
                    row = samp0 + b0 + b2
                    DMA(ao_scr[row:row + 1, :].rearrange("o (g h d) -> (o h) g d", g=2, h=4), o8[4 * b2:4 * b2 + 4, :, :], r=["o8"], w=[("ao", TS)])
            S.barrier()

    if "p4" in phases:
        with contextlib.ExitStack() as e5:
            Wglu = sb("Wglu", [128, 4, 2048], BF16, e5)
            Wao = sb("Wao", [128, 4, 1024], BF16, e5)
            Wo = sb("Wo", [128, 8, 1024], BF16, e5)
            bglu = sb("bglu", [128, 2048], F32, e5)
            with contextlib.ExitStack() as e5w:
                load_weight_bf16(Wglu, w_glu, D_SSM, 2048, e5w, nm="wglu")
                load_weight_bf16(Wao, w_ao, D_ATTN, 1024, e5w, nm="wao")
                load_weight_bf16(Wo, w_o, D_MODEL, 1024, e5w, nm="wo")
                S.barrier()
            DMA(bglu[:], b_glu.rearrange("(o n) -> o n", o=1).partition_broadcast(128), w=["bglu"])
            gyt = [sb(f"gyt{i}", [128, 4, 128], BF16, e5) for i in range(2)]
            aot = [sb(f"aot{i}", [128, 512], BF16, e5) for i in range(2)]
            gat = [sb(f"gat{i}", [128, 2048], F32, e5) for i in range(2)]
            xin = [sb(f"xin{i}", [128, 1024], F32, e5) for i in range(2)]
            zv = sb("zv", [128, 512], F32, e5)
            zg = sb("zg", [128, 512], F32, e5)
            so = sb("so", [128, 1024], F32, e5)
            aoT = sb("aoT", [128, 4, 128], BF16, e5)
            m1 = sb("m1", [128, 1024], F32, e5)
            m2 = sb("m2", [128, 512], F32, e5)
            mixb = sb("mixb", [128, 1024], BF16, e5)
            mixT = sb("mixT", [128, 8, 128], BF16, e5)
            x2t = [sb(f"x2t{i}", [128, 1024], F32, e5) for i in range(2)]
            p_a = [ps(f"p4_a{i}", [128, 512], F32, e5) for i in range(4)]
            p_tr4 = ps("p4_tr", [128, 8, 128], BF16, e5)
            for T in range(NTT):
                r0, nr, is_s, pti = tile_rows(T)
                gy_, ao_, ga_, xi_, x2_ = gyt[T % 2], aot[T % 2], gat[T % 2], xin[T % 2], x2t[T % 2]
                gkeys = [("gyT", ct_, "s") for ct_ in range(4)] if is_s else [("gyT", ct_, (T * 128) // SEQ, ((T * 128) % SEQ) // 512) for ct_ in range(4)]
                DMA(gy_[:], gyT_scr.rearrange("(c p) t -> p c t", p=128)[:, :, T * 128:(T + 1) * 128], r=gkeys, w=[gy_.name])
                if is_s:
                    V(lambda ao_=ao_: nc.vector.memset(ao_[:], 0.0), w=[ao_.name])
                    DMA(ao_[0:NS, :], ao_scr[T * 128:T * 128 + NS, :], r=[("ao", T)], w=[ao_.name])
                    V(lambda xi_=xi_: nc.vector.memset(xi_[:], 0.0), w=[xi_.name])
                    DMA(xi_[0:NS, :], xs[:, :], w=[xi_.name])
                else:
                    DMA(ao_[:], ao_scr[T * 128:(T + 1) * 128, :], r=[("ao", T)], w=[ao_.name])
                    DMA(xi_[:], xp[r0:r0 + 128, :], w=[xi_.name])
                DMA(ga_[:], gate_scr[T * 128:(T + 1) * 128, :], r=[("gate", T)], w=[ga_.name])
                for nh in range(2):
                    pv, pg = p_a[0], p_a[1]
                    for (pp, c0) in ((pv, nh * 512), (pg, 1024 + nh * 512)):
                        for kc in range(4):
                            PE(lambda pp=pp, c0=c0, kc=kc, gy_=gy_: nc.tensor.matmul(pp[:], lhsT=gy_[:, kc, :], rhs=Wglu[:, kc, c0:c0 + 512], start=(kc == 0), stop=(kc == 3)),
                               r=[gy_.name, "Wglu"], w=[pp.name], skip_same=True)
                    V(lambda nh=nh: nc.vector.tensor_tensor(out=zv[:], in0=p_a[0][:], in1=bglu[:, nh * 512:(nh + 1) * 512], op=ALU.add), r=[p_a[0].name, "bglu"], w=["zv"])
                    V(lambda nh=nh: nc.vector.tensor_tensor(out=zg[:], in0=p_a[1][:], in1=bglu[:, 1024 + nh * 512:1024 + (nh + 1) * 512], op=ALU.add),
                      r=[p_a[1].name, "bglu"], w=["zg"])
                    A(lambda: nc.scalar.activation(out=zg[:], in_=zg[:], func=AF.Sigmoid), r=["zg"], w=["zg"])
                    G(lambda nh=nh: nc.gpsimd.tensor_tensor(out=so[:, nh * 512:(nh + 1) * 512], in0=zv[:], in1=zg[:], op=ALU.mult), r=["zv", "zg"], w=["so"])
                for kc in range(4):
                    PE(lambda kc=kc, ao_=ao_: nc.tensor.transpose(out=p_tr4[:, kc, :], in_=ao_[:, kc * 128:(kc + 1) * 128], identity=ident[:]),
                       r=[ao_.name, "ident"], w=["p4_tr"], skip_same=True)
                A(lambda: nc.scalar.copy(out=aoT[:], in_=p_tr4[:, 0:4, :]), r=["p4_tr"], w=["aoT"])
                G(lambda ga_=ga_: nc.gpsimd.tensor_tensor(out=m1[:], in0=so[:], in1=ga_[:, 0:1024], op=ALU.mult), r=["so", ga_.name], w=["m1"])
                for nh in range(2):
                    pp = p_a[2 + nh]
                    for kc in range(4):
                        PE(lambda pp=pp, nh=nh, kc=kc: nc.tensor.matmul(pp[:], lhsT=aoT[:, kc, :], rhs=Wao[:, kc, nh * 512:(nh + 1) * 512], start=(kc == 0), stop=(kc == 3)),
                           r=["aoT", "Wao"], w=[pp.name], skip_same=True)
                    V(lambda pp=pp, nh=nh, ga_=ga_: nc.vector.tensor_tensor(out=m2[:], in0=pp[:], in1=ga_[:, 1024 + nh * 512:1024 + (nh + 1) * 512], op=ALU.mult),
                      r=[pp.name, ga_.name], w=["m2"])
                    V(lambda nh=nh: nc.vector.tensor_tensor(out=mixb[:, nh * 512:(nh + 1) * 512], in0=m1[:, nh * 512:(nh + 1) * 512], in1=m2[:], op=ALU.add),
                      r=["m1", "m2"], w=["mixb"])
                for kc in range(8):
                    PE(lambda kc=kc: nc.tensor.transpose(out=p_tr4[:, kc, :], in_=mixb[:, kc * 128:(kc + 1) * 128], identity=ident[:]),
                       r=["mixb", "ident"], w=["p4_tr"], skip_same=True)
                A(lambda: nc.scalar.copy(out=mixT[:], in_=p_tr4[:]), r=["p4_tr"], w=["mixT"])
                for nh in range(2):
                    pp = p_a[nh]
                    for kc in range(8):
                        PE(lambda pp=pp, nh=nh, kc=kc: nc.tensor.matmul(pp[:], lhsT=mixT[:, kc, :], rhs=Wo[:, kc, nh * 512:(nh + 1) * 512], start=(kc == 0), stop=(kc == 7)),
                           r=["mixT", "Wo"], w=[pp.name], skip_same=True)
                    V(lambda pp=pp, nh=nh, xi_=xi_, x2_=x2_: nc.vector.tensor_tensor(out=x2_[:, nh * 512:(nh + 1) * 512], in0=pp[:], in1=xi_[:, nh * 512:(nh + 1) * 512], op=ALU.add),
                      r=[pp.name, xi_.name], w=[x2_.name])
                DMA(x2_scr[T * 128:(T + 1) * 128, :], x2_[:], r=[x2_.name], w=[("x2", T)])
            S.barrier()

    if "p4" in phases:
        with contextlib.ExitStack() as e6:
            Wup = sb("Wup", [128, 8, D_FF], BF16, e6)
            Wdn = sb("Wdn", [128, 32, 1024], BF16, e6)
            g2c = sb("g2c", [128, 8], F32, e6)
            gfb = sb("gfb", [128, 1024], F32, e6)
            with nc.allow_non_contiguous_dma(reason="tiny gain vector"):
                DMA(g2c[:], norm2_g.rearrange("(k p) -> p k", p=128), w=["g2c"])
            DMA(gfb[:], normf_g.rearrange("(o n) -> o n", o=1).partition_broadcast(128), w=["gfb"])
            with contextlib.ExitStack() as e6w:
                load_weight_bf16(Wup, w_up, D_MODEL, D_FF, e6w, gcol=g2c, nm="wup", NSTG=3)
                load_weight_bf16(Wdn, w_down, D_FF, 1024, e6w, nm="wdn", NSTG=3)
                S.barrier()
            x2i = [sb(f"x2i{i}", [128, 1024], F32, e6) for i in range(2)]
            junk6 = sb("junk6", [128, 1024], F32, e6)
            ss6 = sb("ss6", [128, 1], F32, e6)
            rs6 = sb("rs6", [128, 1], F32, e6)
            hh = sb("hh", [128, 1024], BF16, e6)
            hhT = sb("hhT", [128, 8, 128], BF16, e6)
            rl = [sb(f"rl{i}", [128, 512], F32, e6) for i in range(2)]
            aT = sb("aT", [128, 32, 128], BF16, e6)
            x3 = sb("x3", [128, 1024], F32, e6)
            yt = [sb(f"yt{i}", [128, 1024], F32, e6) for i in range(2)]
            p_tr6 = ps("p6_tr", [128, 8, 128], BF16, e6)
            p_up = [ps(f"p6_up{i}", [128, 4, 128], F32, e6) for i in range(2)]
            p_dn = [ps(f"p6_dn{i}", [128, 512], F32, e6) for i in range(2)]
            for T in range(NTT):
                r0, nr, is_s, pti = tile_rows(T)
                xi_, y_ = x2i[T % 2], yt[T % 2]
                DMA(xi_[:], x2_scr[T * 128:(T + 1) * 128, :], r=[("x2", T)], w=[xi_.name])
                A(lambda xi_=xi_: nc.scalar.activation(out=junk6[:], in_=xi_[:], func=AF.Square, accum_out=ss6[:]), r=[xi_.name], w=["junk6", "ss6"])
                A(lambda: nc.scalar.activation(out=rs6[:], in_=ss6[:], func=AF.Sqrt, bias=eps_t[:], scale=1.0 / D_MODEL), r=["ss6", "eps_t"], w=["rs6"])
                V(lambda: nc.vector.reciprocal(out=rs6[:], in_=rs6[:]), r=["rs6"], w=["rs6"])
                V(lambda xi_=xi_: nc.vector.tensor_scalar(out=hh[:], in0=xi_[:], scalar1=rs6[:, 0:1], scalar2=None, op0=ALU.mult), r=[xi_.name, "rs6"], w=["hh"])
                for kc in range(8):
                    PE(lambda kc=kc: nc.tensor.transpose(out=p_tr6[:, kc, :], in_=hh[:, kc * 128:(kc + 1) * 128], identity=ident[:]),
                       r=["hh", "ident"], w=["p6_tr"], skip_same=True)
                A(lambda: nc.scalar.copy(out=hhT[:], in_=p_tr6[:]), r=["p6_tr"], w=["hhT"])
                for f4 in range(8):
                    pu = p_up[f4 % 2]
                    r_ = rl[f4 % 2]
                    for fi in range(4):
                        f = f4 * 4 + fi
                        for kc in range(8):
                            PE(lambda pu=pu, fi=fi, f=f, kc=kc: nc.tensor.matmul(pu[:, fi, :], lhsT=Wup[:, kc, f * 128:(f + 1) * 128], rhs=hhT[:, kc, :],
                                                                              start=(kc == 0), stop=(kc == 7)), r=["Wup", "hhT"], w=[pu.name], skip_same=True)
                    A(lambda pu=pu, r_=r_: nc.scalar.activation(out=r_[:], in_=pu[:].rearrange("p a b -> p (a b)"), func=AF.Relu), r=[pu.name], w=[r_.name])
                    V(lambda r_=r_, f4=f4: nc.vector.tensor_tensor(out=aT[:, f4 * 4:(f4 + 1) * 4, :].rearrange("p a b -> p (a b)"), in0=r_[:], in1=r_[:], op=ALU.mult),
                      r=[r_.name], w=["aT"])
                for nh in range(2):
                    pd = p_dn[nh]
                    for fk in range(32):
                        PE(lambda pd=pd, nh=nh, fk=fk: nc.tensor.matmul(pd[:], lhsT=aT[:, fk, :], rhs=Wdn[:, fk, nh * 512:(nh + 1) * 512], start=(fk == 0), stop=(fk == 31)),
                           r=["aT", "Wdn"], w=[pd.name], skip_same=True)
                    V(lambda pd=pd, nh=nh, xi_=xi_: nc.vector.tensor_tensor(out=x3[:, nh * 512:(nh + 1) * 512], in0=pd[:], in1=xi_[:, nh * 512:(nh + 1) * 512], op=ALU.add),
                      r=[pd.name, xi_.name], w=["x3"])
                A(lambda: nc.scalar.activation(out=junk6[:], in_=x3[:], func=AF.Square, accum_out=ss6[:]), r=["x3"], w=["junk6", "ss6"])
                A(lambda: nc.scalar.activation(out=rs6[:], in_=ss6[:], func=AF.Sqrt, bias=eps_t[:], scale=1.0 / D_MODEL), r=["ss6", "eps_t"], w=["rs6"])
                V(lambda: nc.vector.reciprocal(out=rs6[:], in_=rs6[:]), r=["rs6"], w=["rs6"])
                V(lambda y_=y_: nc.vector.scalar_tensor_tensor(out=y_[:], in0=x3[:], scalar=rs6[:, 0:1], in1=gfb[:], op0=ALU.mult, op1=ALU.mult),
                  r=["x3", "rs6", "gfb"], w=[y_.name])
                if is_s:
                    DMA(y_s[:, :], y_[0:NS, :], r=[y_.name], q="pool", is_output=True)
                else:
                    DMA(y_p[r0:r0 + 128, :], y_[:], r=[y_.name], q="pool", is_output=True)
            S.barrier()
    S.finish()
    es_all.close()
    dbg = dict(qT_scr=qT_scr, kT_scr=kT_scr)
    return nc


def make_in_map(inp, c, cfg):
    NB, NS = cfg.NB, cfg.NS
    f = lambda a: np.ascontiguousarray(a)
    m = {
        "xp": f(inp["x_prompt"][c * NB:(c + 1) * NB].reshape(NB * cfg.SEQ, D_MODEL)),
        "xs": f(inp["x_sample"][c * NS:(c + 1) * NS].reshape(NS, D_MODEL)),
        "cache_k": inp["cache_k"].reshape(cfg.NPOOL, -1),
        "cache_v": inp["cache_v"].reshape(cfg.NPOOL, -1),
        "cache_ik": inp["cache_idx_k"].reshape(cfg.NPOOL, -1),
        "st_re": f(inp["state_ssm_re"][c * NS:(c + 1) * NS].reshape(NS, -1)),
        "st_im": f(inp["state_ssm_im"][c * NS:(c + 1) * NS].reshape(NS, -1)),
        "ptab": f(inp["page_table"][c * NS:(c + 1) * NS].reshape(-1, 1)),
        "ssm_a_re": inp["ssm_a_re"].reshape(-1), "ssm_a_im": inp["ssm_a_im"].reshape(-1),
        "ssm_b_re": inp["ssm_b_re"].reshape(-1, 16), "ssm_b_im": inp["ssm_b_im"].reshape(-1, 16),
        "ssm_c_re": inp["ssm_c_re"].reshape(-1, 64), "ssm_c_im": inp["ssm_c_im"].reshape(-1, 64),
    }
    for k in ("norm1_g", "w_in", "ssm_log_dt", "ssm_d", "w_glu", "b_glu", "w_attn_out", "w_o", "norm2_g",
              "w_up", "w_down", "normf_g"):
        m[k] = inp[k]
    return m


ALL_PHASES = ("p1", "p2", "p3", "p3s", "p4")
OUT_NAMES = ["y_p", "y_s", "k_p", "v_p", "ki_p", "re_p", "im_p", "k_s", "v_s", "ki_s", "re_s", "im_s"]


def kernel(**inputs):
    inp = {k: np.asarray(v) for k, v in inputs.items()}
    B, SEQ = inp["x_prompt"].shape[0], inp["x_prompt"].shape[1]
    NSAMP = inp["x_sample"].shape[0]
    n_cores = 8
    cfg = Cfg(seq=SEQ, past=inp["page_table"].shape[1] * 128, nb=B // n_cores, ns=NSAMP // n_cores,
              n_pool=inp["cache_k"].shape[0])
    nc = build(cfg, phases=ALL_PHASES)
    shared = {"cache_k": inp["cache_k"].reshape(cfg.NPOOL, -1), "cache_v": inp["cache_v"].reshape(cfg.NPOOL, -1),
              "cache_ik": inp["cache_idx_k"].reshape(cfg.NPOOL, -1)}
    in_maps = []
    for c in range(n_cores):
        m = make_in_map(inp, c, cfg)
        m.update(shared)
        in_maps.append(m)
    res = run_bass_kernel_spmd(nc, in_maps, core_ids=list(range(n_cores)))
    outs = {n: np.concatenate([np.asarray(res.results[c][n]) for c in range(n_cores)], axis=0) for n in OUT_NAMES}
    NB, NS = cfg.NB, cfg.NS
    f32 = np.float32
    return (
        outs["y_p"].reshape(B, SEQ, D_MODEL).astype(f32, copy=False),
        outs["y_s"].reshape(NSAMP, 1, D_MODEL).astype(f32, copy=False),
        outs["k_p"].reshape(B, SEQ, 2, 64).astype(f32, copy=False),
        outs["v_p"].reshape(B, SEQ, 2, 64).astype(f32, copy=False),
        outs["ki_p"].reshape(B, SEQ, 64).astype(f32, copy=False),
        outs["re_p"].reshape(B, N_G, P_ST).astype(f32, copy=False),
        outs["im_p"].reshape(B, N_G, P_ST).astype(f32, copy=False),
        outs["k_s"].reshape(NSAMP, 1, 2, 64).astype(f32, copy=False),
        outs["v_s"].reshape(NSAMP, 1, 2, 64).astype(f32, copy=False),
        outs["ki_s"].reshape(NSAMP, 1, 64).astype(f32, copy=False),
        outs["re_s"].reshape(NSAMP, N_G, P_ST).astype(f32, copy=False),
        outs["im_s"].reshape(NSAMP, N_G, P_ST).astype(f32, copy=False),
    )
```

```python
import math
import contextlib
import numpy as np
import concourse.bass as bass
import concourse.mybir as mybir
from concourse.bass_utils import run_bass_kernel_spmd

F32 = mybir.dt.float32
BF16 = mybir.dt.bfloat16
I32 = mybir.dt.int32
U32 = mybir.dt.uint32
ALU = mybir.AluOpType
AF = mybir.ActivationFunctionType
AX = mybir.AxisListType

D_MODEL = 1024
D_SSM = 512
N_G = 32
P_ST = 64
N_HEADS = 8
N_KV = 2
HD = 64
D_ATTN = 512
N_IH = 8
IDX_D = 64
D_FF = 4096
IN_COLS = 3912
EPS = 1e-6
ROPE_THETA = 500000.0
NEG = -1.0e30
RELAX_SAME = False
RELAX_ENGINES = ('dve', 'act', 'pe')
BIGM = -240000.0
C_U, C_Q, C_K, C_V, C_QI, C_KI, C_WI, C_GS, C_GA = 0, 512, 1024, 1152, 1280, 1792, 1856, 1864, 2888


class Sync:
    def __init__(self, nc):
        self.nc = nc
        self.eng = {"pe": nc.tensor, "dve": nc.vector, "act": nc.scalar, "pool": nc.gpsimd, "sp": nc.sync}
        self.sem = {k: nc.alloc_semaphore("sem_" + k) for k in self.eng}
        self.cnt = {k: 0 for k in self.eng}
        self.waited = {k: {} for k in self.eng}
        self.R = 12
        self.dring = {q: [nc.alloc_semaphore(f"dsem_{q}{i}") for i in range(self.R)] for q in ("sp", "pool", "act")}
        self.dn = {q: 0 for q in self.dring}
        self.lastw = {}
        self.readers = {}
        self.semobj = {}
        for k, s in self.sem.items():
            self.semobj[id(s)] = s
        for q in self.dring:
            for s in self.dring[q]:
                self.semobj[id(s)] = s
        self.out_events = []
        self.relax_same = RELAX_SAME

    def _wait(self, e, ev):
        s, v = ev
        w = self.waited[e]
        if w.get(id(s), 0) >= v:
            return
        self.eng[e].wait_ge(s, v)
        w[id(s)] = v

    def _deps(self, e, reads, writes, skip_same=False):
        evs = []
        for k in reads:
            if k in self.lastw:
                evs.append(self.lastw[k])
        for k in writes:
            if k in self.lastw:
                evs.append(self.lastw[k])
            for r in self.readers.get(k, ()):
                evs.append(r)
        for ev in evs:
            if (skip_same or (self.relax_same and e in RELAX_ENGINES)) and e in self.sem and ev[0] is self.sem[e]:
                continue
            self._wait(e, ev)

    def _record(self, ev, reads, writes):
        for k in reads:
            self.readers.setdefault(k, []).append(ev)
            if len(self.readers[k]) > 24:
                best = {}
                for s, v in self.readers[k]:
                    if id(s) not in best or best[id(s)][1] < v:
                        best[id(s)] = (s, v)
                self.readers[k] = list(best.values())
        for k in writes:
            self.lastw[k] = ev
            self.readers[k] = []

    def op(self, e, fn, reads=(), writes=(), skip_same=False):
        self._deps(e, reads, writes, skip_same)
        ins = fn()
        self.cnt[e] += 1
        ins.then_inc(self.sem[e], 1)
        ev = (self.sem[e], self.cnt[e])
        self._record(ev, reads, writes)
        return ev

    def dma(self, q, fn, reads=(), writes=(), is_output=False):
        n = self.dn[q]
        slot = n % self.R
        s = self.dring[q][slot]
        prev = 16 * (n // self.R)
        if prev > 0:
            self._wait(q, (s, prev))
        self._deps(q, reads, writes)
        ins = fn()
        ins.then_inc(s, 16)
        self.dn[q] = n + 1
        ev = (s, prev + 16)
        self._record(ev, reads, writes)
        if is_output:
            self.out_events.append(ev)
        return ev

    def barrier(self):
        evs = []
        for q in self.dring:
            n = self.dn[q]
            for slot in range(self.R):
                cnt = (n - slot + self.R - 1) // self.R if n > slot else 0
                if cnt > 0:
                    evs.append((self.dring[q][slot], 16 * cnt))
        for e in self.eng:
            if self.cnt[e] > 0:
                evs.append((self.sem[e], self.cnt[e]))
        for e in self.eng:
            for ev in evs:
                if ev[0] is self.sem[e]:
                    continue
                self._wait(e, ev)

    def finish(self):
        for q in self.dring:
            n = self.dn[q]
            for slot in range(self.R):
                cnt = (n - slot + self.R - 1) // self.R if n > slot else 0
                if cnt > 0:
                    self._wait("sp", (self.dring[q][slot], 16 * cnt))
        for e in self.eng:
            if e != "sp" and self.cnt[e] > 0:
                self._wait("sp", (self.sem[e], self.cnt[e]))


class Cfg:
    def __init__(self, seq=4096, past=8192, nb=2, ns=16, n_pool=10240):
        self.SEQ = seq
        self.PAST = past
        self.NB = nb
        self.NS = ns
        self.NPOOL = n_pool
        self.NT = seq // 128
        self.NPG = past // 128
        self.TOPK = min(256, seq // 4)
        self.TOPK_S = min(256, (past + 1) // 4)
        self.NTOK = nb * seq
        self.NTT = nb * self.NT + 1
        self.NTOKP = self.NTT * 128


def build(cfg, phases=("p1",), debug=False):
    nc = bass.Bass("TRN2", target_bir_lowering=False)
    S = Sync(nc)
    NB, SEQ, NS, NT = cfg.NB, cfg.SEQ, cfg.NS, cfg.NT
    NTT, NTOKP = cfg.NTT, cfg.NTOKP

    def din(name, shape, dt=F32):
        return nc.dram_tensor(name, list(shape), dt, kind="ExternalInput").ap()

    def dout(name, shape, dt=F32):
        return nc.dram_tensor(name, list(shape), dt, kind="ExternalOutput").ap()

    def dscr(name, shape, dt=F32):
        return nc.dram_tensor(name, list(shape), dt, kind="ExternalOutput" if debug else "Internal").ap()

    xp = din("xp", [NB * SEQ, D_MODEL])
    xs = din("xs", [NS, D_MODEL])
    cache_k = din("cache_k", [cfg.NPOOL, 128 * 128])
    cache_v = din("cache_v", [cfg.NPOOL, 128 * 128])
    cache_ik = din("cache_ik", [cfg.NPOOL, 128 * 64])
    st_re = din("st_re", [NS, N_G * P_ST])
    st_im = din("st_im", [NS, N_G * P_ST])
    ptab = din("ptab", [NS * cfg.NPG, 1], I32)
    norm1_g = din("norm1_g", [D_MODEL])
    w_in = din("w_in", [D_MODEL, IN_COLS])
    a_re = din("ssm_a_re", [N_G * P_ST])
    a_im = din("ssm_a_im", [N_G * P_ST])
    log_dt = din("ssm_log_dt", [N_G])
    b_re = din("ssm_b_re", [N_G * P_ST, 16])
    b_im = din("ssm_b_im", [N_G * P_ST, 16])
    c_re = din("ssm_c_re", [N_G * 16, P_ST])
    c_im = din("ssm_c_im", [N_G * 16, P_ST])
    ssm_d = din("ssm_d", [D_SSM])
    w_glu = din("w_glu", [D_SSM, 2 * D_MODEL])
    b_glu = din("b_glu", [2 * D_MODEL])
    w_ao = din("w_attn_out", [D_ATTN, D_MODEL])
    w_o = din("w_o", [D_MODEL, D_MODEL])
    norm2_g = din("norm2_g", [D_MODEL])
    w_up = din("w_up", [D_MODEL, D_FF])
    w_down = din("w_down", [D_FF, D_MODEL])
    normf_g = din("normf_g", [D_MODEL])

    y_p = dout("y_p", [NB * SEQ, D_MODEL])
    y_s = dout("y_s", [NS, D_MODEL])
    k_p = dout("k_p", [NB * SEQ, 128])
    v_p = dout("v_p", [NB * SEQ, 128])
    ki_p = dout("ki_p", [NB * SEQ, 64])
    re_p = dout("re_p", [NB, N_G * P_ST])
    im_p = dout("im_p", [NB, N_G * P_ST])
    k_s = dout("k_s", [NS, 128])
    v_s = dout("v_s", [NS, 128])
    ki_s = dout("ki_s", [NS, 64])
    re_s = dout("re_s", [NS, N_G * P_ST])
    im_s = dout("im_s", [NS, N_G * P_ST])

    qT_scr = dscr("qT_scr", [NTT, 65, 8, 128], BF16)
    qiT_scr = dscr("qiT_scr", [NTT, 64, 8, 128], BF16)
    kT_scr = dscr("kT_scr", [64, 2, NTOKP], BF16)
    kiT_scr = dscr("kiT_scr", [64, NTOKP], BF16)
    vb_scr = dscr("vb_scr", [NTOKP, 2, 64], BF16)
    uT_scr = dscr("uT_scr", [D_SSM, NTOKP], F32)
    gate_scr = dscr("gate_scr", [NTOKP, 2048], F32)
    gyT_scr = dscr("gyT_scr", [D_SSM, NTOKP], BF16)
    ao_scr = dscr("ao_scr", [NTOKP, D_ATTN], BF16)
    x2_scr = dscr("x2_scr", [NTOKP, D_MODEL], F32)

    if debug:
        dbg_mb = dscr("dbg_mb", [128, SEQ], BF16)
        dbg_sc = dscr("dbg_sc", [128, SEQ], F32)
        dbg_thr = dscr("dbg_thr", [128, 1], F32)
    ident = nc.alloc_sbuf_tensor("ident", [128, 128], BF16)
    identf = nc.alloc_sbuf_tensor("identf", [128, 128], F32)
    cmask = nc.alloc_sbuf_tensor("cmask", [128, 128], F32)
    eps_t = nc.alloc_sbuf_tensor("eps_t", [128, 1], F32)
    cosT = nc.alloc_sbuf_tensor("cosT", [128, NT + 1, 8], F32)
    sinT = nc.alloc_sbuf_tensor("sinT", [128, NT + 1, 8], F32)
    lohi = nc.alloc_sbuf_tensor("lohi", [128, NTT, 16], F32)
    kn2 = nc.alloc_sbuf_tensor("kn2", [128, NTT, 2], F32)

    def V(fn, r=(), w=(), **kw):
        return S.op("dve", fn, r, w, **kw)

    def A(fn, r=(), w=(), **kw):
        return S.op("act", fn, r, w, **kw)

    def G(fn, r=(), w=(), **kw):
        return S.op("pool", fn, r, w, **kw)

    def PE(fn, r=(), w=(), **kw):
        return S.op("pe", fn, r, w, **kw)

    def DMA(out, in_, r=(), w=(), q="sp", is_output=False, **kw):
        return S.dma(q, lambda: S.eng[q].dma_start(out=out, in_=in_, **kw), r, w, is_output=is_output)

    ones_f = nc.alloc_sbuf_tensor("ones_f", [128, 128], F32)
    G(lambda: nc.gpsimd.memset(ones_f[:], 1.0), w=["ones_f"])
    G(lambda: nc.gpsimd.memset(eps_t[:], EPS), w=["eps_t"])
    G(lambda: nc.gpsimd.affine_select(out=identf[:], in_=ones_f[:], pattern=[[-1, 128]], compare_op=ALU.is_equal,
                                      fill=0.0, base=0, channel_multiplier=1), r=["ones_f"], w=["identf"])
    V(lambda: nc.vector.tensor_copy(out=ident[:], in_=identf[:]), r=["identf"], w=["ident"])
    zer_f = nc.alloc_sbuf_tensor("zer_f", [128, 128], F32)
    G(lambda: nc.gpsimd.memset(zer_f[:], 0.0), w=["zer_f"])
    G(lambda: nc.gpsimd.affine_select(out=cmask[:], in_=zer_f[:], pattern=[[-1, 128]], compare_op=ALU.is_ge,
                                      fill=NEG, base=0, channel_multiplier=1), r=["zer_f"], w=["cmask"])

    with contextlib.ExitStack() as es:
        posi = es.enter_context(nc.sbuf_tensor("posi", [128, NT + 1], I32))
        posf = es.enter_context(nc.sbuf_tensor("posf", [128, NT + 1], F32))
        ang = es.enter_context(nc.sbuf_tensor("ang", [128, NT + 1, 8], F32))
        ki_ = es.enter_context(nc.sbuf_tensor("kint", [128, NT + 1, 8], I32))
        kf = es.enter_context(nc.sbuf_tensor("kf", [128, NT + 1, 8], F32))
        rr = es.enter_context(nc.sbuf_tensor("rr", [128, NT + 1, 8], F32))
        r2 = es.enter_context(nc.sbuf_tensor("r2", [128, NT + 1, 8], F32))
        mk = es.enter_context(nc.sbuf_tensor("mk", [128, NT + 1, 8], F32))
        G(lambda: nc.gpsimd.iota(posi[:, 0:NT], pattern=[[128, NT]], base=0, channel_multiplier=1), w=["posi"])
        G(lambda: nc.gpsimd.iota(posi[:, NT:NT + 1], pattern=[[0, 1]], base=cfg.PAST, channel_multiplier=0), w=["posi"])
        V(lambda: nc.vector.tensor_copy(out=posf[:], in_=posi[:]), r=["posi"], w=["posf"])
        for j in range(8):
            inv = ROPE_THETA ** (-j / 8.0)
            V(lambda j=j, inv=inv: nc.vector.tensor_scalar(out=ang[:, :, j], in0=posf[:], scalar1=float(np.float32(inv)),
                                                           scalar2=float(1.0 / (2 * math.pi)), op0=ALU.mult, op1=ALU.mult),
              r=["posf"], w=["ang"])

        def wrap_sin(dst, off):
            V(lambda: nc.vector.tensor_scalar(out=rr[:], in0=ang[:], scalar1=float(off), scalar2=None, op0=ALU.add),
              r=["ang"], w=["rr"])
            V(lambda: nc.vector.tensor_copy(out=ki_[:], in_=rr[:]), r=["rr"], w=["kint"])
            V(lambda: nc.vector.tensor_copy(out=kf[:], in_=ki_[:]), r=["kint"], w=["kf"])
            V(lambda: nc.vector.tensor_tensor(out=r2[:], in0=rr[:], in1=kf[:], op=ALU.subtract), r=["rr", "kf"], w=["r2"])
            V(lambda: nc.vector.tensor_scalar(out=mk[:], in0=r2[:], scalar1=0.5, scalar2=None, op0=ALU.is_gt), r=["r2"], w=["mk"])
            V(lambda: nc.vector.tensor_tensor(out=r2[:], in0=r2[:], in1=mk[:], op=ALU.subtract), r=["r2", "mk"], w=["r2"])
            V(lambda: nc.vector.tensor_scalar(out=mk[:], in0=r2[:], scalar1=-0.5, scalar2=None, op0=ALU.is_lt), r=["r2"], w=["mk"])
            V(lambda: nc.vector.tensor_tensor(out=r2[:], in0=r2[:], in1=mk[:], op=ALU.add), r=["r2", "mk"], w=["r2"])
            A(lambda: nc.scalar.activation(out=dst[:], in_=r2[:], func=AF.Sin, scale=float(2 * math.pi)), r=["r2"], w=[dst.name])

        wrap_sin(sinT, 0.0)
        wrap_sin(cosT, 0.25)
        S.barrier()

    es_all = contextlib.ExitStack()

    def sb(name, shape, dt=F32, stack=None):
        return (stack or es_all).enter_context(nc.sbuf_tensor(name, list(shape), dt))

    def ps(name, shape, dt=F32, stack=None):
        return (stack or es_all).enter_context(nc.psum_tensor(name, list(shape), dt))

    def load_weight_bf16(dst, src, K, N, stack, gcol=None, nm="w", NSTG=2):
        KC = K // 128
        CH = 2048
        stg = [sb(f"stg_{nm}{i}", [128, CH], F32, stack) for i in range(NSTG)]
        i = 0
        for kc in range(KC):
            for c0 in range(0, N, CH):
                cw = min(CH, N - c0)
                st = stg[i % NSTG]
                DMA(st[:, 0:cw], src[kc * 128:(kc + 1) * 128, c0:c0 + cw], w=[st.name], q=("sp", "act")[i % 2])
                eng = ("dve", "pool")[i % 2]
                if gcol is None:
                    if eng == "dve":
                        V(lambda st=st, kc=kc, c0=c0, cw=cw: nc.vector.tensor_copy(out=dst[:, kc, c0:c0 + cw], in_=st[:, 0:cw]),
                          r=[st.name], w=[dst.name])
                    else:
                        G(lambda st=st, kc=kc, c0=c0, cw=cw: nc.gpsimd.tensor_copy(out=dst[:, kc, c0:c0 + cw], in_=st[:, 0:cw]),
                          r=[st.name], w=[dst.name])
                else:
                    V(lambda st=st, kc=kc, c0=c0, cw=cw: nc.vector.tensor_scalar(out=dst[:, kc, c0:c0 + cw], in0=st[:, 0:cw],
                                                                              scalar1=gcol[:, kc:kc + 1], scalar2=None, op0=ALU.mult),
                      r=[st.name, gcol.name], w=[dst.name])
                i += 1

    def tile_rows(T):
        if T < NB * NT:
            return T * 128, 128, False, T % NT
        return 0, NS, True, NT

    if "p1" in phases:
        with contextlib.ExitStack() as e1:
            Win = sb("Win", [128, 8, IN_COLS], BF16, e1)
            g1c = sb("g1c", [128, 8], F32, e1)
            with nc.allow_non_contiguous_dma(reason="tiny gain vector"):
                DMA(g1c[:], norm1_g.rearrange("(k p) -> p k", p=128), w=["g1c"])
            with contextlib.ExitStack() as e1w:
                load_weight_bf16(Win, w_in, D_MODEL, IN_COLS, e1w, gcol=g1c, nm="win", NSTG=6)
                S.barrier()
            xt = [sb(f"xt{i}", [128, D_MODEL], F32, e1) for i in range(2)]
            junk = sb("junk1", [128, D_MODEL], F32, e1)
            ss = sb("ss", [128, 1], F32, e1)
            rstd = sb("rstd", [128, 1], F32, e1)
            hb = sb("hb", [128, D_MODEL], BF16, e1)
            hT = sb("hT", [128, 8, 128], BF16, e1)
            pt = [sb(f"pt{i}", [128, IN_COLS - 512], F32, e1) for i in range(2)]
            uTs = sb("uTs", [128, 4, 128], F32, e1)
            tA = [sb(f"tA{i}", [128, 10, 8], F32, e1) for i in range(6)]
            qn = sb("qn", [128, 8], F32, e1)
            sq = sb("sq", [128, 10, 64], F32, e1)
            wp = sb("wp", [128, 8], F32, e1)
            qa = sb("qa", [128, 8, 65], BF16, e1)
            kb_ = sb("kb_", [128, 2, 64], BF16, e1)
            qib = sb("qib", [128, 8, 64], BF16, e1)
            kib = sb("kib", [128, 64], BF16, e1)
            vbt = sb("vbt", [128, 2, 64], BF16, e1)
            gts = sb("gts", [128, 2048], F32, e1)
            trs = sb("trs", [65, 19, 128], BF16, e1)
            p_tr = ps("p_tr", [128, 8, 128], BF16, e1)
            p_mm = [ps(f"p_mm{i}", [128, 512], F32, e1) for i in range(3)]
            p_u = ps("p_u", [128, 4, 128], F32, e1)
            p_t2 = ps("p_t2", [65, 19, 128], BF16, e1)

            def p1_front(T):
                r0, nr, is_s, pti = tile_rows(T)
                x_ = xt[T % 2]
                p_ = pt[T % 2]
                xn, pn = x_.name, p_.name
                if is_s:
                    V(lambda x_=x_: nc.vector.memset(x_[:], 0.0), w=[xn])
                    DMA(x_[0:NS, :], xs[:, :], w=[xn])
                else:
                    DMA(x_[:], xp[r0:r0 + 128, :], w=[xn])
                A(lambda x_=x_: nc.scalar.activation(out=junk[:], in_=x_[:], func=AF.Square, accum_out=ss[:]),
                  r=[xn], w=["junk1", "ss"])
                A(lambda: nc.scalar.activation(out=rstd[:], in_=ss[:], func=AF.Sqrt, bias=eps_t[:], scale=1.0 / D_MODEL),
                  r=["ss", "eps_t"], w=["rstd"])
                V(lambda: nc.vector.reciprocal(out=rstd[:], in_=rstd[:]), r=["rstd"], w=["rstd"])
                V(lambda x_=x_: nc.vector.tensor_scalar(out=hb[:], in0=x_[:], scalar1=rstd[:, 0:1], scalar2=None, op0=ALU.mult),
                  r=[xn, "rstd"], w=["hb"])
                for kc in range(8):
                    PE(lambda kc=kc: nc.tensor.transpose(out=p_tr[:, kc, :], in_=hb[:, kc * 128:(kc + 1) * 128], identity=ident[:]),
                       r=["hb", "ident"], w=["p_tr"], skip_same=True)
                A(lambda: nc.scalar.copy(out=hT[:], in_=p_tr[:]), r=["p_tr"], w=["hT"])
                for ct in range(4):
                    for kc in range(8):
                        PE(lambda ct=ct, kc=kc: nc.tensor.matmul(p_u[:, ct, :], lhsT=Win[:, kc, ct * 128:(ct + 1) * 128],
                                                                 rhs=hT[:, kc, :], start=(kc == 0), stop=(kc == 7)),
                           r=["Win", "hT"], w=["p_u"], skip_same=True)
                V(lambda: nc.vector.tensor_copy(out=uTs[:], in_=p_u[:]), r=["p_u"], w=["uTs"])
                DMA(uT_scr.rearrange("(c p) t -> p c t", p=128)[:, :, T * 128:(T + 1) * 128], uTs[:], r=["uTs"], w=[("uT", T)])
                ci = 0
                for c0 in range(512, IN_COLS, 512):
                    cw = min(512, IN_COLS - c0)
                    pm = p_mm[ci % 3]
                    for kc in range(8):
                        PE(lambda pm=pm, kc=kc, c0=c0, cw=cw: nc.tensor.matmul(pm[:, 0:cw], lhsT=hT[:, kc, :], rhs=Win[:, kc, c0:c0 + cw],
                                                                               start=(kc == 0), stop=(kc == 7)),
                           r=["Win", "hT"], w=[pm.name], skip_same=True)
                    if ci % 2 == 0:
                        A(lambda pm=pm, c0=c0, cw=cw, p_=p_: nc.scalar.copy(out=p_[:, c0 - 512:c0 - 512 + cw], in_=pm[:, 0:cw]),
                          r=[pm.name], w=[pn])
                    else:
                        V(lambda pm=pm, c0=c0, cw=cw, p_=p_: nc.vector.tensor_copy(out=p_[:, c0 - 512:c0 - 512 + cw], in_=pm[:, 0:cw]),
                          r=[pm.name], w=[pn])
                    ci += 1

            def p1_back(T):
                r0, nr, is_s, pti = tile_rows(T)
                x_ = xt[T % 2]
                p_ = pt[T % 2]
                xn, pn = x_.name, p_.name
                cs_c = cosT[:, pti, :]
                cs_s = sinT[:, pti, :]
                for (cb, H) in ((C_Q - 512, 10), (C_QI - 512, 9)):
                    X = p_[:, cb:cb + H * 64].rearrange("p (h d) -> p h d", d=64)
                    x1 = X[:, :, 0:8]
                    x2 = X[:, :, 8:16]
                    cB = cs_c.unsqueeze(1).to_broadcast([128, H, 8])
                    sB = cs_s.unsqueeze(1).to_broadcast([128, H, 8])
                    t = [tt[:, 0:H, :] for tt in tA]
                    V(lambda x1=x1, cB=cB, t=t: nc.vector.tensor_tensor(out=t[0], in0=x1, in1=cB, op=ALU.mult), r=[pn, "cosT"], w=["tA0"])
                    V(lambda x2=x2, sB=sB, t=t: nc.vector.tensor_tensor(out=t[1], in0=x2, in1=sB, op=ALU.mult), r=[pn, "sinT"], w=["tA1"])
                    V(lambda x1=x1, sB=sB, t=t: nc.vector.tensor_tensor(out=t[2], in0=x1, in1=sB, op=ALU.mult), r=[pn, "sinT"], w=["tA2"])
                    V(lambda x2=x2, cB=cB, t=t: nc.vector.tensor_tensor(out=t[3], in0=x2, in1=cB, op=ALU.mult), r=[pn, "cosT"], w=["tA3"])
                    V(lambda x1=x1, t=t: nc.vector.tensor_tensor(out=x1, in0=t[0], in1=t[1], op=ALU.subtract), r=["tA0", "tA1"], w=[pn])
                    V(lambda x2=x2, t=t: nc.vector.tensor_tensor(out=x2, in0=t[2], in1=t[3], op=ALU.add), r=["tA2", "tA3"], w=[pn])
                kcol, vcol, kicol = C_K - 512, C_V - 512, C_KI - 512
                if is_s:
                    DMA(k_s[:, :], p_[0:NS, kcol:kcol + 128], r=[pn], q="pool", is_output=True)
                    DMA(v_s[:, :], p_[0:NS, vcol:vcol + 128], r=[pn], q="pool", is_output=True)
                    DMA(ki_s[:, :], p_[0:NS, kicol:kicol + 64], r=[pn], q="pool", is_output=True)
                else:
                    DMA(k_p[r0:r0 + 128, :], p_[:, kcol:kcol + 128], r=[pn], q="pool", is_output=True)
                    DMA(v_p[r0:r0 + 128, :], p_[:, vcol:vcol + 128], r=[pn], q="pool", is_output=True)
                    DMA(ki_p[r0:r0 + 128, :], p_[:, kicol:kicol + 64], r=[pn], q="pool", is_output=True)
                QK = p_[:, C_Q - 512:C_Q - 512 + 640].rearrange("p (h d) -> p h d", d=64)
                G(lambda QK=QK: nc.gpsimd.tensor_tensor(out=sq[:], in0=QK, in1=QK, op=ALU.mult), r=[pn], w=["sq"])
                V(lambda: nc.vector.tensor_reduce(out=qn[:], in_=sq[:, 0:8, :], axis=AX.X, op=ALU.add), r=["sq"], w=["qn"])
                V(lambda T=T: nc.vector.tensor_reduce(out=kn2[:, T, :], in_=sq[:, 8:10, :], axis=AX.X, op=ALU.add), r=["sq"], w=["kn2"])
                A(lambda: nc.scalar.activation(out=qn[:], in_=qn[:], func=AF.Sqrt), r=["qn"], w=["qn"])
                V(lambda: nc.vector.tensor_scalar(out=qa[:, :, 64], in0=qn[:], scalar1=-1.0, scalar2=None, op0=ALU.mult),
                  r=["qn"], w=["qa"])
                V(lambda QK=QK: nc.vector.tensor_copy(out=qa[:, :, 0:64], in_=QK[:, 0:8, :]), r=[pn], w=["qa"])
                G(lambda QK=QK: nc.gpsimd.tensor_copy(out=kb_[:], in_=QK[:, 8:10, :]), r=[pn], w=["kb_"])
                wic = C_WI - 512
                V(lambda p_=p_, wic=wic: nc.vector.tensor_scalar(out=wp[:], in0=p_[:, wic:wic + 8], scalar1=float(8 ** -0.5 * 0.125),
                                                                 scalar2=None, op0=ALU.mult), r=[pn], w=["wp"])
                V(lambda T=T: nc.vector.tensor_scalar(out=lohi[:, T, 0:8], in0=wp[:], scalar1=0.0, scalar2=-3.0e38,
                                                      op0=ALU.is_le, op1=ALU.mult), r=["wp"], w=["lohi"])
                V(lambda T=T: nc.vector.tensor_scalar(out=lohi[:, T, 8:16], in0=wp[:], scalar1=0.0, scalar2=3.0e38,
                                                      op0=ALU.is_gt, op1=ALU.mult), r=["wp"], w=["lohi"])
                QI = p_[:, C_QI - 512:C_QI - 512 + 512].rearrange("p (h d) -> p h d", d=64)
                V(lambda QI=QI: nc.vector.tensor_tensor(out=qib[:], in0=QI, in1=wp[:].unsqueeze(2).to_broadcast([128, 8, 64]), op=ALU.mult),
                  r=[pn, "wp"], w=["qib"])
                G(lambda p_=p_: nc.gpsimd.tensor_copy(out=kib[:], in_=p_[:, C_KI - 512:C_KI - 512 + 64]), r=[pn], w=["kib"])
                G(lambda p_=p_: nc.gpsimd.tensor_copy(out=vbt[:], in_=p_[:, vcol:vcol + 128].rearrange("p (g d) -> p g d", d=64)),
                  r=[pn], w=["vbt"])
                DMA(vb_scr[T * 128:(T + 1) * 128, :, :], vbt[:], r=["vbt"], w=[("vb", T)], q="pool")
                A(lambda p_=p_: nc.scalar.activation(out=gts[:], in_=p_[:, C_GS - 512:C_GS - 512 + 2048], func=AF.Sigmoid),
                  r=[pn], w=["gts"])
                DMA(gate_scr[T * 128:(T + 1) * 128, :], gts[:], r=["gts"], w=[("gate", T)], q="pool")
                for h in range(8):
                    PE(lambda h=h: nc.tensor.transpose(out=p_t2[0:65, h, :], in_=qa[:, h, :], identity=ident[:]),
                       r=["qa", "ident"], w=["p_t2"], skip_same=True)
                for g in range(2):
                    PE(lambda g=g: nc.tensor.transpose(out=p_t2[0:64, 8 + g, :], in_=kb_[:, g, :], identity=ident[:]),
                       r=["kb_", "ident"], w=["p_t2"], skip_same=True)
                for h in range(8):
                    PE(lambda h=h: nc.tensor.transpose(out=p_t2[0:64, 10 + h, :], in_=qib[:, h, :], identity=ident[:]),
                       r=["qib", "ident"], w=["p_t2"], skip_same=True)
                PE(lambda: nc.tensor.transpose(out=p_t2[0:64, 18, :], in_=kib[:], identity=ident[:]),
                   r=["kib", "ident"], w=["p_t2"], skip_same=True)
                V(lambda: nc.vector.tensor_copy(out=trs[0:65, 0:8, :], in_=p_t2[0:65, 0:8, :]), r=["p_t2"], w=["trs"])
                A(lambda: nc.scalar.copy(out=trs[0:64, 8:19, :], in_=p_t2[0:64, 8:19, :]), r=["p_t2"], w=["trs"])
                DMA(qT_scr[T, :, :, :], trs[0:65, 0:8, :], r=["trs"], w=[("qT", T)], q="pool")
                DMA(kT_scr[:, :, T * 128:(T + 1) * 128], trs[0:64, 8:10, :], r=["trs"], w=[("kT", T)], q="pool")
                DMA(qiT_scr[T, :, :, :], trs[0:64, 10:18, :], r=["trs"], w=[("qiT", T)], q="pool")
                DMA(kiT_scr[:, T * 128:(T + 1) * 128], trs[0:64, 18, :], r=["trs"], w=[("kiT", T)], q="pool")

            p1_front(0)
            for T in range(NTT):
                if T + 1 < NTT:
                    p1_front(T + 1)
                p1_back(T)
            S.barrier()


    if "p2" in phases:
        with contextlib.ExitStack() as e2:
            NLEV = int(math.log2(SEQ))
            assert (1 << NLEV) == SEQ
            NCH = SEQ // 512
            sm = {}

            def small(name, shape=(128, 16), dt=F32):
                t_ = sb("s2_" + name, list(shape), dt, e2)
                sm[name] = t_
                return t_

            def vtt(o, a, b, op):
                V(lambda: nc.vector.tensor_tensor(out=o[:], in0=a[:], in1=b[:], op=op), r=[a.name, b.name], w=[o.name])

            def vts(o, a, s1, op, s2=None, op1=None):
                if op1 is None:
                    V(lambda: nc.vector.tensor_scalar(out=o[:], in0=a[:], scalar1=s1, scalar2=None, op0=op), r=[a.name], w=[o.name])
                else:
                    V(lambda: nc.vector.tensor_scalar(out=o[:], in0=a[:], scalar1=s1, scalar2=s2, op0=op, op1=op1), r=[a.name], w=[o.name])

            are_t = small("are"); aim_t = small("aim"); ldt = small("ldt"); dtt = small("dt")
            xre = small("xre"); th = small("th"); lam_abs = small("lam_abs")
            cs_ = small("cs"); sn_ = small("sn"); lam_re = small("lam_re"); lam_im = small("lam_im")
            tq = [small(f"tq{i}") for i in range(6)]
            tqi = small("tqi", dt=I32)
            cf_re = small("cf_re"); cf_im = small("cf_im")
            with nc.allow_non_contiguous_dma(reason="tiny ssm params"):
                DMA(are_t[:], a_re.rearrange("(ft f) -> f ft", f=128), w=[are_t.name])
                DMA(aim_t[:], a_im.rearrange("(ft f) -> f ft", f=128), w=[aim_t.name])
                lv = log_dt.rearrange("(ft two) -> two ft", two=2)
                DMA(ldt[0:64, :], lv[0:1, :].partition_broadcast(64), w=[ldt.name])
                DMA(ldt[64:128, :], lv[1:2, :].partition_broadcast(64), w=[ldt.name])
            A(lambda: nc.scalar.activation(out=dtt[:], in_=ldt[:], func=AF.Exp), r=[ldt.name], w=[dtt.name])
            vtt(xre, are_t, dtt, ALU.mult)
            vtt(th, aim_t, dtt, ALU.mult)
            A(lambda: nc.scalar.activation(out=lam_abs[:], in_=xre[:], func=AF.Exp), r=[xre.name], w=[lam_abs.name])

            def sincos_turns(dst, src_turns, off):
                a_, k_, r_, m_ = tq[0], tq[1], tq[2], tq[3]
                vts(a_, src_turns, float(off), ALU.add)
                V(lambda: nc.vector.tensor_copy(out=tqi[:], in_=a_[:]), r=[a_.name], w=[tqi.name])
                V(lambda: nc.vector.tensor_copy(out=k_[:], in_=tqi[:]), r=[tqi.name], w=[k_.name])
                vtt(r_, a_, k_, ALU.subtract)
                vts(m_, r_, 0.5, ALU.is_gt)
                vtt(r_, r_, m_, ALU.subtract)
                vts(m_, r_, -0.5, ALU.is_lt)
                vtt(r_, r_, m_, ALU.add)
                A(lambda: nc.scalar.activation(out=dst[:], in_=r_[:], func=AF.Sin, scale=float(2 * math.pi)), r=[r_.name], w=[dst.name])

            thn = small("thn")
            vts(thn, th, float(1.0 / (2 * math.pi)), ALU.mult)
            sincos_turns(sn_, thn, 0.0)
            sincos_turns(cs_, thn, 0.25)
            vtt(lam_re, lam_abs, cs_, ALU.mult)
            vtt(lam_im, lam_abs, sn_, ALU.mult)
            nr, den, t1_, t2_ = tq[0], tq[1], tq[2], tq[3]
            vts(nr, lam_re, -1.0, ALU.add)
            vtt(t1_, are_t, are_t, ALU.mult)
            vtt(t2_, aim_t, aim_t, ALU.mult)
            vtt(den, t1_, t2_, ALU.add)
            V(lambda: nc.vector.reciprocal(out=den[:], in_=den[:]), r=[den.name], w=[den.name])
            vtt(t1_, nr, are_t, ALU.mult)
            vtt(t2_, lam_im, aim_t, ALU.mult)
            vtt(t1_, t1_, t2_, ALU.add)
            vtt(cf_re, t1_, den, ALU.mult)
            vtt(t1_, lam_im, are_t, ALU.mult)
            vtt(t2_, nr, aim_t, ALU.mult)
            vtt(t1_, t1_, t2_, ALU.subtract)
            vtt(cf_im, t1_, den, ALU.mult)
            wre = small("wre", (128, 16, NLEV)); wim = small("wim", (128, 16, NLEV))
            V(lambda: nc.vector.tensor_copy(out=wre[:, :, 0], in_=cs_[:]), r=[cs_.name], w=[wre.name])
            V(lambda: nc.vector.tensor_copy(out=wim[:, :, 0], in_=sn_[:]), r=[sn_.name], w=[wim.name])
            for k in range(1, NLEV):
                V(lambda k=k: nc.vector.tensor_tensor(out=tq[0][:], in0=wre[:, :, k - 1], in1=wre[:, :, k - 1], op=ALU.mult), r=[wre.name], w=[tq[0].name])
                V(lambda k=k: nc.vector.tensor_tensor(out=tq[1][:], in0=wim[:, :, k - 1], in1=wim[:, :, k - 1], op=ALU.mult), r=[wim.name], w=[tq[1].name])
                V(lambda k=k: nc.vector.tensor_tensor(out=wre[:, :, k], in0=tq[0][:], in1=tq[1][:], op=ALU.subtract), r=[tq[0].name, tq[1].name], w=[wre.name])
                V(lambda k=k: nc.vector.tensor_tensor(out=tq[2][:], in0=wre[:, :, k - 1], in1=wim[:, :, k - 1], op=ALU.mult), r=[wre.name, wim.name], w=[tq[2].name])
                V(lambda k=k: nc.vector.tensor_scalar(out=wim[:, :, k], in0=tq[2][:], scalar1=2.0, scalar2=None, op0=ALU.mult), r=[tq[2].name], w=[wim.name])
            Bre = small("Bre", (128, 16, 16)); Bim = small("Bim", (128, 16, 16))
            bbr = small("bbr", (128, 16, 16)); bbi = small("bbi", (128, 16, 16)); btmp = small("btmp", (128, 16, 16))
            DMA(Bre[:], b_re.rearrange("(ft f) m -> f ft m", f=128), w=[Bre.name])
            DMA(Bim[:], b_im.rearrange("(ft f) m -> f ft m", f=128), w=[Bim.name])
            cfr_b = cf_re[:].unsqueeze(2).to_broadcast([128, 16, 16])
            cfi_b = cf_im[:].unsqueeze(2).to_broadcast([128, 16, 16])
            V(lambda: nc.vector.tensor_tensor(out=bbr[:], in0=Bre[:], in1=cfr_b, op=ALU.mult), r=[Bre.name, cf_re.name], w=[bbr.name])
            V(lambda: nc.vector.tensor_tensor(out=btmp[:], in0=Bim[:], in1=cfi_b, op=ALU.mult), r=[Bim.name, cf_im.name], w=[btmp.name])
            vtt(bbr, bbr, btmp, ALU.subtract)
            V(lambda: nc.vector.tensor_tensor(out=bbi[:], in0=Bim[:], in1=cfr_b, op=ALU.mult), r=[Bim.name, cf_re.name], w=[bbi.name])
            V(lambda: nc.vector.tensor_tensor(out=btmp[:], in0=Bre[:], in1=cfi_b, op=ALU.mult), r=[Bre.name, cf_im.name], w=[btmp.name])
            vtt(bbi, bbi, btmp, ALU.add)
            BT = small("BT", (128, 16, 2, 128), BF16)
            CT = small("CT", (128, 16, 2, 128), BF16)
            V(lambda: nc.vector.memset(CT[:], 0.0), w=[CT.name])
            Xb = small("Xb", (128, 128))
            p_s = ps("p2_s", [128, 512], F32, e2)
            for ft in range(16):
                ct, q = ft // 4, ft % 4
                for ri, bb in enumerate((bbr, bbi)):
                    V(lambda: nc.vector.memset(Xb[:], 0.0), w=[Xb.name])
                    V(lambda bb=bb, ft=ft, q=q: nc.vector.tensor_copy(out=Xb[0:64, 32 * q:32 * q + 16], in_=bb[0:64, ft, :]), r=[bb.name], w=[Xb.name])
                    V(lambda bb=bb, ft=ft, q=q: nc.vector.tensor_copy(out=Xb[64:128, 32 * q + 16:32 * q + 32], in_=bb[64:128, ft, :]), r=[bb.name], w=[Xb.name])
                    PE(lambda: nc.tensor.transpose(out=p_s[:, 0:128], in_=Xb[:], identity=identf[:]), r=[Xb.name, "identf"], w=[p_s.name])
                    V(lambda ft=ft, ri=ri: nc.vector.tensor_copy(out=BT[:, ft, ri, :], in_=p_s[:, 0:128]),
                      r=[p_s.name], w=[BT.name])
            Cre = small("Cre", (32, 16, 64)); Cim = small("Cim", (32, 16, 64))
            mA = small("mA", (32, 1)); mB = small("mB", (32, 1)); Xc = small("Xc", (32, 128))
            DMA(Cre[:], c_re.rearrange("(ft r) p -> r ft p", r=32), w=[Cre.name])
            DMA(Cim[:], c_im.rearrange("(ft r) p -> r ft p", r=32), w=[Cim.name])
            V(lambda: nc.vector.memset(mA[:], 0.0), w=[mA.name])
            V(lambda: nc.vector.memset(mA[0:16, :], 1.0), w=[mA.name])
            V(lambda: nc.vector.tensor_scalar(out=mB[:], in0=mA[:], scalar1=-1.0, scalar2=1.0, op0=ALU.mult, op1=ALU.add), r=[mA.name], w=[mB.name])
            for ft in range(16):
                for ri, cc in enumerate((Cre, Cim)):
                    sgn = 1.0 if ri == 0 else -1.0
                    V(lambda cc=cc, ft=ft, sgn=sgn: nc.vector.tensor_scalar(out=Xc[:, 0:64], in0=cc[:, ft, :], scalar1=mA[:, 0:1], scalar2=sgn,
                                                                          op0=ALU.mult, op1=ALU.mult), r=[cc.name, mA.name], w=[Xc.name])
                    V(lambda cc=cc, ft=ft, sgn=sgn: nc.vector.tensor_scalar(out=Xc[:, 64:128], in0=cc[:, ft, :], scalar1=mB[:, 0:1], scalar2=sgn,
                                                                          op0=ALU.mult, op1=ALU.mult), r=[cc.name, mB.name], w=[Xc.name])
                    PE(lambda: nc.tensor.transpose(out=p_s[:, 0:32], in_=Xc[:], identity=identf[0:32, 0:32]), r=[Xc.name, "identf"], w=[p_s.name])
                    V(lambda ft=ft, ri=ri: nc.vector.tensor_copy(out=CT[:, ft, ri, 32 * (ft % 4):32 * (ft % 4) + 32], in_=p_s[:, 0:32]), r=[p_s.name], w=[CT.name])
            dcol = small("dcol", (128, 4))
            with nc.allow_non_contiguous_dma(reason="tiny"):
                DMA(dcol[:], ssm_d.rearrange("(c p) -> p c", p=128), w=[dcol.name])
            h0r = small("h0r", (128, 16, NS)); h0i = small("h0i", (128, 16, NS))
            h1r = small("h1r", (128, 16, NS)); h1i = small("h1i", (128, 16, NS))
            sst = small("sst", (NS, 2048))
            for (src, dst) in ((st_re, h0r), (st_im, h0i)):
                DMA(sst[:], src[:, :], w=[sst.name])
                for ft in range(16):
                    PE(lambda ft=ft: nc.tensor.transpose(out=p_s[:, ft * NS:(ft + 1) * NS], in_=sst[:, ft * 128:(ft + 1) * 128],
                                                         identity=identf[0:NS, 0:NS]), r=[sst.name, "identf"], w=[p_s.name])
                V(lambda dst=dst: nc.vector.tensor_copy(out=dst[:].rearrange("p a b -> p (a b)"), in_=p_s[:, 0:16 * NS]), r=[p_s.name], w=[dst.name])

            WTOK = SEQ
            ufst = [sb(f"ufst{i}", [128, 512], F32, e2) for i in range(2)]
            ub = sb("ub", [128, NB, SEQ], BF16, e2)
            ufs = sb("ufs", [128, NS], F32, e2)
            ubs = sb("ubs", [128, NS], BF16, e2)
            Ec = sb("Ec", [128, SEQ], F32, e2)
            Es = sb("Es", [128, SEQ], F32, e2)
            rdec = sb("rdec", [128, 512], F32, e2)
            yT = sb("yT", [128, NB, SEQ], F32, e2)
            yTs = sb("yTs", [128, NS], F32, e2)
            fin = sb("fin", [128, 16, NB, 2], F32, e2)
            tm8 = [sb(f"tm{i}", [128, 512], F32, e2) for i in range(8)]
            tm = tm8[0:4]
            gre = [sb(f"gre{i}", [128, 512], F32, e2) for i in range(2)]
            gim = [sb(f"gim{i}", [128, 512], F32, e2) for i in range(2)]
            Gre = [sb(f"Gre{i}", [128, 512], F32, e2) for i in range(2)]
            Gim = [sb(f"Gim{i}", [128, 512], F32, e2) for i in range(2)]
            dm8 = [sb(f"dm{i}", [128, 512], F32, e2) for i in range(8)]
            dm = dm8[0:4]
            hre = [sb(f"hre{i}", [128, 512], BF16, e2) for i in range(2)]
            him = [sb(f"him{i}", [128, 512], BF16, e2) for i in range(2)]
            gl = [sb(f"gl{i}", [128, 512], F32, e2) for i in range(3)]
            gyb = sb("gyb", [128, 512], BF16, e2)
            p_br = [ps(f"p_br{i}", [128, 512], F32, e2) for i in range(2)]
            p_bi = [ps(f"p_bi{i}", [128, 512], F32, e2) for i in range(2)]
            p_y = [ps(f"p_y{i}", [128, 512], F32, e2) for i in range(2)]
            samp0 = (NTT - 1) * 128
            it = 0
            for ct in range(4):
                for b in range(NB):
                    for c in range(NCH):
                        us_ = ufst[(b * NCH + c) % 2]
                        DMA(us_[:], uT_scr[ct * 128:(ct + 1) * 128, b * SEQ + c * 512: b * SEQ + (c + 1) * 512],
                            r=[("uT", (b * SEQ + c * 512) // 128 + i_) for i_ in range(4)], w=[us_.name])
                        G(lambda b=b, c=c, us_=us_: nc.gpsimd.tensor_copy(out=ub[:, b, c * 512:(c + 1) * 512], in_=us_[:]), r=[us_.name], w=["ub"])
                DMA(ufs[:], uT_scr[ct * 128:(ct + 1) * 128, samp0:samp0 + NS], r=[("uT", NTT - 1)], w=["ufs"])
                G(lambda: nc.gpsimd.tensor_copy(out=ubs[:], in_=ufs[:]), r=["ufs"], w=["ubs"])
                for q in range(4):
                    ft = ct * 4 + q
                    qs = slice(32 * q, 32 * q + 32)
                    V(lambda: nc.vector.memset(Ec[:, 0:1], 1.0), w=["Ec"])
                    V(lambda: nc.vector.memset(Es[:, 0:1], 0.0), w=["Es"])
                    for k in range(NLEV):
                        n = 1 << k
                        wr = wre[:, ft, k:k + 1]
                        wi = wim[:, ft, k:k + 1]
                        for n0 in range(0, n, 512):
                            nn = min(512, n - n0)
                            lo_ = slice(n0, n0 + nn)
                            hi_ = slice(n + n0, n + n0 + nn)
                            A(lambda wi=wi, lo_=lo_, nn=nn: nc.scalar.activation(out=tm[0][:, 0:nn], in_=Es[:, lo_], func=AF.Identity, scale=wi),
                              r=["Es", wim.name], w=[tm[0].name])
                            V(lambda wr=wr, lo_=lo_, hi_=hi_, nn=nn: nc.vector.scalar_tensor_tensor(out=Ec[:, hi_], in0=Ec[:, lo_], scalar=wr, in1=tm[0][:, 0:nn],
                                                                                                  op0=ALU.mult, op1=ALU.subtract),
                              r=["Ec", tm[0].name, wre.name], w=["Ec"])
                            A(lambda wi=wi, lo_=lo_, nn=nn: nc.scalar.activation(out=tm[1][:, 0:nn], in_=Ec[:, lo_], func=AF.Identity, scale=wi),
                              r=["Ec", wim.name], w=[tm[1].name])
                            V(lambda wr=wr, lo_=lo_, hi_=hi_, nn=nn: nc.vector.scalar_tensor_tensor(out=Es[:, hi_], in0=Es[:, lo_], scalar=wr, in1=tm[1][:, 0:nn],
                                                                                                  op0=ALU.mult, op1=ALU.add),
                              r=["Es", tm[1].name, wre.name], w=["Es"])
                    V(lambda ft=ft: nc.vector.tensor_scalar(out=rdec[:], in0=Ec[:, 0:512], scalar1=0.0, scalar2=lam_abs[:, ft:ft + 1],
                                                            op0=ALU.mult, op1=ALU.add), r=["Ec", lam_abs.name], w=["rdec"])
                    chunks = [(b, c) for b in range(NB) for c in range(NCH)]
                    base_it = it

                    def stage_A(j):
                        b, c = chunks[j]
                        itj = base_it + j
                        cs = slice(c * 512, (c + 1) * 512)
                        pr, pi = p_br[itj % 2], p_bi[itj % 2]
                        PE(lambda: nc.tensor.matmul(pr[:], lhsT=BT[:, ft, 0, :], rhs=ub[:, b, cs], start=True, stop=True), r=[BT.name, "ub"], w=[pr.name])
                        PE(lambda: nc.tensor.matmul(pi[:], lhsT=BT[:, ft, 1, :], rhs=ub[:, b, cs], start=True, stop=True), r=[BT.name, "ub"], w=[pi.name])
                        g_r, g_i, G_r, G_i = gre[itj % 2], gim[itj % 2], Gre[itj % 2], Gim[itj % 2]
                        tm = tm8[4 * (itj % 2):4 * (itj % 2) + 4]
                        V(lambda: nc.vector.tensor_tensor(out=tm[0][:], in0=pr[:], in1=Ec[:, cs], op=ALU.mult), r=[pr.name, "Ec"], w=[tm[0].name])
                        V(lambda: nc.vector.tensor_tensor(out=tm[1][:], in0=pi[:], in1=Es[:, cs], op=ALU.mult), r=[pi.name, "Es"], w=[tm[1].name])
                        V(lambda: nc.vector.tensor_tensor(out=tm[2][:], in0=pi[:], in1=Ec[:, cs], op=ALU.mult), r=[pi.name, "Ec"], w=[tm[2].name])
                        V(lambda: nc.vector.tensor_tensor(out=tm[3][:], in0=pr[:], in1=Es[:, cs], op=ALU.mult), r=[pr.name, "Es"], w=[tm[3].name])
                        V(lambda: nc.vector.tensor_tensor(out=g_r[:], in0=tm[0][:], in1=tm[1][:], op=ALU.add), r=[tm[0].name, tm[1].name], w=[g_r.name])
                        V(lambda: nc.vector.tensor_tensor(out=g_i[:], in0=tm[2][:], in1=tm[3][:], op=ALU.subtract), r=[tm[2].name, tm[3].name], w=[g_i.name])
                        if c == 0:
                            ini_r, ini_i, rd_extra = 0.0, 0.0, []
                        else:
                            pG_r, pG_i = Gre[(itj - 1) % 2], Gim[(itj - 1) % 2]
                            ini_r, ini_i = pG_r[:, 511:512], pG_i[:, 511:512]
                            rd_extra = [pG_r.name, pG_i.name]
                        V(lambda: nc.vector.tensor_tensor_scan(out=G_r[:], data0=rdec[:], data1=g_r[:], initial=ini_r, op0=ALU.mult, op1=ALU.add),
                          r=["rdec", g_r.name] + rd_extra, w=[G_r.name])
                        V(lambda: nc.vector.tensor_tensor_scan(out=G_i[:], data0=rdec[:], data1=g_i[:], initial=ini_i, op0=ALU.mult, op1=ALU.add),
                          r=["rdec", g_i.name] + rd_extra, w=[G_i.name])

                    def stage_B(j):
                        b, c = chunks[j]
                        itj = base_it + j
                        cs = slice(c * 512, (c + 1) * 512)
                        G_r, G_i = Gre[itj % 2], Gim[itj % 2]
                        dm = dm8[4 * (itj % 2):4 * (itj % 2) + 4]
                        h_r, h_i = hre[itj % 2], him[itj % 2]
                        G(lambda: nc.gpsimd.tensor_tensor(out=dm[0][:], in0=G_r[:], in1=Ec[:, cs], op=ALU.mult), r=[G_r.name, "Ec"], w=[dm[0].name])
                        G(lambda: nc.gpsimd.tensor_tensor(out=dm[1][:], in0=G_i[:], in1=Es[:, cs], op=ALU.mult), r=[G_i.name, "Es"], w=[dm[1].name])
                        G(lambda: nc.gpsimd.tensor_tensor(out=dm[2][:], in0=G_r[:], in1=Es[:, cs], op=ALU.mult), r=[G_r.name, "Es"], w=[dm[2].name])
                        G(lambda: nc.gpsimd.tensor_tensor(out=dm[3][:], in0=G_i[:], in1=Ec[:, cs], op=ALU.mult), r=[G_i.name, "Ec"], w=[dm[3].name])
                        G(lambda: nc.gpsimd.tensor_tensor(out=h_r[:], in0=dm[0][:], in1=dm[1][:], op=ALU.subtract), r=[dm[0].name, dm[1].name], w=[h_r.name])
                        G(lambda: nc.gpsimd.tensor_tensor(out=h_i[:], in0=dm[2][:], in1=dm[3][:], op=ALU.add), r=[dm[2].name, dm[3].name], w=[h_i.name])
                        if c == NCH - 1:
                            G(lambda: nc.gpsimd.tensor_tensor(out=fin[:, ft, b, 0:1], in0=dm[0][:, 511:512], in1=dm[1][:, 511:512], op=ALU.subtract),
                              r=[dm[0].name, dm[1].name], w=["fin"])
                            G(lambda: nc.gpsimd.tensor_tensor(out=fin[:, ft, b, 1:2], in0=dm[2][:, 511:512], in1=dm[3][:, 511:512], op=ALU.add),
                              r=[dm[2].name, dm[3].name], w=["fin"])
                        py = p_y[itj % 2]
                        PE(lambda: nc.tensor.matmul(py[:, :], lhsT=CT[:, ft, 0, :], rhs=h_r[:], start=True, stop=False), r=[CT.name, h_r.name], w=[py.name])
                        PE(lambda: nc.tensor.matmul(py[:, :], lhsT=CT[:, ft, 1, :], rhs=h_i[:], start=False, stop=True), r=[CT.name, h_i.name], w=[py.name], skip_same=True)
                        if q == 0:
                            A(lambda: nc.scalar.copy(out=yT[:, b, cs], in_=py[:, :]), r=[py.name], w=["yT"])
                        else:
                            V(lambda: nc.vector.tensor_tensor(out=yT[:, b, cs], in0=py[:, :], in1=yT[:, b, cs], op=ALU.add), r=[py.name, "yT"], w=["yT"])

                    stage_A(0)
                    for j in range(len(chunks)):
                        if j + 1 < len(chunks):
                            stage_A(j + 1)
                        stage_B(j)
                    it += len(chunks)
                    pr, pi = p_br[it % 2], p_bi[it % 2]
                    PE(lambda pr=pr, ft=ft: nc.tensor.matmul(pr[:, 0:NS], lhsT=BT[:, ft, 0, :], rhs=ubs[:, :], start=True, stop=True),
                       r=[BT.name, "ubs"], w=[pr.name])
                    PE(lambda pi=pi, ft=ft: nc.tensor.matmul(pi[:, 0:NS], lhsT=BT[:, ft, 1, :], rhs=ubs[:, :], start=True, stop=True),
                       r=[BT.name, "ubs"], w=[pi.name])
                    lr, li = lam_re[:, ft:ft + 1], lam_im[:, ft:ft + 1]
                    V(lambda ft=ft, li=li: nc.vector.tensor_scalar(out=tm[0][:, 0:NS], in0=h0i[:, ft, :], scalar1=li, scalar2=None, op0=ALU.mult),
                      r=[h0i.name, lam_im.name], w=[tm[0].name])
                    V(lambda ft=ft, lr=lr: nc.vector.scalar_tensor_tensor(out=tm[1][:, 0:NS], in0=h0r[:, ft, :], scalar=lr, in1=tm[0][:, 0:NS], op0=ALU.mult, op1=ALU.subtract),
                      r=[h0r.name, lam_re.name, tm[0].name], w=[tm[1].name])
                    V(lambda ft=ft, pr=pr: nc.vector.tensor_tensor(out=h1r[:, ft, :], in0=pr[:, 0:NS], in1=tm[1][:, 0:NS], op=ALU.add), r=[pr.name, tm[1].name], w=[h1r.name])
                    V(lambda ft=ft, li=li: nc.vector.tensor_scalar(out=tm[2][:, 0:NS], in0=h0r[:, ft, :], scalar1=li, scalar2=None, op0=ALU.mult),
                      r=[h0r.name, lam_im.name], w=[tm[2].name])
                    V(lambda ft=ft, lr=lr: nc.vector.scalar_tensor_tensor(out=tm[3][:, 0:NS], in0=h0i[:, ft, :], scalar=lr, in1=tm[2][:, 0:NS], op0=ALU.mult, op1=ALU.add),
                      r=[h0i.name, lam_re.name, tm[2].name], w=[tm[3].name])
                    V(lambda ft=ft, pi=pi: nc.vector.tensor_tensor(out=h1i[:, ft, :], in0=pi[:, 0:NS], in1=tm[3][:, 0:NS], op=ALU.add), r=[pi.name, tm[3].name], w=[h1i.name])
                    h_r, h_i = hre[it % 2], him[it % 2]
                    V(lambda h_r=h_r, ft=ft: nc.vector.tensor_copy(out=h_r[:, 0:NS], in_=h1r[:, ft, :]), r=[h1r.name], w=[h_r.name])
                    V(lambda h_i=h_i, ft=ft: nc.vector.tensor_copy(out=h_i[:, 0:NS], in_=h1i[:, ft, :]), r=[h1i.name], w=[h_i.name])
                    py = p_y[it % 2]
                    PE(lambda py=py, h_r=h_r, ft=ft, qs=qs: nc.tensor.matmul(py[:, 0:NS], lhsT=CT[:, ft, 0, :], rhs=h_r[:, 0:NS], start=True, stop=False),
                       r=[CT.name, h_r.name], w=[py.name])
                    PE(lambda py=py, h_i=h_i, ft=ft, qs=qs: nc.tensor.matmul(py[:, 0:NS], lhsT=CT[:, ft, 1, :], rhs=h_i[:, 0:NS], start=False, stop=True),
                       r=[CT.name, h_i.name], w=[py.name], skip_same=True)
                    if q == 0:
                        A(lambda py=py: nc.scalar.copy(out=yTs[:, :], in_=py[:, 0:NS]), r=[py.name], w=["yTs"])
                    else:
                        V(lambda py=py: nc.vector.tensor_tensor(out=yTs[:, :], in0=py[:, 0:NS], in1=yTs[:, :], op=ALU.add), r=[py.name, "yTs"], w=["yTs"])
                    it += 1
                dsc = dcol[:, ct:ct + 1]

                def gelu_block(ysrc, usrc, n, dst_dram, rkeys, wkey):
                    a0, a1, a2 = gl[0][:, 0:n], gl[1][:, 0:n], gl[2][:, 0:n]
                    V(lambda: nc.vector.scalar_tensor_tensor(out=a0, in0=usrc, scalar=dsc, in1=ysrc, op0=ALU.mult, op1=ALU.add),
                      r=rkeys + [dcol.name], w=[gl[0].name])
                    A(lambda: nc.scalar.activation(out=a1, in_=a0, func=AF.Square), r=[gl[0].name], w=[gl[1].name])
                    V(lambda: nc.vector.tensor_scalar(out=a1, in0=a1, scalar1=0.044715, scalar2=1.0, op0=ALU.mult, op1=ALU.add), r=[gl[1].name], w=[gl[1].name])
                    G(lambda: nc.gpsimd.tensor_tensor(out=a2, in0=a1, in1=a0, op=ALU.mult), r=[gl[0].name, gl[1].name], w=[gl[2].name])
                    A(lambda: nc.scalar.activation(out=a2, in_=a2, func=AF.Sigmoid, scale=1.5957691216057308), r=[gl[2].name], w=[gl[2].name])
                    V(lambda: nc.vector.tensor_tensor(out=gyb[:, 0:n], in0=a0, in1=a2, op=ALU.mult), r=[gl[0].name, gl[2].name], w=["gyb"])
                    DMA(dst_dram, gyb[:, 0:n], r=["gyb"], w=[wkey])

                for b in range(NB):
                    for c in range(NCH):
                        cs = slice(c * 512, (c + 1) * 512)
                        t0 = b * SEQ + c * 512
                        us_ = ufst[(b * NCH + c) % 2]
                        DMA(us_[:], uT_scr[ct * 128:(ct + 1) * 128, t0:t0 + 512], r=[("uT", t0 // 128 + i_) for i_ in range(4)], w=[us_.name])
                        gelu_block(yT[:, b, cs], us_[:], 512, gyT_scr[ct * 128:(ct + 1) * 128, t0:t0 + 512], ["yT", us_.name], ("gyT", ct, b, c))
                gelu_block(yTs[:], ufs[:], NS, gyT_scr[ct * 128:(ct + 1) * 128, samp0:samp0 + NS], ["yTs", "ufs"], ("gyT", ct, "s"))
            with nc.allow_non_contiguous_dma(reason="state out"):
                for b in range(NB):
                    DMA(re_p[b:b + 1, :].rearrange("o (ft f) -> f (o ft)", f=128), fin[:, :, b, 0], r=["fin"], q="pool", is_output=True)
                    DMA(im_p[b:b + 1, :].rearrange("o (ft f) -> f (o ft)", f=128), fin[:, :, b, 1], r=["fin"], q="pool", is_output=True)
            for (src, dst) in ((h1r, re_s), (h1i, im_s)):
                for half in range(4):
                    for j in range(4):
                        ft = half * 4 + j
                        PE(lambda src=src, ft=ft, j=j: nc.tensor.transpose(out=p_s[0:NS, j * 128:(j + 1) * 128], in_=src[:, ft, :], identity=identf[:]),
                           r=[src.name, "identf"], w=[p_s.name])
                    V(lambda half=half: nc.vector.tensor_copy(out=sst[:, half * 512:(half + 1) * 512], in_=p_s[0:NS, :]), r=[p_s.name], w=[sst.name])
                DMA(dst[:, :], sst[:], r=[sst.name], q="pool", is_output=True)
            S.barrier()

    NIT = 18
    if "p3" in phases:
        assert NB == 2
        with contextlib.ExitStack() as e3:
            TOPK = cfg.TOPK
            kTa = [sb(f"kTa{b}", [65, 2, SEQ], BF16, e3) for b in range(NB)]
            kiT = [sb(f"kiT{b}", [64, SEQ], BF16, e3) for b in range(NB)]
            V1 = [sb(f"V1_{b}", [128, NT, 2, 65], BF16, e3) for b in range(NB)]
            Irep = sb("Irep", [128, 4, 128], BF16, e3)
            pw2 = sb("pw2", [128, NIT + 1], F32, e3)
            kmaxb = sb("kmaxb", [128, 1], F32, e3)
            km1 = sb("km1", [128, 1], F32, e3)
            km2 = sb("km2", [1, 128], F32, e3)
            zrow = sb("zrow", [1, 512], BF16, e3)
            qTa = [[sb(f"qTa{b}_{i}", [65, 8, 128], BF16, e3) for i in range(2)] for b in range(NB)]
            qiT = [[sb(f"qiT{b}_{i}", [64, 8, 128], BF16, e3) for i in range(2)] for b in range(NB)]
            score2 = [[sb(f"score{j}_{b}", [128, SEQ], F32, e3) for b in range(NB)] for j in range(2)]
            eps1 = sb("eps1", [128, 1], F32, e3)
            stph = sb("stph", [128, NIT + 1], F32, e3)
            G(lambda: nc.gpsimd.memset(eps1[:], 1.0), w=["eps1"])
            NTC = 4
            tmpc = [sb(f"tmpc{i}", [128, 512], F32, e3) for i in range(NTC)]
            junkb = [sb("junkb0", [128, SEQ], mybir.dt.uint8, e3), sb("junkb1", [128, SEQ], BF16, e3)]
            mb = [sb(f"mb{b}", [128, SEQ], BF16, e3) for b in range(NB)]
            pT = [sb(f"pT{i}", [128, 512], BF16, e3) for i in range(4)]
            ao = [sb(f"ao{b}", [128, 8, 64], BF16, e3) for b in range(NB)]
            amax = [sb(f"amax{b}", [128, 1], F32, e3) for b in range(NB)]
            stp = [sb(f"stp{b}", [128, NIT + 1], F32, e3) for b in range(NB)]
            mid = [sb(f"mid{b}", [128, 1], F32, e3) for b in range(NB)]
            cnt = [sb(f"cnt{b}", [128, 1], F32, e3) for b in range(NB)]
            c2 = [sb(f"c2{b}", [128, 1], F32, e3) for b in range(NB)]
            thr = [sb(f"thr{b}", [128, 1], F32, e3) for b in range(NB)]
            rec = [sb(f"rec{b}", [128, 4], F32, e3) for b in range(NB)]
            p_ix = [ps(f"p_ix{i}", [128, 512], F32, e3) for i in range(2)]
            p_st = [ps(f"p_st{i}", [128, 512], F32, e3) for i in range(3)]
            p_o = [ps(f"p_o{j}", [128, 512], F32, e3) for j in range(2)]
            p_n = ps("p_n", [128, 4, 65], F32, e3)
            oT = [sb(f"oT{b}", [65, 2, 512], F32, e3) for b in range(NB)]
            p_m = p_ix[0]

            for k in range(NIT + 1):
                G(lambda k=k: nc.gpsimd.memset(pw2[:, k:k + 1], float(2.0 ** (-k))), w=["pw2"])
            G(lambda: nc.gpsimd.memset(zrow[:], 0.0), w=["zrow"])
            for j in range(4):
                V(lambda j=j: nc.vector.tensor_copy(out=Irep[:, j, :], in_=ident[:]), r=["ident"], w=["Irep"])
            V(lambda: nc.vector.tensor_reduce(out=km1[:], in_=kn2[:].rearrange("p a b -> p (a b)"), axis=AX.X, op=ALU.max), r=["kn2"], w=["km1"])
            PE(lambda: nc.tensor.transpose(out=p_m[0:1, 0:128], in_=km1[:], identity=identf[:]), r=["km1", "identf"], w=[p_m.name])
            V(lambda: nc.vector.tensor_reduce(out=km2[0:1, 0:1], in_=p_m[0:1, 0:128], axis=AX.X, op=ALU.max), r=[p_m.name], w=["km2"])
            A(lambda: nc.scalar.activation(out=km2[0:1, 0:1], in_=km2[0:1, 0:1], func=AF.Sqrt), r=["km2"], w=["km2"])
            PE(lambda: nc.tensor.matmul(p_m[:, 0:1], lhsT=ones_f[0:1, :], rhs=km2[0:1, 0:1], start=True, stop=True), r=["ones_f", "km2"], w=[p_m.name])
            V(lambda: nc.vector.tensor_copy(out=kmaxb[:], in_=p_m[:, 0:1]), r=[p_m.name], w=["kmaxb"])

            for b in range(NB):
                t0 = b * SEQ
                for g in range(2):
                    DMA(kTa[b][0:64, g, :], kT_scr[:, g, t0:t0 + SEQ], r=[("kT", b * NT + i_) for i_ in range(NT)], w=[kTa[b].name])
                V(lambda b=b: nc.vector.memset(kTa[b][64:65, :, :], 1.0), w=[kTa[b].name])
                DMA(kiT[b][:, :], kiT_scr[:, t0:t0 + SEQ], r=[("kiT", b * NT + i_) for i_ in range(NT)], w=[kiT[b].name])
                for g in range(2):
                    DMA(V1[b][:, :, g, 0:64], vb_scr[t0:t0 + SEQ, g, :].rearrange("(kb p) d -> p kb d", p=128),
                        r=[("vb", b * NT + i_) for i_ in range(NT)], w=[V1[b].name])
                V(lambda b=b: nc.vector.memset(V1[b][:, :, :, 64:65], 1.0), w=[V1[b].name])

            itc3 = [0, 0]
            thrc = [sb(f"thrc{j}", [128, 1], F32, e3) for j in range(2)]

            def slot_bufs(i):
                return [qTa[b][i % 2] for b in range(NB)], [qiT[b][i % 2] for b in range(NB)], [score2[i % 2][b] for b in range(NB)]

            def idx_stage(i, filler=None):
                Sk = (i + 1) * 128
                search = Sk > TOPK
                Ts = [b * NT + i for b in range(NB)]
                qa_, qi_, sc2 = slot_bufs(i)
                for b in range(NB):
                    DMA(qa_[b][:], qT_scr[Ts[b], :, :, :], r=[("qT", Ts[b])], w=[qa_[b].name])
                    DMA(qi_[b][:], qiT_scr[Ts[b], :, :, :], r=[("qiT", Ts[b])], w=[qi_[b].name])
                    V(lambda b=b: nc.vector.tensor_scalar(out=qa_[b][64:65, :, :], in0=qa_[b][64:65, :, :], scalar1=kmaxb[64:65, 0:1], scalar2=None, op0=ALU.mult),
                      r=[qa_[b].name, "kmaxb"], w=[qa_[b].name])
                for b in range(NB):
                    sc_ = sc2[b]
                    for c0 in range(0, Sk, 512):
                        cw = min(512, Sk - c0)
                        for h in range(8):
                            it = itc3[0]
                            px = p_ix[it % 2]
                            PE(lambda px=px, b=b, h=h, c0=c0, cw=cw: nc.tensor.matmul(px[:, 0:cw], lhsT=qi_[b][:, h, :], rhs=kiT[b][:, c0:c0 + cw], start=True, stop=True),
                               r=[qi_[b].name, kiT[b].name], w=[px.name])
                            lo_ = lohi[:, Ts[b], h:h + 1]
                            hi_ = lohi[:, Ts[b], 8 + h:9 + h]
                            if h == 0:
                                V(lambda px=px, sc_=sc_, c0=c0, cw=cw, lo_=lo_, hi_=hi_: nc.vector.tensor_scalar(out=sc_[:, c0:c0 + cw], in0=px[:, 0:cw], scalar1=lo_, scalar2=hi_,
                                                                                                              op0=ALU.max, op1=ALU.min), r=[px.name, "lohi"], w=[sc_.name])
                            else:
                                tc_ = tmpc[it % NTC]
                                V(lambda px=px, tc_=tc_, cw=cw, lo_=lo_, hi_=hi_: nc.vector.tensor_scalar(out=tc_[:, 0:cw], in0=px[:, 0:cw], scalar1=lo_, scalar2=hi_,
                                                                                                       op0=ALU.max, op1=ALU.min), r=[px.name, "lohi"], w=[tc_.name])
                                G(lambda tc_=tc_, sc_=sc_, c0=c0, cw=cw: nc.gpsimd.tensor_tensor(out=sc_[:, c0:c0 + cw], in0=sc_[:, c0:c0 + cw], in1=tc_[:, 0:cw], op=ALU.add),
                                  r=[tc_.name, sc_.name], w=[sc_.name])
                            itc3[0] += 1
                        if filler is not None:
                            filler()
                    if search:
                        G(lambda b=b, sc_=sc_: nc.gpsimd.tensor_reduce(out=amax[b][:], in_=sc_[:, 0:Sk], axis=AX.X, op=ALU.max, apply_absolute_value=True),
                          r=[sc_.name], w=[amax[b].name]) if False else \
                        V(lambda b=b, sc_=sc_: nc.vector.tensor_reduce(out=amax[b][:], in_=sc_[:, 0:Sk], axis=AX.X, op=ALU.max, apply_absolute_value=True),
                          r=[sc_.name], w=[amax[b].name])
                    G(lambda sc_=sc_: nc.gpsimd.tensor_tensor(out=sc_[:, Sk - 128:Sk], in0=sc_[:, Sk - 128:Sk], in1=cmask[:], op=ALU.add),
                      r=[sc_.name, "cmask"], w=[sc_.name])

            def srch_init(i):
                Sk = (i + 1) * 128
                qa_, qi_, sc2 = slot_bufs(i)
                if Sk > TOPK:
                    V(lambda: nc.vector.tensor_scalar(out=amax[0][:], in0=amax[0][:], scalar1=1.0, scalar2=None, op0=ALU.add), r=[amax[0].name], w=[amax[0].name])
                    V(lambda: nc.vector.tensor_scalar(out=stp[0][:], in0=pw2[:], scalar1=amax[0][:, 0:1], scalar2=None, op0=ALU.mult), r=["pw2", amax[0].name], w=[stp[0].name])
                    V(lambda: nc.vector.memset(mid[0][:], 0.0), w=[mid[0].name])
                    A(lambda: nc.scalar.activation(out=amax[1][:], in_=amax[1][:], func=AF.Identity, bias=eps1[:, 0:1], scale=1.0), r=[amax[1].name, "eps1"], w=[amax[1].name])
                    A(lambda: nc.scalar.activation(out=stp[1][:], in_=pw2[:], func=AF.Identity, scale=amax[1][:, 0:1]), r=["pw2", amax[1].name], w=[stp[1].name])
                    A(lambda: nc.scalar.activation(out=stph[:], in_=stp[1][:], func=AF.Identity, scale=-0.5), r=[stp[1].name], w=["stph"])
                    A(lambda: nc.scalar.activation(out=mid[1][:], in_=zer_f[:, 0:1], func=AF.Identity), r=["zer_f"], w=[mid[1].name])
                    tcst = thrc[i % 2]
                    V(lambda tcst=tcst: nc.vector.memset(tcst[:], -(float(2 * TOPK - Sk) - 0.5)), w=[tcst.name])

            def srch_iter(i, k):
                Sk = (i + 1) * 128
                qa_, qi_, sc2 = slot_bufs(i)
                tcst = thrc[i % 2]
                V(lambda: nc.vector.tensor_scalar(out=junkb[0][:, 0:Sk], in0=sc2[0][:, 0:Sk], scalar1=mid[0][:, 0:1], scalar2=None, op0=ALU.is_ge,
                                                  op1=ALU.add, accum_out=cnt[0][:]), r=[sc2[0].name, mid[0].name], w=[junkb[0].name, cnt[0].name])
                A(lambda: nc.scalar.activation(out=junkb[1][:, 0:Sk], in_=sc2[1][:, 0:Sk], func=AF.Sign, bias=mid[1][:, 0:1], scale=1.0, accum_out=cnt[1][:]),
                  r=[sc2[1].name, mid[1].name], w=[junkb[1].name, cnt[1].name])
                V(lambda: nc.vector.tensor_scalar(out=c2[0][:], in0=cnt[0][:], scalar1=float(TOPK), scalar2=-0.5, op0=ALU.is_ge, op1=ALU.add),
                  r=[cnt[0].name], w=[c2[0].name])
                V(lambda: nc.vector.scalar_tensor_tensor(out=mid[0][:], in0=c2[0][:], scalar=stp[0][:, k:k + 1], in1=mid[0][:], op0=ALU.mult, op1=ALU.add),
                  r=[c2[0].name, stp[0].name, mid[0].name], w=[mid[0].name])
                A(lambda: nc.scalar.activation(out=c2[1][:], in_=cnt[1][:], func=AF.Sign, bias=tcst[:, 0:1], scale=1.0), r=[cnt[1].name, tcst.name], w=[c2[1].name])
                A(lambda: nc.scalar.activation(out=mid[1][:], in_=c2[1][:], func=AF.Identity, scale=stph[:, k:k + 1], bias=mid[1][:, 0:1]),
                  r=[c2[1].name, "stph", mid[1].name], w=[mid[1].name])

            def srch_fin(i):
                Sk = (i + 1) * 128
                Ts = [b * NT + i for b in range(NB)]
                qa_, qi_, sc2 = slot_bufs(i)
                if Sk > TOPK:
                    V(lambda: nc.vector.tensor_tensor(out=thr[0][:], in0=mid[0][:], in1=stp[0][:, NIT:NIT + 1], op=ALU.subtract), r=[mid[0].name, stp[0].name], w=[thr[0].name])
                    V(lambda: nc.vector.scalar_tensor_tensor(out=thr[1][:], in0=mid[1][:], scalar=-1.0, in1=stp[1][:, NIT:NIT + 1], op0=ALU.mult, op1=ALU.subtract),
                      r=[mid[1].name, stp[1].name], w=[thr[1].name])
                else:
                    for b in range(NB):
                        V(lambda b=b: nc.vector.memset(thr[b][:], -1.0e29), w=[thr[b].name])
                for b in range(NB):
                    V(lambda b=b: nc.vector.tensor_scalar(out=mb[b][:, 0:Sk], in0=sc2[b][:, 0:Sk], scalar1=thr[b][:, 0:1], scalar2=BIGM, op0=ALU.is_lt, op1=ALU.mult),
                      r=[sc2[b].name, thr[b].name], w=[mb[b].name])
                    if debug and Ts[b] == NT - 1:
                        DMA(dbg_mb[:, 0:Sk], mb[b][:, 0:Sk], r=[mb[b].name], w=["dbg_mb"])
                        DMA(dbg_sc[:, 0:Sk], sc2[b][:, 0:Sk], r=[sc2[b].name], w=["dbg_sc"])
                        DMA(dbg_thr[:, :], thr[b][:], r=[thr[b].name], w=["dbg_thr"])

            def att_stage(i, b, g):
                qa_, qi_, sc2 = slot_bufs(i)
                po = p_o[(2 * b + g) % 2]
                base = itc3[1]

                def qk(kb):
                    ia = base + kb
                    pst = p_st[ia % 3]
                    pt_ = pT[ia % 4]
                    ks = slice(kb * 128, (kb + 1) * 128)
                    PE(lambda: nc.tensor.matmul(pst[:], lhsT=kTa[b][:, g, ks], rhs=qa_[b][:, 4 * g:4 * g + 4, :].rearrange("p a b -> p (a b)"),
                                                start=True, stop=False), r=[kTa[b].name, qa_[b].name], w=[pst.name])
                    PE(lambda: nc.tensor.matmul(pst[:], lhsT=mb[b][:, ks], rhs=Irep[:].rearrange("p a b -> p (a b)"), start=False, stop=True),
                       r=[mb[b].name, "Irep"], w=[pst.name], skip_same=True)
                    A(lambda: nc.scalar.activation(out=pt_[:], in_=pst[:], func=AF.Exp, scale=0.125), r=[pst.name], w=[pt_.name])

                def pv(kb):
                    ia = base + kb
                    pt_ = pT[ia % 4]
                    PE(lambda: nc.tensor.matmul(po[0:65, :], lhsT=V1[b][:, kb, g, :], rhs=pt_[:], start=(kb == 0), stop=(kb == i)),
                       r=[pt_.name, V1[b].name], w=[po.name], skip_same=True)

                qk(0)
                if i >= 1:
                    qk(1)
                for kb in range(i + 1):
                    if kb + 2 <= i:
                        qk(kb + 2)
                    pv(kb)
                itc3[1] += i + 1
                A(lambda po=po: nc.scalar.copy(out=oT[b][0:65, g, :], in_=po[0:65, :]), r=[po.name], w=[oT[b].name])

            def norm_stage(i):
                Ts = [b * NT + i for b in range(NB)]
                for b in range(NB):
                    for g in range(2):
                        for h4 in range(4):
                            PE(lambda b=b, g=g, h4=h4: nc.tensor.transpose(out=p_n[:, h4, :], in_=oT[b][0:65, g, h4 * 128:(h4 + 1) * 128], identity=identf[0:65, 0:65]),
                               r=[oT[b].name, "identf"], w=["p_n"], skip_same=True)
                        V(lambda b=b: nc.vector.reciprocal(out=rec[b][:], in_=p_n[:, :, 64]), r=["p_n"], w=[rec[b].name])
                        V(lambda b=b, g=g: nc.vector.tensor_tensor(out=ao[b][:, 4 * g:4 * g + 4, :], in0=p_n[:, :, 0:64], in1=rec[b][:].unsqueeze(2).to_broadcast([128, 4, 64]),
                                                                   op=ALU.mult), r=["p_n", rec[b].name], w=[ao[b].name])
                    DMA(ao_scr[Ts[b] * 128:(Ts[b] + 1) * 128, :], ao[b][:].rearrange("p a b -> p (a b)"), r=[ao[b].name], w=[("ao", Ts[b])])

            idx_stage(0)
            for i in range(NT):
                Sk_i = (i + 1) * 128
                n_it = NIT if Sk_i > TOPK else 0
                srch_init(i)
                kdone = [0]
                if i + 1 < NT:
                    nslots = NB * (((i + 2) * 128 + 511) // 512)
                    per = -(-n_it // nslots) if n_it else 0

                    def filler(i=i, per=per, kdone=kdone, n_it=n_it):
                        for _ in range(per):
                            if kdone[0] < n_it:
                                srch_iter(i, kdone[0])
                                kdone[0] += 1
                    idx_stage(i + 1, filler)
                while kdone[0] < n_it:
                    srch_iter(i, kdone[0])
                    kdone[0] += 1
                srch_fin(i)
                if i >= 1:
                    norm_stage(i - 1)
                for b in range(NB):
                    for g in range(2):
                        att_stage(i, b, g)
            norm_stage(NT - 1)
            S.barrier()

    if "p3s" in phases:
        NIT = 22
        with contextlib.ExitStack() as e4:
            NPG = cfg.NPG
            assert NPG == 64 and NS % 2 == 0
            TOPK_S = cfg.TOPK_S
            TS = NTT - 1
            samp0 = TS * 128
            NO = 129
            lohi_scr = dscr("lohi_scr", [NS, 16], F32)
            DMA(lohi_scr[:, :], lohi[0:NS, TS, :], r=["lohi"], w=["lohi_scr"])
            lohiB = sb("lohiB", [128, NS, 16], F32, e4)
            DMA(lohiB[:].rearrange("p a b -> p (a b)"), lohi_scr.rearrange("(o a) b -> o (a b)", o=1).partition_broadcast(128),
                r=["lohi_scr"], w=["lohiB"])
            qTs = sb("qTs", [65, 8, 128], BF16, e4)
            qiTs = sb("qiTs", [64, 8, 128], BF16, e4)
            DMA(qTs[:], qT_scr[TS, :, :, :], r=[("qT", TS)], w=["qTs"])
            DMA(qiTs[:], qiT_scr[TS, :, :, :], r=[("qiT", TS)], w=["qiTs"])
            idx = sb("idx", [128, 1], I32, e4)
            idxf = sb("idxf", [128, 1], F32, e4)
            idxa = sb("idxa", [128, 1], I32, e4)
            idxb = sb("idxb", [128, 1], I32, e4)
            KI = sb("KI", [128, 128, 64], F32, e4)
            KIx = sb("KIx", [128, 64], F32, e4)
            Kh = [sb(f"Kh{i}", [128, 64, 128], F32, e4) for i in range(2)]
            Kx = sb("Kx", [128, 128], F32, e4)
            Vh = sb("Vh", [128, 64, 128], F32, e4)
            Vx = sb("Vx", [128, 128], F32, e4)
            V1h = sb("V1h", [128, 64, 2, 65], BF16, e4)
            V1x = sb("V1x", [128, 2, 65], BF16, e4)
            kT4 = [sb(f"kT4_{i}", [65, 4, 128], BF16, e4) for i in range(2)]
            qis = sb("qis", [64, 2, 8], BF16, e4)
            qas = sb("qas", [65, 2, 2, 4], BF16, e4)
            sc = sb("sc_s", [128, NO], F32, e4)
            cl = sb("cl_s", [128, 32, 16], F32, e4)
            red = sb("red_s", [128, 32, 2], F32, e4)
            colmask = sb("colmask", [128, 1], F32, e4)
            cross = sb("cross", [128, 16], F32, e4)
            maskfull = sb("maskfull", [128, NO, 16], F32, e4)
            blk1 = sb("blk1", [128, 128], F32, e4)
            pw2s = sb("pw2s", [128, NIT + 1], F32, e4)
            am1 = sb("am1", [128, 1], F32, e4)
            am2 = sb("am2", [1, 128], F32, e4)
            amb = sb("amb", [128, 1], F32, e4)
            stps = sb("stps", [128, NIT + 1], F32, e4)
            mids = sb("mids", [128, 1], F32, e4)
            cnts = sb("cnts", [128, 1], F32, e4)
            c2s = sb("c2s", [128, 1], F32, e4)
            thrs = sb("thrs", [128, 1], F32, e4)
            junks = sb("junks", [128, NO], F32, e4)
            sqk = Vh
            n2 = sb("n2", [128, 258], F32, e4)
            kmb = sb("kmb", [128, 1], F32, e4)
            tmps = sb("tmps", [128, 32, 16], F32, e4)
            pTs = [sb(f"pTs{i}", [128, 32, 16], BF16, e4) for i in range(2)]
            zrow_s = sb("zrow_s", [1, 512], BF16, e4)
            o8 = sb("o8", [8, 2, 64], BF16, e4)
            rec8 = sb("rec8", [8, 2], F32, e4)
            p_t = [ps(f"p3s_t{i}", [64, 4, 128], F32, e4) for i in range(2)]
            p_sx = [ps(f"p3s_x{i}", [128, 32, 16], F32, e4) for i in range(2)]
            p_os = ps("p3s_o", [8, 2, 65], F32, e4)
            p_ms = ps("p3s_m", [128, 512], F32, e4)

            for k in range(NIT + 1):
                G(lambda k=k: nc.gpsimd.memset(pw2s[:, k:k + 1], float(2.0 ** (-k))), w=["pw2s"])
            G(lambda: nc.gpsimd.memset(zrow_s[:], 0.0), w=["zrow_s"])
            V(lambda: nc.vector.memset(colmask[:], NEG), w=["colmask"])
            V(lambda: nc.vector.memset(colmask[0:1, :], 0.0), w=["colmask"])
            V(lambda: nc.vector.memset(colmask[64:65, :], 0.0), w=["colmask"])
            V(lambda: nc.vector.memset(cross[:], 0.0), w=["cross"])
            crv = cross[:].rearrange("p (g b h) -> p g b h", g=2, b=2)
            V(lambda: nc.vector.memset(crv[0:64, :, 1, :], BIGM), w=["cross"])
            V(lambda: nc.vector.memset(crv[64:128, :, 0, :], BIGM), w=["cross"])
            V(lambda: nc.vector.memset(blk1[:], 0.0), w=["blk1"])
            V(lambda: nc.vector.memset(blk1[0:64, 0:64], 1.0), w=["blk1"])
            V(lambda: nc.vector.memset(blk1[64:128, 64:128], 1.0), w=["blk1"])
            for t_ in kT4:
                V(lambda t_=t_: nc.vector.memset(t_[64:65, :, :], 1.0), w=[t_.name])
            V(lambda: nc.vector.memset(V1h[:, :, :, 64:65], 1.0), w=["V1h"])
            V(lambda: nc.vector.memset(V1x[:, :, 64:65], 1.0), w=["V1x"])

            def gather(dst2d, src2d, idx_t, wkey):
                S.dma("pool", lambda: nc.gpsimd.indirect_dma_start(out=dst2d, out_offset=None, in_=src2d,
                                                                   in_offset=bass.IndirectOffsetOnAxis(ap=idx_t[:, :], axis=0)),
                      reads=[idx_t.name], writes=[wkey])

            cache_kh = cache_k.rearrange("n (h e) -> (n h) e", h=2)
            cache_vh = cache_v.rearrange("n (h e) -> (n h) e", h=2)
            itc = [0]

            def transposed_units(units, consume):
                for u0 in range(0, len(units), 4):
                    grp = units[u0:u0 + 4]
                    pt_ = p_t[itc[0] % 2]
                    kt = kT4[itc[0] % 2]
                    for s_, (src, rk) in enumerate(grp):
                        PE(lambda pt_=pt_, s_=s_, src=src: nc.tensor.transpose(out=pt_[:, s_, :], in_=src, identity=identf[:]),
                           r=list(rk) + ["identf"], w=[pt_.name], skip_same=True)
                    n_ = len(grp)
                    if itc[0] % 2 == 0:
                        V(lambda pt_=pt_, kt=kt, n_=n_: nc.vector.tensor_copy(out=kt[0:64, 0:n_, :], in_=pt_[:, 0:n_, :]), r=[pt_.name], w=[kt.name])
                    else:
                        A(lambda pt_=pt_, kt=kt, n_=n_: nc.scalar.copy(out=kt[0:64, 0:n_, :], in_=pt_[:, 0:n_, :]), r=[pt_.name], w=[kt.name])
                    for s_ in range(n_):
                        consume(kt, s_, u0 + s_)
                    itc[0] += 1

            for pr in range(NS // 2):
                b0 = 2 * pr
                DMA(idx[:], ptab[b0 * NPG:(b0 + 2) * NPG, :], w=["idx"])
                V(lambda: nc.vector.tensor_copy(out=idxf[:], in_=idx[:]), r=["idx"], w=["idxf"])
                V(lambda: nc.vector.tensor_scalar(out=idxa[:], in0=idxf[:], scalar1=2.0, scalar2=None, op0=ALU.mult), r=["idxf"], w=["idxa"])
                V(lambda: nc.vector.tensor_scalar(out=idxb[:], in0=idxf[:], scalar1=2.0, scalar2=1.0, op0=ALU.mult, op1=ALU.add), r=["idxf"], w=["idxb"])
                gather(KI[:].rearrange("p a b -> p (a b)"), cache_ik[:, :], idx, "KI")
                gather(Kh[0][:].rearrange("p a b -> p (a b)"), cache_kh[:, :], idxa, "Kh0")
                gather(Kh[1][:].rearrange("p a b -> p (a b)"), cache_kh[:, :], idxb, "Kh1")
                for (xt_, src_) in ((KIx, ki_s), (Kx, k_s), (Vx, v_s)):
                    V(lambda xt_=xt_: nc.vector.memset(xt_[:], 0.0), w=[xt_.name])
                    DMA(xt_[0:1, :], src_[b0:b0 + 1, :], w=[xt_.name])
                    DMA(xt_[64:65, :], src_[b0 + 1:b0 + 2, :], w=[xt_.name])
                V(lambda b0=b0: nc.vector.tensor_copy(out=qis[:], in_=qiTs[:, :, b0:b0 + 2].rearrange("p h t -> p t h")), r=["qiTs"], w=["qis"])
                for g in range(2):
                    V(lambda b0=b0, g=g: nc.vector.tensor_copy(out=qas[:, g, :, :], in_=qTs[:, 4 * g:4 * g + 4, b0:b0 + 2].rearrange("p h t -> p t h")),
                      r=["qTs"], w=["qas"])
                for hf in range(2):
                    G(lambda hf=hf: nc.gpsimd.tensor_tensor(out=sqk[:], in0=Kh[hf][:], in1=Kh[hf][:], op=ALU.mult), r=[f"Kh{hf}"], w=["Vh"])
                    V(lambda hf=hf: nc.vector.tensor_reduce(out=n2[:, hf * 128:(hf + 1) * 128], in_=sqk[:].rearrange("p o (g d) -> p (o g) d", g=2), axis=AX.X, op=ALU.add),
                      r=["Vh"], w=["n2"])
                G(lambda: nc.gpsimd.tensor_tensor(out=sqk[:, 0, :], in0=Kx[:], in1=Kx[:], op=ALU.mult), r=["Kx"], w=["Vh"])
                V(lambda: nc.vector.tensor_reduce(out=n2[:, 256:258], in_=sqk[:, 0, :].rearrange("p (g d) -> p g d", g=2), axis=AX.X, op=ALU.add), r=["Vh"], w=["n2"])
                V(lambda: nc.vector.tensor_reduce(out=am1[:], in_=n2[:], axis=AX.X, op=ALU.max), r=["n2"], w=["am1"])
                PE(lambda: nc.tensor.transpose(out=p_ms[0:1, 0:128], in_=am1[:], identity=identf[:]), r=["am1", "identf"], w=[p_ms.name])
                V(lambda: nc.vector.tensor_reduce(out=am2[0:1, 0:1], in_=p_ms[0:1, 0:128], axis=AX.X, op=ALU.max), r=[p_ms.name], w=["am2"])
                A(lambda: nc.scalar.activation(out=am2[0:1, 0:1], in_=am2[0:1, 0:1], func=AF.Sqrt), r=["am2"], w=["am2"])
                PE(lambda: nc.tensor.matmul(p_ms[:, 0:1], lhsT=ones_f[0:1, :], rhs=am2[0:1, 0:1], start=True, stop=True), r=["ones_f", "am2"], w=[p_ms.name])
                V(lambda: nc.vector.tensor_copy(out=kmb[:], in_=p_ms[:, 0:1]), r=[p_ms.name], w=["kmb"])
                V(lambda: nc.vector.tensor_scalar(out=qas[64:65, :, :, :], in0=qas[64:65, :, :, :], scalar1=kmb[64:65, 0:1], scalar2=None, op0=ALU.mult),
                  r=["qas", "kmb"], w=["qas"])

                loB = lohiB[:, b0:b0 + 2, 0:8]
                hiB = lohiB[:, b0:b0 + 2, 8:16]

                def idx_group(units, o_lo, n_o):
                    px = p_sx[itc[0] % 2]

                    def consume(kt, s_, ui, px=px):
                        PE(lambda: nc.tensor.matmul(px[:, ui, :], lhsT=kt[0:64, s_, :], rhs=qis[:].rearrange("p a b -> p (a b)"), start=True, stop=True),
                           r=[kt.name, "qis"], w=[px.name], skip_same=True)
                    transposed_units(units, consume)
                    V(lambda: nc.vector.tensor_tensor(out=cl[:, 0:n_o, :].rearrange("p o (b h) -> p o b h", b=2), in0=px[:, 0:n_o, :].rearrange("p o (b h) -> p o b h", b=2),
                                                      in1=loB.unsqueeze(1).to_broadcast([128, n_o, 2, 8]), op=ALU.max), r=[px.name, "lohiB"], w=["cl_s"])
                    V(lambda: nc.vector.tensor_tensor(out=cl[:, 0:n_o, :].rearrange("p o (b h) -> p o b h", b=2), in0=cl[:, 0:n_o, :].rearrange("p o (b h) -> p o b h", b=2),
                                                      in1=hiB.unsqueeze(1).to_broadcast([128, n_o, 2, 8]), op=ALU.min), r=["cl_s", "lohiB"], w=["cl_s"])
                    V(lambda: nc.vector.tensor_reduce(out=red[:, 0:n_o, :], in_=cl[:, 0:n_o, :].rearrange("p o (b h) -> p o b h", b=2), axis=AX.X, op=ALU.add),
                      r=["cl_s"], w=["red_s"])
                    V(lambda: nc.vector.tensor_copy(out=sc[0:64, o_lo:o_lo + n_o], in_=red[0:64, 0:n_o, 0]), r=["red_s"], w=["sc_s"])
                    V(lambda: nc.vector.tensor_copy(out=sc[64:128, o_lo:o_lo + n_o], in_=red[64:128, 0:n_o, 1]), r=["red_s"], w=["sc_s"])

                for og in range(4):
                    idx_group([(KI[:, og * 32 + oo, :], ["KI"]) for oo in range(32)], og * 32, 32)
                idx_group([(KIx[:], ["KIx"])], 128, 1)
                V(lambda: nc.vector.tensor_reduce(out=am1[:], in_=sc[:], axis=AX.X, op=ALU.max, apply_absolute_value=True), r=["sc_s"], w=["am1"])
                V(lambda: nc.vector.tensor_tensor(out=sc[:, 128:129], in0=sc[:, 128:129], in1=colmask[:], op=ALU.add), r=["sc_s", "colmask"], w=["sc_s"])
                PE(lambda: nc.tensor.transpose(out=p_ms[0:1, 0:128], in_=am1[:], identity=identf[:]), r=["am1", "identf"], w=[p_ms.name])
                V(lambda: nc.vector.tensor_reduce(out=am2[0:1, 0:1], in_=p_ms[0:1, 0:128], axis=AX.X, op=ALU.max), r=[p_ms.name], w=["am2"])
                V(lambda: nc.vector.tensor_scalar(out=am2[0:1, 0:1], in0=am2[0:1, 0:1], scalar1=1.0, scalar2=None, op0=ALU.add), r=["am2"], w=["am2"])
                PE(lambda: nc.tensor.matmul(p_ms[:, 0:1], lhsT=ones_f[0:1, :], rhs=am2[0:1, 0:1], start=True, stop=True), r=["ones_f", "am2"], w=[p_ms.name])
                V(lambda: nc.vector.tensor_copy(out=amb[:], in_=p_ms[:, 0:1]), r=[p_ms.name], w=["amb"])
                V(lambda: nc.vector.tensor_scalar(out=stps[:], in0=pw2s[:], scalar1=amb[:, 0:1], scalar2=None, op0=ALU.mult), r=["pw2s", "amb"], w=["stps"])
                V(lambda: nc.vector.memset(mids[:], 0.0), w=["mids"])
                for k in range(NIT):
                    V(lambda: nc.vector.tensor_scalar(out=junks[:], in0=sc[:], scalar1=mids[:, 0:1], scalar2=None, op0=ALU.is_ge, op1=ALU.add, accum_out=cnts[:]),
                      r=["sc_s", "mids"], w=["junks", "cnts"])
                    PE(lambda: nc.tensor.matmul(p_ms[:, 0:1], lhsT=blk1[:], rhs=cnts[:], start=True, stop=True), r=["blk1", "cnts"], w=[p_ms.name])
                    V(lambda: nc.vector.tensor_scalar(out=c2s[:], in0=p_ms[:, 0:1], scalar1=float(TOPK_S), scalar2=-0.5, op0=ALU.is_ge, op1=ALU.add), r=[p_ms.name], w=["c2s"])
                    V(lambda k=k: nc.vector.scalar_tensor_tensor(out=mids[:], in0=c2s[:], scalar=stps[:, k:k + 1], in1=mids[:], op0=ALU.mult, op1=ALU.add),
                      r=["c2s", "stps", "mids"], w=["mids"])
                V(lambda: nc.vector.tensor_tensor(out=thrs[:], in0=mids[:], in1=stps[:, NIT:NIT + 1], op=ALU.subtract), r=["mids", "stps"], w=["thrs"])
                V(lambda: nc.vector.tensor_scalar(out=junks[:], in0=sc[:], scalar1=thrs[:, 0:1], scalar2=BIGM, op0=ALU.is_lt, op1=ALU.mult), r=["sc_s", "thrs"], w=["junks"])
                V(lambda: nc.vector.tensor_tensor(out=maskfull[:], in0=junks[:].unsqueeze(2).to_broadcast([128, NO, 16]),
                                                  in1=cross[:].unsqueeze(1).to_broadcast([128, NO, 16]), op=ALU.add), r=["junks", "cross"], w=["maskfull"])
                PE(lambda: nc.tensor.matmul(p_os[:].rearrange("p a b -> p (a b)"), lhsT=zrow_s[0:1, 0:8], rhs=zrow_s[0:1, 0:130], start=True, stop=False,
                                            skip_group_check=True), r=["zrow_s"], w=["p3s_o"])

                def att_group(kunits, vsrc_fn, o_lo, n_o, last):
                    px = p_sx[itc[0] % 2]
                    pts = pTs[itc[0] % 2]

                    def consume(kt, s_, ui, px=px):
                        ol, g = ui // 2, ui % 2
                        PE(lambda: nc.tensor.matmul(px[:, ol, g * 8:(g + 1) * 8], lhsT=kt[:, s_, :], rhs=qas[:, g, :, :].rearrange("p a b -> p (a b)"), start=True, stop=True),
                           r=[kt.name, "qas"], w=[px.name], skip_same=True)
                    transposed_units(kunits, consume)
                    V(lambda: nc.vector.tensor_tensor(out=tmps[:, 0:n_o, :], in0=px[:, 0:n_o, :], in1=maskfull[:, o_lo:o_lo + n_o, :], op=ALU.add),
                      r=[px.name, "maskfull"], w=["tmps"])
                    A(lambda: nc.scalar.activation(out=pts[:, 0:n_o, :], in_=tmps[:, 0:n_o, :], func=AF.Exp, scale=0.125), r=["tmps"], w=[pts.name])
                    for ol in range(n_o):
                        for g in range(2):
                            vap, vk = vsrc_fn(ol, g)
                            PE(lambda ol=ol, g=g, vap=vap: nc.tensor.matmul(p_os[:, g, :], lhsT=pts[:, ol, g * 8:(g + 1) * 8], rhs=vap, start=False,
                                                                         stop=(last and ol == n_o - 1), skip_group_check=True),
                               r=[pts.name, vk], w=["p3s_o"], skip_same=True)

                for hf in range(2):
                    gather(Vh[:].rearrange("p a b -> p (a b)"), cache_vh[:, :], idxa if hf == 0 else idxb, "Vh")
                    for g in range(2):
                        G(lambda g=g: nc.gpsimd.tensor_copy(out=V1h[:, :, g, 0:64], in_=Vh[:, :, g * 64:(g + 1) * 64]), r=["Vh"], w=["V1h"])
                    for og in range(2):
                        units = []
                        for oo in range(32):
                            for g in range(2):
                                units.append((Kh[hf][:, og * 32 + oo, g * 64:(g + 1) * 64], [f"Kh{hf}"]))
                        att_group(units, lambda ol, g, og=og: (V1h[:, og * 32 + ol, g, :], "V1h"), hf * 64 + og * 32, 32, False)
                for g in range(2):
                    V(lambda g=g: nc.vector.tensor_copy(out=V1x[:, g, 0:64], in_=Vx[:, g * 64:(g + 1) * 64]), r=["Vx"], w=["V1x"])
                att_group([(Kx[:, g * 64:(g + 1) * 64], ["Kx"]) for g in range(2)], lambda ol, g: (V1x[:, g, :], "V1x"), 128, 1, True)
                V(lambda: nc.vector.reciprocal(out=rec8[:], in_=p_os[:, :, 64]), r=["p3s_o"], w=["rec8"])
                V(lambda: nc.vector.tensor_tensor(out=o8[:], in0=p_os[:, :, 0:64], in1=rec8[:].unsqueeze(2).to_broadcast([8, 2, 64]), op=ALU.mult),
                  r=["p3s_o", "rec8"], w=["o8"])
                for b2 in range(2):
                    row = samp0 + b0 + b2
                    DMA(ao_scr[row:row + 1, :].rearrange("o (g h d) -> (o h) g d", g=2, h=4), o8[4 * b2:4 * b2 + 4, :, :], r=["o8"], w=[("ao", TS)])
            S.barrier()

    if "p4" in phases:
        with contextlib.ExitStack() as e5:
            Wglu = sb("Wglu", [128, 4, 2048], BF16, e5)
            Wao = sb("Wao", [128, 4, 1024], BF16, e5)
            Wo = sb("Wo", [128, 8, 1024], BF16, e5)
            bglu = sb("bglu", [128, 2048], F32, e5)
            with contextlib.ExitStack() as e5w:
                load_weight_bf16(Wglu, w_glu, D_SSM, 2048, e5w, nm="wglu")
                load_weight_bf16(Wao, w_ao, D_ATTN, 1024, e5w, nm="wao")
                load_weight_bf16(Wo, w_o, D_MODEL, 1024, e5w, nm="wo")
                S.barrier()
            DMA(bglu[:], b_glu.rearrange("(o n) -> o n", o=1).partition_broadcast(128), w=["bglu"])
            gyt = [sb(f"gyt{i}", [128, 4, 128], BF16, e5) for i in range(2)]
            aot = [sb(f"aot{i}", [128, 512], BF16, e5) for i in range(2)]
            gat = [sb(f"gat{i}", [128, 2048], F32, e5) for i in range(2)]
            xin = [sb(f"xin{i}", [128, 1024], F32, e5) for i in range(2)]
            zv = sb("zv", [128, 512], F32, e5)
            zg = sb("zg", [128, 512], F32, e5)
            so = sb("so", [128, 1024], F32, e5)
            aoT = sb("aoT", [128, 4, 128], BF16, e5)
            m1 = sb("m1", [128, 1024], F32, e5)
            m2 = sb("m2", [128, 512], F32, e5)
            mixb2 = [sb(f"mixb{i}", [128, 1024], BF16, e5) for i in range(2)]
            mixT = sb("mixT", [128, 8, 128], BF16, e5)
            x2t = [sb(f"x2t{i}", [128, 1024], F32, e5) for i in range(2)]
            p_a = [ps(f"p4_a{i}", [128, 512], F32, e5) for i in range(4)]
            p_tr4 = ps("p4_tr", [128, 8, 128], BF16, e5)
            def p4a_front(T):
                r0, nr, is_s, pti = tile_rows(T)
                gy_, ao_, ga_, xi_, x2_ = gyt[T % 2], aot[T % 2], gat[T % 2], xin[T % 2], x2t[T % 2]
                mixb = mixb2[T % 2]
                gkeys = [("gyT", ct_, "s") for ct_ in range(4)] if is_s else [("gyT", ct_, (T * 128) // SEQ, ((T * 128) % SEQ) // 512) for ct_ in range(4)]
                DMA(gy_[:], gyT_scr.rearrange("(c p) t -> p c t", p=128)[:, :, T * 128:(T + 1) * 128], r=gkeys, w=[gy_.name])
                if is_s:
                    V(lambda ao_=ao_: nc.vector.memset(ao_[:], 0.0), w=[ao_.name])
                    DMA(ao_[0:NS, :], ao_scr[T * 128:T * 128 + NS, :], r=[("ao", T)], w=[ao_.name])
                    V(lambda xi_=xi_: nc.vector.memset(xi_[:], 0.0), w=[xi_.name])
                    DMA(xi_[0:NS, :], xs[:, :], w=[xi_.name])
                else:
                    DMA(ao_[:], ao_scr[T * 128:(T + 1) * 128, :], r=[("ao", T)], w=[ao_.name])
                    DMA(xi_[:], xp[r0:r0 + 128, :], w=[xi_.name])
                DMA(ga_[:], gate_scr[T * 128:(T + 1) * 128, :], r=[("gate", T)], w=[ga_.name])
                for nh in range(2):
                    pv, pg = p_a[0], p_a[1]
                    for (pp, c0) in ((pv, nh * 512), (pg, 1024 + nh * 512)):
                        for kc in range(4):
                            PE(lambda pp=pp, c0=c0, kc=kc, gy_=gy_: nc.tensor.matmul(pp[:], lhsT=gy_[:, kc, :], rhs=Wglu[:, kc, c0:c0 + 512], start=(kc == 0), stop=(kc == 3)),
                               r=[gy_.name, "Wglu"], w=[pp.name], skip_same=True)
                    V(lambda nh=nh: nc.vector.tensor_tensor(out=zv[:], in0=p_a[0][:], in1=bglu[:, nh * 512:(nh + 1) * 512], op=ALU.add), r=[p_a[0].name, "bglu"], w=["zv"])
                    V(lambda nh=nh: nc.vector.tensor_tensor(out=zg[:], in0=p_a[1][:], in1=bglu[:, 1024 + nh * 512:1024 + (nh + 1) * 512], op=ALU.add),
                      r=[p_a[1].name, "bglu"], w=["zg"])
                    A(lambda: nc.scalar.activation(out=zg[:], in_=zg[:], func=AF.Sigmoid), r=["zg"], w=["zg"])
                    G(lambda nh=nh: nc.gpsimd.tensor_tensor(out=so[:, nh * 512:(nh + 1) * 512], in0=zv[:], in1=zg[:], op=ALU.mult), r=["zv", "zg"], w=["so"])
                for kc in range(4):
                    PE(lambda kc=kc, ao_=ao_: nc.tensor.transpose(out=p_tr4[:, kc, :], in_=ao_[:, kc * 128:(kc + 1) * 128], identity=ident[:]),
                       r=[ao_.name, "ident"], w=["p4_tr"], skip_same=True)
                A(lambda: nc.scalar.copy(out=aoT[:], in_=p_tr4[:, 0:4, :]), r=["p4_tr"], w=["aoT"])
                G(lambda ga_=ga_: nc.gpsimd.tensor_tensor(out=m1[:], in0=so[:], in1=ga_[:, 0:1024], op=ALU.mult), r=["so", ga_.name], w=["m1"])
                for nh in range(2):
                    pp = p_a[2 + nh]
                    for kc in range(4):
                        PE(lambda pp=pp, nh=nh, kc=kc: nc.tensor.matmul(pp[:], lhsT=aoT[:, kc, :], rhs=Wao[:, kc, nh * 512:(nh + 1) * 512], start=(kc == 0), stop=(kc == 3)),
                           r=["aoT", "Wao"], w=[pp.name], skip_same=True)
                    V(lambda pp=pp, nh=nh, ga_=ga_: nc.vector.tensor_tensor(out=m2[:], in0=pp[:], in1=ga_[:, 1024 + nh * 512:1024 + (nh + 1) * 512], op=ALU.mult),
                      r=[pp.name, ga_.name], w=["m2"])
                    V(lambda nh=nh: nc.vector.tensor_tensor(out=mixb[:, nh * 512:(nh + 1) * 512], in0=m1[:, nh * 512:(nh + 1) * 512], in1=m2[:], op=ALU.add),
                      r=["m1", "m2"], w=[mixb.name])

            def p4a_back(T):
                r0, nr, is_s, pti = tile_rows(T)
                gy_, ao_, ga_, xi_, x2_ = gyt[T % 2], aot[T % 2], gat[T % 2], xin[T % 2], x2t[T % 2]
                mixb = mixb2[T % 2]
                for kc in range(8):
                    PE(lambda kc=kc: nc.tensor.transpose(out=p_tr4[:, kc, :], in_=mixb[:, kc * 128:(kc + 1) * 128], identity=ident[:]),
                       r=[mixb.name, "ident"], w=["p4_tr"], skip_same=True)
                A(lambda: nc.scalar.copy(out=mixT[:], in_=p_tr4[:]), r=["p4_tr"], w=["mixT"])
                for nh in range(2):
                    pp = p_a[nh]
                    for kc in range(8):
                        PE(lambda pp=pp, nh=nh, kc=kc: nc.tensor.matmul(pp[:], lhsT=mixT[:, kc, :], rhs=Wo[:, kc, nh * 512:(nh + 1) * 512], start=(kc == 0), stop=(kc == 7)),
                           r=["mixT", "Wo"], w=[pp.name], skip_same=True)
                    V(lambda pp=pp, nh=nh, xi_=xi_, x2_=x2_: nc.vector.tensor_tensor(out=x2_[:, nh * 512:(nh + 1) * 512], in0=pp[:], in1=xi_[:, nh * 512:(nh + 1) * 512], op=ALU.add),
                      r=[pp.name, xi_.name], w=[x2_.name])
                DMA(x2_scr[T * 128:(T + 1) * 128, :], x2_[:], r=[x2_.name], w=[("x2", T)], q="pool")

            p4a_front(0)
            for T in range(NTT):
                if T + 1 < NTT:
                    p4a_front(T + 1)
                p4a_back(T)
            S.barrier()

    if "p4" in phases:
        with contextlib.ExitStack() as e6:
            Wup = sb("Wup", [128, 8, D_FF], BF16, e6)
            Wdn = sb("Wdn", [128, 32, 1024], BF16, e6)
            g2c = sb("g2c", [128, 8], F32, e6)
            gfb = sb("gfb", [128, 1024], F32, e6)
            with nc.allow_non_contiguous_dma(reason="tiny gain vector"):
                DMA(g2c[:], norm2_g.rearrange("(k p) -> p k", p=128), w=["g2c"])
            DMA(gfb[:], normf_g.rearrange("(o n) -> o n", o=1).partition_broadcast(128), w=["gfb"])
            with contextlib.ExitStack() as e6w:
                load_weight_bf16(Wup, w_up, D_MODEL, D_FF, e6w, gcol=g2c, nm="wup", NSTG=3)
                load_weight_bf16(Wdn, w_down, D_FF, 1024, e6w, nm="wdn", NSTG=3)
                S.barrier()
            x2i = [sb(f"x2i{i}", [128, 1024], F32, e6) for i in range(2)]
            junk6 = sb("junk6", [128, 1024], F32, e6)
            ss6 = sb("ss6", [128, 1], F32, e6)
            rs6 = sb("rs6", [128, 1], F32, e6)
            hh = sb("hh", [128, 1024], BF16, e6)
            hhT = sb("hhT", [128, 8, 128], BF16, e6)
            rl = [sb(f"rl{i}", [128, 512], F32, e6) for i in range(2)]
            aT = sb("aT", [128, 32, 128], BF16, e6)
            x3 = sb("x3", [128, 1024], F32, e6)
            yt = [sb(f"yt{i}", [128, 1024], F32, e6) for i in range(2)]
            p_tr6 = ps("p6_tr", [128, 8, 128], BF16, e6)
            p_up = [ps(f"p6_up{i}", [128, 4, 128], F32, e6) for i in range(2)]
            p_dn = [ps(f"p6_dn{i}", [128, 512], F32, e6) for i in range(2)]
            for T in range(NTT):
                r0, nr, is_s, pti = tile_rows(T)
                xi_, y_ = x2i[T % 2], yt[T % 2]
                DMA(xi_[:], x2_scr[T * 128:(T + 1) * 128, :], r=[("x2", T)], w=[xi_.name])
                A(lambda xi_=xi_: nc.scalar.activation(out=junk6[:], in_=xi_[:], func=AF.Square, accum_out=ss6[:]), r=[xi_.name], w=["junk6", "ss6"])
                A(lambda: nc.scalar.activation(out=rs6[:], in_=ss6[:], func=AF.Sqrt, bias=eps_t[:], scale=1.0 / D_MODEL), r=["ss6", "eps_t"], w=["rs6"])
                V(lambda: nc.vector.reciprocal(out=rs6[:], in_=rs6[:]), r=["rs6"], w=["rs6"])
                V(lambda xi_=xi_: nc.vector.tensor_scalar(out=hh[:], in0=xi_[:], scalar1=rs6[:, 0:1], scalar2=None, op0=ALU.mult), r=[xi_.name, "rs6"], w=["hh"])
                for kc in range(8):
                    PE(lambda kc=kc: nc.tensor.transpose(out=p_tr6[:, kc, :], in_=hh[:, kc * 128:(kc + 1) * 128], identity=ident[:]),
                       r=["hh", "ident"], w=["p6_tr"], skip_same=True)
                A(lambda: nc.scalar.copy(out=hhT[:], in_=p_tr6[:]), r=["p6_tr"], w=["hhT"])
                for f4 in range(8):
                    pu = p_up[f4 % 2]
                    r_ = rl[f4 % 2]
                    for fi in range(4):
                        f = f4 * 4 + fi
                        for kc in range(8):
                            PE(lambda pu=pu, fi=fi, f=f, kc=kc: nc.tensor.matmul(pu[:, fi, :], lhsT=Wup[:, kc, f * 128:(f + 1) * 128], rhs=hhT[:, kc, :],
                                                                              start=(kc == 0), stop=(kc == 7)), r=["Wup", "hhT"], w=[pu.name], skip_same=True)
                    A(lambda pu=pu, r_=r_: nc.scalar.activation(out=r_[:], in_=pu[:].rearrange("p a b -> p (a b)"), func=AF.Relu), r=[pu.name], w=[r_.name])
                    V(lambda r_=r_, f4=f4: nc.vector.tensor_tensor(out=aT[:, f4 * 4:(f4 + 1) * 4, :].rearrange("p a b -> p (a b)"), in0=r_[:], in1=r_[:], op=ALU.mult),
                      r=[r_.name], w=["aT"])
                for nh in range(2):
                    pd = p_dn[nh]
                    for fk in range(32):
                        PE(lambda pd=pd, nh=nh, fk=fk: nc.tensor.matmul(pd[:], lhsT=aT[:, fk, :], rhs=Wdn[:, fk, nh * 512:(nh + 1) * 512], start=(fk == 0), stop=(fk == 31)),
                           r=["aT", "Wdn"], w=[pd.name], skip_same=True)
                    V(lambda pd=pd, nh=nh, xi_=xi_: nc.vector.tensor_tensor(out=x3[:, nh * 512:(nh + 1) * 512], in0=pd[:], in1=xi_[:, nh * 512:(nh + 1) * 512], op=ALU.add),
                      r=[pd.name, xi_.name], w=["x3"])
                A(lambda: nc.scalar.activation(out=junk6[:], in_=x3[:], func=AF.Square, accum_out=ss6[:]), r=["x3"], w=["junk6", "ss6"])
                A(lambda: nc.scalar.activation(out=rs6[:], in_=ss6[:], func=AF.Sqrt, bias=eps_t[:], scale=1.0 / D_MODEL), r=["ss6", "eps_t"], w=["rs6"])
                V(lambda: nc.vector.reciprocal(out=rs6[:], in_=rs6[:]), r=["rs6"], w=["rs6"])
                V(lambda y_=y_: nc.vector.scalar_tensor_tensor(out=y_[:], in0=x3[:], scalar=rs6[:, 0:1], in1=gfb[:], op0=ALU.mult, op1=ALU.mult),
                  r=["x3", "rs6", "gfb"], w=[y_.name])
                if is_s:
                    DMA(y_s[:, :], y_[0:NS, :], r=[y_.name], q="pool", is_output=True)
                else:
                    DMA(y_p[r0:r0 + 128, :], y_[:], r=[y_.name], q="pool", is_output=True)
            S.barrier()
    S.finish()
    es_all.close()
    dbg = dict(qT_scr=qT_scr, kT_scr=kT_scr)
    return nc


def make_in_map(inp, c, cfg):
    NB, NS = cfg.NB, cfg.NS
    f = lambda a: np.ascontiguousarray(a)
    m = {
        "xp": f(inp["x_prompt"][c * NB:(c + 1) * NB].reshape(NB * cfg.SEQ, D_MODEL)),
        "xs": f(inp["x_sample"][c * NS:(c + 1) * NS].reshape(NS, D_MODEL)),
        "cache_k": inp["cache_k"].reshape(cfg.NPOOL, -1),
        "cache_v": inp["cache_v"].reshape(cfg.NPOOL, -1),
        "cache_ik": inp["cache_idx_k"].reshape(cfg.NPOOL, -1),
        "st_re": f(inp["state_ssm_re"][c * NS:(c + 1) * NS].reshape(NS, -1)),
        "st_im": f(inp["state_ssm_im"][c * NS:(c + 1) * NS].reshape(NS, -1)),
        "ptab": f(inp["page_table"][c * NS:(c + 1) * NS].reshape(-1, 1)),
        "ssm_a_re": inp["ssm_a_re"].reshape(-1), "ssm_a_im": inp["ssm_a_im"].reshape(-1),
        "ssm_b_re": inp["ssm_b_re"].reshape(-1, 16), "ssm_b_im": inp["ssm_b_im"].reshape(-1, 16),
        "ssm_c_re": inp["ssm_c_re"].reshape(-1, 64), "ssm_c_im": inp["ssm_c_im"].reshape(-1, 64),
    }
    for k in ("norm1_g", "w_in", "ssm_log_dt", "ssm_d", "w_glu", "b_glu", "w_attn_out", "w_o", "norm2_g",
              "w_up", "w_down", "normf_g"):
        m[k] = inp[k]
    return m


ALL_PHASES = ("p1", "p2", "p3", "p3s", "p4")
OUT_NAMES = ["y_p", "y_s", "k_p", "v_p", "ki_p", "re_p", "im_p", "k_s", "v_s", "ki_s", "re_s", "im_s"]


def kernel(**inputs):
    inp = {k: np.asarray(v) for k, v in inputs.items()}
    B, SEQ = inp["x_prompt"].shape[0], inp["x_prompt"].shape[1]
    NSAMP = inp["x_sample"].shape[0]
    n_cores = 8
    cfg = Cfg(seq=SEQ, past=inp["page_table"].shape[1] * 128, nb=B // n_cores, ns=NSAMP // n_cores,
              n_pool=inp["cache_k"].shape[0])
    nc = build(cfg, phases=ALL_PHASES)
    shared = {"cache_k": inp["cache_k"].reshape(cfg.NPOOL, -1), "cache_v": inp["cache_v"].reshape(cfg.NPOOL, -1),
              "cache_ik": inp["cache_idx_k"].reshape(cfg.NPOOL, -1)}
    in_maps = []
    for c in range(n_cores):
        m = make_in_map(inp, c, cfg)
        m.update(shared)
        in_maps.append(m)
    res = run_bass_kernel_spmd(nc, in_maps, core_ids=list(range(n_cores)))
    outs = {n: np.concatenate([np.asarray(res.results[c][n]) for c in range(n_cores)], axis=0) for n in OUT_NAMES}
    NB, NS = cfg.NB, cfg.NS
    f32 = np.float32
    return (
        outs["y_p"].reshape(B, SEQ, D_MODEL).astype(f32, copy=False),
        outs["y_s"].reshape(NSAMP, 1, D_MODEL).astype(f32, copy=False),
        outs["k_p"].reshape(B, SEQ, 2, 64).astype(f32, copy=False),
        outs["v_p"].reshape(B, SEQ, 2, 64).astype(f32, copy=False),
        outs["ki_p"].reshape(B, SEQ, 64).astype(f32, copy=False),
        outs["re_p"].reshape(B, N_G, P_ST).astype(f32, copy=False),
        outs["im_p"].reshape(B, N_G, P_ST).astype(f32, copy=False),
        outs["k_s"].reshape(NSAMP, 1, 2, 64).astype(f32, copy=False),
        outs["v_s"].reshape(NSAMP, 1, 2, 64).astype(f32, copy=False),
        outs["ki_s"].reshape(NSAMP, 1, 64).astype(f32, copy=False),
        outs["re_s"].reshape(NSAMP, N_G, P_ST).astype(f32, copy=False),
        outs["im_s"].reshape(NSAMP, N_G, P_ST).astype(f32, copy=False),
    )
```

```python
import math
import contextlib
import numpy as np
import concourse.bass as bass
import concourse.mybir as mybir
from concourse.bass_utils import run_bass_kernel_spmd

F32 = mybir.dt.float32
BF16 = mybir.dt.bfloat16
I32 = mybir.dt.int32
U32 = mybir.dt.uint32
ALU = mybir.AluOpType
AF = mybir.ActivationFunctionType
AX = mybir.AxisListType

D_MODEL = 1024
D_SSM = 512
N_G = 32
P_ST = 64
N_HEADS = 8
N_KV = 2
HD = 64
D_ATTN = 512
N_IH = 8
IDX_D = 64
D_FF = 4096
IN_COLS = 3912
EPS = 1e-6
ROPE_THETA = 500000.0
NEG = -1.0e30
RELAX_SAME = False
RELAX_ENGINES = ('dve', 'act', 'pe')
BIGM = -240000.0
C_U, C_Q, C_K, C_V, C_QI, C_KI, C_WI, C_GS, C_GA = 0, 512, 1024, 1152, 1280, 1792, 1856, 1864, 2888


class Sync:
    def __init__(self, nc):
        self.nc = nc
        self.eng = {"pe": nc.tensor, "dve": nc.vector, "act": nc.scalar, "pool": nc.gpsimd, "sp": nc.sync}
        self.sem = {k: nc.alloc_semaphore("sem_" + k) for k in self.eng}
        self.cnt = {k: 0 for k in self.eng}
        self.waited = {k: {} for k in self.eng}
        self.R = 12
        self.dring = {q: [nc.alloc_semaphore(f"dsem_{q}{i}") for i in range(self.R)] for q in ("sp", "pool", "act")}
        self.dn = {q: 0 for q in self.dring}
        self.lastw = {}
        self.readers = {}
        self.semobj = {}
        for k, s in self.sem.items():
            self.semobj[id(s)] = s
        for q in self.dring:
            for s in self.dring[q]:
                self.semobj[id(s)] = s
        self.out_events = []
        self.relax_same = RELAX_SAME

    def _wait(self, e, ev):
        s, v = ev
        w = self.waited[e]
        if w.get(id(s), 0) >= v:
            return
        self.eng[e].wait_ge(s, v)
        w[id(s)] = v

    def _deps(self, e, reads, writes, skip_same=False):
        evs = []
        for k in reads:
            if k in self.lastw:
                evs.append(self.lastw[k])
        for k in writes:
            if k in self.lastw:
                evs.append(self.lastw[k])
            for r in self.readers.get(k, ()):
                evs.append(r)
        for ev in evs:
            if (skip_same or (self.relax_same and e in RELAX_ENGINES)) and e in self.sem and ev[0] is self.sem[e]:
                continue
            self._wait(e, ev)

    def _record(self, ev, reads, writes):
        for k in reads:
            self.readers.setdefault(k, []).append(ev)
            if len(self.readers[k]) > 24:
                best = {}
                for s, v in self.readers[k]:
                    if id(s) not in best or best[id(s)][1] < v:
                        best[id(s)] = (s, v)
                self.readers[k] = list(best.values())
        for k in writes:
            self.lastw[k] = ev
            self.readers[k] = []

    def op(self, e, fn, reads=(), writes=(), skip_same=False):
        self._deps(e, reads, writes, skip_same)
        ins = fn()
        self.cnt[e] += 1
        ins.then_inc(self.sem[e], 1)
        ev = (self.sem[e], self.cnt[e])
        self._record(ev, reads, writes)
        return ev

    def dma(self, q, fn, reads=(), writes=(), is_output=False):
        n = self.dn[q]
        slot = n % self.R
        s = self.dring[q][slot]
        prev = 16 * (n // self.R)
        if prev > 0:
            self._wait(q, (s, prev))
        self._deps(q, reads, writes)
        ins = fn()
        ins.then_inc(s, 16)
        self.dn[q] = n + 1
        ev = (s, prev + 16)
        self._record(ev, reads, writes)
        if is_output:
            self.out_events.append(ev)
        return ev

    def barrier(self):
        evs = []
        for q in self.dring:
            n = self.dn[q]
            for slot in range(self.R):
                cnt = (n - slot + self.R - 1) // self.R if n > slot else 0
                if cnt > 0:
                    evs.append((self.dring[q][slot], 16 * cnt))
        for e in self.eng:
            if self.cnt[e] > 0:
                evs.append((self.sem[e], self.cnt[e]))
        for e in self.eng:
            for ev in evs:
                if ev[0] is self.sem[e]:
                    continue
                self._wait(e, ev)

    def finish(self):
        for q in self.dring:
            n = self.dn[q]
            for slot in range(self.R):
                cnt = (n - slot + self.R - 1) // self.R if n > slot else 0
                if cnt > 0:
                    self._wait("sp", (self.dring[q][slot], 16 * cnt))
        for e in self.eng:
            if e != "sp" and self.cnt[e] > 0:
                self._wait("sp", (self.sem[e], self.cnt[e]))


class Cfg:
    def __init__(self, seq=4096, past=8192, nb=2, ns=16, n_pool=10240):
        self.SEQ = seq
        self.PAST = past
        self.NB = nb
        self.NS = ns
        self.NPOOL = n_pool
        self.NT = seq // 128
        self.NPG = past // 128
        self.TOPK = min(256, seq // 4)
        self.TOPK_S = min(256, (past + 1) // 4)
        self.NTOK = nb * seq
        self.NTT = nb * self.NT + 1
        self.NTOKP = self.NTT * 128


def build(cfg, phases=("p1",), debug=False):
    nc = bass.Bass("TRN2", target_bir_lowering=False)
    S = Sync(nc)
    NB, SEQ, NS, NT = cfg.NB, cfg.SEQ, cfg.NS, cfg.NT
    NTT, NTOKP = cfg.NTT, cfg.NTOKP

    def din(name, shape, dt=F32):
        return nc.dram_tensor(name, list(shape), dt, kind="ExternalInput").ap()

    def dout(name, shape, dt=F32):
        return nc.dram_tensor(name, list(shape), dt, kind="ExternalOutput").ap()

    def dscr(name, shape, dt=F32):
        return nc.dram_tensor(name, list(shape), dt, kind="ExternalOutput" if debug else "Internal").ap()

    xp = din("xp", [NB * SEQ, D_MODEL])
    xs = din("xs", [NS, D_MODEL])
    cache_k = din("cache_k", [cfg.NPOOL, 128 * 128])
    cache_v = din("cache_v", [cfg.NPOOL, 128 * 128])
    cache_ik = din("cache_ik", [cfg.NPOOL, 128 * 64])
    st_re = din("st_re", [NS, N_G * P_ST])
    st_im = din("st_im", [NS, N_G * P_ST])
    ptab = din("ptab", [NS * cfg.NPG, 1], I32)
    norm1_g = din("norm1_g", [D_MODEL])
    w_in = din("w_in", [D_MODEL, IN_COLS])
    a_re = din("ssm_a_re", [N_G * P_ST])
    a_im = din("ssm_a_im", [N_G * P_ST])
    log_dt = din("ssm_log_dt", [N_G])
    b_re = din("ssm_b_re", [N_G * P_ST, 16])
    b_im = din("ssm_b_im", [N_G * P_ST, 16])
    c_re = din("ssm_c_re", [N_G * 16, P_ST])
    c_im = din("ssm_c_im", [N_G * 16, P_ST])
    ssm_d = din("ssm_d", [D_SSM])
    w_glu = din("w_glu", [D_SSM, 2 * D_MODEL])
    b_glu = din("b_glu", [2 * D_MODEL])
    w_ao = din("w_attn_out", [D_ATTN, D_MODEL])
    w_o = din("w_o", [D_MODEL, D_MODEL])
    norm2_g = din("norm2_g", [D_MODEL])
    w_up = din("w_up", [D_MODEL, D_FF])
    w_down = din("w_down", [D_FF, D_MODEL])
    normf_g = din("normf_g", [D_MODEL])

    y_p = dout("y_p", [NB * SEQ, D_MODEL])
    y_s = dout("y_s", [NS, D_MODEL])
    k_p = dout("k_p", [NB * SEQ, 128])
    v_p = dout("v_p", [NB * SEQ, 128])
    ki_p = dout("ki_p", [NB * SEQ, 64])
    re_p = dout("re_p", [NB, N_G * P_ST])
    im_p = dout("im_p", [NB, N_G * P_ST])
    k_s = dout("k_s", [NS, 128])
    v_s = dout("v_s", [NS, 128])
    ki_s = dout("ki_s", [NS, 64])
    re_s = dout("re_s", [NS, N_G * P_ST])
    im_s = dout("im_s", [NS, N_G * P_ST])

    qT_scr = dscr("qT_scr", [NTT, 65, 8, 128], BF16)
    qiT_scr = dscr("qiT_scr", [NTT, 64, 8, 128], BF16)
    kT_scr = dscr("kT_scr", [64, 2, NTOKP], BF16)
    kiT_scr = dscr("kiT_scr", [64, NTOKP], BF16)
    vb_scr = dscr("vb_scr", [NTOKP, 2, 64], BF16)
    uT_scr = dscr("uT_scr", [D_SSM, NTOKP], F32)
    gate_scr = dscr("gate_scr", [NTOKP, 2048], F32)
    gyT_scr = dscr("gyT_scr", [D_SSM, NTOKP], BF16)
    ao_scr = dscr("ao_scr", [NTOKP, D_ATTN], BF16)
    x2_scr = dscr("x2_scr", [NTOKP, D_MODEL], F32)

    if debug:
        dbg_mb = dscr("dbg_mb", [128, SEQ], BF16)
        dbg_sc = dscr("dbg_sc", [128, SEQ], F32)
        dbg_thr = dscr("dbg_thr", [128, 1], F32)
    ident = nc.alloc_sbuf_tensor("ident", [128, 128], BF16)
    identf = nc.alloc_sbuf_tensor("identf", [128, 128], F32)
    cmask = nc.alloc_sbuf_tensor("cmask", [128, 128], F32)
    eps_t = nc.alloc_sbuf_tensor("eps_t", [128, 1], F32)
    cosT = nc.alloc_sbuf_tensor("cosT", [128, NT + 1, 8], F32)
    sinT = nc.alloc_sbuf_tensor("sinT", [128, NT + 1, 8], F32)
    lohi = nc.alloc_sbuf_tensor("lohi", [128, NTT, 16], F32)
    kn2 = nc.alloc_sbuf_tensor("kn2", [128, NTT, 2], F32)

    def V(fn, r=(), w=(), **kw):
        return S.op("dve", fn, r, w, **kw)

    def A(fn, r=(), w=(), **kw):
        return S.op("act", fn, r, w, **kw)

    def G(fn, r=(), w=(), **kw):
        return S.op("pool", fn, r, w, **kw)

    def PE(fn, r=(), w=(), **kw):
        return S.op("pe", fn, r, w, **kw)

    def DMA(out, in_, r=(), w=(), q="sp", is_output=False, **kw):
        return S.dma(q, lambda: S.eng[q].dma_start(out=out, in_=in_, **kw), r, w, is_output=is_output)

    ones_f = nc.alloc_sbuf_tensor("ones_f", [128, 128], F32)
    G(lambda: nc.gpsimd.memset(ones_f[:], 1.0), w=["ones_f"])
    G(lambda: nc.gpsimd.memset(eps_t[:], EPS), w=["eps_t"])
    G(lambda: nc.gpsimd.affine_select(out=identf[:], in_=ones_f[:], pattern=[[-1, 128]], compare_op=ALU.is_equal,
                                      fill=0.0, base=0, channel_multiplier=1), r=["ones_f"], w=["identf"])
    V(lambda: nc.vector.tensor_copy(out=ident[:], in_=identf[:]), r=["identf"], w=["ident"])
    zer_f = nc.alloc_sbuf_tensor("zer_f", [128, 128], F32)
    G(lambda: nc.gpsimd.memset(zer_f[:], 0.0), w=["zer_f"])
    G(lambda: nc.gpsimd.affine_select(out=cmask[:], in_=zer_f[:], pattern=[[-1, 128]], compare_op=ALU.is_ge,
                                      fill=NEG, base=0, channel_multiplier=1), r=["zer_f"], w=["cmask"])

    with contextlib.ExitStack() as es:
        posi = es.enter_context(nc.sbuf_tensor("posi", [128, NT + 1], I32))
        posf = es.enter_context(nc.sbuf_tensor("posf", [128, NT + 1], F32))
        ang = es.enter_context(nc.sbuf_tensor("ang", [128, NT + 1, 8], F32))
        ki_ = es.enter_context(nc.sbuf_tensor("kint", [128, NT + 1, 8], I32))
        kf = es.enter_context(nc.sbuf_tensor("kf", [128, NT + 1, 8], F32))
        rr = es.enter_context(nc.sbuf_tensor("rr", [128, NT + 1, 8], F32))
        r2 = es.enter_context(nc.sbuf_tensor("r2", [128, NT + 1, 8], F32))
        mk = es.enter_context(nc.sbuf_tensor("mk", [128, NT + 1, 8], F32))
        G(lambda: nc.gpsimd.iota(posi[:, 0:NT], pattern=[[128, NT]], base=0, channel_multiplier=1), w=["posi"])
        G(lambda: nc.gpsimd.iota(posi[:, NT:NT + 1], pattern=[[0, 1]], base=cfg.PAST, channel_multiplier=0), w=["posi"])
        V(lambda: nc.vector.tensor_copy(out=posf[:], in_=posi[:]), r=["posi"], w=["posf"])
        for j in range(8):
            inv = ROPE_THETA ** (-j / 8.0)
            V(lambda j=j, inv=inv: nc.vector.tensor_scalar(out=ang[:, :, j], in0=posf[:], scalar1=float(np.float32(inv)),
                                                           scalar2=float(1.0 / (2 * math.pi)), op0=ALU.mult, op1=ALU.mult),
              r=["posf"], w=["ang"])

        def wrap_sin(dst, off):
            V(lambda: nc.vector.tensor_scalar(out=rr[:], in0=ang[:], scalar1=float(off), scalar2=None, op0=ALU.add),
              r=["ang"], w=["rr"])
            V(lambda: nc.vector.tensor_copy(out=ki_[:], in_=rr[:]), r=["rr"], w=["kint"])
            V(lambda: nc.vector.tensor_copy(out=kf[:], in_=ki_[:]), r=["kint"], w=["kf"])
            V(lambda: nc.vector.tensor_tensor(out=r2[:], in0=rr[:], in1=kf[:], op=ALU.subtract), r=["rr", "kf"], w=["r2"])
            V(lambda: nc.vector.tensor_scalar(out=mk[:], in0=r2[:], scalar1=0.5, scalar2=None, op0=ALU.is_gt), r=["r2"], w=["mk"])
            V(lambda: nc.vector.tensor_tensor(out=r2[:], in0=r2[:], in1=mk[:], op=ALU.subtract), r=["r2", "mk"], w=["r2"])
            V(lambda: nc.vector.tensor_scalar(out=mk[:], in0=r2[:], scalar1=-0.5, scalar2=None, op0=ALU.is_lt), r=["r2"], w=["mk"])
            V(lambda: nc.vector.tensor_tensor(out=r2[:], in0=r2[:], in1=mk[:], op=ALU.add), r=["r2", "mk"], w=["r2"])
            A(lambda: nc.scalar.activation(out=dst[:], in_=r2[:], func=AF.Sin, scale=float(2 * math.pi)), r=["r2"], w=[dst.name])

        wrap_sin(sinT, 0.0)
        wrap_sin(cosT, 0.25)
        S.barrier()

    es_all = contextlib.ExitStack()

    def sb(name, shape, dt=F32, stack=None):
        return (stack or es_all).enter_context(nc.sbuf_tensor(name, list(shape), dt))

    def ps(name, shape, dt=F32, stack=None):
        return (stack or es_all).enter_context(nc.psum_tensor(name, list(shape), dt))

    def load_weight_bf16(dst, src, K, N, stack, gcol=None, nm="w", NSTG=2):
        KC = K // 128
        CH = 2048
        stg = [sb(f"stg_{nm}{i}", [128, CH], F32, stack) for i in range(NSTG)]
        i = 0
        for kc in range(KC):
            for c0 in range(0, N, CH):
                cw = min(CH, N - c0)
                st = stg[i % NSTG]
                DMA(st[:, 0:cw], src[kc * 128:(kc + 1) * 128, c0:c0 + cw], w=[st.name], q=("sp", "act")[i % 2])
                eng = ("dve", "pool")[i % 2]
                if gcol is None:
                    if eng == "dve":
                        V(lambda st=st, kc=kc, c0=c0, cw=cw: nc.vector.tensor_copy(out=dst[:, kc, c0:c0 + cw], in_=st[:, 0:cw]),
                          r=[st.name], w=[dst.name])
                    else:
                        G(lambda st=st, kc=kc, c0=c0, cw=cw: nc.gpsimd.tensor_copy(out=dst[:, kc, c0:c0 + cw], in_=st[:, 0:cw]),
                          r=[st.name], w=[dst.name])
                else:
                    V(lambda st=st, kc=kc, c0=c0, cw=cw: nc.vector.tensor_scalar(out=dst[:, kc, c0:c0 + cw], in0=st[:, 0:cw],
                                                                              scalar1=gcol[:, kc:kc + 1], scalar2=None, op0=ALU.mult),
                      r=[st.name, gcol.name], w=[dst.name])
                i += 1

    def tile_rows(T):
        if T < NB * NT:
            return T * 128, 128, False, T % NT
        return 0, NS, True, NT

    if "p1" in phases:
        with contextlib.ExitStack() as e1:
            Win = sb("Win", [128, 8, IN_COLS], BF16, e1)
            g1c = sb("g1c", [128, 8], F32, e1)
            with nc.allow_non_contiguous_dma(reason="tiny gain vector"):
                DMA(g1c[:], norm1_g.rearrange("(k p) -> p k", p=128), w=["g1c"])
            with contextlib.ExitStack() as e1w:
                load_weight_bf16(Win, w_in, D_MODEL, IN_COLS, e1w, gcol=g1c, nm="win", NSTG=6)
                S.barrier()
            xt = [sb(f"xt{i}", [128, D_MODEL], F32, e1) for i in range(2)]
            junk = sb("junk1", [128, D_MODEL], F32, e1)
            ss = sb("ss", [128, 1], F32, e1)
            rstd = sb("rstd", [128, 1], F32, e1)
            hb = sb("hb", [128, D_MODEL], BF16, e1)
            hT = sb("hT", [128, 8, 128], BF16, e1)
            pt = [sb(f"pt{i}", [128, IN_COLS - 512], F32, e1) for i in range(2)]
            uTs = sb("uTs", [128, 4, 128], F32, e1)
            tA = [sb(f"tA{i}", [128, 10, 8], F32, e1) for i in range(6)]
            qn = sb("qn", [128, 8], F32, e1)
            sq = sb("sq", [128, 10, 64], F32, e1)
            wp = sb("wp", [128, 8], F32, e1)
            qa = sb("qa", [128, 8, 65], BF16, e1)
            kb_ = sb("kb_", [128, 2, 64], BF16, e1)
            qib = sb("qib", [128, 8, 64], BF16, e1)
            kib = sb("kib", [128, 64], BF16, e1)
            vbt = sb("vbt", [128, 2, 64], BF16, e1)
            gts = sb("gts", [128, 2048], F32, e1)
            trs = sb("trs", [65, 19, 128], BF16, e1)
            p_tr = ps("p_tr", [128, 8, 128], BF16, e1)
            p_mm = [ps(f"p_mm{i}", [128, 512], F32, e1) for i in range(3)]
            p_u = ps("p_u", [128, 4, 128], F32, e1)
            p_t2 = ps("p_t2", [65, 19, 128], BF16, e1)

            def p1_front(T):
                r0, nr, is_s, pti = tile_rows(T)
                x_ = xt[T % 2]
                p_ = pt[T % 2]
                xn, pn = x_.name, p_.name
                if is_s:
                    V(lambda x_=x_: nc.vector.memset(x_[:], 0.0), w=[xn])
                    DMA(x_[0:NS, :], xs[:, :], w=[xn])
                else:
                    DMA(x_[:], xp[r0:r0 + 128, :], w=[xn])
                A(lambda x_=x_: nc.scalar.activation(out=junk[:], in_=x_[:], func=AF.Square, accum_out=ss[:]),
                  r=[xn], w=["junk1", "ss"])
                A(lambda: nc.scalar.activation(out=rstd[:], in_=ss[:], func=AF.Sqrt, bias=eps_t[:], scale=1.0 / D_MODEL),
                  r=["ss", "eps_t"], w=["rstd"])
                V(lambda: nc.vector.reciprocal(out=rstd[:], in_=rstd[:]), r=["rstd"], w=["rstd"])
                V(lambda x_=x_: nc.vector.tensor_scalar(out=hb[:], in0=x_[:], scalar1=rstd[:, 0:1], scalar2=None, op0=ALU.mult),
                  r=[xn, "rstd"], w=["hb"])
                for kc in range(8):
                    PE(lambda kc=kc: nc.tensor.transpose(out=p_tr[:, kc, :], in_=hb[:, kc * 128:(kc + 1) * 128], identity=ident[:]),
                       r=["hb", "ident"], w=["p_tr"], skip_same=True)
                A(lambda: nc.scalar.copy(out=hT[:], in_=p_tr[:]), r=["p_tr"], w=["hT"])
                for ct in range(4):
                    for kc in range(8):
                        PE(lambda ct=ct, kc=kc: nc.tensor.matmul(p_u[:, ct, :], lhsT=Win[:, kc, ct * 128:(ct + 1) * 128],
                                                                 rhs=hT[:, kc, :], start=(kc == 0), stop=(kc == 7)),
                           r=["Win", "hT"], w=["p_u"], skip_same=True)
                V(lambda: nc.vector.tensor_copy(out=uTs[:], in_=p_u[:]), r=["p_u"], w=["uTs"])
                DMA(uT_scr.rearrange("(c p) t -> p c t", p=128)[:, :, T * 128:(T + 1) * 128], uTs[:], r=["uTs"], w=[("uT", T)])
                ci = 0
                for c0 in range(512, IN_COLS, 512):
                    cw = min(512, IN_COLS - c0)
                    pm = p_mm[ci % 3]
                    for kc in range(8):
                        PE(lambda pm=pm, kc=kc, c0=c0, cw=cw: nc.tensor.matmul(pm[:, 0:cw], lhsT=hT[:, kc, :], rhs=Win[:, kc, c0:c0 + cw],
                                                                               start=(kc == 0), stop=(kc == 7)),
                           r=["Win", "hT"], w=[pm.name], skip_same=True)
                    if ci % 2 == 0:
                        A(lambda pm=pm, c0=c0, cw=cw, p_=p_: nc.scalar.copy(out=p_[:, c0 - 512:c0 - 512 + cw], in_=pm[:, 0:cw]),
                          r=[pm.name], w=[pn])
                    else:
                        V(lambda pm=pm, c0=c0, cw=cw, p_=p_: nc.vector.tensor_copy(out=p_[:, c0 - 512:c0 - 512 + cw], in_=pm[:, 0:cw]),
                          r=[pm.name], w=[pn])
                    ci += 1

            def p1_back(T):
                r0, nr, is_s, pti = tile_rows(T)
                x_ = xt[T % 2]
                p_ = pt[T % 2]
                xn, pn = x_.name, p_.name
                cs_c = cosT[:, pti, :]
                cs_s = sinT[:, pti, :]
                for (cb, H) in ((C_Q - 512, 10), (C_QI - 512, 9)):
                    X = p_[:, cb:cb + H * 64].rearrange("p (h d) -> p h d", d=64)
                    x1 = X[:, :, 0:8]
                    x2 = X[:, :, 8:16]
                    cB = cs_c.unsqueeze(1).to_broadcast([128, H, 8])
                    sB = cs_s.unsqueeze(1).to_broadcast([128, H, 8])
                    t = [tt[:, 0:H, :] for tt in tA]
                    V(lambda x1=x1, cB=cB, t=t: nc.vector.tensor_tensor(out=t[0], in0=x1, in1=cB, op=ALU.mult), r=[pn, "cosT"], w=["tA0"])
                    V(lambda x2=x2, sB=sB, t=t: nc.vector.tensor_tensor(out=t[1], in0=x2, in1=sB, op=ALU.mult), r=[pn, "sinT"], w=["tA1"])
                    V(lambda x1=x1, sB=sB, t=t: nc.vector.tensor_tensor(out=t[2], in0=x1, in1=sB, op=ALU.mult), r=[pn, "sinT"], w=["tA2"])
                    V(lambda x2=x2, cB=cB, t=t: nc.vector.tensor_tensor(out=t[3], in0=x2, in1=cB, op=ALU.mult), r=[pn, "cosT"], w=["tA3"])
                    V(lambda x1=x1, t=t: nc.vector.tensor_tensor(out=x1, in0=t[0], in1=t[1], op=ALU.subtract), r=["tA0", "tA1"], w=[pn])
                    V(lambda x2=x2, t=t: nc.vector.tensor_tensor(out=x2, in0=t[2], in1=t[3], op=ALU.add), r=["tA2", "tA3"], w=[pn])
                kcol, vcol, kicol = C_K - 512, C_V - 512, C_KI - 512
                if is_s:
                    DMA(k_s[:, :], p_[0:NS, kcol:kcol + 128], r=[pn], q="pool", is_output=True)
                    DMA(v_s[:, :], p_[0:NS, vcol:vcol + 128], r=[pn], q="pool", is_output=True)
                    DMA(ki_s[:, :], p_[0:NS, kicol:kicol + 64], r=[pn], q="pool", is_output=True)
                else:
                    DMA(k_p[r0:r0 + 128, :], p_[:, kcol:kcol + 128], r=[pn], q="pool", is_output=True)
                    DMA(v_p[r0:r0 + 128, :], p_[:, vcol:vcol + 128], r=[pn], q="pool", is_output=True)
                    DMA(ki_p[r0:r0 + 128, :], p_[:, kicol:kicol + 64], r=[pn], q="pool", is_output=True)
                QK = p_[:, C_Q - 512:C_Q - 512 + 640].rearrange("p (h d) -> p h d", d=64)
                G(lambda QK=QK: nc.gpsimd.tensor_tensor(out=sq[:], in0=QK, in1=QK, op=ALU.mult), r=[pn], w=["sq"])
                V(lambda: nc.vector.tensor_reduce(out=qn[:], in_=sq[:, 0:8, :], axis=AX.X, op=ALU.add), r=["sq"], w=["qn"])
                V(lambda T=T: nc.vector.tensor_reduce(out=kn2[:, T, :], in_=sq[:, 8:10, :], axis=AX.X, op=ALU.add), r=["sq"], w=["kn2"])
                A(lambda: nc.scalar.activation(out=qn[:], in_=qn[:], func=AF.Sqrt), r=["qn"], w=["qn"])
                V(lambda: nc.vector.tensor_scalar(out=qa[:, :, 64], in0=qn[:], scalar1=-1.0, scalar2=None, op0=ALU.mult),
                  r=["qn"], w=["qa"])
                V(lambda QK=QK: nc.vector.tensor_copy(out=qa[:, :, 0:64], in_=QK[:, 0:8, :]), r=[pn], w=["qa"])
                G(lambda QK=QK: nc.gpsimd.tensor_copy(out=kb_[:], in_=QK[:, 8:10, :]), r=[pn], w=["kb_"])
                wic = C_WI - 512
                V(lambda p_=p_, wic=wic: nc.vector.tensor_scalar(out=wp[:], in0=p_[:, wic:wic + 8], scalar1=float(8 ** -0.5 * 0.125),
                                                                 scalar2=None, op0=ALU.mult), r=[pn], w=["wp"])
                V(lambda T=T: nc.vector.tensor_scalar(out=lohi[:, T, 0:8], in0=wp[:], scalar1=0.0, scalar2=-3.0e38,
                                                      op0=ALU.is_le, op1=ALU.mult), r=["wp"], w=["lohi"])
                V(lambda T=T: nc.vector.tensor_scalar(out=lohi[:, T, 8:16], in0=wp[:], scalar1=0.0, scalar2=3.0e38,
                                                      op0=ALU.is_gt, op1=ALU.mult), r=["wp"], w=["lohi"])
                QI = p_[:, C_QI - 512:C_QI - 512 + 512].rearrange("p (h d) -> p h d", d=64)
                V(lambda QI=QI: nc.vector.tensor_tensor(out=qib[:], in0=QI, in1=wp[:].unsqueeze(2).to_broadcast([128, 8, 64]), op=ALU.mult),
                  r=[pn, "wp"], w=["qib"])
                G(lambda p_=p_: nc.gpsimd.tensor_copy(out=kib[:], in_=p_[:, C_KI - 512:C_KI - 512 + 64]), r=[pn], w=["kib"])
                G(lambda p_=p_: nc.gpsimd.tensor_copy(out=vbt[:], in_=p_[:, vcol:vcol + 128].rearrange("p (g d) -> p g d", d=64)),
                  r=[pn], w=["vbt"])
                DMA(vb_scr[T * 128:(T + 1) * 128, :, :], vbt[:], r=["vbt"], w=[("vb", T)], q="pool")
                A(lambda p_=p_: nc.scalar.activation(out=gts[:], in_=p_[:, C_GS - 512:C_GS - 512 + 2048], func=AF.Sigmoid),
                  r=[pn], w=["gts"])
                DMA(gate_scr[T * 128:(T + 1) * 128, :], gts[:], r=["gts"], w=[("gate", T)], q="pool")
                for h in range(8):
                    PE(lambda h=h: nc.tensor.transpose(out=p_t2[0:65, h, :], in_=qa[:, h, :], identity=ident[:]),
                       r=["qa", "ident"], w=["p_t2"], skip_same=True)
                for g in range(2):
                    PE(lambda g=g: nc.tensor.transpose(out=p_t2[0:64, 8 + g, :], in_=kb_[:, g, :], identity=ident[:]),
                       r=["kb_", "ident"], w=["p_t2"], skip_same=True)
                for h in range(8):
                    PE(lambda h=h: nc.tensor.transpose(out=p_t2[0:64, 10 + h, :], in_=qib[:, h, :], identity=ident[:]),
                       r=["qib", "ident"], w=["p_t2"], skip_same=True)
                PE(lambda: nc.tensor.transpose(out=p_t2[0:64, 18, :], in_=kib[:], identity=ident[:]),
                   r=["kib", "ident"], w=["p_t2"], skip_same=True)
                V(lambda: nc.vector.tensor_copy(out=trs[0:65, 0:8, :], in_=p_t2[0:65, 0:8, :]), r=["p_t2"], w=["trs"])
                A(lambda: nc.scalar.copy(out=trs[0:64, 8:19, :], in_=p_t2[0:64, 8:19, :]), r=["p_t2"], w=["trs"])
                DMA(qT_scr[T, :, :, :], trs[0:65, 0:8, :], r=["trs"], w=[("qT", T)], q="pool")
                DMA(kT_scr[:, :, T * 128:(T + 1) * 128], trs[0:64, 8:10, :], r=["trs"], w=[("kT", T)], q="pool")
                DMA(qiT_scr[T, :, :, :], trs[0:64, 10:18, :], r=["trs"], w=[("qiT", T)], q="pool")
                DMA(kiT_scr[:, T * 128:(T + 1) * 128], trs[0:64, 18, :], r=["trs"], w=[("kiT", T)], q="pool")

            p1_front(0)
            for T in range(NTT):
                if T + 1 < NTT:
                    p1_front(T + 1)
                p1_back(T)
            S.barrier()


    if "p2" in phases:
        with contextlib.ExitStack() as e2:
            NLEV = int(math.log2(SEQ))
            assert (1 << NLEV) == SEQ
            NCH = SEQ // 512
            sm = {}

            def small(name, shape=(128, 16), dt=F32):
                t_ = sb("s2_" + name, list(shape), dt, e2)
                sm[name] = t_
                return t_

            def vtt(o, a, b, op):
                V(lambda: nc.vector.tensor_tensor(out=o[:], in0=a[:], in1=b[:], op=op), r=[a.name, b.name], w=[o.name])

            def vts(o, a, s1, op, s2=None, op1=None):
                if op1 is None:
                    V(lambda: nc.vector.tensor_scalar(out=o[:], in0=a[:], scalar1=s1, scalar2=None, op0=op), r=[a.name], w=[o.name])
                else:
                    V(lambda: nc.vector.tensor_scalar(out=o[:], in0=a[:], scalar1=s1, scalar2=s2, op0=op, op1=op1), r=[a.name], w=[o.name])

            are_t = small("are"); aim_t = small("aim"); ldt = small("ldt"); dtt = small("dt")
            xre = small("xre"); th = small("th"); lam_abs = small("lam_abs")
            cs_ = small("cs"); sn_ = small("sn"); lam_re = small("lam_re"); lam_im = small("lam_im")
            tq = [small(f"tq{i}") for i in range(6)]
            tqi = small("tqi", dt=I32)
            cf_re = small("cf_re"); cf_im = small("cf_im")
            with nc.allow_non_contiguous_dma(reason="tiny ssm params"):
                DMA(are_t[:], a_re.rearrange("(ft f) -> f ft", f=128), w=[are_t.name])
                DMA(aim_t[:], a_im.rearrange("(ft f) -> f ft", f=128), w=[aim_t.name])
                lv = log_dt.rearrange("(ft two) -> two ft", two=2)
                DMA(ldt[0:64, :], lv[0:1, :].partition_broadcast(64), w=[ldt.name])
                DMA(ldt[64:128, :], lv[1:2, :].partition_broadcast(64), w=[ldt.name])
            A(lambda: nc.scalar.activation(out=dtt[:], in_=ldt[:], func=AF.Exp), r=[ldt.name], w=[dtt.name])
            vtt(xre, are_t, dtt, ALU.mult)
            vtt(th, aim_t, dtt, ALU.mult)
            A(lambda: nc.scalar.activation(out=lam_abs[:], in_=xre[:], func=AF.Exp), r=[xre.name], w=[lam_abs.name])

            def sincos_turns(dst, src_turns, off):
                a_, k_, r_, m_ = tq[0], tq[1], tq[2], tq[3]
                vts(a_, src_turns, float(off), ALU.add)
                V(lambda: nc.vector.tensor_copy(out=tqi[:], in_=a_[:]), r=[a_.name], w=[tqi.name])
                V(lambda: nc.vector.tensor_copy(out=k_[:], in_=tqi[:]), r=[tqi.name], w=[k_.name])
                vtt(r_, a_, k_, ALU.subtract)
                vts(m_, r_, 0.5, ALU.is_gt)
                vtt(r_, r_, m_, ALU.subtract)
                vts(m_, r_, -0.5, ALU.is_lt)
                vtt(r_, r_, m_, ALU.add)
                A(lambda: nc.scalar.activation(out=dst[:], in_=r_[:], func=AF.Sin, scale=float(2 * math.pi)), r=[r_.name], w=[dst.name])

            thn = small("thn")
            vts(thn, th, float(1.0 / (2 * math.pi)), ALU.mult)
            sincos_turns(sn_, thn, 0.0)
            sincos_turns(cs_, thn, 0.25)
            vtt(lam_re, lam_abs, cs_, ALU.mult)
            vtt(lam_im, lam_abs, sn_, ALU.mult)
            nr, den, t1_, t2_ = tq[0], tq[1], tq[2], tq[3]
            vts(nr, lam_re, -1.0, ALU.add)
            vtt(t1_, are_t, are_t, ALU.mult)
            vtt(t2_, aim_t, aim_t, ALU.mult)
            vtt(den, t1_, t2_, ALU.add)
            V(lambda: nc.vector.reciprocal(out=den[:], in_=den[:]), r=[den.name], w=[den.name])
            vtt(t1_, nr, are_t, ALU.mult)
            vtt(t2_, lam_im, aim_t, ALU.mult)
            vtt(t1_, t1_, t2_, ALU.add)
            vtt(cf_re, t1_, den, ALU.mult)
            vtt(t1_, lam_im, are_t, ALU.mult)
            vtt(t2_, nr, aim_t, ALU.mult)
            vtt(t1_, t1_, t2_, ALU.subtract)
            vtt(cf_im, t1_, den, ALU.mult)
            wre = small("wre", (128, 16, NLEV)); wim = small("wim", (128, 16, NLEV))
            V(lambda: nc.vector.tensor_copy(out=wre[:, :, 0], in_=cs_[:]), r=[cs_.name], w=[wre.name])
            V(lambda: nc.vector.tensor_copy(out=wim[:, :, 0], in_=sn_[:]), r=[sn_.name], w=[wim.name])
            for k in range(1, NLEV):
                V(lambda k=k: nc.vector.tensor_tensor(out=tq[0][:], in0=wre[:, :, k - 1], in1=wre[:, :, k - 1], op=ALU.mult), r=[wre.name], w=[tq[0].name])
                V(lambda k=k: nc.vector.tensor_tensor(out=tq[1][:], in0=wim[:, :, k - 1], in1=wim[:, :, k - 1], op=ALU.mult), r=[wim.name], w=[tq[1].name])
                V(lambda k=k: nc.vector.tensor_tensor(out=wre[:, :, k], in0=tq[0][:], in1=tq[1][:], op=ALU.subtract), r=[tq[0].name, tq[1].name], w=[wre.name])
                V(lambda k=k: nc.vector.tensor_tensor(out=tq[2][:], in0=wre[:, :, k - 1], in1=wim[:, :, k - 1], op=ALU.mult), r=[wre.name, wim.name], w=[tq[2].name])
                V(lambda k=k: nc.vector.tensor_scalar(out=wim[:, :, k], in0=tq[2][:], scalar1=2.0, scalar2=None, op0=ALU.mult), r=[tq[2].name], w=[wim.name])
            Bre = small("Bre", (128, 16, 16)); Bim = small("Bim", (128, 16, 16))
            bbr = small("bbr", (128, 16, 16)); bbi = small("bbi", (128, 16, 16)); btmp = small("btmp", (128, 16, 16))
            DMA(Bre[:], b_re.rearrange("(ft f) m -> f ft m", f=128), w=[Bre.name])
            DMA(Bim[:], b_im.rearrange("(ft f) m -> f ft m", f=128), w=[Bim.name])
            cfr_b = cf_re[:].unsqueeze(2).to_broadcast([128, 16, 16])
            cfi_b = cf_im[:].unsqueeze(2).to_broadcast([128, 16, 16])
            V(lambda: nc.vector.tensor_tensor(out=bbr[:], in0=Bre[:], in1=cfr_b, op=ALU.mult), r=[Bre.name, cf_re.name], w=[bbr.name])
            V(lambda: nc.vector.tensor_tensor(out=btmp[:], in0=Bim[:], in1=cfi_b, op=ALU.mult), r=[Bim.name, cf_im.name], w=[btmp.name])
            vtt(bbr, bbr, btmp, ALU.subtract)
            V(lambda: nc.vector.tensor_tensor(out=bbi[:], in0=Bim[:], in1=cfr_b, op=ALU.mult), r=[Bim.name, cf_re.name], w=[bbi.name])
            V(lambda: nc.vector.tensor_tensor(out=btmp[:], in0=Bre[:], in1=cfi_b, op=ALU.mult), r=[Bre.name, cf_im.name], w=[btmp.name])
            vtt(bbi, bbi, btmp, ALU.add)
            BT = small("BT", (128, 16, 2, 128), BF16)
            CT = small("CT", (128, 16, 2, 128), BF16)
            V(lambda: nc.vector.memset(CT[:], 0.0), w=[CT.name])
            Xb = small("Xb", (128, 128))
            p_s = ps("p2_s", [128, 512], F32, e2)
            for ft in range(16):
                ct, q = ft // 4, ft % 4
                for ri, bb in enumerate((bbr, bbi)):
                    V(lambda: nc.vector.memset(Xb[:], 0.0), w=[Xb.name])
                    V(lambda bb=bb, ft=ft, q=q: nc.vector.tensor_copy(out=Xb[0:64, 32 * q:32 * q + 16], in_=bb[0:64, ft, :]), r=[bb.name], w=[Xb.name])
                    V(lambda bb=bb, ft=ft, q=q: nc.vector.tensor_copy(out=Xb[64:128, 32 * q + 16:32 * q + 32], in_=bb[64:128, ft, :]), r=[bb.name], w=[Xb.name])
                    PE(lambda: nc.tensor.transpose(out=p_s[:, 0:128], in_=Xb[:], identity=identf[:]), r=[Xb.name, "identf"], w=[p_s.name])
                    V(lambda ft=ft, ri=ri: nc.vector.tensor_copy(out=BT[:, ft, ri, :], in_=p_s[:, 0:128]),
                      r=[p_s.name], w=[BT.name])
            Cre = small("Cre", (32, 16, 64)); Cim = small("Cim", (32, 16, 64))
            mA = small("mA", (32, 1)); mB = small("mB", (32, 1)); Xc = small("Xc", (32, 128))
            DMA(Cre[:], c_re.rearrange("(ft r) p -> r ft p", r=32), w=[Cre.name])
            DMA(Cim[:], c_im.rearrange("(ft r) p -> r ft p", r=32), w=[Cim.name])
            V(lambda: nc.vector.memset(mA[:], 0.0), w=[mA.name])
            V(lambda: nc.vector.memset(mA[0:16, :], 1.0), w=[mA.name])
            V(lambda: nc.vector.tensor_scalar(out=mB[:], in0=mA[:], scalar1=-1.0, scalar2=1.0, op0=ALU.mult, op1=ALU.add), r=[mA.name], w=[mB.name])
            for ft in range(16):
                for ri, cc in enumerate((Cre, Cim)):
                    sgn = 1.0 if ri == 0 else -1.0
                    V(lambda cc=cc, ft=ft, sgn=sgn: nc.vector.tensor_scalar(out=Xc[:, 0:64], in0=cc[:, ft, :], scalar1=mA[:, 0:1], scalar2=sgn,
                                                                          op0=ALU.mult, op1=ALU.mult), r=[cc.name, mA.name], w=[Xc.name])
                    V(lambda cc=cc, ft=ft, sgn=sgn: nc.vector.tensor_scalar(out=Xc[:, 64:128], in0=cc[:, ft, :], scalar1=mB[:, 0:1], scalar2=sgn,
                                                                          op0=ALU.mult, op1=ALU.mult), r=[cc.name, mB.name], w=[Xc.name])
                    PE(lambda: nc.tensor.transpose(out=p_s[:, 0:32], in_=Xc[:], identity=identf[0:32, 0:32]), r=[Xc.name, "identf"], w=[p_s.name])
                    V(lambda ft=ft, ri=ri: nc.vector.tensor_copy(out=CT[:, ft, ri, 32 * (ft % 4):32 * (ft % 4) + 32], in_=p_s[:, 0:32]), r=[p_s.name], w=[CT.name])
            dcol = small("dcol", (128, 4))
            with nc.allow_non_contiguous_dma(reason="tiny"):
                DMA(dcol[:], ssm_d.rearrange("(c p) -> p c", p=128), w=[dcol.name])
            h0r = small("h0r", (128, 16, NS)); h0i = small("h0i", (128, 16, NS))
            h1r = small("h1r", (128, 16, NS)); h1i = small("h1i", (128, 16, NS))
            sst = small("sst", (NS, 2048))
            for (src, dst) in ((st_re, h0r), (st_im, h0i)):
                DMA(sst[:], src[:, :], w=[sst.name])
                for ft in range(16):
                    PE(lambda ft=ft: nc.tensor.transpose(out=p_s[:, ft * NS:(ft + 1) * NS], in_=sst[:, ft * 128:(ft + 1) * 128],
                                                         identity=identf[0:NS, 0:NS]), r=[sst.name, "identf"], w=[p_s.name])
                V(lambda dst=dst: nc.vector.tensor_copy(out=dst[:].rearrange("p a b -> p (a b)"), in_=p_s[:, 0:16 * NS]), r=[p_s.name], w=[dst.name])

            WTOK = SEQ
            ufst = [sb(f"ufst{i}", [128, 512], F32, e2) for i in range(2)]
            ub = sb("ub", [128, NB, SEQ], BF16, e2)
            ufs = sb("ufs", [128, NS], F32, e2)
            ubs = sb("ubs", [128, NS], BF16, e2)
            Ec = sb("Ec", [128, SEQ], F32, e2)
            Es = sb("Es", [128, SEQ], F32, e2)
            rdec = sb("rdec", [128, 512], F32, e2)
            yT = sb("yT", [128, NB, SEQ], F32, e2)
            yTs = sb("yTs", [128, NS], F32, e2)
            fin = sb("fin", [128, 16, NB, 2], F32, e2)
            tm8 = [sb(f"tm{i}", [128, 512], F32, e2) for i in range(8)]
            tm = tm8[0:4]
            gre = [sb(f"gre{i}", [128, 512], F32, e2) for i in range(2)]
            gim = [sb(f"gim{i}", [128, 512], F32, e2) for i in range(2)]
            Gre = [sb(f"Gre{i}", [128, 512], F32, e2) for i in range(2)]
            Gim = [sb(f"Gim{i}", [128, 512], F32, e2) for i in range(2)]
            dm8 = [sb(f"dm{i}", [128, 512], F32, e2) for i in range(8)]
            dm = dm8[0:4]
            hre = [sb(f"hre{i}", [128, 512], BF16, e2) for i in range(2)]
            him = [sb(f"him{i}", [128, 512], BF16, e2) for i in range(2)]
            gl = [sb(f"gl{i}", [128, 512], F32, e2) for i in range(3)]
            gyb = sb("gyb", [128, 512], BF16, e2)
            p_br = [ps(f"p_br{i}", [128, 512], F32, e2) for i in range(2)]
            p_bi = [ps(f"p_bi{i}", [128, 512], F32, e2) for i in range(2)]
            p_y = [ps(f"p_y{i}", [128, 512], F32, e2) for i in range(2)]
            samp0 = (NTT - 1) * 128
            it = 0
            for ct in range(4):
                for b in range(NB):
                    for c in range(NCH):
                        us_ = ufst[(b * NCH + c) % 2]
                        DMA(us_[:], uT_scr[ct * 128:(ct + 1) * 128, b * SEQ + c * 512: b * SEQ + (c + 1) * 512],
                            r=[("uT", (b * SEQ + c * 512) // 128 + i_) for i_ in range(4)], w=[us_.name])
                        G(lambda b=b, c=c, us_=us_: nc.gpsimd.tensor_copy(out=ub[:, b, c * 512:(c + 1) * 512], in_=us_[:]), r=[us_.name], w=["ub"])
                DMA(ufs[:], uT_scr[ct * 128:(ct + 1) * 128, samp0:samp0 + NS], r=[("uT", NTT - 1)], w=["ufs"])
                G(lambda: nc.gpsimd.tensor_copy(out=ubs[:], in_=ufs[:]), r=["ufs"], w=["ubs"])
                for q in range(4):
                    ft = ct * 4 + q
                    qs = slice(32 * q, 32 * q + 32)
                    V(lambda: nc.vector.memset(Ec[:, 0:1], 1.0), w=["Ec"])
                    V(lambda: nc.vector.memset(Es[:, 0:1], 0.0), w=["Es"])
                    for k in range(NLEV):
                        n = 1 << k
                        wr = wre[:, ft, k:k + 1]
                        wi = wim[:, ft, k:k + 1]
                        for n0 in range(0, n, 512):
                            nn = min(512, n - n0)
                            lo_ = slice(n0, n0 + nn)
                            hi_ = slice(n + n0, n + n0 + nn)
                            A(lambda wi=wi, lo_=lo_, nn=nn: nc.scalar.activation(out=tm[0][:, 0:nn], in_=Es[:, lo_], func=AF.Identity, scale=wi),
                              r=["Es", wim.name], w=[tm[0].name])
                            V(lambda wr=wr, lo_=lo_, hi_=hi_, nn=nn: nc.vector.scalar_tensor_tensor(out=Ec[:, hi_], in0=Ec[:, lo_], scalar=wr, in1=tm[0][:, 0:nn],
                                                                                                  op0=ALU.mult, op1=ALU.subtract),
                              r=["Ec", tm[0].name, wre.name], w=["Ec"])
                            A(lambda wi=wi, lo_=lo_, nn=nn: nc.scalar.activation(out=tm[1][:, 0:nn], in_=Ec[:, lo_], func=AF.Identity, scale=wi),
                              r=["Ec", wim.name], w=[tm[1].name])
                            V(lambda wr=wr, lo_=lo_, hi_=hi_, nn=nn: nc.vector.scalar_tensor_tensor(out=Es[:, hi_], in0=Es[:, lo_], scalar=wr, in1=tm[1][:, 0:nn],
                                                                                                  op0=ALU.mult, op1=ALU.add),
                              r=["Es", tm[1].name, wre.name], w=["Es"])
                    V(lambda ft=ft: nc.vector.tensor_scalar(out=rdec[:], in0=Ec[:, 0:512], scalar1=0.0, scalar2=lam_abs[:, ft:ft + 1],
                                                            op0=ALU.mult, op1=ALU.add), r=["Ec", lam_abs.name], w=["rdec"])
                    chunks = [(b, c) for b in range(NB) for c in range(NCH)]
                    base_it = it

                    def stage_A(j):
                        b, c = chunks[j]
                        itj = base_it + j
                        cs = slice(c * 512, (c + 1) * 512)
                        pr, pi = p_br[itj % 2], p_bi[itj % 2]
                        PE(lambda: nc.tensor.matmul(pr[:], lhsT=BT[:, ft, 0, :], rhs=ub[:, b, cs], start=True, stop=True), r=[BT.name, "ub"], w=[pr.name])
                        PE(lambda: nc.tensor.matmul(pi[:], lhsT=BT[:, ft, 1, :], rhs=ub[:, b, cs], start=True, stop=True), r=[BT.name, "ub"], w=[pi.name])
                        g_r, g_i, G_r, G_i = gre[itj % 2], gim[itj % 2], Gre[itj % 2], Gim[itj % 2]
                        tm = tm8[4 * (itj % 2):4 * (itj % 2) + 4]
                        V(lambda: nc.vector.tensor_tensor(out=tm[0][:], in0=pr[:], in1=Ec[:, cs], op=ALU.mult), r=[pr.name, "Ec"], w=[tm[0].name])
                        V(lambda: nc.vector.tensor_tensor(out=tm[1][:], in0=pi[:], in1=Es[:, cs], op=ALU.mult), r=[pi.name, "Es"], w=[tm[1].name])
                        V(lambda: nc.vector.tensor_tensor(out=tm[2][:], in0=pi[:], in1=Ec[:, cs], op=ALU.mult), r=[pi.name, "Ec"], w=[tm[2].name])
                        V(lambda: nc.vector.tensor_tensor(out=tm[3][:], in0=pr[:], in1=Es[:, cs], op=ALU.mult), r=[pr.name, "Es"], w=[tm[3].name])
                        V(lambda: nc.vector.tensor_tensor(out=g_r[:], in0=tm[0][:], in1=tm[1][:], op=ALU.add), r=[tm[0].name, tm[1].name], w=[g_r.name])
                        V(lambda: nc.vector.tensor_tensor(out=g_i[:], in0=tm[2][:], in1=tm[3][:], op=ALU.subtract), r=[tm[2].name, tm[3].name], w=[g_i.name])
                        if c == 0:
                            ini_r, ini_i, rd_extra = 0.0, 0.0, []
                        else:
                            pG_r, pG_i = Gre[(itj - 1) % 2], Gim[(itj - 1) % 2]
                            ini_r, ini_i = pG_r[:, 511:512], pG_i[:, 511:512]
                            rd_extra = [pG_r.name, pG_i.name]
                        V(lambda: nc.vector.tensor_tensor_scan(out=G_r[:], data0=rdec[:], data1=g_r[:], initial=ini_r, op0=ALU.mult, op1=ALU.add),
                          r=["rdec", g_r.name] + rd_extra, w=[G_r.name])
                        V(lambda: nc.vector.tensor_tensor_scan(out=G_i[:], data0=rdec[:], data1=g_i[:], initial=ini_i, op0=ALU.mult, op1=ALU.add),
                          r=["rdec", g_i.name] + rd_extra, w=[G_i.name])

                    def stage_B(j):
                        b, c = chunks[j]
                        itj = base_it + j
                        cs = slice(c * 512, (c + 1) * 512)
                        G_r, G_i = Gre[itj % 2], Gim[itj % 2]
                        dm = dm8[4 * (itj % 2):4 * (itj % 2) + 4]
                        h_r, h_i = hre[itj % 2], him[itj % 2]
                        G(lambda: nc.gpsimd.tensor_tensor(out=dm[0][:], in0=G_r[:], in1=Ec[:, cs], op=ALU.mult), r=[G_r.name, "Ec"], w=[dm[0].name])
                        G(lambda: nc.gpsimd.tensor_tensor(out=dm[1][:], in0=G_i[:], in1=Es[:, cs], op=ALU.mult), r=[G_i.name, "Es"], w=[dm[1].name])
                        G(lambda: nc.gpsimd.tensor_tensor(out=dm[2][:], in0=G_r[:], in1=Es[:, cs], op=ALU.mult), r=[G_r.name, "Es"], w=[dm[2].name])
                        G(lambda: nc.gpsimd.tensor_tensor(out=dm[3][:], in0=G_i[:], in1=Ec[:, cs], op=ALU.mult), r=[G_i.name, "Ec"], w=[dm[3].name])
                        G(lambda: nc.gpsimd.tensor_tensor(out=h_r[:], in0=dm[0][:], in1=dm[1][:], op=ALU.subtract), r=[dm[0].name, dm[1].name], w=[h_r.name])
                        G(lambda: nc.gpsimd.tensor_tensor(out=h_i[:], in0=dm[2][:], in1=dm[3][:], op=ALU.add), r=[dm[2].name, dm[3].name], w=[h_i.name])
                        if c == NCH - 1:
                            G(lambda: nc.gpsimd.tensor_tensor(out=fin[:, ft, b, 0:1], in0=dm[0][:, 511:512], in1=dm[1][:, 511:512], op=ALU.subtract),
                              r=[dm[0].name, dm[1].name], w=["fin"])
                            G(lambda: nc.gpsimd.tensor_tensor(out=fin[:, ft, b, 1:2], in0=dm[2][:, 511:512], in1=dm[3][:, 511:512], op=ALU.add),
                              r=[dm[2].name, dm[3].name], w=["fin"])
                        py = p_y[itj % 2]
                        PE(lambda: nc.tensor.matmul(py[:, :], lhsT=CT[:, ft, 0, :], rhs=h_r[:], start=True, stop=False), r=[CT.name, h_r.name], w=[py.name])
                        PE(lambda: nc.tensor.matmul(py[:, :], lhsT=CT[:, ft, 1, :], rhs=h_i[:], start=False, stop=True), r=[CT.name, h_i.name], w=[py.name], skip_same=True)
                        if q == 0:
                            A(lambda: nc.scalar.copy(out=yT[:, b, cs], in_=py[:, :]), r=[py.name], w=["yT"])
                        else:
                            V(lambda: nc.vector.tensor_tensor(out=yT[:, b, cs], in0=py[:, :], in1=yT[:, b, cs], op=ALU.add), r=[py.name, "yT"], w=["yT"])

                    stage_A(0)
                    for j in range(len(chunks)):
                        if j + 1 < len(chunks):
                            stage_A(j + 1)
                        stage_B(j)
                    it += len(chunks)
                    pr, pi = p_br[it % 2], p_bi[it % 2]
                    PE(lambda pr=pr, ft=ft: nc.tensor.matmul(pr[:, 0:NS], lhsT=BT[:, ft, 0, :], rhs=ubs[:, :], start=True, stop=True),
                       r=[BT.name, "ubs"], w=[pr.name])
                    PE(lambda pi=pi, ft=ft: nc.tensor.matmul(pi[:, 0:NS], lhsT=BT[:, ft, 1, :], rhs=ubs[:, :], start=True, stop=True),
                       r=[BT.name, "ubs"], w=[pi.name])
                    lr, li = lam_re[:, ft:ft + 1], lam_im[:, ft:ft + 1]
                    V(lambda ft=ft, li=li: nc.vector.tensor_scalar(out=tm[0][:, 0:NS], in0=h0i[:, ft, :], scalar1=li, scalar2=None, op0=ALU.mult),
                      r=[h0i.name, lam_im.name], w=[tm[0].name])
                    V(lambda ft=ft, lr=lr: nc.vector.scalar_tensor_tensor(out=tm[1][:, 0:NS], in0=h0r[:, ft, :], scalar=lr, in1=tm[0][:, 0:NS], op0=ALU.mult, op1=ALU.subtract),
                      r=[h0r.name, lam_re.name, tm[0].name], w=[tm[1].name])
                    V(lambda ft=ft, pr=pr: nc.vector.tensor_tensor(out=h1r[:, ft, :], in0=pr[:, 0:NS], in1=tm[1][:, 0:NS], op=ALU.add), r=[pr.name, tm[1].name], w=[h1r.name])
                    V(lambda ft=ft, li=li: nc.vector.tensor_scalar(out=tm[2][:, 0:NS], in0=h0r[:, ft, :], scalar1=li, scalar2=None, op0=ALU.mult),
                      r=[h0r.name, lam_im.name], w=[tm[2].name])
                    V(lambda ft=ft, lr=lr: nc.vector.scalar_tensor_tensor(out=tm[3][:, 0:NS], in0=h0i[:, ft, :], scalar=lr, in1=tm[2][:, 0:NS], op0=ALU.mult, op1=ALU.add),
                      r=[h0i.name, lam_re.name, tm[2].name], w=[tm[3].name])
                    V(lambda ft=ft, pi=pi: nc.vector.tensor_tensor(out=h1i[:, ft, :], in0=pi[:, 0:NS], in1=tm[3][:, 0:NS], op=ALU.add), r=[pi.name, tm[3].name], w=[h1i.name])
                    h_r, h_i = hre[it % 2], him[it % 2]
                    V(lambda h_r=h_r, ft=ft: nc.vector.tensor_copy(out=h_r[:, 0:NS], in_=h1r[:, ft, :]), r=[h1r.name], w=[h_r.name])
                    V(lambda h_i=h_i, ft=ft: nc.vector.tensor_copy(out=h_i[:, 0:NS], in_=h1i[:, ft, :]), r=[h1i.name], w=[h_i.name])
                    py = p_y[it % 2]
                    PE(lambda py=py, h_r=h_r, ft=ft, qs=qs: nc.tensor.matmul(py[:, 0:NS], lhsT=CT[:, ft, 0, :], rhs=h_r[:, 0:NS], start=True, stop=False),
                       r=[CT.name, h_r.name], w=[py.name])
                    PE(lambda py=py, h_i=h_i, ft=ft, qs=qs: nc.tensor.matmul(py[:, 0:NS], lhsT=CT[:, ft, 1, :], rhs=h_i[:, 0:NS], start=False, stop=True),
                       r=[CT.name, h_i.name], w=[py.name], skip_same=True)
                    if q == 0:
                        A(lambda py=py: nc.scalar.copy(out=yTs[:, :], in_=py[:, 0:NS]), r=[py.name], w=["yTs"])
                    else:
                        V(lambda py=py: nc.vector.tensor_tensor(out=yTs[:, :], in0=py[:, 0:NS], in1=yTs[:, :], op=ALU.add), r=[py.name, "yTs"], w=["yTs"])
                    it += 1
                dsc = dcol[:, ct:ct + 1]

                def gelu_block(ysrc, usrc, n, dst_dram, rkeys, wkey):
                    a0, a1, a2 = gl[0][:, 0:n], gl[1][:, 0:n], gl[2][:, 0:n]
                    V(lambda: nc.vector.scalar_tensor_tensor(out=a0, in0=usrc, scalar=dsc, in1=ysrc, op0=ALU.mult, op1=ALU.add),
                      r=rkeys + [dcol.name], w=[gl[0].name])
                    A(lambda: nc.scalar.activation(out=a1, in_=a0, func=AF.Square), r=[gl[0].name], w=[gl[1].name])
                    V(lambda: nc.vector.tensor_scalar(out=a1, in0=a1, scalar1=0.044715, scalar2=1.0, op0=ALU.mult, op1=ALU.add), r=[gl[1].name], w=[gl[1].name])
                    G(lambda: nc.gpsimd.tensor_tensor(out=a2, in0=a1, in1=a0, op=ALU.mult), r=[gl[0].name, gl[1].name], w=[gl[2].name])
                    A(lambda: nc.scalar.activation(out=a2, in_=a2, func=AF.Sigmoid, scale=1.5957691216057308), r=[gl[2].name], w=[gl[2].name])
                    V(lambda: nc.vector.tensor_tensor(out=gyb[:, 0:n], in0=a0, in1=a2, op=ALU.mult), r=[gl[0].name, gl[2].name], w=["gyb"])
                    DMA(dst_dram, gyb[:, 0:n], r=["gyb"], w=[wkey])

                for b in range(NB):
                    for c in range(NCH):
                        cs = slice(c * 512, (c + 1) * 512)
                        t0 = b * SEQ + c * 512
                        us_ = ufst[(b * NCH + c) % 2]
                        DMA(us_[:], uT_scr[ct * 128:(ct + 1) * 128, t0:t0 + 512], r=[("uT", t0 // 128 + i_) for i_ in range(4)], w=[us_.name])
                        gelu_block(yT[:, b, cs], us_[:], 512, gyT_scr[ct * 128:(ct + 1) * 128, t0:t0 + 512], ["yT", us_.name], ("gyT", ct, b, c))
                gelu_block(yTs[:], ufs[:], NS, gyT_scr[ct * 128:(ct + 1) * 128, samp0:samp0 + NS], ["yTs", "ufs"], ("gyT", ct, "s"))
            with nc.allow_non_contiguous_dma(reason="state out"):
                for b in range(NB):
                    DMA(re_p[b:b + 1, :].rearrange("o (ft f) -> f (o ft)", f=128), fin[:, :, b, 0], r=["fin"], q="pool", is_output=True)
                    DMA(im_p[b:b + 1, :].rearrange("o (ft f) -> f (o ft)", f=128), fin[:, :, b, 1], r=["fin"], q="pool", is_output=True)
            for (src, dst) in ((h1r, re_s), (h1i, im_s)):
                for half in range(4):
                    for j in range(4):
                        ft = half * 4 + j
                        PE(lambda src=src, ft=ft, j=j: nc.tensor.transpose(out=p_s[0:NS, j * 128:(j + 1) * 128], in_=src[:, ft, :], identity=identf[:]),
                           r=[src.name, "identf"], w=[p_s.name])
                    V(lambda half=half: nc.vector.tensor_copy(out=sst[:, half * 512:(half + 1) * 512], in_=p_s[0:NS, :]), r=[p_s.name], w=[sst.name])
                DMA(dst[:, :], sst[:], r=[sst.name], q="pool", is_output=True)
            S.barrier()

    NIT = 18
    if "p3" in phases:
        assert NB == 2
        with contextlib.ExitStack() as e3:
            TOPK = cfg.TOPK
            kTa = [sb(f"kTa{b}", [65, 2, SEQ], BF16, e3) for b in range(NB)]
            kiT = [sb(f"kiT{b}", [64, SEQ], BF16, e3) for b in range(NB)]
            V1 = [sb(f"V1_{b}", [128, NT, 2, 65], BF16, e3) for b in range(NB)]
            Irep = sb("Irep", [128, 4, 128], BF16, e3)
            pw2 = sb("pw2", [128, NIT + 1], F32, e3)
            kmaxb = sb("kmaxb", [128, 1], F32, e3)
            km1 = sb("km1", [128, 1], F32, e3)
            km2 = sb("km2", [1, 128], F32, e3)
            zrow = sb("zrow", [1, 512], BF16, e3)
            qTa = [[sb(f"qTa{b}_{i}", [65, 8, 128], BF16, e3) for i in range(2)] for b in range(NB)]
            qiT = [[sb(f"qiT{b}_{i}", [64, 8, 128], BF16, e3) for i in range(2)] for b in range(NB)]
            score2 = [[sb(f"score{j}_{b}", [128, SEQ], F32, e3) for b in range(NB)] for j in range(2)]
            eps1 = sb("eps1", [128, 1], F32, e3)
            stph = sb("stph", [128, NIT + 1], F32, e3)
            G(lambda: nc.gpsimd.memset(eps1[:], 1.0), w=["eps1"])
            NTC = 4
            tmpc = [sb(f"tmpc{i}", [128, 512], F32, e3) for i in range(NTC)]
            junkb = [sb("junkb0", [128, SEQ], mybir.dt.uint8, e3), sb("junkb1", [128, SEQ], BF16, e3)]
            mb = [sb(f"mb{b}", [128, SEQ], BF16, e3) for b in range(NB)]
            pT = [sb(f"pT{i}", [128, 512], BF16, e3) for i in range(4)]
            ao = [sb(f"ao{b}", [128, 8, 64], BF16, e3) for b in range(NB)]
            amax = [sb(f"amax{b}", [128, 1], F32, e3) for b in range(NB)]
            stp = [sb(f"stp{b}", [128, NIT + 1], F32, e3) for b in range(NB)]
            mid = [sb(f"mid{b}", [128, 1], F32, e3) for b in range(NB)]
            cnt = [sb(f"cnt{b}", [128, 1], F32, e3) for b in range(NB)]
            c2 = [sb(f"c2{b}", [128, 1], F32, e3) for b in range(NB)]
            thr = [sb(f"thr{b}", [128, 1], F32, e3) for b in range(NB)]
            rec = [sb(f"rec{b}", [128, 4], F32, e3) for b in range(NB)]
            p_ix = [ps(f"p_ix{i}", [128, 512], F32, e3) for i in range(2)]
            p_st = [ps(f"p_st{i}", [128, 512], F32, e3) for i in range(3)]
            p_o = [ps(f"p_o{j}", [128, 512], F32, e3) for j in range(2)]
            p_n = ps("p_n", [128, 4, 65], F32, e3)
            oT = [sb(f"oT{b}", [65, 2, 512], F32, e3) for b in range(NB)]
            p_m = p_ix[0]

            for k in range(NIT + 1):
                G(lambda k=k: nc.gpsimd.memset(pw2[:, k:k + 1], float(2.0 ** (-k))), w=["pw2"])
            G(lambda: nc.gpsimd.memset(zrow[:], 0.0), w=["zrow"])
            for j in range(4):
                V(lambda j=j: nc.vector.tensor_copy(out=Irep[:, j, :], in_=ident[:]), r=["ident"], w=["Irep"])
            V(lambda: nc.vector.tensor_reduce(out=km1[:], in_=kn2[:].rearrange("p a b -> p (a b)"), axis=AX.X, op=ALU.max), r=["kn2"], w=["km1"])
            PE(lambda: nc.tensor.transpose(out=p_m[0:1, 0:128], in_=km1[:], identity=identf[:]), r=["km1", "identf"], w=[p_m.name])
            V(lambda: nc.vector.tensor_reduce(out=km2[0:1, 0:1], in_=p_m[0:1, 0:128], axis=AX.X, op=ALU.max), r=[p_m.name], w=["km2"])
            A(lambda: nc.scalar.activation(out=km2[0:1, 0:1], in_=km2[0:1, 0:1], func=AF.Sqrt), r=["km2"], w=["km2"])
            PE(lambda: nc.tensor.matmul(p_m[:, 0:1], lhsT=ones_f[0:1, :], rhs=km2[0:1, 0:1], start=True, stop=True), r=["ones_f", "km2"], w=[p_m.name])
            V(lambda: nc.vector.tensor_copy(out=kmaxb[:], in_=p_m[:, 0:1]), r=[p_m.name], w=["kmaxb"])

            for b in range(NB):
                t0 = b * SEQ
                for g in range(2):
                    DMA(kTa[b][0:64, g, :], kT_scr[:, g, t0:t0 + SEQ], r=[("kT", b * NT + i_) for i_ in range(NT)], w=[kTa[b].name])
                V(lambda b=b: nc.vector.memset(kTa[b][64:65, :, :], 1.0), w=[kTa[b].name])
                DMA(kiT[b][:, :], kiT_scr[:, t0:t0 + SEQ], r=[("kiT", b * NT + i_) for i_ in range(NT)], w=[kiT[b].name])
                for g in range(2):
                    DMA(V1[b][:, :, g, 0:64], vb_scr[t0:t0 + SEQ, g, :].rearrange("(kb p) d -> p kb d", p=128),
                        r=[("vb", b * NT + i_) for i_ in range(NT)], w=[V1[b].name])
                V(lambda b=b: nc.vector.memset(V1[b][:, :, :, 64:65], 1.0), w=[V1[b].name])

            itc3 = [0, 0]
            thrc = [sb(f"thrc{j}", [128, 1], F32, e3) for j in range(2)]

            def slot_bufs(i):
                return [qTa[b][i % 2] for b in range(NB)], [qiT[b][i % 2] for b in range(NB)], [score2[i % 2][b] for b in range(NB)]

            def idx_stage(i, filler=None):
                Sk = (i + 1) * 128
                search = Sk > TOPK
                Ts = [b * NT + i for b in range(NB)]
                qa_, qi_, sc2 = slot_bufs(i)
                for b in range(NB):
                    DMA(qa_[b][:], qT_scr[Ts[b], :, :, :], r=[("qT", Ts[b])], w=[qa_[b].name])
                    DMA(qi_[b][:], qiT_scr[Ts[b], :, :, :], r=[("qiT", Ts[b])], w=[qi_[b].name])
                    V(lambda b=b: nc.vector.tensor_scalar(out=qa_[b][64:65, :, :], in0=qa_[b][64:65, :, :], scalar1=kmaxb[64:65, 0:1], scalar2=None, op0=ALU.mult),
                      r=[qa_[b].name, "kmaxb"], w=[qa_[b].name])
                for b in range(NB):
                    sc_ = sc2[b]
                    for c0 in range(0, Sk, 512):
                        cw = min(512, Sk - c0)
                        for h in range(8):
                            it = itc3[0]
                            px = p_ix[it % 2]
                            PE(lambda px=px, b=b, h=h, c0=c0, cw=cw: nc.tensor.matmul(px[:, 0:cw], lhsT=qi_[b][:, h, :], rhs=kiT[b][:, c0:c0 + cw], start=True, stop=True),
                               r=[qi_[b].name, kiT[b].name], w=[px.name])
                            lo_ = lohi[:, Ts[b], h:h + 1]
                            hi_ = lohi[:, Ts[b], 8 + h:9 + h]
                            if h == 0:
                                V(lambda px=px, sc_=sc_, c0=c0, cw=cw, lo_=lo_, hi_=hi_: nc.vector.tensor_scalar(out=sc_[:, c0:c0 + cw], in0=px[:, 0:cw], scalar1=lo_, scalar2=hi_,
                                                                                                              op0=ALU.max, op1=ALU.min), r=[px.name, "lohi"], w=[sc_.name])
                            else:
                                tc_ = tmpc[it % NTC]
                                V(lambda px=px, tc_=tc_, cw=cw, lo_=lo_, hi_=hi_: nc.vector.tensor_scalar(out=tc_[:, 0:cw], in0=px[:, 0:cw], scalar1=lo_, scalar2=hi_,
                                                                                                       op0=ALU.max, op1=ALU.min), r=[px.name, "lohi"], w=[tc_.name])
                                G(lambda tc_=tc_, sc_=sc_, c0=c0, cw=cw: nc.gpsimd.tensor_tensor(out=sc_[:, c0:c0 + cw], in0=sc_[:, c0:c0 + cw], in1=tc_[:, 0:cw], op=ALU.add),
                                  r=[tc_.name, sc_.name], w=[sc_.name])
                            itc3[0] += 1
                        if filler is not None:
                            filler()
                    if search:
                        G(lambda b=b, sc_=sc_: nc.gpsimd.tensor_reduce(out=amax[b][:], in_=sc_[:, 0:Sk], axis=AX.X, op=ALU.max, apply_absolute_value=True),
                          r=[sc_.name], w=[amax[b].name]) if False else \
                        V(lambda b=b, sc_=sc_: nc.vector.tensor_reduce(out=amax[b][:], in_=sc_[:, 0:Sk], axis=AX.X, op=ALU.max, apply_absolute_value=True),
                          r=[sc_.name], w=[amax[b].name])
                    G(lambda sc_=sc_: nc.gpsimd.tensor_tensor(out=sc_[:, Sk - 128:Sk], in0=sc_[:, Sk - 128:Sk], in1=cmask[:], op=ALU.add),
                      r=[sc_.name, "cmask"], w=[sc_.name])

            def srch_init(i):
                Sk = (i + 1) * 128
                qa_, qi_, sc2 = slot_bufs(i)
                if Sk > TOPK:
                    V(lambda: nc.vector.tensor_scalar(out=amax[0][:], in0=amax[0][:], scalar1=1.0, scalar2=None, op0=ALU.add), r=[amax[0].name], w=[amax[0].name])
                    V(lambda: nc.vector.tensor_scalar(out=stp[0][:], in0=pw2[:], scalar1=amax[0][:, 0:1], scalar2=None, op0=ALU.mult), r=["pw2", amax[0].name], w=[stp[0].name])
                    V(lambda: nc.vector.memset(mid[0][:], 0.0), w=[mid[0].name])
                    A(lambda: nc.scalar.activation(out=amax[1][:], in_=amax[1][:], func=AF.Identity, bias=eps1[:, 0:1], scale=1.0), r=[amax[1].name, "eps1"], w=[amax[1].name])
                    A(lambda: nc.scalar.activation(out=stp[1][:], in_=pw2[:], func=AF.Identity, scale=amax[1][:, 0:1]), r=["pw2", amax[1].name], w=[stp[1].name])
                    A(lambda: nc.scalar.activation(out=stph[:], in_=stp[1][:], func=AF.Identity, scale=-0.5), r=[stp[1].name], w=["stph"])
                    A(lambda: nc.scalar.activation(out=mid[1][:], in_=zer_f[:, 0:1], func=AF.Identity), r=["zer_f"], w=[mid[1].name])
                    tcst = thrc[i % 2]
                    V(lambda tcst=tcst: nc.vector.memset(tcst[:], -(float(2 * TOPK - Sk) - 0.5)), w=[tcst.name])

            def srch_iter(i, k):
                Sk = (i + 1) * 128
                qa_, qi_, sc2 = slot_bufs(i)
                tcst = thrc[i % 2]
                V(lambda: nc.vector.tensor_scalar(out=junkb[0][:, 0:Sk], in0=sc2[0][:, 0:Sk], scalar1=mid[0][:, 0:1], scalar2=None, op0=ALU.is_ge,
                                                  op1=ALU.add, accum_out=cnt[0][:]), r=[sc2[0].name, mid[0].name], w=[junkb[0].name, cnt[0].name])
                A(lambda: nc.scalar.activation(out=junkb[1][:, 0:Sk], in_=sc2[1][:, 0:Sk], func=AF.Sign, bias=mid[1][:, 0:1], scale=1.0, accum_out=cnt[1][:]),
                  r=[sc2[1].name, mid[1].name], w=[junkb[1].name, cnt[1].name])
                V(lambda: nc.vector.tensor_scalar(out=c2[0][:], in0=cnt[0][:], scalar1=float(TOPK), scalar2=-0.5, op0=ALU.is_ge, op1=ALU.add),
                  r=[cnt[0].name], w=[c2[0].name])
                V(lambda: nc.vector.scalar_tensor_tensor(out=mid[0][:], in0=c2[0][:], scalar=stp[0][:, k:k + 1], in1=mid[0][:], op0=ALU.mult, op1=ALU.add),
                  r=[c2[0].name, stp[0].name, mid[0].name], w=[mid[0].name])
                A(lambda: nc.scalar.activation(out=c2[1][:], in_=cnt[1][:], func=AF.Sign, bias=tcst[:, 0:1], scale=1.0), r=[cnt[1].name, tcst.name], w=[c2[1].name])
                A(lambda: nc.scalar.activation(out=mid[1][:], in_=c2[1][:], func=AF.Identity, scale=stph[:, k:k + 1], bias=mid[1][:, 0:1]),
                  r=[c2[1].name, "stph", mid[1].name], w=[mid[1].name])

            def srch_fin(i):
                Sk = (i + 1) * 128
                Ts = [b * NT + i for b in range(NB)]
                qa_, qi_, sc2 = slot_bufs(i)
                if Sk > TOPK:
                    V(lambda: nc.vector.tensor_tensor(out=thr[0][:], in0=mid[0][:], in1=stp[0][:, NIT:NIT + 1], op=ALU.subtract), r=[mid[0].name, stp[0].name], w=[thr[0].name])
                    V(lambda: nc.vector.scalar_tensor_tensor(out=thr[1][:], in0=mid[1][:], scalar=-1.0, in1=stp[1][:, NIT:NIT + 1], op0=ALU.mult, op1=ALU.subtract),
                      r=[mid[1].name, stp[1].name], w=[thr[1].name])
                else:
                    for b in range(NB):
                        V(lambda b=b: nc.vector.memset(thr[b][:], -1.0e29), w=[thr[b].name])
                for b in range(NB):
                    V(lambda b=b: nc.vector.tensor_scalar(out=mb[b][:, 0:Sk], in0=sc2[b][:, 0:Sk], scalar1=thr[b][:, 0:1], scalar2=BIGM, op0=ALU.is_lt, op1=ALU.mult),
                      r=[sc2[b].name, thr[b].name], w=[mb[b].name])
                    if debug and Ts[b] == NT - 1:
                        DMA(dbg_mb[:, 0:Sk], mb[b][:, 0:Sk], r=[mb[b].name], w=["dbg_mb"])
                        DMA(dbg_sc[:, 0:Sk], sc2[b][:, 0:Sk], r=[sc2[b].name], w=["dbg_sc"])
                        DMA(dbg_thr[:, :], thr[b][:], r=[thr[b].name], w=["dbg_thr"])

            def att_stage(i, b, g):
                qa_, qi_, sc2 = slot_bufs(i)
                po = p_o[(2 * b + g) % 2]
                base = itc3[1]

                def qk(kb):
                    ia = base + kb
                    pst = p_st[ia % 3]
                    pt_ = pT[ia % 4]
                    ks = slice(kb * 128, (kb + 1) * 128)
                    PE(lambda: nc.tensor.matmul(pst[:], lhsT=kTa[b][:, g, ks], rhs=qa_[b][:, 4 * g:4 * g + 4, :].rearrange("p a b -> p (a b)"),
                                                start=True, stop=False), r=[kTa[b].name, qa_[b].name], w=[pst.name])
                    PE(lambda: nc.tensor.matmul(pst[:], lhsT=mb[b][:, ks], rhs=Irep[:].rearrange("p a b -> p (a b)"), start=False, stop=True),
                       r=[mb[b].name, "Irep"], w=[pst.name], skip_same=True)
                    A(lambda: nc.scalar.activation(out=pt_[:], in_=pst[:], func=AF.Exp, scale=0.125), r=[pst.name], w=[pt_.name])

                def pv(kb):
                    ia = base + kb
                    pt_ = pT[ia % 4]
                    PE(lambda: nc.tensor.matmul(po[0:65, :], lhsT=V1[b][:, kb, g, :], rhs=pt_[:], start=(kb == 0), stop=(kb == i)),
                       r=[pt_.name, V1[b].name], w=[po.name], skip_same=True)

                qk(0)
                if i >= 1:
                    qk(1)
                for kb in range(i + 1):
                    if kb + 2 <= i:
                        qk(kb + 2)
                    pv(kb)
                itc3[1] += i + 1
                A(lambda po=po: nc.scalar.copy(out=oT[b][0:65, g, :], in_=po[0:65, :]), r=[po.name], w=[oT[b].name])

            def norm_stage(i):
                Ts = [b * NT + i for b in range(NB)]
                for b in range(NB):
                    for g in range(2):
                        for h4 in range(4):
                            PE(lambda b=b, g=g, h4=h4: nc.tensor.transpose(out=p_n[:, h4, :], in_=oT[b][0:65, g, h4 * 128:(h4 + 1) * 128], identity=identf[0:65, 0:65]),
                               r=[oT[b].name, "identf"], w=["p_n"], skip_same=True)
                        V(lambda b=b: nc.vector.reciprocal(out=rec[b][:], in_=p_n[:, :, 64]), r=["p_n"], w=[rec[b].name])
                        V(lambda b=b, g=g: nc.vector.tensor_tensor(out=ao[b][:, 4 * g:4 * g + 4, :], in0=p_n[:, :, 0:64], in1=rec[b][:].unsqueeze(2).to_broadcast([128, 4, 64]),
                                                                   op=ALU.mult), r=["p_n", rec[b].name], w=[ao[b].name])
                    DMA(ao_scr[Ts[b] * 128:(Ts[b] + 1) * 128, :], ao[b][:].rearrange("p a b -> p (a b)"), r=[ao[b].name], w=[("ao", Ts[b])])

            idx_stage(0)
            for i in range(NT):
                Sk_i = (i + 1) * 128
                n_it = NIT if Sk_i > TOPK else 0
                srch_init(i)
                kdone = [0]
                if i + 1 < NT:
                    nslots = NB * (((i + 2) * 128 + 511) // 512)
                    per = -(-n_it // nslots) if n_it else 0

                    def filler(i=i, per=per, kdone=kdone, n_it=n_it):
                        for _ in range(per):
                            if kdone[0] < n_it:
                                srch_iter(i, kdone[0])
                                kdone[0] += 1
                    idx_stage(i + 1, filler)
                while kdone[0] < n_it:
                    srch_iter(i, kdone[0])
                    kdone[0] += 1
                srch_fin(i)
                if i >= 1:
                    norm_stage(i - 1)
                for b in range(NB):
                    for g in range(2):
                        att_stage(i, b, g)
            norm_stage(NT - 1)
            S.barrier()

    if "p3s" in phases:
        NIT = 22
        with contextlib.ExitStack() as e4:
            NPG = cfg.NPG
            assert NPG == 64 and NS % 2 == 0
            TOPK_S = cfg.TOPK_S
            TS = NTT - 1
            samp0 = TS * 128
            NO = 129
            lohi_scr = dscr("lohi_scr", [NS, 16], F32)
            DMA(lohi_scr[:, :], lohi[0:NS, TS, :], r=["lohi"], w=["lohi_scr"])
            lohiB = sb("lohiB", [128, NS, 16], F32, e4)
            DMA(lohiB[:].rearrange("p a b -> p (a b)"), lohi_scr.rearrange("(o a) b -> o (a b)", o=1).partition_broadcast(128),
                r=["lohi_scr"], w=["lohiB"])
            qTs = sb("qTs", [65, 8, 128], BF16, e4)
            qiTs = sb("qiTs", [64, 8, 128], BF16, e4)
            DMA(qTs[:], qT_scr[TS, :, :, :], r=[("qT", TS)], w=["qTs"])
            DMA(qiTs[:], qiT_scr[TS, :, :, :], r=[("qiT", TS)], w=["qiTs"])
            idx = sb("idx", [128, 1], I32, e4)
            idxf = sb("idxf", [128, 1], F32, e4)
            idxa = sb("idxa", [128, 1], I32, e4)
            idxb = sb("idxb", [128, 1], I32, e4)
            KI = sb("KI", [128, 128, 64], F32, e4)
            KIx = sb("KIx", [128, 64], F32, e4)
            Kh = [sb(f"Kh{i}", [128, 64, 128], F32, e4) for i in range(2)]
            Kx = sb("Kx", [128, 128], F32, e4)
            Vh = sb("Vh", [128, 64, 128], F32, e4)
            Vx = sb("Vx", [128, 128], F32, e4)
            V1h = sb("V1h", [128, 64, 2, 65], BF16, e4)
            V1x = sb("V1x", [128, 2, 65], BF16, e4)
            kT4 = [sb(f"kT4_{i}", [65, 4, 128], BF16, e4) for i in range(2)]
            qis = sb("qis", [64, 2, 8], BF16, e4)
            qas = sb("qas", [65, 2, 2, 4], BF16, e4)
            sc = sb("sc_s", [128, NO], F32, e4)
            cl = sb("cl_s", [128, 32, 16], F32, e4)
            red = sb("red_s", [128, 32, 2], F32, e4)
            colmask = sb("colmask", [128, 1], F32, e4)
            cross = sb("cross", [128, 16], F32, e4)
            maskfull = sb("maskfull", [128, NO, 16], F32, e4)
            blk1 = sb("blk1", [128, 128], F32, e4)
            pw2s = sb("pw2s", [128, NIT + 1], F32, e4)
            am1 = sb("am1", [128, 1], F32, e4)
            am2 = sb("am2", [1, 128], F32, e4)
            amb = sb("amb", [128, 1], F32, e4)
            stps = sb("stps", [128, NIT + 1], F32, e4)
            mids = sb("mids", [128, 1], F32, e4)
            cnts = sb("cnts", [128, 1], F32, e4)
            c2s = sb("c2s", [128, 1], F32, e4)
            thrs = sb("thrs", [128, 1], F32, e4)
            junks = sb("junks", [128, NO], F32, e4)
            sqk = Vh
            n2 = sb("n2", [128, 258], F32, e4)
            kmb = sb("kmb", [128, 1], F32, e4)
            tmps = sb("tmps", [128, 32, 16], F32, e4)
            pTs = [sb(f"pTs{i}", [128, 32, 16], BF16, e4) for i in range(2)]
            zrow_s = sb("zrow_s", [1, 512], BF16, e4)
            o8 = sb("o8", [8, 2, 64], BF16, e4)
            rec8 = sb("rec8", [8, 2], F32, e4)
            p_t = [ps(f"p3s_t{i}", [64, 4, 128], F32, e4) for i in range(2)]
            p_sx = [ps(f"p3s_x{i}", [128, 32, 16], F32, e4) for i in range(2)]
            p_os = ps("p3s_o", [8, 2, 65], F32, e4)
            p_ms = ps("p3s_m", [128, 512], F32, e4)

            for k in range(NIT + 1):
                G(lambda k=k: nc.gpsimd.memset(pw2s[:, k:k + 1], float(2.0 ** (-k))), w=["pw2s"])
            G(lambda: nc.gpsimd.memset(zrow_s[:], 0.0), w=["zrow_s"])
            V(lambda: nc.vector.memset(colmask[:], NEG), w=["colmask"])
            V(lambda: nc.vector.memset(colmask[0:1, :], 0.0), w=["colmask"])
            V(lambda: nc.vector.memset(colmask[64:65, :], 0.0), w=["colmask"])
            V(lambda: nc.vector.memset(cross[:], 0.0), w=["cross"])
            crv = cross[:].rearrange("p (g b h) -> p g b h", g=2, b=2)
            V(lambda: nc.vector.memset(crv[0:64, :, 1, :], BIGM), w=["cross"])
            V(lambda: nc.vector.memset(crv[64:128, :, 0, :], BIGM), w=["cross"])
            V(lambda: nc.vector.memset(blk1[:], 0.0), w=["blk1"])
            V(lambda: nc.vector.memset(blk1[0:64, 0:64], 1.0), w=["blk1"])
            V(lambda: nc.vector.memset(blk1[64:128, 64:128], 1.0), w=["blk1"])
            for t_ in kT4:
                V(lambda t_=t_: nc.vector.memset(t_[64:65, :, :], 1.0), w=[t_.name])
            V(lambda: nc.vector.memset(V1h[:, :, :, 64:65], 1.0), w=["V1h"])
            V(lambda: nc.vector.memset(V1x[:, :, 64:65], 1.0), w=["V1x"])

            def gather(dst2d, src2d, idx_t, wkey):
                S.dma("pool", lambda: nc.gpsimd.indirect_dma_start(out=dst2d, out_offset=None, in_=src2d,
                                                                   in_offset=bass.IndirectOffsetOnAxis(ap=idx_t[:, :], axis=0)),
                      reads=[idx_t.name], writes=[wkey])

            cache_kh = cache_k.rearrange("n (h e) -> (n h) e", h=2)
            cache_vh = cache_v.rearrange("n (h e) -> (n h) e", h=2)
            itc = [0]

            def transposed_units(units, consume):
                for u0 in range(0, len(units), 4):
                    grp = units[u0:u0 + 4]
                    pt_ = p_t[itc[0] % 2]
                    kt = kT4[itc[0] % 2]
                    for s_, (src, rk) in enumerate(grp):
                        PE(lambda pt_=pt_, s_=s_, src=src: nc.tensor.transpose(out=pt_[:, s_, :], in_=src, identity=identf[:]),
                           r=list(rk) + ["identf"], w=[pt_.name], skip_same=True)
                    n_ = len(grp)
                    if itc[0] % 2 == 0:
                        V(lambda pt_=pt_, kt=kt, n_=n_: nc.vector.tensor_copy(out=kt[0:64, 0:n_, :], in_=pt_[:, 0:n_, :]), r=[pt_.name], w=[kt.name])
                    else:
                        A(lambda pt_=pt_, kt=kt, n_=n_: nc.scalar.copy(out=kt[0:64, 0:n_, :], in_=pt_[:, 0:n_, :]), r=[pt_.name], w=[kt.name])
                    for s_ in range(n_):
                        consume(kt, s_, u0 + s_)
                    itc[0] += 1

            DMA(idx[:], ptab[0:2 * NPG, :], w=["idx"])
            gather(KI[:].rearrange("p a b -> p (a b)"), cache_ik[:, :], idx, "KI")
            for pr in range(NS // 2):
                b0 = 2 * pr
                V(lambda: nc.vector.tensor_copy(out=idxf[:], in_=idx[:]), r=["idx"], w=["idxf"])
                V(lambda: nc.vector.tensor_scalar(out=idxa[:], in0=idxf[:], scalar1=2.0, scalar2=None, op0=ALU.mult), r=["idxf"], w=["idxa"])
                V(lambda: nc.vector.tensor_scalar(out=idxb[:], in0=idxf[:], scalar1=2.0, scalar2=1.0, op0=ALU.mult, op1=ALU.add), r=["idxf"], w=["idxb"])
                gather(Kh[0][:].rearrange("p a b -> p (a b)"), cache_kh[:, :], idxa, "Kh0")
                gather(Kh[1][:].rearrange("p a b -> p (a b)"), cache_kh[:, :], idxb, "Kh1")
                for (xt_, src_) in ((KIx, ki_s), (Kx, k_s), (Vx, v_s)):
                    V(lambda xt_=xt_: nc.vector.memset(xt_[:], 0.0), w=[xt_.name])
                    DMA(xt_[0:1, :], src_[b0:b0 + 1, :], w=[xt_.name])
                    DMA(xt_[64:65, :], src_[b0 + 1:b0 + 2, :], w=[xt_.name])
                V(lambda b0=b0: nc.vector.tensor_copy(out=qis[:], in_=qiTs[:, :, b0:b0 + 2].rearrange("p h t -> p t h")), r=["qiTs"], w=["qis"])
                for g in range(2):
                    V(lambda b0=b0, g=g: nc.vector.tensor_copy(out=qas[:, g, :, :], in_=qTs[:, 4 * g:4 * g + 4, b0:b0 + 2].rearrange("p h t -> p t h")),
                      r=["qTs"], w=["qas"])
                for hf in range(2):
                    G(lambda hf=hf: nc.gpsimd.tensor_tensor(out=sqk[:], in0=Kh[hf][:], in1=Kh[hf][:], op=ALU.mult), r=[f"Kh{hf}"], w=["Vh"])
                    V(lambda hf=hf: nc.vector.tensor_reduce(out=n2[:, hf * 128:(hf + 1) * 128], in_=sqk[:].rearrange("p o (g d) -> p (o g) d", g=2), axis=AX.X, op=ALU.add),
                      r=["Vh"], w=["n2"])
                G(lambda: nc.gpsimd.tensor_tensor(out=sqk[:, 0, :], in0=Kx[:], in1=Kx[:], op=ALU.mult), r=["Kx"], w=["Vh"])
                V(lambda: nc.vector.tensor_reduce(out=n2[:, 256:258], in_=sqk[:, 0, :].rearrange("p (g d) -> p g d", g=2), axis=AX.X, op=ALU.add), r=["Vh"], w=["n2"])
                V(lambda: nc.vector.tensor_reduce(out=am1[:], in_=n2[:], axis=AX.X, op=ALU.max), r=["n2"], w=["am1"])
                PE(lambda: nc.tensor.transpose(out=p_ms[0:1, 0:128], in_=am1[:], identity=identf[:]), r=["am1", "identf"], w=[p_ms.name])
                V(lambda: nc.vector.tensor_reduce(out=am2[0:1, 0:1], in_=p_ms[0:1, 0:128], axis=AX.X, op=ALU.max), r=[p_ms.name], w=["am2"])
                A(lambda: nc.scalar.activation(out=am2[0:1, 0:1], in_=am2[0:1, 0:1], func=AF.Sqrt), r=["am2"], w=["am2"])
                PE(lambda: nc.tensor.matmul(p_ms[:, 0:1], lhsT=ones_f[0:1, :], rhs=am2[0:1, 0:1], start=True, stop=True), r=["ones_f", "am2"], w=[p_ms.name])
                V(lambda: nc.vector.tensor_copy(out=kmb[:], in_=p_ms[:, 0:1]), r=[p_ms.name], w=["kmb"])
                V(lambda: nc.vector.tensor_scalar(out=qas[64:65, :, :, :], in0=qas[64:65, :, :, :], scalar1=kmb[64:65, 0:1], scalar2=None, op0=ALU.mult),
                  r=["qas", "kmb"], w=["qas"])

                loB = lohiB[:, b0:b0 + 2, 0:8]
                hiB = lohiB[:, b0:b0 + 2, 8:16]

                def idx_group(units, o_lo, n_o):
                    px = p_sx[itc[0] % 2]

                    def consume(kt, s_, ui, px=px):
                        PE(lambda: nc.tensor.matmul(px[:, ui, :], lhsT=kt[0:64, s_, :], rhs=qis[:].rearrange("p a b -> p (a b)"), start=True, stop=True),
                           r=[kt.name, "qis"], w=[px.name], skip_same=True)
                    transposed_units(units, consume)
                    V(lambda: nc.vector.tensor_tensor(out=cl[:, 0:n_o, :].rearrange("p o (b h) -> p o b h", b=2), in0=px[:, 0:n_o, :].rearrange("p o (b h) -> p o b h", b=2),
                                                      in1=loB.unsqueeze(1).to_broadcast([128, n_o, 2, 8]), op=ALU.max), r=[px.name, "lohiB"], w=["cl_s"])
                    V(lambda: nc.vector.tensor_tensor(out=cl[:, 0:n_o, :].rearrange("p o (b h) -> p o b h", b=2), in0=cl[:, 0:n_o, :].rearrange("p o (b h) -> p o b h", b=2),
                                                      in1=hiB.unsqueeze(1).to_broadcast([128, n_o, 2, 8]), op=ALU.min), r=["cl_s", "lohiB"], w=["cl_s"])
                    V(lambda: nc.vector.tensor_reduce(out=red[:, 0:n_o, :], in_=cl[:, 0:n_o, :].rearrange("p o (b h) -> p o b h", b=2), axis=AX.X, op=ALU.add),
                      r=["cl_s"], w=["red_s"])
                    V(lambda: nc.vector.tensor_copy(out=sc[0:64, o_lo:o_lo + n_o], in_=red[0:64, 0:n_o, 0]), r=["red_s"], w=["sc_s"])
                    V(lambda: nc.vector.tensor_copy(out=sc[64:128, o_lo:o_lo + n_o], in_=red[64:128, 0:n_o, 1]), r=["red_s"], w=["sc_s"])

                for og in range(4):
                    idx_group([(KI[:, og * 32 + oo, :], ["KI"]) for oo in range(32)], og * 32, 32)
                idx_group([(KIx[:], ["KIx"])], 128, 1)
                if pr + 1 < NS // 2:
                    DMA(idx[:], ptab[(b0 + 2) * NPG:(b0 + 4) * NPG, :], r=["idxf"], w=["idx"])
                    gather(KI[:].rearrange("p a b -> p (a b)"), cache_ik[:, :], idx, "KI")
                V(lambda: nc.vector.tensor_reduce(out=am1[:], in_=sc[:], axis=AX.X, op=ALU.max, apply_absolute_value=True), r=["sc_s"], w=["am1"])
                V(lambda: nc.vector.tensor_tensor(out=sc[:, 128:129], in0=sc[:, 128:129], in1=colmask[:], op=ALU.add), r=["sc_s", "colmask"], w=["sc_s"])
                PE(lambda: nc.tensor.transpose(out=p_ms[0:1, 0:128], in_=am1[:], identity=identf[:]), r=["am1", "identf"], w=[p_ms.name])
                V(lambda: nc.vector.tensor_reduce(out=am2[0:1, 0:1], in_=p_ms[0:1, 0:128], axis=AX.X, op=ALU.max), r=[p_ms.name], w=["am2"])
                V(lambda: nc.vector.tensor_scalar(out=am2[0:1, 0:1], in0=am2[0:1, 0:1], scalar1=1.0, scalar2=None, op0=ALU.add), r=["am2"], w=["am2"])
                PE(lambda: nc.tensor.matmul(p_ms[:, 0:1], lhsT=ones_f[0:1, :], rhs=am2[0:1, 0:1], start=True, stop=True), r=["ones_f", "am2"], w=[p_ms.name])
                V(lambda: nc.vector.tensor_copy(out=amb[:], in_=p_ms[:, 0:1]), r=[p_ms.name], w=["amb"])
                V(lambda: nc.vector.tensor_scalar(out=stps[:], in0=pw2s[:], scalar1=amb[:, 0:1], scalar2=None, op0=ALU.mult), r=["pw2s", "amb"], w=["stps"])
                V(lambda: nc.vector.memset(mids[:], 0.0), w=["mids"])
                for k in range(NIT):
                    V(lambda: nc.vector.tensor_scalar(out=junks[:], in0=sc[:], scalar1=mids[:, 0:1], scalar2=None, op0=ALU.is_ge, op1=ALU.add, accum_out=cnts[:]),
                      r=["sc_s", "mids"], w=["junks", "cnts"])
                    PE(lambda: nc.tensor.matmul(p_ms[:, 0:1], lhsT=blk1[:], rhs=cnts[:], start=True, stop=True), r=["blk1", "cnts"], w=[p_ms.name])
                    V(lambda: nc.vector.tensor_scalar(out=c2s[:], in0=p_ms[:, 0:1], scalar1=float(TOPK_S), scalar2=-0.5, op0=ALU.is_ge, op1=ALU.add), r=[p_ms.name], w=["c2s"])
                    V(lambda k=k: nc.vector.scalar_tensor_tensor(out=mids[:], in0=c2s[:], scalar=stps[:, k:k + 1], in1=mids[:], op0=ALU.mult, op1=ALU.add),
                      r=["c2s", "stps", "mids"], w=["mids"])
                V(lambda: nc.vector.tensor_tensor(out=thrs[:], in0=mids[:], in1=stps[:, NIT:NIT + 1], op=ALU.subtract), r=["mids", "stps"], w=["thrs"])
                V(lambda: nc.vector.tensor_scalar(out=junks[:], in0=sc[:], scalar1=thrs[:, 0:1], scalar2=BIGM, op0=ALU.is_lt, op1=ALU.mult), r=["sc_s", "thrs"], w=["junks"])
                V(lambda: nc.vector.tensor_tensor(out=maskfull[:], in0=junks[:].unsqueeze(2).to_broadcast([128, NO, 16]),
                                                  in1=cross[:].unsqueeze(1).to_broadcast([128, NO, 16]), op=ALU.add), r=["junks", "cross"], w=["maskfull"])
                PE(lambda: nc.tensor.matmul(p_os[:].rearrange("p a b -> p (a b)"), lhsT=zrow_s[0:1, 0:8], rhs=zrow_s[0:1, 0:130], start=True, stop=False,
                                            skip_group_check=True), r=["zrow_s"], w=["p3s_o"])

                def att_group(kunits, vsrc_fn, o_lo, n_o, last):
                    px = p_sx[itc[0] % 2]
                    pts = pTs[itc[0] % 2]

                    def consume(kt, s_, ui, px=px):
                        ol, g = ui // 2, ui % 2
                        PE(lambda: nc.tensor.matmul(px[:, ol, g * 8:(g + 1) * 8], lhsT=kt[:, s_, :], rhs=qas[:, g, :, :].rearrange("p a b -> p (a b)"), start=True, stop=True),
                           r=[kt.name, "qas"], w=[px.name], skip_same=True)
                    transposed_units(kunits, consume)
                    V(lambda: nc.vector.tensor_tensor(out=tmps[:, 0:n_o, :], in0=px[:, 0:n_o, :], in1=maskfull[:, o_lo:o_lo + n_o, :], op=ALU.add),
                      r=[px.name, "maskfull"], w=["tmps"])
                    A(lambda: nc.scalar.activation(out=pts[:, 0:n_o, :], in_=tmps[:, 0:n_o, :], func=AF.Exp, scale=0.125), r=["tmps"], w=[pts.name])
                    for ol in range(n_o):
                        for g in range(2):
                            vap, vk = vsrc_fn(ol, g)
                            PE(lambda ol=ol, g=g, vap=vap: nc.tensor.matmul(p_os[:, g, :], lhsT=pts[:, ol, g * 8:(g + 1) * 8], rhs=vap, start=False,
                                                                         stop=(last and ol == n_o - 1), skip_group_check=True),
                               r=[pts.name, vk], w=["p3s_o"], skip_same=True)

                for hf in range(2):
                    gather(Vh[:].rearrange("p a b -> p (a b)"), cache_vh[:, :], idxa if hf == 0 else idxb, "Vh")
                    for g in range(2):
                        G(lambda g=g: nc.gpsimd.tensor_copy(out=V1h[:, :, g, 0:64], in_=Vh[:, :, g * 64:(g + 1) * 64]), r=["Vh"], w=["V1h"])
                    for og in range(2):
                        units = []
                        for oo in range(32):
                            for g in range(2):
                                units.append((Kh[hf][:, og * 32 + oo, g * 64:(g + 1) * 64], [f"Kh{hf}"]))
                        att_group(units, lambda ol, g, og=og: (V1h[:, og * 32 + ol, g, :], "V1h"), hf * 64 + og * 32, 32, False)
                for g in range(2):
                    V(lambda g=g: nc.vector.tensor_copy(out=V1x[:, g, 0:64], in_=Vx[:, g * 64:(g + 1) * 64]), r=["Vx"], w=["V1x"])
                att_group([(Kx[:, g * 64:(g + 1) * 64], ["Kx"]) for g in range(2)], lambda ol, g: (V1x[:, g, :], "V1x"), 128, 1, True)
                V(lambda: nc.vector.reciprocal(out=rec8[:], in_=p_os[:, :, 64]), r=["p3s_o"], w=["rec8"])
                V(lambda: nc.vector.tensor_tensor(out=o8[:], in0=p_os[:, :, 0:64], in1=rec8[:].unsqueeze(2).to_broadcast([8, 2, 64]), op=ALU.mult),
                  r=["p3s_o", "rec8"], w=["o8"])
                for b2 in range(2):
                    row = samp0 + b0 + b2
                    DMA(ao_scr[row:row + 1, :].rearrange("o (g h d) -> (o h) g d", g=2, h=4), o8[4 * b2:4 * b2 + 4, :, :], r=["o8"], w=[("ao", TS)])
            S.barrier()

    if "p4" in phases:
        with contextlib.ExitStack() as e5:
            Wglu = sb("Wglu", [128, 4, 2048], BF16, e5)
            Wao = sb("Wao", [128, 4, 1024], BF16, e5)
            Wo = sb("Wo", [128, 8, 1024], BF16, e5)
            bglu = sb("bglu", [128, 2048], F32, e5)
            with contextlib.ExitStack() as e5w:
                load_weight_bf16(Wglu, w_glu, D_SSM, 2048, e5w, nm="wglu")
                load_weight_bf16(Wao, w_ao, D_ATTN, 1024, e5w, nm="wao")
                load_weight_bf16(Wo, w_o, D_MODEL, 1024, e5w, nm="wo")
                S.barrier()
            DMA(bglu[:], b_glu.rearrange("(o n) -> o n", o=1).partition_broadcast(128), w=["bglu"])
            gyt = [sb(f"gyt{i}", [128, 4, 128], BF16, e5) for i in range(2)]
            aot = [sb(f"aot{i}", [128, 512], BF16, e5) for i in range(2)]
            gat = [sb(f"gat{i}", [128, 2048], F32, e5) for i in range(2)]
            xin = [sb(f"xin{i}", [128, 1024], F32, e5) for i in range(2)]
            zv = sb("zv", [128, 512], F32, e5)
            zg = sb("zg", [128, 512], F32, e5)
            so = sb("so", [128, 1024], F32, e5)
            aoT = sb("aoT", [128, 4, 128], BF16, e5)
            m1 = sb("m1", [128, 1024], F32, e5)
            m2 = sb("m2", [128, 512], F32, e5)
            mixb2 = [sb(f"mixb{i}", [128, 1024], BF16, e5) for i in range(2)]
            mixT = sb("mixT", [128, 8, 128], BF16, e5)
            x2t = [sb(f"x2t{i}", [128, 1024], F32, e5) for i in range(2)]
            p_a = [ps(f"p4_a{i}", [128, 512], F32, e5) for i in range(4)]
            p_tr4 = ps("p4_tr", [128, 8, 128], BF16, e5)
            def p4a_front(T):
                r0, nr, is_s, pti = tile_rows(T)
                gy_, ao_, ga_, xi_, x2_ = gyt[T % 2], aot[T % 2], gat[T % 2], xin[T % 2], x2t[T % 2]
                mixb = mixb2[T % 2]
                gkeys = [("gyT", ct_, "s") for ct_ in range(4)] if is_s else [("gyT", ct_, (T * 128) // SEQ, ((T * 128) % SEQ) // 512) for ct_ in range(4)]
                DMA(gy_[:], gyT_scr.rearrange("(c p) t -> p c t", p=128)[:, :, T * 128:(T + 1) * 128], r=gkeys, w=[gy_.name])
                if is_s:
                    V(lambda ao_=ao_: nc.vector.memset(ao_[:], 0.0), w=[ao_.name])
                    DMA(ao_[0:NS, :], ao_scr[T * 128:T * 128 + NS, :], r=[("ao", T)], w=[ao_.name])
                    V(lambda xi_=xi_: nc.vector.memset(xi_[:], 0.0), w=[xi_.name])
                    DMA(xi_[0:NS, :], xs[:, :], w=[xi_.name])
                else:
                    DMA(ao_[:], ao_scr[T * 128:(T + 1) * 128, :], r=[("ao", T)], w=[ao_.name])
                    DMA(xi_[:], xp[r0:r0 + 128, :], w=[xi_.name])
                DMA(ga_[:], gate_scr[T * 128:(T + 1) * 128, :], r=[("gate", T)], w=[ga_.name])
                for nh in range(2):
                    pv, pg = p_a[0], p_a[1]
                    for (pp, c0) in ((pv, nh * 512), (pg, 1024 + nh * 512)):
                        for kc in range(4):
                            PE(lambda pp=pp, c0=c0, kc=kc, gy_=gy_: nc.tensor.matmul(pp[:], lhsT=gy_[:, kc, :], rhs=Wglu[:, kc, c0:c0 + 512], start=(kc == 0), stop=(kc == 3)),
                               r=[gy_.name, "Wglu"], w=[pp.name], skip_same=True)
                    V(lambda nh=nh: nc.vector.tensor_tensor(out=zv[:], in0=p_a[0][:], in1=bglu[:, nh * 512:(nh + 1) * 512], op=ALU.add), r=[p_a[0].name, "bglu"], w=["zv"])
                    V(lambda nh=nh: nc.vector.tensor_tensor(out=zg[:], in0=p_a[1][:], in1=bglu[:, 1024 + nh * 512:1024 + (nh + 1) * 512], op=ALU.add),
                      r=[p_a[1].name, "bglu"], w=["zg"])
                    A(lambda: nc.scalar.activation(out=zg[:], in_=zg[:], func=AF.Sigmoid), r=["zg"], w=["zg"])
                    G(lambda nh=nh: nc.gpsimd.tensor_tensor(out=so[:, nh * 512:(nh + 1) * 512], in0=zv[:], in1=zg[:], op=ALU.mult), r=["zv", "zg"], w=["so"])
                for kc in range(4):
                    PE(lambda kc=kc, ao_=ao_: nc.tensor.transpose(out=p_tr4[:, kc, :], in_=ao_[:, kc * 128:(kc + 1) * 128], identity=ident[:]),
                       r=[ao_.name, "ident"], w=["p4_tr"], skip_same=True)
                A(lambda: nc.scalar.copy(out=aoT[:], in_=p_tr4[:, 0:4, :]), r=["p4_tr"], w=["aoT"])
                G(lambda ga_=ga_: nc.gpsimd.tensor_tensor(out=m1[:], in0=so[:], in1=ga_[:, 0:1024], op=ALU.mult), r=["so", ga_.name], w=["m1"])
                for nh in range(2):
                    pp = p_a[2 + nh]
                    for kc in range(4):
                        PE(lambda pp=pp, nh=nh, kc=kc: nc.tensor.matmul(pp[:], lhsT=aoT[:, kc, :], rhs=Wao[:, kc, nh * 512:(nh + 1) * 512], start=(kc == 0), stop=(kc == 3)),
                           r=["aoT", "Wao"], w=[pp.name], skip_same=True)
                    V(lambda pp=pp, nh=nh, ga_=ga_: nc.vector.tensor_tensor(out=m2[:], in0=pp[:], in1=ga_[:, 1024 + nh * 512:1024 + (nh + 1) * 512], op=ALU.mult),
                      r=[pp.name, ga_.name], w=["m2"])
                    V(lambda nh=nh: nc.vector.tensor_tensor(out=mixb[:, nh * 512:(nh + 1) * 512], in0=m1[:, nh * 512:(nh + 1) * 512], in1=m2[:], op=ALU.add),
                      r=["m1", "m2"], w=[mixb.name])

            def p4a_back(T):
                r0, nr, is_s, pti = tile_rows(T)
                gy_, ao_, ga_, xi_, x2_ = gyt[T % 2], aot[T % 2], gat[T % 2], xin[T % 2], x2t[T % 2]
                mixb = mixb2[T % 2]
                for kc in range(8):
                    PE(lambda kc=kc: nc.tensor.transpose(out=p_tr4[:, kc, :], in_=mixb[:, kc * 128:(kc + 1) * 128], identity=ident[:]),
                       r=[mixb.name, "ident"], w=["p4_tr"], skip_same=True)
                A(lambda: nc.scalar.copy(out=mixT[:], in_=p_tr4[:]), r=["p4_tr"], w=["mixT"])
                for nh in range(2):
                    pp = p_a[nh]
                    for kc in range(8):
                        PE(lambda pp=pp, nh=nh, kc=kc: nc.tensor.matmul(pp[:], lhsT=mixT[:, kc, :], rhs=Wo[:, kc, nh * 512:(nh + 1) * 512], start=(kc == 0), stop=(kc == 7)),
                           r=["mixT", "Wo"], w=[pp.name], skip_same=True)
                    V(lambda pp=pp, nh=nh, xi_=xi_, x2_=x2_: nc.vector.tensor_tensor(out=x2_[:, nh * 512:(nh + 1) * 512], in0=pp[:], in1=xi_[:, nh * 512:(nh + 1) * 512], op=ALU.add),
                      r=[pp.name, xi_.name], w=[x2_.name])
                DMA(x2_scr[T * 128:(T + 1) * 128, :], x2_[:], r=[x2_.name], w=[("x2", T)], q="pool")

            p4a_front(0)
            for T in range(NTT):
                if T + 1 < NTT:
                    p4a_front(T + 1)
                p4a_back(T)
            S.barrier()

    if "p4" in phases:
        with contextlib.ExitStack() as e6:
            Wup = sb("Wup", [128, 8, D_FF], BF16, e6)
            Wdn = sb("Wdn", [128, 32, 1024], BF16, e6)
            g2c = sb("g2c", [128, 8], F32, e6)
            gfb = sb("gfb", [128, 1024], F32, e6)
            with nc.allow_non_contiguous_dma(reason="tiny gain vector"):
                DMA(g2c[:], norm2_g.rearrange("(k p) -> p k", p=128), w=["g2c"])
            DMA(gfb[:], normf_g.rearrange("(o n) -> o n", o=1).partition_broadcast(128), w=["gfb"])
            with contextlib.ExitStack() as e6w:
                load_weight_bf16(Wup, w_up, D_MODEL, D_FF, e6w, gcol=g2c, nm="wup", NSTG=3)
                load_weight_bf16(Wdn, w_down, D_FF, 1024, e6w, nm="wdn", NSTG=3)
                S.barrier()
            x2i = [sb(f"x2i{i}", [128, 1024], F32, e6) for i in range(2)]
            junk6 = sb("junk6", [128, 1024], F32, e6)
            ss6 = sb("ss6", [128, 1], F32, e6)
            rs6 = sb("rs6", [128, 1], F32, e6)
            hh = sb("hh", [128, 1024], BF16, e6)
            hhT = sb("hhT", [128, 8, 128], BF16, e6)
            rl = [sb(f"rl{i}", [128, 512], F32, e6) for i in range(2)]
            aT = sb("aT", [128, 32, 128], BF16, e6)
            x3 = sb("x3", [128, 1024], F32, e6)
            yt = [sb(f"yt{i}", [128, 1024], F32, e6) for i in range(2)]
            p_tr6 = ps("p6_tr", [128, 8, 128], BF16, e6)
            p_up = [ps(f"p6_up{i}", [128, 4, 128], F32, e6) for i in range(2)]
            p_dn = [ps(f"p6_dn{i}", [128, 512], F32, e6) for i in range(2)]
            for T in range(NTT):
                r0, nr, is_s, pti = tile_rows(T)
                xi_, y_ = x2i[T % 2], yt[T % 2]
                DMA(xi_[:], x2_scr[T * 128:(T + 1) * 128, :], r=[("x2", T)], w=[xi_.name])
                A(lambda xi_=xi_: nc.scalar.activation(out=junk6[:], in_=xi_[:], func=AF.Square, accum_out=ss6[:]), r=[xi_.name], w=["junk6", "ss6"])
                A(lambda: nc.scalar.activation(out=rs6[:], in_=ss6[:], func=AF.Sqrt, bias=eps_t[:], scale=1.0 / D_MODEL), r=["ss6", "eps_t"], w=["rs6"])
                V(lambda: nc.vector.reciprocal(out=rs6[:], in_=rs6[:]), r=["rs6"], w=["rs6"])
                V(lambda xi_=xi_: nc.vector.tensor_scalar(out=hh[:], in0=xi_[:], scalar1=rs6[:, 0:1], scalar2=None, op0=ALU.mult), r=[xi_.name, "rs6"], w=["hh"])
                for kc in range(8):
                    PE(lambda kc=kc: nc.tensor.transpose(out=p_tr6[:, kc, :], in_=hh[:, kc * 128:(kc + 1) * 128], identity=ident[:]),
                       r=["hh", "ident"], w=["p6_tr"], skip_same=True)
                A(lambda: nc.scalar.copy(out=hhT[:], in_=p_tr6[:]), r=["p6_tr"], w=["hhT"])
                for f4 in range(8):
                    pu = p_up[f4 % 2]
                    r_ = rl[f4 % 2]
                    for fi in range(4):
                        f = f4 * 4 + fi
                        for kc in range(8):
                            PE(lambda pu=pu, fi=fi, f=f, kc=kc: nc.tensor.matmul(pu[:, fi, :], lhsT=Wup[:, kc, f * 128:(f + 1) * 128], rhs=hhT[:, kc, :],
                                                                              start=(kc == 0), stop=(kc == 7)), r=["Wup", "hhT"], w=[pu.name], skip_same=True)
                    A(lambda pu=pu, r_=r_: nc.scalar.activation(out=r_[:], in_=pu[:].rearrange("p a b -> p (a b)"), func=AF.Relu), r=[pu.name], w=[r_.name])
                    V(lambda r_=r_, f4=f4: nc.vector.tensor_tensor(out=aT[:, f4 * 4:(f4 + 1) * 4, :].rearrange("p a b -> p (a b)"), in0=r_[:], in1=r_[:], op=ALU.mult),
                      r=[r_.name], w=["aT"])
                for nh in range(2):
                    pd = p_dn[nh]
                    for fk in range(32):
                        PE(lambda pd=pd, nh=nh, fk=fk: nc.tensor.matmul(pd[:], lhsT=aT[:, fk, :], rhs=Wdn[:, fk, nh * 512:(nh + 1) * 512], start=(fk == 0), stop=(fk == 31)),
                           r=["aT", "Wdn"], w=[pd.name], skip_same=True)
                    V(lambda pd=pd, nh=nh, xi_=xi_: nc.vector.tensor_tensor(out=x3[:, nh * 512:(nh + 1) * 512], in0=pd[:], in1=xi_[:, nh * 512:(nh + 1) * 512], op=ALU.add),
                      r=[pd.name, xi_.name], w=["x3"])
                A(lambda: nc.scalar.activation(out=junk6[:], in_=x3[:], func=AF.Square, accum_out=ss6[:]), r=["x3"], w=["junk6", "ss6"])
                A(lambda: nc.scalar.activation(out=rs6[:], in_=ss6[:], func=AF.Sqrt, bias=eps_t[:], scale=1.0 / D_MODEL), r=["ss6", "eps_t"], w=["rs6"])
                V(lambda: nc.vector.reciprocal(out=rs6[:], in_=rs6[:]), r=["rs6"], w=["rs6"])
                V(lambda y_=y_: nc.vector.scalar_tensor_tensor(out=y_[:], in0=x3[:], scalar=rs6[:, 0:1], in1=gfb[:], op0=ALU.mult, op1=ALU.mult),
                  r=["x3", "rs6", "gfb"], w=[y_.name])
                if is_s:
                    DMA(y_s[:, :], y_[0:NS, :], r=[y_.name], q="pool", is_output=True)
                else:
                    DMA(y_p[r0:r0 + 128, :], y_[:], r=[y_.name], q="pool", is_output=True)
            S.barrier()
    S.finish()
    es_all.close()
    dbg = dict(qT_scr=qT_scr, kT_scr=kT_scr)
    return nc


def make_in_map(inp, c, cfg):
    NB, NS = cfg.NB, cfg.NS
    f = lambda a: np.ascontiguousarray(a)
    m = {
        "xp": f(inp["x_prompt"][c * NB:(c + 1) * NB].reshape(NB * cfg.SEQ, D_MODEL)),
        "xs": f(inp["x_sample"][c * NS:(c + 1) * NS].reshape(NS, D_MODEL)),
        "cache_k": inp["cache_k"].reshape(cfg.NPOOL, -1),
        "cache_v": inp["cache_v"].reshape(cfg.NPOOL, -1),
        "cache_ik": inp["cache_idx_k"].reshape(cfg.NPOOL, -1),
        "st_re": f(inp["state_ssm_re"][c * NS:(c + 1) * NS].reshape(NS, -1)),
        "st_im": f(inp["state_ssm_im"][c * NS:(c + 1) * NS].reshape(NS, -1)),
        "ptab": f(inp["page_table"][c * NS:(c + 1) * NS].reshape(-1, 1)),
        "ssm_a_re": inp["ssm_a_re"].reshape(-1), "ssm_a_im": inp["ssm_a_im"].reshape(-1),
        "ssm_b_re": inp["ssm_b_re"].reshape(-1, 16), "ssm_b_im": inp["ssm_b_im"].reshape(-1, 16),
        "ssm_c_re": inp["ssm_c_re"].reshape(-1, 64), "ssm_c_im": inp["ssm_c_im"].reshape(-1, 64),
    }
    for k in ("norm1_g", "w_in", "ssm_log_dt", "ssm_d", "w_glu", "b_glu", "w_attn_out", "w_o", "norm2_g",
              "w_up", "w_down", "normf_g"):
        m[k] = inp[k]
    return m


ALL_PHASES = ("p1", "p2", "p3", "p3s", "p4")
OUT_NAMES = ["y_p", "y_s", "k_p", "v_p", "ki_p", "re_p", "im_p", "k_s", "v_s", "ki_s", "re_s", "im_s"]


def kernel(**inputs):
    inp = {k: np.asarray(v) for k, v in inputs.items()}
    B, SEQ = inp["x_prompt"].shape[0], inp["x_prompt"].shape[1]
    NSAMP = inp["x_sample"].shape[0]
    n_cores = 8
    cfg = Cfg(seq=SEQ, past=inp["page_table"].shape[1] * 128, nb=B // n_cores, ns=NSAMP // n_cores,
              n_pool=inp["cache_k"].shape[0])
    nc = build(cfg, phases=ALL_PHASES)
    shared = {"cache_k": inp["cache_k"].reshape(cfg.NPOOL, -1), "cache_v": inp["cache_v"].reshape(cfg.NPOOL, -1),
              "cache_ik": inp["cache_idx_k"].reshape(cfg.NPOOL, -1)}
    in_maps = []
    for c in range(n_cores):
        m = make_in_map(inp, c, cfg)
        m.update(shared)
        in_maps.append(m)
    res = run_bass_kernel_spmd(nc, in_maps, core_ids=list(range(n_cores)))
    outs = {n: np.concatenate([np.asarray(res.results[c][n]) for c in range(n_cores)], axis=0) for n in OUT_NAMES}
    NB, NS = cfg.NB, cfg.NS
    f32 = np.float32
    return (
        outs["y_p"].reshape(B, SEQ, D_MODEL).astype(f32, copy=False),
        outs["y_s"].reshape(NSAMP, 1, D_MODEL).astype(f32, copy=False),
        outs["k_p"].reshape(B, SEQ, 2, 64).astype(f32, copy=False),
        outs["v_p"].reshape(B, SEQ, 2, 64).astype(f32, copy=False),
        outs["ki_p"].reshape(B, SEQ, 64).astype(f32, copy=False),
        outs["re_p"].reshape(B, N_G, P_ST).astype(f32, copy=False),
        outs["im_p"].reshape(B, N_G, P_ST).astype(f32, copy=False),
        outs["k_s"].reshape(NSAMP, 1, 2, 64).astype(f32, copy=False),
        outs["v_s"].reshape(NSAMP, 1, 2, 64).astype(f32, copy=False),
        outs["ki_s"].reshape(NSAMP, 1, 64).astype(f32, copy=False),
        outs["re_s"].reshape(NSAMP, N_G, P_ST).astype(f32, copy=False),
        outs["im_s"].reshape(NSAMP, N_G, P_ST).astype(f32, copy=False),
    )
```

```python
import math
import contextlib
import numpy as np
import concourse.bass as bass
import concourse.mybir as mybir
from concourse.bass_utils import run_bass_kernel_spmd

F32 = mybir.dt.float32
BF16 = mybir.dt.bfloat16
I32 = mybir.dt.int32
U32 = mybir.dt.uint32
ALU = mybir.AluOpType
AF = mybir.ActivationFunctionType
AX = mybir.AxisListType

D_MODEL = 1024
D_SSM = 512
N_G = 32
P_ST = 64
N_HEADS = 8
N_KV = 2
HD = 64
D_ATTN = 512
N_IH = 8
IDX_D = 64
D_FF = 4096
IN_COLS = 3912
EPS = 1e-6
ROPE_THETA = 500000.0
NEG = -1.0e30
RELAX_SAME = False
RELAX_ENGINES = ('dve', 'act', 'pe')
BIGM = -240000.0
C_U, C_Q, C_K, C_V, C_QI, C_KI, C_WI, C_GS, C_GA = 0, 512, 1024, 1152, 1280, 1792, 1856, 1864, 2888


class Sync:
    def __init__(self, nc):
        self.nc = nc
        self.eng = {"pe": nc.tensor, "dve": nc.vector, "act": nc.scalar, "pool": nc.gpsimd, "sp": nc.sync}
        self.sem = {k: nc.alloc_semaphore("sem_" + k) for k in self.eng}
        self.cnt = {k: 0 for k in self.eng}
        self.waited = {k: {} for k in self.eng}
        self.R = 12
        self.dring = {q: [nc.alloc_semaphore(f"dsem_{q}{i}") for i in range(self.R)] for q in ("sp", "pool", "act")}
        self.dn = {q: 0 for q in self.dring}
        self.lastw = {}
        self.readers = {}
        self.semobj = {}
        for k, s in self.sem.items():
            self.semobj[id(s)] = s
        for q in self.dring:
            for s in self.dring[q]:
                self.semobj[id(s)] = s
        self.out_events = []
        self.relax_same = RELAX_SAME

    def _wait(self, e, ev):
        s, v = ev
        w = self.waited[e]
        if w.get(id(s), 0) >= v:
            return
        self.eng[e].wait_ge(s, v)
        w[id(s)] = v

    def _deps(self, e, reads, writes, skip_same=False):
        evs = []
        for k in reads:
            if k in self.lastw:
                evs.append(self.lastw[k])
        for k in writes:
            if k in self.lastw:
                evs.append(self.lastw[k])
            for r in self.readers.get(k, ()):
                evs.append(r)
        for ev in evs:
            if (skip_same or (self.relax_same and e in RELAX_ENGINES)) and e in self.sem and ev[0] is self.sem[e]:
                continue
            self._wait(e, ev)

    def _record(self, ev, reads, writes):
        for k in reads:
            self.readers.setdefault(k, []).append(ev)
            if len(self.readers[k]) > 24:
                best = {}
                for s, v in self.readers[k]:
                    if id(s) not in best or best[id(s)][1] < v:
                        best[id(s)] = (s, v)
                self.readers[k] = list(best.values())
        for k in writes:
            self.lastw[k] = ev
            self.readers[k] = []

    def op(self, e, fn, reads=(), writes=(), skip_same=False):
        self._deps(e, reads, writes, skip_same)
        ins = fn()
        self.cnt[e] += 1
        ins.then_inc(self.sem[e], 1)
        ev = (self.sem[e], self.cnt[e])
        self._record(ev, reads, writes)
        return ev

    def dma(self, q, fn, reads=(), writes=(), is_output=False):
        n = self.dn[q]
        slot = n % self.R
        s = self.dring[q][slot]
        prev = 16 * (n // self.R)
        if prev > 0:
            self._wait(q, (s, prev))
        self._deps(q, reads, writes)
        ins = fn()
        ins.then_inc(s, 16)
        self.dn[q] = n + 1
        ev = (s, prev + 16)
        self._record(ev, reads, writes)
        if is_output:
            self.out_events.append(ev)
        return ev

    def barrier(self):
        evs = []
        for q in self.dring:
            n = self.dn[q]
            for slot in range(self.R):
                cnt = (n - slot + self.R - 1) // self.R if n > slot else 0
                if cnt > 0:
                    evs.append((self.dring[q][slot], 16 * cnt))
        for e in self.eng:
            if self.cnt[e] > 0:
                evs.append((self.sem[e], self.cnt[e]))
        for e in self.eng:
            for ev in evs:
                if ev[0] is self.sem[e]:
                    continue
                self._wait(e, ev)

    def finish(self):
        for q in self.dring:
            n = self.dn[q]
            for slot in range(self.R):
                cnt = (n - slot + self.R - 1) // self.R if n > slot else 0
                if cnt > 0:
                    self._wait("sp", (self.dring[q][slot], 16 * cnt))
        for e in self.eng:
            if e != "sp" and self.cnt[e] > 0:
                self._wait("sp", (self.sem[e], self.cnt[e]))


class Cfg:
    def __init__(self, seq=4096, past=8192, nb=2, ns=16, n_pool=10240):
        self.SEQ = seq
        self.PAST = past
        self.NB = nb
        self.NS = ns
        self.NPOOL = n_pool
        self.NT = seq // 128
        self.NPG = past // 128
        self.TOPK = min(256, seq // 4)
        self.TOPK_S = min(256, (past + 1) // 4)
        self.NTOK = nb * seq
        self.NTT = nb * self.NT + 1
        self.NTOKP = self.NTT * 128


def build(cfg, phases=("p1",), debug=False):
    nc = bass.Bass("TRN2", target_bir_lowering=False)
    S = Sync(nc)
    NB, SEQ, NS, NT = cfg.NB, cfg.SEQ, cfg.NS, cfg.NT
    NTT, NTOKP = cfg.NTT, cfg.NTOKP

    def din(name, shape, dt=F32):
        return nc.dram_tensor(name, list(shape), dt, kind="ExternalInput").ap()

    def dout(name, shape, dt=F32):
        return nc.dram_tensor(name, list(shape), dt, kind="ExternalOutput").ap()

    def dscr(name, shape, dt=F32):
        return nc.dram_tensor(name, list(shape), dt, kind="ExternalOutput" if debug else "Internal").ap()

    xp = din("xp", [NB * SEQ, D_MODEL])
    xs = din("xs", [NS, D_MODEL])
    cache_k = din("cache_k", [cfg.NPOOL, 128 * 128])
    cache_v = din("cache_v", [cfg.NPOOL, 128 * 128])
    cache_ik = din("cache_ik", [cfg.NPOOL, 128 * 64])
    st_re = din("st_re", [NS, N_G * P_ST])
    st_im = din("st_im", [NS, N_G * P_ST])
    ptab = din("ptab", [NS * cfg.NPG, 1], I32)
    norm1_g = din("norm1_g", [D_MODEL])
    w_in = din("w_in", [D_MODEL, IN_COLS])
    a_re = din("ssm_a_re", [N_G * P_ST])
    a_im = din("ssm_a_im", [N_G * P_ST])
    log_dt = din("ssm_log_dt", [N_G])
    b_re = din("ssm_b_re", [N_G * P_ST, 16])
    b_im = din("ssm_b_im", [N_G * P_ST, 16])
    c_re = din("ssm_c_re", [N_G * 16, P_ST])
    c_im = din("ssm_c_im", [N_G * 16, P_ST])
    ssm_d = din("ssm_d", [D_SSM])
    w_glu = din("w_glu", [D_SSM, 2 * D_MODEL])
    b_glu = din("b_glu", [2 * D_MODEL])
    w_ao = din("w_attn_out", [D_ATTN, D_MODEL])
    w_o = din("w_o", [D_MODEL, D_MODEL])
    norm2_g = din("norm2_g", [D_MODEL])
    w_up = din("w_up", [D_MODEL, D_FF])
    w_down = din("w_down", [D_FF, D_MODEL])
    normf_g = din("normf_g", [D_MODEL])

    y_p = dout("y_p", [NB * SEQ, D_MODEL])
    y_s = dout("y_s", [NS, D_MODEL])
    k_p = dout("k_p", [NB * SEQ, 128])
    v_p = dout("v_p", [NB * SEQ, 128])
    ki_p = dout("ki_p", [NB * SEQ, 64])
    re_p = dout("re_p", [NB, N_G * P_ST])
    im_p = dout("im_p", [NB, N_G * P_ST])
    k_s = dout("k_s", [NS, 128])
    v_s = dout("v_s", [NS, 128])
    ki_s = dout("ki_s", [NS, 64])
    re_s = dout("re_s", [NS, N_G * P_ST])
    im_s = dout("im_s", [NS, N_G * P_ST])

    qT_scr = dscr("qT_scr", [NTT, 65, 8, 128], BF16)
    qiT_scr = dscr("qiT_scr", [NTT, 64, 8, 128], BF16)
    kT_scr = dscr("kT_scr", [64, 2, NTOKP], BF16)
    kiT_scr = dscr("kiT_scr", [64, NTOKP], BF16)
    vb_scr = dscr("vb_scr", [NTOKP, 2, 64], BF16)
    uT_scr = dscr("uT_scr", [D_SSM, NTOKP], F32)
    gate_scr = dscr("gate_scr", [NTOKP, 2048], F32)
    gyT_scr = dscr("gyT_scr", [D_SSM, NTOKP], BF16)
    ao_scr = dscr("ao_scr", [NTOKP, D_ATTN], BF16)
    x2_scr = dscr("x2_scr", [NTOKP, D_MODEL], F32)

    if debug:
        dbg_mb = dscr("dbg_mb", [128, SEQ], BF16)
        dbg_sc = dscr("dbg_sc", [128, SEQ], F32)
        dbg_thr = dscr("dbg_thr", [128, 1], F32)
    ident = nc.alloc_sbuf_tensor("ident", [128, 128], BF16)
    identf = nc.alloc_sbuf_tensor("identf", [128, 128], F32)
    cmask = nc.alloc_sbuf_tensor("cmask", [128, 128], F32)
    eps_t = nc.alloc_sbuf_tensor("eps_t", [128, 1], F32)
    cosT = nc.alloc_sbuf_tensor("cosT", [128, NT + 1, 8], F32)
    sinT = nc.alloc_sbuf_tensor("sinT", [128, NT + 1, 8], F32)
    lohi = nc.alloc_sbuf_tensor("lohi", [128, NTT, 16], F32)
    kn2 = nc.alloc_sbuf_tensor("kn2", [128, NTT, 2], F32)

    def V(fn, r=(), w=(), **kw):
        return S.op("dve", fn, r, w, **kw)

    def A(fn, r=(), w=(), **kw):
        return S.op("act", fn, r, w, **kw)

    def G(fn, r=(), w=(), **kw):
        return S.op("pool", fn, r, w, **kw)

    def PE(fn, r=(), w=(), **kw):
        return S.op("pe", fn, r, w, **kw)

    def DMA(out, in_, r=(), w=(), q="sp", is_output=False, **kw):
        return S.dma(q, lambda: S.eng[q].dma_start(out=out, in_=in_, **kw), r, w, is_output=is_output)

    ones_f = nc.alloc_sbuf_tensor("ones_f", [128, 128], F32)
    G(lambda: nc.gpsimd.memset(ones_f[:], 1.0), w=["ones_f"])
    G(lambda: nc.gpsimd.memset(eps_t[:], EPS), w=["eps_t"])
    G(lambda: nc.gpsimd.affine_select(out=identf[:], in_=ones_f[:], pattern=[[-1, 128]], compare_op=ALU.is_equal,
                                      fill=0.0, base=0, channel_multiplier=1), r=["ones_f"], w=["identf"])
    V(lambda: nc.vector.tensor_copy(out=ident[:], in_=identf[:]), r=["identf"], w=["ident"])
    zer_f = nc.alloc_sbuf_tensor("zer_f", [128, 128], F32)
    G(lambda: nc.gpsimd.memset(zer_f[:], 0.0), w=["zer_f"])
    G(lambda: nc.gpsimd.affine_select(out=cmask[:], in_=zer_f[:], pattern=[[-1, 128]], compare_op=ALU.is_ge,
                                      fill=NEG, base=0, channel_multiplier=1), r=["zer_f"], w=["cmask"])

    with contextlib.ExitStack() as es:
        posi = es.enter_context(nc.sbuf_tensor("posi", [128, NT + 1], I32))
        posf = es.enter_context(nc.sbuf_tensor("posf", [128, NT + 1], F32))
        ang = es.enter_context(nc.sbuf_tensor("ang", [128, NT + 1, 8], F32))
        ki_ = es.enter_context(nc.sbuf_tensor("kint", [128, NT + 1, 8], I32))
        kf = es.enter_context(nc.sbuf_tensor("kf", [128, NT + 1, 8], F32))
        rr = es.enter_context(nc.sbuf_tensor("rr", [128, NT + 1, 8], F32))
        r2 = es.enter_context(nc.sbuf_tensor("r2", [128, NT + 1, 8], F32))
        mk = es.enter_context(nc.sbuf_tensor("mk", [128, NT + 1, 8], F32))
        G(lambda: nc.gpsimd.iota(posi[:, 0:NT], pattern=[[128, NT]], base=0, channel_multiplier=1), w=["posi"])
        G(lambda: nc.gpsimd.iota(posi[:, NT:NT + 1], pattern=[[0, 1]], base=cfg.PAST, channel_multiplier=0), w=["posi"])
        V(lambda: nc.vector.tensor_copy(out=posf[:], in_=posi[:]), r=["posi"], w=["posf"])
        for j in range(8):
            inv = ROPE_THETA ** (-j / 8.0)
            V(lambda j=j, inv=inv: nc.vector.tensor_scalar(out=ang[:, :, j], in0=posf[:], scalar1=float(np.float32(inv)),
                                                           scalar2=float(1.0 / (2 * math.pi)), op0=ALU.mult, op1=ALU.mult),
              r=["posf"], w=["ang"])

        def wrap_sin(dst, off):
            V(lambda: nc.vector.tensor_scalar(out=rr[:], in0=ang[:], scalar1=float(off), scalar2=None, op0=ALU.add),
              r=["ang"], w=["rr"])
            V(lambda: nc.vector.tensor_copy(out=ki_[:], in_=rr[:]), r=["rr"], w=["kint"])
            V(lambda: nc.vector.tensor_copy(out=kf[:], in_=ki_[:]), r=["kint"], w=["kf"])
            V(lambda: nc.vector.tensor_tensor(out=r2[:], in0=rr[:], in1=kf[:], op=ALU.subtract), r=["rr", "kf"], w=["r2"])
            V(lambda: nc.vector.tensor_scalar(out=mk[:], in0=r2[:], scalar1=0.5, scalar2=None, op0=ALU.is_gt), r=["r2"], w=["mk"])
            V(lambda: nc.vector.tensor_tensor(out=r2[:], in0=r2[:], in1=mk[:], op=ALU.subtract), r=["r2", "mk"], w=["r2"])
            V(lambda: nc.vector.tensor_scalar(out=mk[:], in0=r2[:], scalar1=-0.5, scalar2=None, op0=ALU.is_lt), r=["r2"], w=["mk"])
            V(lambda: nc.vector.tensor_tensor(out=r2[:], in0=r2[:], in1=mk[:], op=ALU.add), r=["r2", "mk"], w=["r2"])
            A(lambda: nc.scalar.activation(out=dst[:], in_=r2[:], func=AF.Sin, scale=float(2 * math.pi)), r=["r2"], w=[dst.name])

        wrap_sin(sinT, 0.0)
        wrap_sin(cosT, 0.25)
        S.barrier()

    es_all = contextlib.ExitStack()

    def sb(name, shape, dt=F32, stack=None):
        return (stack or es_all).enter_context(nc.sbuf_tensor(name, list(shape), dt))

    def ps(name, shape, dt=F32, stack=None):
        return (stack or es_all).enter_context(nc.psum_tensor(name, list(shape), dt))

    def load_weight_bf16(dst, src, K, N, stack, gcol=None, nm="w", NSTG=2):
        KC = K // 128
        CH = 2048
        stg = [sb(f"stg_{nm}{i}", [128, CH], F32, stack) for i in range(NSTG)]
        i = 0
        for kc in range(KC):
            for c0 in range(0, N, CH):
                cw = min(CH, N - c0)
                st = stg[i % NSTG]
                DMA(st[:, 0:cw], src[kc * 128:(kc + 1) * 128, c0:c0 + cw], w=[st.name], q=("sp", "act")[i % 2])
                eng = ("dve", "pool")[i % 2]
                if gcol is None:
                    if eng == "dve":
                        V(lambda st=st, kc=kc, c0=c0, cw=cw: nc.vector.tensor_copy(out=dst[:, kc, c0:c0 + cw], in_=st[:, 0:cw]),
                          r=[st.name], w=[dst.name])
                    else:
                        G(lambda st=st, kc=kc, c0=c0, cw=cw: nc.gpsimd.tensor_copy(out=dst[:, kc, c0:c0 + cw], in_=st[:, 0:cw]),
                          r=[st.name], w=[dst.name])
                else:
                    V(lambda st=st, kc=kc, c0=c0, cw=cw: nc.vector.tensor_scalar(out=dst[:, kc, c0:c0 + cw], in0=st[:, 0:cw],
                                                                              scalar1=gcol[:, kc:kc + 1], scalar2=None, op0=ALU.mult),
                      r=[st.name, gcol.name], w=[dst.name])
                i += 1

    def tile_rows(T):
        if T < NB * NT:
            return T * 128, 128, False, T % NT
        return 0, NS, True, NT

    if "p1" in phases:
        with contextlib.ExitStack() as e1:
            Win = sb("Win", [128, 8, IN_COLS], BF16, e1)
            g1c = sb("g1c", [128, 8], F32, e1)
            with nc.allow_non_contiguous_dma(reason="tiny gain vector"):
                DMA(g1c[:], norm1_g.rearrange("(k p) -> p k", p=128), w=["g1c"])
            with contextlib.ExitStack() as e1w:
                load_weight_bf16(Win, w_in, D_MODEL, IN_COLS, e1w, gcol=g1c, nm="win", NSTG=6)
                S.barrier()
            xt = [sb(f"xt{i}", [128, D_MODEL], F32, e1) for i in range(2)]
            junk = sb("junk1", [128, D_MODEL], F32, e1)
            ss = sb("ss", [128, 1], F32, e1)
            rstd = sb("rstd", [128, 1], F32, e1)
            hb = sb("hb", [128, D_MODEL], BF16, e1)
            hT = sb("hT", [128, 8, 128], BF16, e1)
            pt = [sb(f"pt{i}", [128, IN_COLS - 512], F32, e1) for i in range(2)]
            uTs = sb("uTs", [128, 4, 128], F32, e1)
            tA = [sb(f"tA{i}", [128, 10, 8], F32, e1) for i in range(6)]
            qn = sb("qn", [128, 8], F32, e1)
            sq = sb("sq", [128, 10, 64], F32, e1)
            wp = sb("wp", [128, 8], F32, e1)
            qa = sb("qa", [128, 8, 65], BF16, e1)
            kb_ = sb("kb_", [128, 2, 64], BF16, e1)
            qib = sb("qib", [128, 8, 64], BF16, e1)
            kib = sb("kib", [128, 64], BF16, e1)
            vbt = sb("vbt", [128, 2, 64], BF16, e1)
            gts = sb("gts", [128, 2048], F32, e1)
            trs = sb("trs", [65, 19, 128], BF16, e1)
            p_tr = ps("p_tr", [128, 8, 128], BF16, e1)
            p_mm = [ps(f"p_mm{i}", [128, 512], F32, e1) for i in range(3)]
            p_u = ps("p_u", [128, 4, 128], F32, e1)
            p_t2 = ps("p_t2", [65, 19, 128], BF16, e1)

            def p1_front(T):
                r0, nr, is_s, pti = tile_rows(T)
                x_ = xt[T % 2]
                p_ = pt[T % 2]
                xn, pn = x_.name, p_.name
                if is_s:
                    V(lambda x_=x_: nc.vector.memset(x_[:], 0.0), w=[xn])
                    DMA(x_[0:NS, :], xs[:, :], w=[xn])
                else:
                    DMA(x_[:], xp[r0:r0 + 128, :], w=[xn])
                A(lambda x_=x_: nc.scalar.activation(out=junk[:], in_=x_[:], func=AF.Square, accum_out=ss[:]),
                  r=[xn], w=["junk1", "ss"])
                A(lambda: nc.scalar.activation(out=rstd[:], in_=ss[:], func=AF.Sqrt, bias=eps_t[:], scale=1.0 / D_MODEL),
                  r=["ss", "eps_t"], w=["rstd"])
                V(lambda: nc.vector.reciprocal(out=rstd[:], in_=rstd[:]), r=["rstd"], w=["rstd"])
                V(lambda x_=x_: nc.vector.tensor_scalar(out=hb[:], in0=x_[:], scalar1=rstd[:, 0:1], scalar2=None, op0=ALU.mult),
                  r=[xn, "rstd"], w=["hb"])
                for kc in range(8):
                    PE(lambda kc=kc: nc.tensor.transpose(out=p_tr[:, kc, :], in_=hb[:, kc * 128:(kc + 1) * 128], identity=ident[:]),
                       r=["hb", "ident"], w=["p_tr"], skip_same=True)
                A(lambda: nc.scalar.copy(out=hT[:], in_=p_tr[:]), r=["p_tr"], w=["hT"])
                for ct in range(4):
                    for kc in range(8):
                        PE(lambda ct=ct, kc=kc: nc.tensor.matmul(p_u[:, ct, :], lhsT=Win[:, kc, ct * 128:(ct + 1) * 128],
                                                                 rhs=hT[:, kc, :], start=(kc == 0), stop=(kc == 7)),
                           r=["Win", "hT"], w=["p_u"], skip_same=True)
                V(lambda: nc.vector.tensor_copy(out=uTs[:], in_=p_u[:]), r=["p_u"], w=["uTs"])
                DMA(uT_scr.rearrange("(c p) t -> p c t", p=128)[:, :, T * 128:(T + 1) * 128], uTs[:], r=["uTs"], w=[("uT", T)])
                ci = 0
                for c0 in range(512, IN_COLS, 512):
                    cw = min(512, IN_COLS - c0)
                    pm = p_mm[ci % 3]
                    for kc in range(8):
                        PE(lambda pm=pm, kc=kc, c0=c0, cw=cw: nc.tensor.matmul(pm[:, 0:cw], lhsT=hT[:, kc, :], rhs=Win[:, kc, c0:c0 + cw],
                                                                               start=(kc == 0), stop=(kc == 7)),
                           r=["Win", "hT"], w=[pm.name], skip_same=True)
                    if ci % 2 == 0:
                        A(lambda pm=pm, c0=c0, cw=cw, p_=p_: nc.scalar.copy(out=p_[:, c0 - 512:c0 - 512 + cw], in_=pm[:, 0:cw]),
                          r=[pm.name], w=[pn])
                    else:
                        V(lambda pm=pm, c0=c0, cw=cw, p_=p_: nc.vector.tensor_copy(out=p_[:, c0 - 512:c0 - 512 + cw], in_=pm[:, 0:cw]),
                          r=[pm.name], w=[pn])
                    ci += 1

            def p1_back(T):
                r0, nr, is_s, pti = tile_rows(T)
                x_ = xt[T % 2]
                p_ = pt[T % 2]
                xn, pn = x_.name, p_.name
                cs_c = cosT[:, pti, :]
                cs_s = sinT[:, pti, :]
                for (cb, H) in ((C_Q - 512, 10), (C_QI - 512, 9)):
                    X = p_[:, cb:cb + H * 64].rearrange("p (h d) -> p h d", d=64)
                    x1 = X[:, :, 0:8]
                    x2 = X[:, :, 8:16]
                    cB = cs_c.unsqueeze(1).to_broadcast([128, H, 8])
                    sB = cs_s.unsqueeze(1).to_broadcast([128, H, 8])
                    t = [tt[:, 0:H, :] for tt in tA]
                    V(lambda x1=x1, cB=cB, t=t: nc.vector.tensor_tensor(out=t[0], in0=x1, in1=cB, op=ALU.mult), r=[pn, "cosT"], w=["tA0"])
                    V(lambda x2=x2, sB=sB, t=t: nc.vector.tensor_tensor(out=t[1], in0=x2, in1=sB, op=ALU.mult), r=[pn, "sinT"], w=["tA1"])
                    V(lambda x1=x1, sB=sB, t=t: nc.vector.tensor_tensor(out=t[2], in0=x1, in1=sB, op=ALU.mult), r=[pn, "sinT"], w=["tA2"])
                    V(lambda x2=x2, cB=cB, t=t: nc.vector.tensor_tensor(out=t[3], in0=x2, in1=cB, op=ALU.mult), r=[pn, "cosT"], w=["tA3"])
                    V(lambda x1=x1, t=t: nc.vector.tensor_tensor(out=x1, in0=t[0], in1=t[1], op=ALU.subtract), r=["tA0", "tA1"], w=[pn])
                    V(lambda x2=x2, t=t: nc.vector.tensor_tensor(out=x2, in0=t[2], in1=t[3], op=ALU.add), r=["tA2", "tA3"], w=[pn])
                kcol, vcol, kicol = C_K - 512, C_V - 512, C_KI - 512
                if is_s:
                    DMA(k_s[:, :], p_[0:NS, kcol:kcol + 128], r=[pn], q="pool", is_output=True)
                    DMA(v_s[:, :], p_[0:NS, vcol:vcol + 128], r=[pn], q="pool", is_output=True)
                    DMA(ki_s[:, :], p_[0:NS, kicol:kicol + 64], r=[pn], q="pool", is_output=True)
                else:
                    DMA(k_p[r0:r0 + 128, :], p_[:, kcol:kcol + 128], r=[pn], q="pool", is_output=True)
                    DMA(v_p[r0:r0 + 128, :], p_[:, vcol:vcol + 128], r=[pn], q="pool", is_output=True)
                    DMA(ki_p[r0:r0 + 128, :], p_[:, kicol:kicol + 64], r=[pn], q="pool", is_output=True)
                QK = p_[:, C_Q - 512:C_Q - 512 + 640].rearrange("p (h d) -> p h d", d=64)
                G(lambda QK=QK: nc.gpsimd.tensor_tensor(out=sq[:], in0=QK, in1=QK, op=ALU.mult), r=[pn], w=["sq"])
                V(lambda: nc.vector.tensor_reduce(out=qn[:], in_=sq[:, 0:8, :], axis=AX.X, op=ALU.add), r=["sq"], w=["qn"])
                V(lambda T=T: nc.vector.tensor_reduce(out=kn2[:, T, :], in_=sq[:, 8:10, :], axis=AX.X, op=ALU.add), r=["sq"], w=["kn2"])
                A(lambda: nc.scalar.activation(out=qn[:], in_=qn[:], func=AF.Sqrt), r=["qn"], w=["qn"])
                V(lambda: nc.vector.tensor_scalar(out=qa[:, :, 64], in0=qn[:], scalar1=-1.0, scalar2=None, op0=ALU.mult),
                  r=["qn"], w=["qa"])
                V(lambda QK=QK: nc.vector.tensor_copy(out=qa[:, :, 0:64], in_=QK[:, 0:8, :]), r=[pn], w=["qa"])
                G(lambda QK=QK: nc.gpsimd.tensor_copy(out=kb_[:], in_=QK[:, 8:10, :]), r=[pn], w=["kb_"])
                wic = C_WI - 512
                V(lambda p_=p_, wic=wic: nc.vector.tensor_scalar(out=wp[:], in0=p_[:, wic:wic + 8], scalar1=float(8 ** -0.5 * 0.125),
                                                                 scalar2=None, op0=ALU.mult), r=[pn], w=["wp"])
                V(lambda T=T: nc.vector.tensor_scalar(out=lohi[:, T, 0:8], in0=wp[:], scalar1=0.0, scalar2=-3.0e38,
                                                      op0=ALU.is_le, op1=ALU.mult), r=["wp"], w=["lohi"])
                V(lambda T=T: nc.vector.tensor_scalar(out=lohi[:, T, 8:16], in0=wp[:], scalar1=0.0, scalar2=3.0e38,
                                                      op0=ALU.is_gt, op1=ALU.mult), r=["wp"], w=["lohi"])
                QI = p_[:, C_QI - 512:C_QI - 512 + 512].rearrange("p (h d) -> p h d", d=64)
                V(lambda QI=QI: nc.vector.tensor_tensor(out=qib[:], in0=QI, in1=wp[:].unsqueeze(2).to_broadcast([128, 8, 64]), op=ALU.mult),
                  r=[pn, "wp"], w=["qib"])
                G(lambda p_=p_: nc.gpsimd.tensor_copy(out=kib[:], in_=p_[:, C_KI - 512:C_KI - 512 + 64]), r=[pn], w=["kib"])
                G(lambda p_=p_: nc.gpsimd.tensor_copy(out=vbt[:], in_=p_[:, vcol:vcol + 128].rearrange("p (g d) -> p g d", d=64)),
                  r=[pn], w=["vbt"])
                DMA(vb_scr[T * 128:(T + 1) * 128, :, :], vbt[:], r=["vbt"], w=[("vb", T)], q="pool")
                A(lambda p_=p_: nc.scalar.activation(out=gts[:], in_=p_[:, C_GS - 512:C_GS - 512 + 2048], func=AF.Sigmoid),
                  r=[pn], w=["gts"])
                DMA(gate_scr[T * 128:(T + 1) * 128, :], gts[:], r=["gts"], w=[("gate", T)], q="pool")
                for h in range(8):
                    PE(lambda h=h: nc.tensor.transpose(out=p_t2[0:65, h, :], in_=qa[:, h, :], identity=ident[:]),
                       r=["qa", "ident"], w=["p_t2"], skip_same=True)
                for g in range(2):
                    PE(lambda g=g: nc.tensor.transpose(out=p_t2[0:64, 8 + g, :], in_=kb_[:, g, :], identity=ident[:]),
                       r=["kb_", "ident"], w=["p_t2"], skip_same=True)
                for h in range(8):
                    PE(lambda h=h: nc.tensor.transpose(out=p_t2[0:64, 10 + h, :], in_=qib[:, h, :], identity=ident[:]),
                       r=["qib", "ident"], w=["p_t2"], skip_same=True)
                PE(lambda: nc.tensor.transpose(out=p_t2[0:64, 18, :], in_=kib[:], identity=ident[:]),
                   r=["kib", "ident"], w=["p_t2"], skip_same=True)
                V(lambda: nc.vector.tensor_copy(out=trs[0:65, 0:8, :], in_=p_t2[0:65, 0:8, :]), r=["p_t2"], w=["trs"])
                A(lambda: nc.scalar.copy(out=trs[0:64, 8:19, :], in_=p_t2[0:64, 8:19, :]), r=["p_t2"], w=["trs"])
                DMA(qT_scr[T, :, :, :], trs[0:65, 0:8, :], r=["trs"], w=[("qT", T)], q="pool")
                DMA(kT_scr[:, :, T * 128:(T + 1) * 128], trs[0:64, 8:10, :], r=["trs"], w=[("kT", T)], q="pool")
                DMA(qiT_scr[T, :, :, :], trs[0:64, 10:18, :], r=["trs"], w=[("qiT", T)], q="pool")
                DMA(kiT_scr[:, T * 128:(T + 1) * 128], trs[0:64, 18, :], r=["trs"], w=[("kiT", T)], q="pool")

            p1_front(0)
            for T in range(NTT):
                if T + 1 < NTT:
                    p1_front(T + 1)
                p1_back(T)
            S.barrier()


    if "p2" in phases:
        with contextlib.ExitStack() as e2:
            NLEV = int(math.log2(SEQ))
            assert (1 << NLEV) == SEQ
            NCH = SEQ // 512
            sm = {}

            def small(name, shape=(128, 16), dt=F32):
                t_ = sb("s2_" + name, list(shape), dt, e2)
                sm[name] = t_
                return t_

            def vtt(o, a, b, op):
                V(lambda: nc.vector.tensor_tensor(out=o[:], in0=a[:], in1=b[:], op=op), r=[a.name, b.name], w=[o.name])

            def vts(o, a, s1, op, s2=None, op1=None):
                if op1 is None:
                    V(lambda: nc.vector.tensor_scalar(out=o[:], in0=a[:], scalar1=s1, scalar2=None, op0=op), r=[a.name], w=[o.name])
                else:
                    V(lambda: nc.vector.tensor_scalar(out=o[:], in0=a[:], scalar1=s1, scalar2=s2, op0=op, op1=op1), r=[a.name], w=[o.name])

            are_t = small("are"); aim_t = small("aim"); ldt = small("ldt"); dtt = small("dt")
            xre = small("xre"); th = small("th"); lam_abs = small("lam_abs")
            cs_ = small("cs"); sn_ = small("sn"); lam_re = small("lam_re"); lam_im = small("lam_im")
            tq = [small(f"tq{i}") for i in range(6)]
            tqi = small("tqi", dt=I32)
            cf_re = small("cf_re"); cf_im = small("cf_im")
            with nc.allow_non_contiguous_dma(reason="tiny ssm params"):
                DMA(are_t[:], a_re.rearrange("(ft f) -> f ft", f=128), w=[are_t.name])
                DMA(aim_t[:], a_im.rearrange("(ft f) -> f ft", f=128), w=[aim_t.name])
                lv = log_dt.rearrange("(ft two) -> two ft", two=2)
                DMA(ldt[0:64, :], lv[0:1, :].partition_broadcast(64), w=[ldt.name])
                DMA(ldt[64:128, :], lv[1:2, :].partition_broadcast(64), w=[ldt.name])
            A(lambda: nc.scalar.activation(out=dtt[:], in_=ldt[:], func=AF.Exp), r=[ldt.name], w=[dtt.name])
            vtt(xre, are_t, dtt, ALU.mult)
            vtt(th, aim_t, dtt, ALU.mult)
            A(lambda: nc.scalar.activation(out=lam_abs[:], in_=xre[:], func=AF.Exp), r=[xre.name], w=[lam_abs.name])

            def sincos_turns(dst, src_turns, off):
                a_, k_, r_, m_ = tq[0], tq[1], tq[2], tq[3]
                vts(a_, src_turns, float(off), ALU.add)
                V(lambda: nc.vector.tensor_copy(out=tqi[:], in_=a_[:]), r=[a_.name], w=[tqi.name])
                V(lambda: nc.vector.tensor_copy(out=k_[:], in_=tqi[:]), r=[tqi.name], w=[k_.name])
                vtt(r_, a_, k_, ALU.subtract)
                vts(m_, r_, 0.5, ALU.is_gt)
                vtt(r_, r_, m_, ALU.subtract)
                vts(m_, r_, -0.5, ALU.is_lt)
                vtt(r_, r_, m_, ALU.add)
                A(lambda: nc.scalar.activation(out=dst[:], in_=r_[:], func=AF.Sin, scale=float(2 * math.pi)), r=[r_.name], w=[dst.name])

            thn = small("thn")
            vts(thn, th, float(1.0 / (2 * math.pi)), ALU.mult)
            sincos_turns(sn_, thn, 0.0)
            sincos_turns(cs_, thn, 0.25)
            vtt(lam_re, lam_abs, cs_, ALU.mult)
            vtt(lam_im, lam_abs, sn_, ALU.mult)
            nr, den, t1_, t2_ = tq[0], tq[1], tq[2], tq[3]
            vts(nr, lam_re, -1.0, ALU.add)
            vtt(t1_, are_t, are_t, ALU.mult)
            vtt(t2_, aim_t, aim_t, ALU.mult)
            vtt(den, t1_, t2_, ALU.add)
            V(lambda: nc.vector.reciprocal(out=den[:], in_=den[:]), r=[den.name], w=[den.name])
            vtt(t1_, nr, are_t, ALU.mult)
            vtt(t2_, lam_im, aim_t, ALU.mult)
            vtt(t1_, t1_, t2_, ALU.add)
            vtt(cf_re, t1_, den, ALU.mult)
            vtt(t1_, lam_im, are_t, ALU.mult)
            vtt(t2_, nr, aim_t, ALU.mult)
            vtt(t1_, t1_, t2_, ALU.subtract)
            vtt(cf_im, t1_, den, ALU.mult)
            wre = small("wre", (128, 16, NLEV)); wim = small("wim", (128, 16, NLEV))
            V(lambda: nc.vector.tensor_copy(out=wre[:, :, 0], in_=cs_[:]), r=[cs_.name], w=[wre.name])
            V(lambda: nc.vector.tensor_copy(out=wim[:, :, 0], in_=sn_[:]), r=[sn_.name], w=[wim.name])
            for k in range(1, NLEV):
                V(lambda k=k: nc.vector.tensor_tensor(out=tq[0][:], in0=wre[:, :, k - 1], in1=wre[:, :, k - 1], op=ALU.mult), r=[wre.name], w=[tq[0].name])
                V(lambda k=k: nc.vector.tensor_tensor(out=tq[1][:], in0=wim[:, :, k - 1], in1=wim[:, :, k - 1], op=ALU.mult), r=[wim.name], w=[tq[1].name])
                V(lambda k=k: nc.vector.tensor_tensor(out=wre[:, :, k], in0=tq[0][:], in1=tq[1][:], op=ALU.subtract), r=[tq[0].name, tq[1].name], w=[wre.name])
                V(lambda k=k: nc.vector.tensor_tensor(out=tq[2][:], in0=wre[:, :, k - 1], in1=wim[:, :, k - 1], op=ALU.mult), r=[wre.name, wim.name], w=[tq[2].name])
                V(lambda k=k: nc.vector.tensor_scalar(out=wim[:, :, k], in0=tq[2][:], scalar1=2.0, scalar2=None, op0=ALU.mult), r=[tq[2].name], w=[wim.name])
            Bre = small("Bre", (128, 16, 16)); Bim = small("Bim", (128, 16, 16))
            bbr = small("bbr", (128, 16, 16)); bbi = small("bbi", (128, 16, 16)); btmp = small("btmp", (128, 16, 16))
            DMA(Bre[:], b_re.rearrange("(ft f) m -> f ft m", f=128), w=[Bre.name])
            DMA(Bim[:], b_im.rearrange("(ft f) m -> f ft m", f=128), w=[Bim.name])
            cfr_b = cf_re[:].unsqueeze(2).to_broadcast([128, 16, 16])
            cfi_b = cf_im[:].unsqueeze(2).to_broadcast([128, 16, 16])
            V(lambda: nc.vector.tensor_tensor(out=bbr[:], in0=Bre[:], in1=cfr_b, op=ALU.mult), r=[Bre.name, cf_re.name], w=[bbr.name])
            V(lambda: nc.vector.tensor_tensor(out=btmp[:], in0=Bim[:], in1=cfi_b, op=ALU.mult), r=[Bim.name, cf_im.name], w=[btmp.name])
            vtt(bbr, bbr, btmp, ALU.subtract)
            V(lambda: nc.vector.tensor_tensor(out=bbi[:], in0=Bim[:], in1=cfr_b, op=ALU.mult), r=[Bim.name, cf_re.name], w=[bbi.name])
            V(lambda: nc.vector.tensor_tensor(out=btmp[:], in0=Bre[:], in1=cfi_b, op=ALU.mult), r=[Bre.name, cf_im.name], w=[btmp.name])
            vtt(bbi, bbi, btmp, ALU.add)
            BT = small("BT", (128, 16, 2, 128), BF16)
            CT = small("CT", (128, 16, 2, 128), BF16)
            V(lambda: nc.vector.memset(CT[:], 0.0), w=[CT.name])
            Xb = small("Xb", (128, 128))
            p_s = ps("p2_s", [128, 512], F32, e2)
            for ft in range(16):
                ct, q = ft // 4, ft % 4
                for ri, bb in enumerate((bbr, bbi)):
                    V(lambda: nc.vector.memset(Xb[:], 0.0), w=[Xb.name])
                    V(lambda bb=bb, ft=ft, q=q: nc.vector.tensor_copy(out=Xb[0:64, 32 * q:32 * q + 16], in_=bb[0:64, ft, :]), r=[bb.name], w=[Xb.name])
                    V(lambda bb=bb, ft=ft, q=q: nc.vector.tensor_copy(out=Xb[64:128, 32 * q + 16:32 * q + 32], in_=bb[64:128, ft, :]), r=[bb.name], w=[Xb.name])
                    PE(lambda: nc.tensor.transpose(out=p_s[:, 0:128], in_=Xb[:], identity=identf[:]), r=[Xb.name, "identf"], w=[p_s.name])
                    V(lambda ft=ft, ri=ri: nc.vector.tensor_copy(out=BT[:, ft, ri, :], in_=p_s[:, 0:128]),
                      r=[p_s.name], w=[BT.name])
            Cre = small("Cre", (32, 16, 64)); Cim = small("Cim", (32, 16, 64))
            mA = small("mA", (32, 1)); mB = small("mB", (32, 1)); Xc = small("Xc", (32, 128))
            DMA(Cre[:], c_re.rearrange("(ft r) p -> r ft p", r=32), w=[Cre.name])
            DMA(Cim[:], c_im.rearrange("(ft r) p -> r ft p", r=32), w=[Cim.name])
            V(lambda: nc.vector.memset(mA[:], 0.0), w=[mA.name])
            V(lambda: nc.vector.memset(mA[0:16, :], 1.0), w=[mA.name])
            V(lambda: nc.vector.tensor_scalar(out=mB[:], in0=mA[:], scalar1=-1.0, scalar2=1.0, op0=ALU.mult, op1=ALU.add), r=[mA.name], w=[mB.name])
            for ft in range(16):
                for ri, cc in enumerate((Cre, Cim)):
                    sgn = 1.0 if ri == 0 else -1.0
                    V(lambda cc=cc, ft=ft, sgn=sgn: nc.vector.tensor_scalar(out=Xc[:, 0:64], in0=cc[:, ft, :], scalar1=mA[:, 0:1], scalar2=sgn,
                                                                          op0=ALU.mult, op1=ALU.mult), r=[cc.name, mA.name], w=[Xc.name])
                    V(lambda cc=cc, ft=ft, sgn=sgn: nc.vector.tensor_scalar(out=Xc[:, 64:128], in0=cc[:, ft, :], scalar1=mB[:, 0:1], scalar2=sgn,
                                                                          op0=ALU.mult, op1=ALU.mult), r=[cc.name, mB.name], w=[Xc.name])
                    PE(lambda: nc.tensor.transpose(out=p_s[:, 0:32], in_=Xc[:], identity=identf[0:32, 0:32]), r=[Xc.name, "identf"], w=[p_s.name])
                    V(lambda ft=ft, ri=ri: nc.vector.tensor_copy(out=CT[:, ft, ri, 32 * (ft % 4):32 * (ft % 4) + 32], in_=p_s[:, 0:32]), r=[p_s.name], w=[CT.name])
            dcol = small("dcol", (128, 4))
            with nc.allow_non_contiguous_dma(reason="tiny"):
                DMA(dcol[:], ssm_d.rearrange("(c p) -> p c", p=128), w=[dcol.name])
            h0r = small("h0r", (128, 16, NS)); h0i = small("h0i", (128, 16, NS))
            h1r = small("h1r", (128, 16, NS)); h1i = small("h1i", (128, 16, NS))
            sst = small("sst", (NS, 2048))
            for (src, dst) in ((st_re, h0r), (st_im, h0i)):
                DMA(sst[:], src[:, :], w=[sst.name])
                for ft in range(16):
                    PE(lambda ft=ft: nc.tensor.transpose(out=p_s[:, ft * NS:(ft + 1) * NS], in_=sst[:, ft * 128:(ft + 1) * 128],
                                                         identity=identf[0:NS, 0:NS]), r=[sst.name, "identf"], w=[p_s.name])
                V(lambda dst=dst: nc.vector.tensor_copy(out=dst[:].rearrange("p a b -> p (a b)"), in_=p_s[:, 0:16 * NS]), r=[p_s.name], w=[dst.name])

            WTOK = SEQ
            ufst = [sb(f"ufst{i}", [128, 512], F32, e2) for i in range(2)]
            ub = sb("ub", [128, NB, SEQ], BF16, e2)
            ufs = sb("ufs", [128, NS], F32, e2)
            ubs = sb("ubs", [128, NS], BF16, e2)
            Ec = sb("Ec", [128, SEQ], F32, e2)
            Es = sb("Es", [128, SEQ], F32, e2)
            rdec = sb("rdec", [128, 512], F32, e2)
            yT = sb("yT", [128, NB, SEQ], F32, e2)
            yTs = sb("yTs", [128, NS], F32, e2)
            fin = sb("fin", [128, 16, NB, 2], F32, e2)
            tm8 = [sb(f"tm{i}", [128, 512], F32, e2) for i in range(8)]
            tm = tm8[0:4]
            gre = [sb(f"gre{i}", [128, 512], F32, e2) for i in range(2)]
            gim = [sb(f"gim{i}", [128, 512], F32, e2) for i in range(2)]
            Gre = [sb(f"Gre{i}", [128, 512], F32, e2) for i in range(2)]
            Gim = [sb(f"Gim{i}", [128, 512], F32, e2) for i in range(2)]
            dm8 = [sb(f"dm{i}", [128, 512], F32, e2) for i in range(8)]
            dm = dm8[0:4]
            hre = [sb(f"hre{i}", [128, 512], BF16, e2) for i in range(2)]
            him = [sb(f"him{i}", [128, 512], BF16, e2) for i in range(2)]
            gl = [sb(f"gl{i}", [128, 512], F32, e2) for i in range(3)]
            gyb = sb("gyb", [128, 512], BF16, e2)
            p_br = [ps(f"p_br{i}", [128, 512], F32, e2) for i in range(2)]
            p_bi = [ps(f"p_bi{i}", [128, 512], F32, e2) for i in range(2)]
            p_y = [ps(f"p_y{i}", [128, 512], F32, e2) for i in range(2)]
            samp0 = (NTT - 1) * 128
            it = 0
            for ct in range(4):
                for b in range(NB):
                    for c in range(NCH):
                        us_ = ufst[(b * NCH + c) % 2]
                        DMA(us_[:], uT_scr[ct * 128:(ct + 1) * 128, b * SEQ + c * 512: b * SEQ + (c + 1) * 512],
                            r=[("uT", (b * SEQ + c * 512) // 128 + i_) for i_ in range(4)], w=[us_.name])
                        G(lambda b=b, c=c, us_=us_: nc.gpsimd.tensor_copy(out=ub[:, b, c * 512:(c + 1) * 512], in_=us_[:]), r=[us_.name], w=["ub"])
                DMA(ufs[:], uT_scr[ct * 128:(ct + 1) * 128, samp0:samp0 + NS], r=[("uT", NTT - 1)], w=["ufs"])
                G(lambda: nc.gpsimd.tensor_copy(out=ubs[:], in_=ufs[:]), r=["ufs"], w=["ubs"])
                for q in range(4):
                    ft = ct * 4 + q
                    qs = slice(32 * q, 32 * q + 32)
                    V(lambda: nc.vector.memset(Ec[:, 0:1], 1.0), w=["Ec"])
                    V(lambda: nc.vector.memset(Es[:, 0:1], 0.0), w=["Es"])
                    for k in range(NLEV):
                        n = 1 << k
                        wr = wre[:, ft, k:k + 1]
                        wi = wim[:, ft, k:k + 1]
                        for n0 in range(0, n, 512):
                            nn = min(512, n - n0)
                            lo_ = slice(n0, n0 + nn)
                            hi_ = slice(n + n0, n + n0 + nn)
                            A(lambda wi=wi, lo_=lo_, nn=nn: nc.scalar.activation(out=tm[0][:, 0:nn], in_=Es[:, lo_], func=AF.Identity, scale=wi),
                              r=["Es", wim.name], w=[tm[0].name])
                            V(lambda wr=wr, lo_=lo_, hi_=hi_, nn=nn: nc.vector.scalar_tensor_tensor(out=Ec[:, hi_], in0=Ec[:, lo_], scalar=wr, in1=tm[0][:, 0:nn],
                                                                                                  op0=ALU.mult, op1=ALU.subtract),
                              r=["Ec", tm[0].name, wre.name], w=["Ec"])
                            A(lambda wi=wi, lo_=lo_, nn=nn: nc.scalar.activation(out=tm[1][:, 0:nn], in_=Ec[:, lo_], func=AF.Identity, scale=wi),
                              r=["Ec", wim.name], w=[tm[1].name])
                            V(lambda wr=wr, lo_=lo_, hi_=hi_, nn=nn: nc.vector.scalar_tensor_tensor(out=Es[:, hi_], in0=Es[:, lo_], scalar=wr, in1=tm[1][:, 0:nn],
                                                                                                  op0=ALU.mult, op1=ALU.add),
                              r=["Es", tm[1].name, wre.name], w=["Es"])
                    V(lambda ft=ft: nc.vector.tensor_scalar(out=rdec[:], in0=Ec[:, 0:512], scalar1=0.0, scalar2=lam_abs[:, ft:ft + 1],
                                                            op0=ALU.mult, op1=ALU.add), r=["Ec", lam_abs.name], w=["rdec"])
                    chunks = [(b, c) for b in range(NB) for c in range(NCH)]
                    base_it = it

                    def stage_A(j):
                        b, c = chunks[j]
                        itj = base_it + j
                        cs = slice(c * 512, (c + 1) * 512)
                        pr, pi = p_br[itj % 2], p_bi[itj % 2]
                        PE(lambda: nc.tensor.matmul(pr[:], lhsT=BT[:, ft, 0, :], rhs=ub[:, b, cs], start=True, stop=True), r=[BT.name, "ub"], w=[pr.name])
                        PE(lambda: nc.tensor.matmul(pi[:], lhsT=BT[:, ft, 1, :], rhs=ub[:, b, cs], start=True, stop=True), r=[BT.name, "ub"], w=[pi.name])
                        g_r, g_i, G_r, G_i = gre[itj % 2], gim[itj % 2], Gre[itj % 2], Gim[itj % 2]
                        tm = tm8[4 * (itj % 2):4 * (itj % 2) + 4]
                        V(lambda: nc.vector.tensor_tensor(out=tm[0][:], in0=pr[:], in1=Ec[:, cs], op=ALU.mult), r=[pr.name, "Ec"], w=[tm[0].name])
                        V(lambda: nc.vector.tensor_tensor(out=tm[1][:], in0=pi[:], in1=Es[:, cs], op=ALU.mult), r=[pi.name, "Es"], w=[tm[1].name])
                        V(lambda: nc.vector.tensor_tensor(out=tm[2][:], in0=pi[:], in1=Ec[:, cs], op=ALU.mult), r=[pi.name, "Ec"], w=[tm[2].name])
                        V(lambda: nc.vector.tensor_tensor(out=tm[3][:], in0=pr[:], in1=Es[:, cs], op=ALU.mult), r=[pr.name, "Es"], w=[tm[3].name])
                        V(lambda: nc.vector.tensor_tensor(out=g_r[:], in0=tm[0][:], in1=tm[1][:], op=ALU.add), r=[tm[0].name, tm[1].name], w=[g_r.name])
                        V(lambda: nc.vector.tensor_tensor(out=g_i[:], in0=tm[2][:], in1=tm[3][:], op=ALU.subtract), r=[tm[2].name, tm[3].name], w=[g_i.name])
                        if c == 0:
                            ini_r, ini_i, rd_extra = 0.0, 0.0, []
                        else:
                            pG_r, pG_i = Gre[(itj - 1) % 2], Gim[(itj - 1) % 2]
                            ini_r, ini_i = pG_r[:, 511:512], pG_i[:, 511:512]
                            rd_extra = [pG_r.name, pG_i.name]
                        V(lambda: nc.vector.tensor_tensor_scan(out=G_r[:], data0=rdec[:], data1=g_r[:], initial=ini_r, op0=ALU.mult, op1=ALU.add),
                          r=["rdec", g_r.name] + rd_extra, w=[G_r.name])
                        V(lambda: nc.vector.tensor_tensor_scan(out=G_i[:], data0=rdec[:], data1=g_i[:], initial=ini_i, op0=ALU.mult, op1=ALU.add),
                          r=["rdec", g_i.name] + rd_extra, w=[G_i.name])

                    def stage_B(j):
                        b, c = chunks[j]
                        itj = base_it + j
                        cs = slice(c * 512, (c + 1) * 512)
                        G_r, G_i = Gre[itj % 2], Gim[itj % 2]
                        dm = dm8[4 * (itj % 2):4 * (itj % 2) + 4]
                        h_r, h_i = hre[itj % 2], him[itj % 2]
                        G(lambda: nc.gpsimd.tensor_tensor(out=dm[0][:], in0=G_r[:], in1=Ec[:, cs], op=ALU.mult), r=[G_r.name, "Ec"], w=[dm[0].name])
                        G(lambda: nc.gpsimd.tensor_tensor(out=dm[1][:], in0=G_i[:], in1=Es[:, cs], op=ALU.mult), r=[G_i.name, "Es"], w=[dm[1].name])
                        G(lambda: nc.gpsimd.tensor_tensor(out=dm[2][:], in0=G_r[:], in1=Es[:, cs], op=ALU.mult), r=[G_r.name, "Es"], w=[dm[2].name])
                        G(lambda: nc.gpsimd.tensor_tensor(out=dm[3][:], in0=G_i[:], in1=Ec[:, cs], op=ALU.mult), r=[G_i.name, "Ec"], w=[dm[3].name])
                        G(lambda: nc.gpsimd.tensor_tensor(out=h_r[:], in0=dm[0][:], in1=dm[1][:], op=ALU.subtract), r=[dm[0].name, dm[1].name], w=[h_r.name])
                        G(lambda: nc.gpsimd.tensor_tensor(out=h_i[:], in0=dm[2][:], in1=dm[3][:], op=ALU.add), r=[dm[2].name, dm[3].name], w=[h_i.name])
                        if c == NCH - 1:
                            G(lambda: nc.gpsimd.tensor_tensor(out=fin[:, ft, b, 0:1], in0=dm[0][:, 511:512], in1=dm[1][:, 511:512], op=ALU.subtract),
                              r=[dm[0].name, dm[1].name], w=["fin"])
                            G(lambda: nc.gpsimd.tensor_tensor(out=fin[:, ft, b, 1:2], in0=dm[2][:, 511:512], in1=dm[3][:, 511:512], op=ALU.add),
                              r=[dm[2].name, dm[3].name], w=["fin"])
                        py = p_y[itj % 2]
                        PE(lambda: nc.tensor.matmul(py[:, :], lhsT=CT[:, ft, 0, :], rhs=h_r[:], start=True, stop=False), r=[CT.name, h_r.name], w=[py.name])
                        PE(lambda: nc.tensor.matmul(py[:, :], lhsT=CT[:, ft, 1, :], rhs=h_i[:], start=False, stop=True), r=[CT.name, h_i.name], w=[py.name], skip_same=True)
                        if q == 0:
                            A(lambda: nc.scalar.copy(out=yT[:, b, cs], in_=py[:, :]), r=[py.name], w=["yT"])
                        else:
                            V(lambda: nc.vector.tensor_tensor(out=yT[:, b, cs], in0=py[:, :], in1=yT[:, b, cs], op=ALU.add), r=[py.name, "yT"], w=["yT"])

                    stage_A(0)
                    for j in range(len(chunks)):
                        if j + 1 < len(chunks):
                            stage_A(j + 1)
                        stage_B(j)
                    it += len(chunks)
                    pr, pi = p_br[it % 2], p_bi[it % 2]
                    PE(lambda pr=pr, ft=ft: nc.tensor.matmul(pr[:, 0:NS], lhsT=BT[:, ft, 0, :], rhs=ubs[:, :], start=True, stop=True),
                       r=[BT.name, "ubs"], w=[pr.name])
                    PE(lambda pi=pi, ft=ft: nc.tensor.matmul(pi[:, 0:NS], lhsT=BT[:, ft, 1, :], rhs=ubs[:, :], start=True, stop=True),
                       r=[BT.name, "ubs"], w=[pi.name])
                    lr, li = lam_re[:, ft:ft + 1], lam_im[:, ft:ft + 1]
                    V(lambda ft=ft, li=li: nc.vector.tensor_scalar(out=tm[0][:, 0:NS], in0=h0i[:, ft, :], scalar1=li, scalar2=None, op0=ALU.mult),
                      r=[h0i.name, lam_im.name], w=[tm[0].name])
                    V(lambda ft=ft, lr=lr: nc.vector.scalar_tensor_tensor(out=tm[1][:, 0:NS], in0=h0r[:, ft, :], scalar=lr, in1=tm[0][:, 0:NS], op0=ALU.mult, op1=ALU.subtract),
                      r=[h0r.name, lam_re.name, tm[0].name], w=[tm[1].name])
                    V(lambda ft=ft, pr=pr: nc.vector.tensor_tensor(out=h1r[:, ft, :], in0=pr[:, 0:NS], in1=tm[1][:, 0:NS], op=ALU.add), r=[pr.name, tm[1].name], w=[h1r.name])
                    V(lambda ft=ft, li=li: nc.vector.tensor_scalar(out=tm[2][:, 0:NS], in0=h0r[:, ft, :], scalar1=li, scalar2=None, op0=ALU.mult),
                      r=[h0r.name, lam_im.name], w=[tm[2].name])
                    V(lambda ft=ft, lr=lr: nc.vector.scalar_tensor_tensor(out=tm[3][:, 0:NS], in0=h0i[:, ft, :], scalar=lr, in1=tm[2][:, 0:NS], op0=ALU.mult, op1=ALU.add),
                      r=[h0i.name, lam_re.name, tm[2].name], w=[tm[3].name])
                    V(lambda ft=ft, pi=pi: nc.vector.tensor_tensor(out=h1i[:, ft, :], in0=pi[:, 0:NS], in1=tm[3][:, 0:NS], op=ALU.add), r=[pi.name, tm[3].name], w=[h1i.name])
                    h_r, h_i = hre[it % 2], him[it % 2]
                    V(lambda h_r=h_r, ft=ft: nc.vector.tensor_copy(out=h_r[:, 0:NS], in_=h1r[:, ft, :]), r=[h1r.name], w=[h_r.name])
                    V(lambda h_i=h_i, ft=ft: nc.vector.tensor_copy(out=h_i[:, 0:NS], in_=h1i[:, ft, :]), r=[h1i.name], w=[h_i.name])
                    py = p_y[it % 2]
                    PE(lambda py=py, h_r=h_r, ft=ft, qs=qs: nc.tensor.matmul(py[:, 0:NS], lhsT=CT[:, ft, 0, :], rhs=h_r[:, 0:NS], start=True, stop=False),
                       r=[CT.name, h_r.name], w=[py.name])
                    PE(lambda py=py, h_i=h_i, ft=ft, qs=qs: nc.tensor.matmul(py[:, 0:NS], lhsT=CT[:, ft, 1, :], rhs=h_i[:, 0:NS], start=False, stop=True),
                       r=[CT.name, h_i.name], w=[py.name], skip_same=True)
                    if q == 0:
                        A(lambda py=py: nc.scalar.copy(out=yTs[:, :], in_=py[:, 0:NS]), r=[py.name], w=["yTs"])
                    else:
                        V(lambda py=py: nc.vector.tensor_tensor(out=yTs[:, :], in0=py[:, 0:NS], in1=yTs[:, :], op=ALU.add), r=[py.name, "yTs"], w=["yTs"])
                    it += 1
                dsc = dcol[:, ct:ct + 1]

                def gelu_block(ysrc, usrc, n, dst_dram, rkeys, wkey):
                    a0, a1, a2 = gl[0][:, 0:n], gl[1][:, 0:n], gl[2][:, 0:n]
                    V(lambda: nc.vector.scalar_tensor_tensor(out=a0, in0=usrc, scalar=dsc, in1=ysrc, op0=ALU.mult, op1=ALU.add),
                      r=rkeys + [dcol.name], w=[gl[0].name])
                    A(lambda: nc.scalar.activation(out=a1, in_=a0, func=AF.Square), r=[gl[0].name], w=[gl[1].name])
                    V(lambda: nc.vector.tensor_scalar(out=a1, in0=a1, scalar1=0.044715, scalar2=1.0, op0=ALU.mult, op1=ALU.add), r=[gl[1].name], w=[gl[1].name])
                    G(lambda: nc.gpsimd.tensor_tensor(out=a2, in0=a1, in1=a0, op=ALU.mult), r=[gl[0].name, gl[1].name], w=[gl[2].name])
                    A(lambda: nc.scalar.activation(out=a2, in_=a2, func=AF.Sigmoid, scale=1.5957691216057308), r=[gl[2].name], w=[gl[2].name])
                    V(lambda: nc.vector.tensor_tensor(out=gyb[:, 0:n], in0=a0, in1=a2, op=ALU.mult), r=[gl[0].name, gl[2].name], w=["gyb"])
                    DMA(dst_dram, gyb[:, 0:n], r=["gyb"], w=[wkey])

                for b in range(NB):
                    for c in range(NCH):
                        cs = slice(c * 512, (c + 1) * 512)
                        t0 = b * SEQ + c * 512
                        us_ = ufst[(b * NCH + c) % 2]
                        DMA(us_[:], uT_scr[ct * 128:(ct + 1) * 128, t0:t0 + 512], r=[("uT", t0 // 128 + i_) for i_ in range(4)], w=[us_.name])
                        gelu_block(yT[:, b, cs], us_[:], 512, gyT_scr[ct * 128:(ct + 1) * 128, t0:t0 + 512], ["yT", us_.name], ("gyT", ct, b, c))
                gelu_block(yTs[:], ufs[:], NS, gyT_scr[ct * 128:(ct + 1) * 128, samp0:samp0 + NS], ["yTs", "ufs"], ("gyT", ct, "s"))
            with nc.allow_non_contiguous_dma(reason="state out"):
                for b in range(NB):
                    DMA(re_p[b:b + 1, :].rearrange("o (ft f) -> f (o ft)", f=128), fin[:, :, b, 0], r=["fin"], q="pool", is_output=True)
                    DMA(im_p[b:b + 1, :].rearrange("o (ft f) -> f (o ft)", f=128), fin[:, :, b, 1], r=["fin"], q="pool", is_output=True)
            for (src, dst) in ((h1r, re_s), (h1i, im_s)):
                for half in range(4):
                    for j in range(4):
                        ft = half * 4 + j
                        PE(lambda src=src, ft=ft, j=j: nc.tensor.transpose(out=p_s[0:NS, j * 128:(j + 1) * 128], in_=src[:, ft, :], identity=identf[:]),
                           r=[src.name, "identf"], w=[p_s.name])
                    V(lambda half=half: nc.vector.tensor_copy(out=sst[:, half * 512:(half + 1) * 512], in_=p_s[0:NS, :]), r=[p_s.name], w=[sst.name])
                DMA(dst[:, :], sst[:], r=[sst.name], q="pool", is_output=True)
            S.barrier()

    NIT = 18
    if "p3" in phases:
        assert NB == 2
        with contextlib.ExitStack() as e3:
            TOPK = cfg.TOPK
            kTa = [sb(f"kTa{b}", [65, 2, SEQ], BF16, e3) for b in range(NB)]
            kiT = [sb(f"kiT{b}", [64, SEQ], BF16, e3) for b in range(NB)]
            V1 = [sb(f"V1_{b}", [128, NT, 2, 65], BF16, e3) for b in range(NB)]
            Irep = sb("Irep", [128, 4, 128], BF16, e3)
            pw2 = sb("pw2", [128, NIT + 1], F32, e3)
            kmaxb = sb("kmaxb", [128, 1], F32, e3)
            km1 = sb("km1", [128, 1], F32, e3)
            km2 = sb("km2", [1, 128], F32, e3)
            zrow = sb("zrow", [1, 512], BF16, e3)
            qTa = [[sb(f"qTa{b}_{i}", [65, 8, 128], BF16, e3) for i in range(2)] for b in range(NB)]
            qiT = [[sb(f"qiT{b}_{i}", [64, 8, 128], BF16, e3) for i in range(2)] for b in range(NB)]
            score2 = [[sb(f"score{j}_{b}", [128, SEQ], F32, e3) for b in range(NB)] for j in range(2)]
            eps1 = sb("eps1", [128, 1], F32, e3)
            stph = sb("stph", [128, NIT + 1], F32, e3)
            G(lambda: nc.gpsimd.memset(eps1[:], 1.0), w=["eps1"])
            NTC = 4
            tmpc = [sb(f"tmpc{i}", [128, 512], F32, e3) for i in range(NTC)]
            junkb = [sb("junkb0", [128, SEQ], mybir.dt.uint8, e3), sb("junkb1", [128, SEQ], BF16, e3)]
            mb = [sb(f"mb{b}", [128, SEQ], BF16, e3) for b in range(NB)]
            pT = [sb(f"pT{i}", [128, 512], BF16, e3) for i in range(4)]
            ao = [sb(f"ao{b}", [128, 8, 64], BF16, e3) for b in range(NB)]
            amax = [sb(f"amax{b}", [128, 1], F32, e3) for b in range(NB)]
            stp = [sb(f"stp{b}", [128, NIT + 1], F32, e3) for b in range(NB)]
            mid = [sb(f"mid{b}", [128, 1], F32, e3) for b in range(NB)]
            cnt = [sb(f"cnt{b}", [128, 1], F32, e3) for b in range(NB)]
            c2 = [sb(f"c2{b}", [128, 1], F32, e3) for b in range(NB)]
            thr = [sb(f"thr{b}", [128, 1], F32, e3) for b in range(NB)]
            rec = [sb(f"rec{b}", [128, 4], F32, e3) for b in range(NB)]
            p_ix = [ps(f"p_ix{i}", [128, 512], F32, e3) for i in range(2)]
            p_st = [ps(f"p_st{i}", [128, 512], F32, e3) for i in range(3)]
            p_o = [ps(f"p_o{j}", [128, 512], F32, e3) for j in range(2)]
            p_n = ps("p_n", [128, 4, 65], F32, e3)
            oT = [sb(f"oT{b}", [65, 2, 512], F32, e3) for b in range(NB)]
            p_m = p_ix[0]

            for k in range(NIT + 1):
                G(lambda k=k: nc.gpsimd.memset(pw2[:, k:k + 1], float(2.0 ** (-k))), w=["pw2"])
            G(lambda: nc.gpsimd.memset(zrow[:], 0.0), w=["zrow"])
            for j in range(4):
                V(lambda j=j: nc.vector.tensor_copy(out=Irep[:, j, :], in_=ident[:]), r=["ident"], w=["Irep"])
            V(lambda: nc.vector.tensor_reduce(out=km1[:], in_=kn2[:].rearrange("p a b -> p (a b)"), axis=AX.X, op=ALU.max), r=["kn2"], w=["km1"])
            PE(lambda: nc.tensor.transpose(out=p_m[0:1, 0:128], in_=km1[:], identity=identf[:]), r=["km1", "identf"], w=[p_m.name])
            V(lambda: nc.vector.tensor_reduce(out=km2[0:1, 0:1], in_=p_m[0:1, 0:128], axis=AX.X, op=ALU.max), r=[p_m.name], w=["km2"])
            A(lambda: nc.scalar.activation(out=km2[0:1, 0:1], in_=km2[0:1, 0:1], func=AF.Sqrt), r=["km2"], w=["km2"])
            PE(lambda: nc.tensor.matmul(p_m[:, 0:1], lhsT=ones_f[0:1, :], rhs=km2[0:1, 0:1], start=True, stop=True), r=["ones_f", "km2"], w=[p_m.name])
            V(lambda: nc.vector.tensor_copy(out=kmaxb[:], in_=p_m[:, 0:1]), r=[p_m.name], w=["kmaxb"])

            for b in range(NB):
                t0 = b * SEQ
                for g in range(2):
                    DMA(kTa[b][0:64, g, :], kT_scr[:, g, t0:t0 + SEQ], r=[("kT", b * NT + i_) for i_ in range(NT)], w=[kTa[b].name])
                V(lambda b=b: nc.vector.memset(kTa[b][64:65, :, :], 1.0), w=[kTa[b].name])
                DMA(kiT[b][:, :], kiT_scr[:, t0:t0 + SEQ], r=[("kiT", b * NT + i_) for i_ in range(NT)], w=[kiT[b].name])
                for g in range(2):
                    DMA(V1[b][:, :, g, 0:64], vb_scr[t0:t0 + SEQ, g, :].rearrange("(kb p) d -> p kb d", p=128),
                        r=[("vb", b * NT + i_) for i_ in range(NT)], w=[V1[b].name])
                V(lambda b=b: nc.vector.memset(V1[b][:, :, :, 64:65], 1.0), w=[V1[b].name])

            itc3 = [0, 0]
            thrc = [sb(f"thrc{j}", [128, 1], F32, e3) for j in range(2)]

            def slot_bufs(i):
                return [qTa[b][i % 2] for b in range(NB)], [qiT[b][i % 2] for b in range(NB)], [score2[i % 2][b] for b in range(NB)]

            def idx_stage(i, filler=None):
                Sk = (i + 1) * 128
                search = Sk > TOPK
                Ts = [b * NT + i for b in range(NB)]
                qa_, qi_, sc2 = slot_bufs(i)
                for b in range(NB):
                    DMA(qa_[b][:], qT_scr[Ts[b], :, :, :], r=[("qT", Ts[b])], w=[qa_[b].name])
                    DMA(qi_[b][:], qiT_scr[Ts[b], :, :, :], r=[("qiT", Ts[b])], w=[qi_[b].name])
                    V(lambda b=b: nc.vector.tensor_scalar(out=qa_[b][64:65, :, :], in0=qa_[b][64:65, :, :], scalar1=kmaxb[64:65, 0:1], scalar2=None, op0=ALU.mult),
                      r=[qa_[b].name, "kmaxb"], w=[qa_[b].name])
                for b in range(NB):
                    sc_ = sc2[b]
                    for c0 in range(0, Sk, 512):
                        cw = min(512, Sk - c0)
                        for h in range(8):
                            it = itc3[0]
                            px = p_ix[it % 2]
                            PE(lambda px=px, b=b, h=h, c0=c0, cw=cw: nc.tensor.matmul(px[:, 0:cw], lhsT=qi_[b][:, h, :], rhs=kiT[b][:, c0:c0 + cw], start=True, stop=True),
                               r=[qi_[b].name, kiT[b].name], w=[px.name])
                            lo_ = lohi[:, Ts[b], h:h + 1]
                            hi_ = lohi[:, Ts[b], 8 + h:9 + h]
                            if h == 0:
                                V(lambda px=px, sc_=sc_, c0=c0, cw=cw, lo_=lo_, hi_=hi_: nc.vector.tensor_scalar(out=sc_[:, c0:c0 + cw], in0=px[:, 0:cw], scalar1=lo_, scalar2=hi_,
                                                                                                              op0=ALU.max, op1=ALU.min), r=[px.name, "lohi"], w=[sc_.name])
                            else:
                                tc_ = tmpc[it % NTC]
                                V(lambda px=px, tc_=tc_, cw=cw, lo_=lo_, hi_=hi_: nc.vector.tensor_scalar(out=tc_[:, 0:cw], in0=px[:, 0:cw], scalar1=lo_, scalar2=hi_,
                                                                                                       op0=ALU.max, op1=ALU.min), r=[px.name, "lohi"], w=[tc_.name])
                                G(lambda tc_=tc_, sc_=sc_, c0=c0, cw=cw: nc.gpsimd.tensor_tensor(out=sc_[:, c0:c0 + cw], in0=sc_[:, c0:c0 + cw], in1=tc_[:, 0:cw], op=ALU.add),
                                  r=[tc_.name, sc_.name], w=[sc_.name])
                            itc3[0] += 1
                        if filler is not None:
                            filler()
                    if search:
                        G(lambda b=b, sc_=sc_: nc.gpsimd.tensor_reduce(out=amax[b][:], in_=sc_[:, 0:Sk], axis=AX.X, op=ALU.max, apply_absolute_value=True),
                          r=[sc_.name], w=[amax[b].name]) if False else \
                        V(lambda b=b, sc_=sc_: nc.vector.tensor_reduce(out=amax[b][:], in_=sc_[:, 0:Sk], axis=AX.X, op=ALU.max, apply_absolute_value=True),
                          r=[sc_.name], w=[amax[b].name])
                    G(lambda sc_=sc_: nc.gpsimd.tensor_tensor(out=sc_[:, Sk - 128:Sk], in0=sc_[:, Sk - 128:Sk], in1=cmask[:], op=ALU.add),
                      r=[sc_.name, "cmask"], w=[sc_.name])

            def srch_init(i):
                Sk = (i + 1) * 128
                qa_, qi_, sc2 = slot_bufs(i)
                if Sk > TOPK:
                    V(lambda: nc.vector.tensor_scalar(out=amax[0][:], in0=amax[0][:], scalar1=1.0, scalar2=None, op0=ALU.add), r=[amax[0].name], w=[amax[0].name])
                    V(lambda: nc.vector.tensor_scalar(out=stp[0][:], in0=pw2[:], scalar1=amax[0][:, 0:1], scalar2=None, op0=ALU.mult), r=["pw2", amax[0].name], w=[stp[0].name])
                    V(lambda: nc.vector.memset(mid[0][:], 0.0), w=[mid[0].name])
                    A(lambda: nc.scalar.activation(out=amax[1][:], in_=amax[1][:], func=AF.Identity, bias=eps1[:, 0:1], scale=1.0), r=[amax[1].name, "eps1"], w=[amax[1].name])
                    A(lambda: nc.scalar.activation(out=stp[1][:], in_=pw2[:], func=AF.Identity, scale=amax[1][:, 0:1]), r=["pw2", amax[1].name], w=[stp[1].name])
                    A(lambda: nc.scalar.activation(out=stph[:], in_=stp[1][:], func=AF.Identity, scale=-0.5), r=[stp[1].name], w=["stph"])
                    A(lambda: nc.scalar.activation(out=mid[1][:], in_=zer_f[:, 0:1], func=AF.Identity), r=["zer_f"], w=[mid[1].name])
                    tcst = thrc[i % 2]
                    V(lambda tcst=tcst: nc.vector.memset(tcst[:], -(float(2 * TOPK - Sk) - 0.5)), w=[tcst.name])

            def srch_iter(i, k):
                Sk = (i + 1) * 128
                qa_, qi_, sc2 = slot_bufs(i)
                tcst = thrc[i % 2]
                V(lambda: nc.vector.tensor_scalar(out=junkb[0][:, 0:Sk], in0=sc2[0][:, 0:Sk], scalar1=mid[0][:, 0:1], scalar2=None, op0=ALU.is_ge,
                                                  op1=ALU.add, accum_out=cnt[0][:]), r=[sc2[0].name, mid[0].name], w=[junkb[0].name, cnt[0].name])
                A(lambda: nc.scalar.activation(out=junkb[1][:, 0:Sk], in_=sc2[1][:, 0:Sk], func=AF.Sign, bias=mid[1][:, 0:1], scale=1.0, accum_out=cnt[1][:]),
                  r=[sc2[1].name, mid[1].name], w=[junkb[1].name, cnt[1].name])
                V(lambda: nc.vector.tensor_scalar(out=c2[0][:], in0=cnt[0][:], scalar1=float(TOPK), scalar2=-0.5, op0=ALU.is_ge, op1=ALU.add),
                  r=[cnt[0].name], w=[c2[0].name])
                V(lambda: nc.vector.scalar_tensor_tensor(out=mid[0][:], in0=c2[0][:], scalar=stp[0][:, k:k + 1], in1=mid[0][:], op0=ALU.mult, op1=ALU.add),
                  r=[c2[0].name, stp[0].name, mid[0].name], w=[mid[0].name])
                A(lambda: nc.scalar.activation(out=c2[1][:], in_=cnt[1][:], func=AF.Sign, bias=tcst[:, 0:1], scale=1.0), r=[cnt[1].name, tcst.name], w=[c2[1].name])
                A(lambda: nc.scalar.activation(out=mid[1][:], in_=c2[1][:], func=AF.Identity, scale=stph[:, k:k + 1], bias=mid[1][:, 0:1]),
                  r=[c2[1].name, "stph", mid[1].name], w=[mid[1].name])

            def srch_fin(i):
                Sk = (i + 1) * 128
                Ts = [b * NT + i for b in range(NB)]
                qa_, qi_, sc2 = slot_bufs(i)
                if Sk > TOPK:
                    V(lambda: nc.vector.tensor_tensor(out=thr[0][:], in0=mid[0][:], in1=stp[0][:, NIT:NIT + 1], op=ALU.subtract), r=[mid[0].name, stp[0].name], w=[thr[0].name])
                    V(lambda: nc.vector.scalar_tensor_tensor(out=thr[1][:], in0=mid[1][:], scalar=-1.0, in1=stp[1][:, NIT:NIT + 1], op0=ALU.mult, op1=ALU.subtract),
                      r=[mid[1].name, stp[1].name], w=[thr[1].name])
                else:
                    for b in range(NB):
                        V(lambda b=b: nc.vector.memset(thr[b][:], -1.0e29), w=[thr[b].name])
                for b in range(NB):
                    V(lambda b=b: nc.vector.tensor_scalar(out=mb[b][:, 0:Sk], in0=sc2[b][:, 0:Sk], scalar1=thr[b][:, 0:1], scalar2=BIGM, op0=ALU.is_lt, op1=ALU.mult),
                      r=[sc2[b].name, thr[b].name], w=[mb[b].name])
                    if debug and Ts[b] == NT - 1:
                        DMA(dbg_mb[:, 0:Sk], mb[b][:, 0:Sk], r=[mb[b].name], w=["dbg_mb"])
                        DMA(dbg_sc[:, 0:Sk], sc2[b][:, 0:Sk], r=[sc2[b].name], w=["dbg_sc"])
                        DMA(dbg_thr[:, :], thr[b][:], r=[thr[b].name], w=["dbg_thr"])

            def att_stage(i, b, g):
                qa_, qi_, sc2 = slot_bufs(i)
                po = p_o[(2 * b + g) % 2]
                base = itc3[1]

                def qk(kb):
                    ia = base + kb
                    pst = p_st[ia % 3]
                    pt_ = pT[ia % 4]
                    ks = slice(kb * 128, (kb + 1) * 128)
                    PE(lambda: nc.tensor.matmul(pst[:], lhsT=kTa[b][:, g, ks], rhs=qa_[b][:, 4 * g:4 * g + 4, :].rearrange("p a b -> p (a b)"),
                                                start=True, stop=False), r=[kTa[b].name, qa_[b].name], w=[pst.name])
                    PE(lambda: nc.tensor.matmul(pst[:], lhsT=mb[b][:, ks], rhs=Irep[:].rearrange("p a b -> p (a b)"), start=False, stop=True),
                       r=[mb[b].name, "Irep"], w=[pst.name], skip_same=True)
                    A(lambda: nc.scalar.activation(out=pt_[:], in_=pst[:], func=AF.Exp, scale=0.125), r=[pst.name], w=[pt_.name])

                def pv(kb):
                    ia = base + kb
                    pt_ = pT[ia % 4]
                    PE(lambda: nc.tensor.matmul(po[0:65, :], lhsT=V1[b][:, kb, g, :], rhs=pt_[:], start=(kb == 0), stop=(kb == i)),
                       r=[pt_.name, V1[b].name], w=[po.name], skip_same=True)

                qk(0)
                if i >= 1:
                    qk(1)
                for kb in range(i + 1):
                    if kb + 2 <= i:
                        qk(kb + 2)
                    pv(kb)
                itc3[1] += i + 1
                A(lambda po=po: nc.scalar.copy(out=oT[b][0:65, g, :], in_=po[0:65, :]), r=[po.name], w=[oT[b].name])

            def norm_stage(i):
                Ts = [b * NT + i for b in range(NB)]
                for b in range(NB):
                    for g in range(2):
                        for h4 in range(4):
                            PE(lambda b=b, g=g, h4=h4: nc.tensor.transpose(out=p_n[:, h4, :], in_=oT[b][0:65, g, h4 * 128:(h4 + 1) * 128], identity=identf[0:65, 0:65]),
                               r=[oT[b].name, "identf"], w=["p_n"], skip_same=True)
                        V(lambda b=b: nc.vector.reciprocal(out=rec[b][:], in_=p_n[:, :, 64]), r=["p_n"], w=[rec[b].name])
                        V(lambda b=b, g=g: nc.vector.tensor_tensor(out=ao[b][:, 4 * g:4 * g + 4, :], in0=p_n[:, :, 0:64], in1=rec[b][:].unsqueeze(2).to_broadcast([128, 4, 64]),
                                                                   op=ALU.mult), r=["p_n", rec[b].name], w=[ao[b].name])
                    DMA(ao_scr[Ts[b] * 128:(Ts[b] + 1) * 128, :], ao[b][:].rearrange("p a b -> p (a b)"), r=[ao[b].name], w=[("ao", Ts[b])])

            idx_stage(0)
            for i in range(NT):
                Sk_i = (i + 1) * 128
                n_it = NIT if Sk_i > TOPK else 0
                srch_init(i)
                kdone = [0]
                if i + 1 < NT:
                    nslots = NB * (((i + 2) * 128 + 511) // 512)
                    per = -(-n_it // nslots) if n_it else 0

                    def filler(i=i, per=per, kdone=kdone, n_it=n_it):
                        for _ in range(per):
                            if kdone[0] < n_it:
                                srch_iter(i, kdone[0])
                                kdone[0] += 1
                    idx_stage(i + 1, filler)
                while kdone[0] < n_it:
                    srch_iter(i, kdone[0])
                    kdone[0] += 1
                srch_fin(i)
                if i >= 1:
                    norm_stage(i - 1)
                for b in range(NB):
                    for g in range(2):
                        att_stage(i, b, g)
            norm_stage(NT - 1)
            S.barrier()

    if "p3s" in phases:
        NIT = 22
        with contextlib.ExitStack() as e4:
            NPG = cfg.NPG
            assert NPG == 64 and NS % 2 == 0
            TOPK_S = cfg.TOPK_S
            TS = NTT - 1
            samp0 = TS * 128
            NO = 129
            lohi_scr = dscr("lohi_scr", [NS, 16], F32)
            DMA(lohi_scr[:, :], lohi[0:NS, TS, :], r=["lohi"], w=["lohi_scr"])
            lohiB = sb("lohiB", [128, NS, 16], F32, e4)
            DMA(lohiB[:].rearrange("p a b -> p (a b)"), lohi_scr.rearrange("(o a) b -> o (a b)", o=1).partition_broadcast(128),
                r=["lohi_scr"], w=["lohiB"])
            qTs = sb("qTs", [65, 8, 128], BF16, e4)
            qiTs = sb("qiTs", [64, 8, 128], BF16, e4)
            DMA(qTs[:], qT_scr[TS, :, :, :], r=[("qT", TS)], w=["qTs"])
            DMA(qiTs[:], qiT_scr[TS, :, :, :], r=[("qiT", TS)], w=["qiTs"])
            idx = sb("idx", [128, 1], I32, e4)
            idxf = sb("idxf", [128, 1], F32, e4)
            idxa = sb("idxa", [128, 1], I32, e4)
            idxb = sb("idxb", [128, 1], I32, e4)
            KI = sb("KI", [128, 128, 64], F32, e4)
            KIx = sb("KIx", [128, 64], F32, e4)
            Kh = [sb(f"Kh{i}", [128, 64, 128], F32, e4) for i in range(2)]
            Kx = sb("Kx", [128, 128], F32, e4)
            Vh = sb("Vh", [128, 64, 128], F32, e4)
            Vx = sb("Vx", [128, 128], F32, e4)
            V1h = sb("V1h", [128, 64, 2, 65], BF16, e4)
            V1x = sb("V1x", [128, 2, 65], BF16, e4)
            kT4 = [sb(f"kT4_{i}", [65, 4, 128], BF16, e4) for i in range(2)]
            qis = sb("qis", [64, 2, 8], BF16, e4)
            qas = sb("qas", [65, 2, 2, 4], BF16, e4)
            sc = sb("sc_s", [128, NO], F32, e4)
            cl = sb("cl_s", [128, 32, 16], F32, e4)
            red = sb("red_s", [128, 32, 2], F32, e4)
            colmask = sb("colmask", [128, 1], F32, e4)
            cross = sb("cross", [128, 16], F32, e4)
            maskfull = sb("maskfull", [128, NO, 16], F32, e4)
            blk1 = sb("blk1", [128, 128], F32, e4)
            pw2s = sb("pw2s", [128, NIT + 1], F32, e4)
            am1 = sb("am1", [128, 1], F32, e4)
            am2 = sb("am2", [1, 128], F32, e4)
            amb = sb("amb", [128, 1], F32, e4)
            stps = sb("stps", [128, NIT + 1], F32, e4)
            mids = sb("mids", [128, 1], F32, e4)
            cnts = sb("cnts", [128, 1], F32, e4)
            c2s = sb("c2s", [128, 1], F32, e4)
            thrs = sb("thrs", [128, 1], F32, e4)
            junks = sb("junks", [128, NO], F32, e4)
            sqk = Vh
            n2 = sb("n2", [128, 258], F32, e4)
            kmb = sb("kmb", [128, 1], F32, e4)
            tmps = sb("tmps", [128, 32, 16], F32, e4)
            pTs = [sb(f"pTs{i}", [128, 32, 16], BF16, e4) for i in range(2)]
            zrow_s = sb("zrow_s", [1, 512], BF16, e4)
            o8 = sb("o8", [8, 2, 64], BF16, e4)
            rec8 = sb("rec8", [8, 2], F32, e4)
            p_t = [ps(f"p3s_t{i}", [64, 4, 128], F32, e4) for i in range(2)]
            p_sx = [ps(f"p3s_x{i}", [128, 32, 16], F32, e4) for i in range(2)]
            p_os = ps("p3s_o", [8, 2, 65], F32, e4)
            p_ms = ps("p3s_m", [128, 512], F32, e4)

            for k in range(NIT + 1):
                G(lambda k=k: nc.gpsimd.memset(pw2s[:, k:k + 1], float(2.0 ** (-k))), w=["pw2s"])
            G(lambda: nc.gpsimd.memset(zrow_s[:], 0.0), w=["zrow_s"])
            V(lambda: nc.vector.memset(colmask[:], NEG), w=["colmask"])
            V(lambda: nc.vector.memset(colmask[0:1, :], 0.0), w=["colmask"])
            V(lambda: nc.vector.memset(colmask[64:65, :], 0.0), w=["colmask"])
            V(lambda: nc.vector.memset(cross[:], 0.0), w=["cross"])
            crv = cross[:].rearrange("p (g b h) -> p g b h", g=2, b=2)
            V(lambda: nc.vector.memset(crv[0:64, :, 1, :], BIGM), w=["cross"])
            V(lambda: nc.vector.memset(crv[64:128, :, 0, :], BIGM), w=["cross"])
            V(lambda: nc.vector.memset(blk1[:], 0.0), w=["blk1"])
            V(lambda: nc.vector.memset(blk1[0:64, 0:64], 1.0), w=["blk1"])
            V(lambda: nc.vector.memset(blk1[64:128, 64:128], 1.0), w=["blk1"])
            for t_ in kT4:
                V(lambda t_=t_: nc.vector.memset(t_[64:65, :, :], 1.0), w=[t_.name])
            V(lambda: nc.vector.memset(V1h[:, :, :, 64:65], 1.0), w=["V1h"])
            V(lambda: nc.vector.memset(V1x[:, :, 64:65], 1.0), w=["V1x"])

            def gather(dst2d, src2d, idx_t, wkey):
                S.dma("pool", lambda: nc.gpsimd.indirect_dma_start(out=dst2d, out_offset=None, in_=src2d,
                                                                   in_offset=bass.IndirectOffsetOnAxis(ap=idx_t[:, :], axis=0)),
                      reads=[idx_t.name], writes=[wkey])

            cache_kh = cache_k.rearrange("n (h e) -> (n h) e", h=2)
            cache_vh = cache_v.rearrange("n (h e) -> (n h) e", h=2)
            itc = [0]

            def transposed_units(units, consume):
                for u0 in range(0, len(units), 4):
                    grp = units[u0:u0 + 4]
                    pt_ = p_t[itc[0] % 2]
                    kt = kT4[itc[0] % 2]
                    for s_, (src, rk) in enumerate(grp):
                        PE(lambda pt_=pt_, s_=s_, src=src: nc.tensor.transpose(out=pt_[:, s_, :], in_=src, identity=identf[:]),
                           r=list(rk) + ["identf"], w=[pt_.name], skip_same=True)
                    n_ = len(grp)
                    if itc[0] % 2 == 0:
                        V(lambda pt_=pt_, kt=kt, n_=n_: nc.vector.tensor_copy(out=kt[0:64, 0:n_, :], in_=pt_[:, 0:n_, :]), r=[pt_.name], w=[kt.name])
                    else:
                        A(lambda pt_=pt_, kt=kt, n_=n_: nc.scalar.copy(out=kt[0:64, 0:n_, :], in_=pt_[:, 0:n_, :]), r=[pt_.name], w=[kt.name])
                    for s_ in range(n_):
                        consume(kt, s_, u0 + s_)
                    itc[0] += 1

            DMA(idx[:], ptab[0:2 * NPG, :], w=["idx"])
            gather(KI[:].rearrange("p a b -> p (a b)"), cache_ik[:, :], idx, "KI")
            for pr in range(NS // 2):
                b0 = 2 * pr
                V(lambda: nc.vector.tensor_copy(out=idxf[:], in_=idx[:]), r=["idx"], w=["idxf"])
                V(lambda: nc.vector.tensor_scalar(out=idxa[:], in0=idxf[:], scalar1=2.0, scalar2=None, op0=ALU.mult), r=["idxf"], w=["idxa"])
                V(lambda: nc.vector.tensor_scalar(out=idxb[:], in0=idxf[:], scalar1=2.0, scalar2=1.0, op0=ALU.mult, op1=ALU.add), r=["idxf"], w=["idxb"])
                gather(Kh[0][:].rearrange("p a b -> p (a b)"), cache_kh[:, :], idxa, "Kh0")
                gather(Kh[1][:].rearrange("p a b -> p (a b)"), cache_kh[:, :], idxb, "Kh1")
                for (xt_, src_) in ((KIx, ki_s), (Kx, k_s), (Vx, v_s)):
                    V(lambda xt_=xt_: nc.vector.memset(xt_[:], 0.0), w=[xt_.name])
                    DMA(xt_[0:1, :], src_[b0:b0 + 1, :], w=[xt_.name])
                    DMA(xt_[64:65, :], src_[b0 + 1:b0 + 2, :], w=[xt_.name])
                V(lambda b0=b0: nc.vector.tensor_copy(out=qis[:], in_=qiTs[:, :, b0:b0 + 2].rearrange("p h t -> p t h")), r=["qiTs"], w=["qis"])
                for g in range(2):
                    V(lambda b0=b0, g=g: nc.vector.tensor_copy(out=qas[:, g, :, :], in_=qTs[:, 4 * g:4 * g + 4, b0:b0 + 2].rearrange("p h t -> p t h")),
                      r=["qTs"], w=["qas"])
                for hf in range(2):
                    A(lambda hf=hf: nc.scalar.activation(out=sqk[:], in_=Kh[hf][:], func=AF.Square), r=[f"Kh{hf}"], w=["Vh"])
                    V(lambda hf=hf: nc.vector.tensor_reduce(out=n2[:, hf * 128:(hf + 1) * 128], in_=sqk[:].rearrange("p o (g d) -> p (o g) d", g=2), axis=AX.X, op=ALU.add),
                      r=["Vh"], w=["n2"])
                G(lambda: nc.gpsimd.tensor_tensor(out=sqk[:, 0, :], in0=Kx[:], in1=Kx[:], op=ALU.mult), r=["Kx"], w=["Vh"])
                V(lambda: nc.vector.tensor_reduce(out=n2[:, 256:258], in_=sqk[:, 0, :].rearrange("p (g d) -> p g d", g=2), axis=AX.X, op=ALU.add), r=["Vh"], w=["n2"])
                V(lambda: nc.vector.tensor_reduce(out=am1[:], in_=n2[:], axis=AX.X, op=ALU.max), r=["n2"], w=["am1"])
                PE(lambda: nc.tensor.transpose(out=p_ms[0:1, 0:128], in_=am1[:], identity=identf[:]), r=["am1", "identf"], w=[p_ms.name])
                V(lambda: nc.vector.tensor_reduce(out=am2[0:1, 0:1], in_=p_ms[0:1, 0:128], axis=AX.X, op=ALU.max), r=[p_ms.name], w=["am2"])
                A(lambda: nc.scalar.activation(out=am2[0:1, 0:1], in_=am2[0:1, 0:1], func=AF.Sqrt), r=["am2"], w=["am2"])
                PE(lambda: nc.tensor.matmul(p_ms[:, 0:1], lhsT=ones_f[0:1, :], rhs=am2[0:1, 0:1], start=True, stop=True), r=["ones_f", "am2"], w=[p_ms.name])
                V(lambda: nc.vector.tensor_copy(out=kmb[:], in_=p_ms[:, 0:1]), r=[p_ms.name], w=["kmb"])
                V(lambda: nc.vector.tensor_scalar(out=qas[64:65, :, :, :], in0=qas[64:65, :, :, :], scalar1=kmb[64:65, 0:1], scalar2=None, op0=ALU.mult),
                  r=["qas", "kmb"], w=["qas"])

                loB = lohiB[:, b0:b0 + 2, 0:8]
                hiB = lohiB[:, b0:b0 + 2, 8:16]

                def idx_group(units, o_lo, n_o):
                    px = p_sx[itc[0] % 2]

                    def consume(kt, s_, ui, px=px):
                        PE(lambda: nc.tensor.matmul(px[:, ui, :], lhsT=kt[0:64, s_, :], rhs=qis[:].rearrange("p a b -> p (a b)"), start=True, stop=True),
                           r=[kt.name, "qis"], w=[px.name], skip_same=True)
                    transposed_units(units, consume)
                    V(lambda: nc.vector.tensor_tensor(out=cl[:, 0:n_o, :].rearrange("p o (b h) -> p o b h", b=2), in0=px[:, 0:n_o, :].rearrange("p o (b h) -> p o b h", b=2),
                                                      in1=loB.unsqueeze(1).to_broadcast([128, n_o, 2, 8]), op=ALU.max), r=[px.name, "lohiB"], w=["cl_s"])
                    V(lambda: nc.vector.tensor_tensor(out=cl[:, 0:n_o, :].rearrange("p o (b h) -> p o b h", b=2), in0=cl[:, 0:n_o, :].rearrange("p o (b h) -> p o b h", b=2),
                                                      in1=hiB.unsqueeze(1).to_broadcast([128, n_o, 2, 8]), op=ALU.min), r=["cl_s", "lohiB"], w=["cl_s"])
                    V(lambda: nc.vector.tensor_reduce(out=red[:, 0:n_o, :], in_=cl[:, 0:n_o, :].rearrange("p o (b h) -> p o b h", b=2), axis=AX.X, op=ALU.add),
                      r=["cl_s"], w=["red_s"])
                    V(lambda: nc.vector.tensor_copy(out=sc[0:64, o_lo:o_lo + n_o], in_=red[0:64, 0:n_o, 0]), r=["red_s"], w=["sc_s"])
                    V(lambda: nc.vector.tensor_copy(out=sc[64:128, o_lo:o_lo + n_o], in_=red[64:128, 0:n_o, 1]), r=["red_s"], w=["sc_s"])

                for og in range(4):
                    idx_group([(KI[:, og * 32 + oo, :], ["KI"]) for oo in range(32)], og * 32, 32)
                idx_group([(KIx[:], ["KIx"])], 128, 1)
                if pr + 1 < NS // 2:
                    DMA(idx[:], ptab[(b0 + 2) * NPG:(b0 + 4) * NPG, :], r=["idxf"], w=["idx"])
                    gather(KI[:].rearrange("p a b -> p (a b)"), cache_ik[:, :], idx, "KI")
                V(lambda: nc.vector.tensor_reduce(out=am1[:], in_=sc[:], axis=AX.X, op=ALU.max, apply_absolute_value=True), r=["sc_s"], w=["am1"])
                V(lambda: nc.vector.tensor_tensor(out=sc[:, 128:129], in0=sc[:, 128:129], in1=colmask[:], op=ALU.add), r=["sc_s", "colmask"], w=["sc_s"])
                PE(lambda: nc.tensor.transpose(out=p_ms[0:1, 0:128], in_=am1[:], identity=identf[:]), r=["am1", "identf"], w=[p_ms.name])
                V(lambda: nc.vector.tensor_reduce(out=am2[0:1, 0:1], in_=p_ms[0:1, 0:128], axis=AX.X, op=ALU.max), r=[p_ms.name], w=["am2"])
                V(lambda: nc.vector.tensor_scalar(out=am2[0:1, 0:1], in0=am2[0:1, 0:1], scalar1=1.0, scalar2=None, op0=ALU.add), r=["am2"], w=["am2"])
                PE(lambda: nc.tensor.matmul(p_ms[:, 0:1], lhsT=ones_f[0:1, :], rhs=am2[0:1, 0:1], start=True, stop=True), r=["ones_f", "am2"], w=[p_ms.name])
                V(lambda: nc.vector.tensor_copy(out=amb[:], in_=p_ms[:, 0:1]), r=[p_ms.name], w=["amb"])
                V(lambda: nc.vector.tensor_scalar(out=stps[:], in0=pw2s[:], scalar1=amb[:, 0:1], scalar2=None, op0=ALU.mult), r=["pw2s", "amb"], w=["stps"])
                V(lambda: nc.vector.memset(mids[:], 0.0), w=["mids"])
                for k in range(NIT):
                    V(lambda: nc.vector.tensor_scalar(out=junks[:], in0=sc[:], scalar1=mids[:, 0:1], scalar2=None, op0=ALU.is_ge, op1=ALU.add, accum_out=cnts[:]),
                      r=["sc_s", "mids"], w=["junks", "cnts"])
                    PE(lambda: nc.tensor.matmul(p_ms[:, 0:1], lhsT=blk1[:], rhs=cnts[:], start=True, stop=True), r=["blk1", "cnts"], w=[p_ms.name])
                    V(lambda: nc.vector.tensor_scalar(out=c2s[:], in0=p_ms[:, 0:1], scalar1=float(TOPK_S), scalar2=-0.5, op0=ALU.is_ge, op1=ALU.add), r=[p_ms.name], w=["c2s"])
                    V(lambda k=k: nc.vector.scalar_tensor_tensor(out=mids[:], in0=c2s[:], scalar=stps[:, k:k + 1], in1=mids[:], op0=ALU.mult, op1=ALU.add),
                      r=["c2s", "stps", "mids"], w=["mids"])
                V(lambda: nc.vector.tensor_tensor(out=thrs[:], in0=mids[:], in1=stps[:, NIT:NIT + 1], op=ALU.subtract), r=["mids", "stps"], w=["thrs"])
                V(lambda: nc.vector.tensor_scalar(out=junks[:], in0=sc[:], scalar1=thrs[:, 0:1], scalar2=BIGM, op0=ALU.is_lt, op1=ALU.mult), r=["sc_s", "thrs"], w=["junks"])
                V(lambda: nc.vector.tensor_tensor(out=maskfull[:], in0=junks[:].unsqueeze(2).to_broadcast([128, NO, 16]),
                                                  in1=cross[:].unsqueeze(1).to_broadcast([128, NO, 16]), op=ALU.add), r=["junks", "cross"], w=["maskfull"])
                PE(lambda: nc.tensor.matmul(p_os[:].rearrange("p a b -> p (a b)"), lhsT=zrow_s[0:1, 0:8], rhs=zrow_s[0:1, 0:130], start=True, stop=False,
                                            skip_group_check=True), r=["zrow_s"], w=["p3s_o"])

                def att_group(kunits, vsrc_fn, o_lo, n_o, last):
                    px = p_sx[itc[0] % 2]
                    pts = pTs[itc[0] % 2]

                    def consume(kt, s_, ui, px=px):
                        ol, g = ui // 2, ui % 2
                        PE(lambda: nc.tensor.matmul(px[:, ol, g * 8:(g + 1) * 8], lhsT=kt[:, s_, :], rhs=qas[:, g, :, :].rearrange("p a b -> p (a b)"), start=True, stop=True),
                           r=[kt.name, "qas"], w=[px.name], skip_same=True)
                    transposed_units(kunits, consume)
                    V(lambda: nc.vector.tensor_tensor(out=tmps[:, 0:n_o, :], in0=px[:, 0:n_o, :], in1=maskfull[:, o_lo:o_lo + n_o, :], op=ALU.add),
                      r=[px.name, "maskfull"], w=["tmps"])
                    A(lambda: nc.scalar.activation(out=pts[:, 0:n_o, :], in_=tmps[:, 0:n_o, :], func=AF.Exp, scale=0.125), r=["tmps"], w=[pts.name])
                    for ol in range(n_o):
                        for g in range(2):
                            vap, vk = vsrc_fn(ol, g)
                            PE(lambda ol=ol, g=g, vap=vap: nc.tensor.matmul(p_os[:, g, :], lhsT=pts[:, ol, g * 8:(g + 1) * 8], rhs=vap, start=False,
                                                                         stop=(last and ol == n_o - 1), skip_group_check=True),
                               r=[pts.name, vk], w=["p3s_o"], skip_same=True)

                for hf in range(2):
                    gather(Vh[:].rearrange("p a b -> p (a b)"), cache_vh[:, :], idxa if hf == 0 else idxb, "Vh")
                    for g in range(2):
                        A(lambda g=g: nc.scalar.copy(out=V1h[:, :, g, 0:64], in_=Vh[:, :, g * 64:(g + 1) * 64]), r=["Vh"], w=["V1h"])
                    for og in range(2):
                        units = []
                        for oo in range(32):
                            for g in range(2):
                                units.append((Kh[hf][:, og * 32 + oo, g * 64:(g + 1) * 64], [f"Kh{hf}"]))
                        att_group(units, lambda ol, g, og=og: (V1h[:, og * 32 + ol, g, :], "V1h"), hf * 64 + og * 32, 32, False)
                for g in range(2):
                    V(lambda g=g: nc.vector.tensor_copy(out=V1x[:, g, 0:64], in_=Vx[:, g * 64:(g + 1) * 64]), r=["Vx"], w=["V1x"])
                att_group([(Kx[:, g * 64:(g + 1) * 64], ["Kx"]) for g in range(2)], lambda ol, g: (V1x[:, g, :], "V1x"), 128, 1, True)
                V(lambda: nc.vector.reciprocal(out=rec8[:], in_=p_os[:, :, 64]), r=["p3s_o"], w=["rec8"])
                V(lambda: nc.vector.tensor_tensor(out=o8[:], in0=p_os[:, :, 0:64], in1=rec8[:].unsqueeze(2).to_broadcast([8, 2, 64]), op=ALU.mult),
                  r=["p3s_o", "rec8"], w=["o8"])
                for b2 in range(2):
                    row = samp0 + b0 + b2
                    DMA(ao_scr[row:row + 1, :].rearrange("o (g h d) -> (o h) g d", g=2, h=4), o8[4 * b2:4 * b2 + 4, :, :], r=["o8"], w=[("ao", TS)])
            S.barrier()

    if "p4" in phases:
        with contextlib.ExitStack() as e5:
            Wglu = sb("Wglu", [128, 4, 2048], BF16, e5)
            Wao = sb("Wao", [128, 4, 1024], BF16, e5)
            Wo = sb("Wo", [128, 8, 1024], BF16, e5)
            bglu = sb("bglu", [128, 2048], F32, e5)
            with contextlib.ExitStack() as e5w:
                load_weight_bf16(Wglu, w_glu, D_SSM, 2048, e5w, nm="wglu")
                load_weight_bf16(Wao, w_ao, D_ATTN, 1024, e5w, nm="wao")
                load_weight_bf16(Wo, w_o, D_MODEL, 1024, e5w, nm="wo")
                S.barrier()
            DMA(bglu[:], b_glu.rearrange("(o n) -> o n", o=1).partition_broadcast(128), w=["bglu"])
            gyt = [sb(f"gyt{i}", [128, 4, 128], BF16, e5) for i in range(2)]
            aot = [sb(f"aot{i}", [128, 512], BF16, e5) for i in range(2)]
            gat = [sb(f"gat{i}", [128, 2048], F32, e5) for i in range(2)]
            xin = [sb(f"xin{i}", [128, 1024], F32, e5) for i in range(2)]
            zv = sb("zv", [128, 512], F32, e5)
            zg = sb("zg", [128, 512], F32, e5)
            so = sb("so", [128, 1024], F32, e5)
            aoT = sb("aoT", [128, 4, 128], BF16, e5)
            m1 = sb("m1", [128, 1024], F32, e5)
            m2 = sb("m2", [128, 512], F32, e5)
            mixb2 = [sb(f"mixb{i}", [128, 1024], BF16, e5) for i in range(2)]
            mixT = sb("mixT", [128, 8, 128], BF16, e5)
            x2t = [sb(f"x2t{i}", [128, 1024], F32, e5) for i in range(2)]
            p_a = [ps(f"p4_a{i}", [128, 512], F32, e5) for i in range(4)]
            p_tr4 = ps("p4_tr", [128, 8, 128], BF16, e5)
            def p4a_front(T):
                r0, nr, is_s, pti = tile_rows(T)
                gy_, ao_, ga_, xi_, x2_ = gyt[T % 2], aot[T % 2], gat[T % 2], xin[T % 2], x2t[T % 2]
                mixb = mixb2[T % 2]
                gkeys = [("gyT", ct_, "s") for ct_ in range(4)] if is_s else [("gyT", ct_, (T * 128) // SEQ, ((T * 128) % SEQ) // 512) for ct_ in range(4)]
                DMA(gy_[:], gyT_scr.rearrange("(c p) t -> p c t", p=128)[:, :, T * 128:(T + 1) * 128], r=gkeys, w=[gy_.name])
                if is_s:
                    V(lambda ao_=ao_: nc.vector.memset(ao_[:], 0.0), w=[ao_.name])
                    DMA(ao_[0:NS, :], ao_scr[T * 128:T * 128 + NS, :], r=[("ao", T)], w=[ao_.name])
                    V(lambda xi_=xi_: nc.vector.memset(xi_[:], 0.0), w=[xi_.name])
                    DMA(xi_[0:NS, :], xs[:, :], w=[xi_.name])
                else:
                    DMA(ao_[:], ao_scr[T * 128:(T + 1) * 128, :], r=[("ao", T)], w=[ao_.name])
                    DMA(xi_[:], xp[r0:r0 + 128, :], w=[xi_.name])
                DMA(ga_[:], gate_scr[T * 128:(T + 1) * 128, :], r=[("gate", T)], w=[ga_.name])
                for nh in range(2):
                    pv, pg = p_a[0], p_a[1]
                    for (pp, c0) in ((pv, nh * 512), (pg, 1024 + nh * 512)):
                        for kc in range(4):
                            PE(lambda pp=pp, c0=c0, kc=kc, gy_=gy_: nc.tensor.matmul(pp[:], lhsT=gy_[:, kc, :], rhs=Wglu[:, kc, c0:c0 + 512], start=(kc == 0), stop=(kc == 3)),
                               r=[gy_.name, "Wglu"], w=[pp.name], skip_same=True)
                    V(lambda nh=nh: nc.vector.tensor_tensor(out=zv[:], in0=p_a[0][:], in1=bglu[:, nh * 512:(nh + 1) * 512], op=ALU.add), r=[p_a[0].name, "bglu"], w=["zv"])
                    V(lambda nh=nh: nc.vector.tensor_tensor(out=zg[:], in0=p_a[1][:], in1=bglu[:, 1024 + nh * 512:1024 + (nh + 1) * 512], op=ALU.add),
                      r=[p_a[1].name, "bglu"], w=["zg"])
                    A(lambda: nc.scalar.activation(out=zg[:], in_=zg[:], func=AF.Sigmoid), r=["zg"], w=["zg"])
                    G(lambda nh=nh: nc.gpsimd.tensor_tensor(out=so[:, nh * 512:(nh + 1) * 512], in0=zv[:], in1=zg[:], op=ALU.mult), r=["zv", "zg"], w=["so"])
                for kc in range(4):
                    PE(lambda kc=kc, ao_=ao_: nc.tensor.transpose(out=p_tr4[:, kc, :], in_=ao_[:, kc * 128:(kc + 1) * 128], identity=ident[:]),
                       r=[ao_.name, "ident"], w=["p4_tr"], skip_same=True)
                A(lambda: nc.scalar.copy(out=aoT[:], in_=p_tr4[:, 0:4, :]), r=["p4_tr"], w=["aoT"])
                G(lambda ga_=ga_: nc.gpsimd.tensor_tensor(out=m1[:], in0=so[:], in1=ga_[:, 0:1024], op=ALU.mult), r=["so", ga_.name], w=["m1"])
                for nh in range(2):
                    pp = p_a[2 + nh]
                    for kc in range(4):
                        PE(lambda pp=pp, nh=nh, kc=kc: nc.tensor.matmul(pp[:], lhsT=aoT[:, kc, :], rhs=Wao[:, kc, nh * 512:(nh + 1) * 512], start=(kc == 0), stop=(kc == 3)),
                           r=["aoT", "Wao"], w=[pp.name], skip_same=True)
                    V(lambda pp=pp, nh=nh, ga_=ga_: nc.vector.tensor_tensor(out=m2[:], in0=pp[:], in1=ga_[:, 1024 + nh * 512:1024 + (nh + 1) * 512], op=ALU.mult),
                      r=[pp.name, ga_.name], w=["m2"])
                    V(lambda nh=nh: nc.vector.tensor_tensor(out=mixb[:, nh * 512:(nh + 1) * 512], in0=m1[:, nh * 512:(nh + 1) * 512], in1=m2[:], op=ALU.add),
                      r=["m1", "m2"], w=[mixb.name])

            def p4a_back(T):
                r0, nr, is_s, pti = tile_rows(T)
                gy_, ao_, ga_, xi_, x2_ = gyt[T % 2], aot[T % 2], gat[T % 2], xin[T % 2], x2t[T % 2]
                mixb = mixb2[T % 2]
                for kc in range(8):
                    PE(lambda kc=kc: nc.tensor.transpose(out=p_tr4[:, kc, :], in_=mixb[:, kc * 128:(kc + 1) * 128], identity=ident[:]),
                       r=[mixb.name, "ident"], w=["p4_tr"], skip_same=True)
                A(lambda: nc.scalar.copy(out=mixT[:], in_=p_tr4[:]), r=["p4_tr"], w=["mixT"])
                for nh in range(2):
                    pp = p_a[nh]
                    for kc in range(8):
                        PE(lambda pp=pp, nh=nh, kc=kc: nc.tensor.matmul(pp[:], lhsT=mixT[:, kc, :], rhs=Wo[:, kc, nh * 512:(nh + 1) * 512], start=(kc == 0), stop=(kc == 7)),
                           r=["mixT", "Wo"], w=[pp.name], skip_same=True)
                    V(lambda pp=pp, nh=nh, xi_=xi_, x2_=x2_: nc.vector.tensor_tensor(out=x2_[:, nh * 512:(nh + 1) * 512], in0=pp[:], in1=xi_[:, nh * 512:(nh + 1) * 512], op=ALU.add),
                      r=[pp.name, xi_.name], w=[x2_.name])
                DMA(x2_scr[T * 128:(T + 1) * 128, :], x2_[:], r=[x2_.name], w=[("x2", T)], q="pool")

            p4a_front(0)
            for T in range(NTT):
                if T + 1 < NTT:
                    p4a_front(T + 1)
                p4a_back(T)
            S.barrier()

    if "p4" in phases:
        with contextlib.ExitStack() as e6:
            Wup = sb("Wup", [128, 8, D_FF], BF16, e6)
            Wdn = sb("Wdn", [128, 32, 1024], BF16, e6)
            g2c = sb("g2c", [128, 8], F32, e6)
            gfb = sb("gfb", [128, 1024], F32, e6)
            with nc.allow_non_contiguous_dma(reason="tiny gain vector"):
                DMA(g2c[:], norm2_g.rearrange("(k p) -> p k", p=128), w=["g2c"])
            DMA(gfb[:], normf_g.rearrange("(o n) -> o n", o=1).partition_broadcast(128), w=["gfb"])
            with contextlib.ExitStack() as e6w:
                load_weight_bf16(Wup, w_up, D_MODEL, D_FF, e6w, gcol=g2c, nm="wup", NSTG=3)
                load_weight_bf16(Wdn, w_down, D_FF, 1024, e6w, nm="wdn", NSTG=3)
                S.barrier()
            x2i = [sb(f"x2i{i}", [128, 1024], F32, e6) for i in range(2)]
            junk6 = sb("junk6", [128, 1024], F32, e6)
            ss6 = sb("ss6", [128, 1], F32, e6)
            rs6 = sb("rs6", [128, 1], F32, e6)
            hh = sb("hh", [128, 1024], BF16, e6)
            hhT = sb("hhT", [128, 8, 128], BF16, e6)
            rl = [sb(f"rl{i}", [128, 512], F32, e6) for i in range(2)]
            aT = sb("aT", [128, 32, 128], BF16, e6)
            x3 = sb("x3", [128, 1024], F32, e6)
            yt = [sb(f"yt{i}", [128, 1024], F32, e6) for i in range(2)]
            p_tr6 = ps("p6_tr", [128, 8, 128], BF16, e6)
            p_up = [ps(f"p6_up{i}", [128, 4, 128], F32, e6) for i in range(2)]
            p_dn = [ps(f"p6_dn{i}", [128, 512], F32, e6) for i in range(2)]
            for T in range(NTT):
                r0, nr, is_s, pti = tile_rows(T)
                xi_, y_ = x2i[T % 2], yt[T % 2]
                DMA(xi_[:], x2_scr[T * 128:(T + 1) * 128, :], r=[("x2", T)], w=[xi_.name])
                A(lambda xi_=xi_: nc.scalar.activation(out=junk6[:], in_=xi_[:], func=AF.Square, accum_out=ss6[:]), r=[xi_.name], w=["junk6", "ss6"])
                A(lambda: nc.scalar.activation(out=rs6[:], in_=ss6[:], func=AF.Sqrt, bias=eps_t[:], scale=1.0 / D_MODEL), r=["ss6", "eps_t"], w=["rs6"])
                V(lambda: nc.vector.reciprocal(out=rs6[:], in_=rs6[:]), r=["rs6"], w=["rs6"])
                V(lambda xi_=xi_: nc.vector.tensor_scalar(out=hh[:], in0=xi_[:], scalar1=rs6[:, 0:1], scalar2=None, op0=ALU.mult), r=[xi_.name, "rs6"], w=["hh"])
                for kc in range(8):
                    PE(lambda kc=kc: nc.tensor.transpose(out=p_tr6[:, kc, :], in_=hh[:, kc * 128:(kc + 1) * 128], identity=ident[:]),
                       r=["hh", "ident"], w=["p6_tr"], skip_same=True)
                A(lambda: nc.scalar.copy(out=hhT[:], in_=p_tr6[:]), r=["p6_tr"], w=["hhT"])
                for f4 in range(8):
                    pu = p_up[f4 % 2]
                    r_ = rl[f4 % 2]
                    for fi in range(4):
                        f = f4 * 4 + fi
                        for kc in range(8):
                            PE(lambda pu=pu, fi=fi, f=f, kc=kc: nc.tensor.matmul(pu[:, fi, :], lhsT=Wup[:, kc, f * 128:(f + 1) * 128], rhs=hhT[:, kc, :],
                                                                              start=(kc == 0), stop=(kc == 7)), r=["Wup", "hhT"], w=[pu.name], skip_same=True)
                    A(lambda pu=pu, r_=r_: nc.scalar.activation(out=r_[:], in_=pu[:].rearrange("p a b -> p (a b)"), func=AF.Relu), r=[pu.name], w=[r_.name])
                    V(lambda r_=r_, f4=f4: nc.vector.tensor_tensor(out=aT[:, f4 * 4:(f4 + 1) * 4, :].rearrange("p a b -> p (a b)"), in0=r_[:], in1=r_[:], op=ALU.mult),
                      r=[r_.name], w=["aT"])
                for nh in range(2):
                    pd = p_dn[nh]
                    for fk in range(32):
                        PE(lambda pd=pd, nh=nh, fk=fk: nc.tensor.matmul(pd[:], lhsT=aT[:, fk, :], rhs=Wdn[:, fk, nh * 512:(nh + 1) * 512], start=(fk == 0), stop=(fk == 31)),
                           r=["aT", "Wdn"], w=[pd.name], skip_same=True)
                    V(lambda pd=pd, nh=nh, xi_=xi_: nc.vector.tensor_tensor(out=x3[:, nh * 512:(nh + 1) * 512], in0=pd[:], in1=xi_[:, nh * 512:(nh + 1) * 512], op=ALU.add),
                      r=[pd.name, xi_.name], w=["x3"])
                A(lambda: nc.scalar.activation(out=junk6[:], in_=x3[:], func=AF.Square, accum_out=ss6[:]), r=["x3"], w=["junk6", "ss6"])
                A(lambda: nc.scalar.activation(out=rs6[:], in_=ss6[:], func=AF.Sqrt, bias=eps_t[:], scale=1.0 / D_MODEL), r=["ss6", "eps_t"], w=["rs6"])
                V(lambda: nc.vector.reciprocal(out=rs6[:], in_=rs6[:]), r=["rs6"], w=["rs6"])
                V(lambda y_=y_: nc.vector.scalar_tensor_tensor(out=y_[:], in0=x3[:], scalar=rs6[:, 0:1], in1=gfb[:], op0=ALU.mult, op1=ALU.mult),
                  r=["x3", "rs6", "gfb"], w=[y_.name])
                if is_s:
                    DMA(y_s[:, :], y_[0:NS, :], r=[y_.name], q="pool", is_output=True)
                else:
                    DMA(y_p[r0:r0 + 128, :], y_[:], r=[y_.name], q="pool", is_output=True)
            S.barrier()
    S.finish()
    es_all.close()
    dbg = dict(qT_scr=qT_scr, kT_scr=kT_scr)
    return nc


def make_in_map(inp, c, cfg):
    NB, NS = cfg.NB, cfg.NS
    f = lambda a: np.ascontiguousarray(a)
    m = {
        "xp": f(inp["x_prompt"][c * NB:(c + 1) * NB].reshape(NB * cfg.SEQ, D_MODEL)),
        "xs": f(inp["x_sample"][c * NS:(c + 1) * NS].reshape(NS, D_MODEL)),
        "cache_k": inp["cache_k"].reshape(cfg.NPOOL, -1),
        "cache_v": inp["cache_v"].reshape(cfg.NPOOL, -1),
        "cache_ik": inp["cache_idx_k"].reshape(cfg.NPOOL, -1),
        "st_re": f(inp["state_ssm_re"][c * NS:(c + 1) * NS].reshape(NS, -1)),
        "st_im": f(inp["state_ssm_im"][c * NS:(c + 1) * NS].reshape(NS, -1)),
        "ptab": f(inp["page_table"][c * NS:(c + 1) * NS].reshape(-1, 1)),
        "ssm_a_re": inp["ssm_a_re"].reshape(-1), "ssm_a_im": inp["ssm_a_im"].reshape(-1),
        "ssm_b_re": inp["ssm_b_re"].reshape(-1, 16), "ssm_b_im": inp["ssm_b_im"].reshape(-1, 16),
        "ssm_c_re": inp["ssm_c_re"].reshape(-1, 64), "ssm_c_im": inp["ssm_c_im"].reshape(-1, 64),
    }
    for k in ("norm1_g", "w_in", "ssm_log_dt", "ssm_d", "w_glu", "b_glu", "w_attn_out", "w_o", "norm2_g",
              "w_up", "w_down", "normf_g"):
        m[k] = inp[k]
    return m


ALL_PHASES = ("p1", "p2", "p3", "p3s", "p4")
OUT_NAMES = ["y_p", "y_s", "k_p", "v_p", "ki_p", "re_p", "im_p", "k_s", "v_s", "ki_s", "re_s", "im_s"]


def kernel(**inputs):
    inp = {k: np.asarray(v) for k, v in inputs.items()}
    B, SEQ = inp["x_prompt"].shape[0], inp["x_prompt"].shape[1]
    NSAMP = inp["x_sample"].shape[0]
    n_cores = 8
    cfg = Cfg(seq=SEQ, past=inp["page_table"].shape[1] * 128, nb=B // n_cores, ns=NSAMP // n_cores,
              n_pool=inp["cache_k"].shape[0])
    nc = build(cfg, phases=ALL_PHASES)
    shared = {"cache_k": inp["cache_k"].reshape(cfg.NPOOL, -1), "cache_v": inp["cache_v"].reshape(cfg.NPOOL, -1),
              "cache_ik": inp["cache_idx_k"].reshape(cfg.NPOOL, -1)}
    in_maps = []
    for c in range(n_cores):
        m = make_in_map(inp, c, cfg)
        m.update(shared)
        in_maps.append(m)
    res = run_bass_kernel_spmd(nc, in_maps, core_ids=list(range(n_cores)))
    outs = {n: np.concatenate([np.asarray(res.results[c][n]) for c in range(n_cores)], axis=0) for n in OUT_NAMES}
    NB, NS = cfg.NB, cfg.NS
    f32 = np.float32
    return (
        outs["y_p"].reshape(B, SEQ, D_MODEL).astype(f32, copy=False),
        outs["y_s"].reshape(NSAMP, 1, D_MODEL).astype(f32, copy=False),
        outs["k_p"].reshape(B, SEQ, 2, 64).astype(f32, copy=False),
        outs["v_p"].reshape(B, SEQ, 2, 64).astype(f32, copy=False),
        outs["ki_p"].reshape(B, SEQ, 64).astype(f32, copy=False),
        outs["re_p"].reshape(B, N_G, P_ST).astype(f32, copy=False),
        outs["im_p"].reshape(B, N_G, P_ST).astype(f32, copy=False),
        outs["k_s"].reshape(NSAMP, 1, 2, 64).astype(f32, copy=False),
        outs["v_s"].reshape(NSAMP, 1, 2, 64).astype(f32, copy=False),
        outs["ki_s"].reshape(NSAMP, 1, 64).astype(f32, copy=False),
        outs["re_s"].reshape(NSAMP, N_G, P_ST).astype(f32, copy=False),
        outs["im_s"].reshape(NSAMP, N_G, P_ST).astype(f32, copy=False),
    )
```
